# Optimizing a Trainium2 kernel written in Bass

```python
import jax
import jax.numpy as jnp
from jax import lax
import numpy as np

D_MODEL = 1024
BATCH = 1
SEQ = 16384
DEPTH = 4

GRID_W = 64
CTX_LEN = 256
N_MIXERS = 2
N_MOD = 6
DN_ALPHA = (2.0 * DEPTH) ** 0.25
DN_BETA = (8.0 * DEPTH) ** -0.25
LN_EPS = 1e-5

MLSTM_HEADS = 4
MLSTM_DH = D_MODEL // MLSTM_HEADS
MLSTM_CHUNK = 128
MLSTM_PROJ = 4 * D_MODEL + 4 * MLSTM_HEADS
CONV_K = 3
M_INIT = -1e30

RWKV_N = 64
RWKV_HEADS = D_MODEL // RWKV_N
DECAY_LORA = 64
AAA_LORA = 64
GATE_LORA = 160
RWKV_GN_EPS = 64e-5

N_KEYS = 128
N_EXPERTS = N_KEYS * N_KEYS
PEER_HEADS = 8
PEER_DQ = 256
PEER_TOPK = 16
PEER_BLOCK = 128

N_MLSTM_LAYERS = (DEPTH + 1) // 2
N_RWKV_LAYERS = DEPTH // 2

kernel_name = "hybrid_mlstm_rwkv7_peer_flow_block"


def _layernorm(x, g, b):
    xf = x.astype(jnp.float32)
    mu = jnp.mean(xf, -1, keepdims=True)
    var = jnp.mean(jnp.square(xf - mu), -1, keepdims=True)
    return ((xf - mu) * lax.rsqrt(var + LN_EPS)).astype(x.dtype) * g + b


def _head_norm(h, eps):
    hf = h.astype(jnp.float32)
    mu = jnp.mean(hf, -1, keepdims=True)
    var = jnp.mean(jnp.square(hf - mu), -1, keepdims=True)
    return (hf - mu) * lax.rsqrt(var + eps)


def _grid_dwconv(u, w, b, rows):
    bsz, n, ch = u.shape
    img = u.reshape(bsz, rows, GRID_W, ch)
    out = lax.conv_general_dilated(img, w[:, :, None, :].astype(u.dtype), (1, 1), "SAME",
                                   dimension_numbers=("NHWC", "HWIO", "NHWC"),
                                   feature_group_count=ch)
    return out.reshape(bsz, n, ch) + b


def _seq_dwconv(u, w, b):
    p = jnp.pad(u, ((0, 0), (1, 1), (0, 0)))
    wc = w[CONV_K // 2]
    return p[:, :-2] * wc[0] + p[:, 1:-1] * wc[1] + p[:, 2:] * wc[2] + b


def _qshift_grid(u, rows):
    bsz, n, d = u.shape
    q = d // 4
    p = jnp.pad(u.reshape(bsz, rows, GRID_W, d), ((0, 0), (1, 1), (1, 1), (0, 0)))
    out = jnp.concatenate([p[:, 1:-1, :-2, :q], p[:, 1:-1, 2:, q:2 * q],
                           p[:, :-2, 1:-1, 2 * q:3 * q], p[:, 2:, 1:-1, 3 * q:]], axis=-1)
    return out.reshape(bsz, n, d)


def _shift_seq(u):
    h = u.shape[-1] // 2
    p = jnp.pad(u, ((0, 0), (1, 1), (0, 0)))
    return jnp.concatenate([p[:, :-2, :h], p[:, 2:, h:]], axis=-1)


def _mlstm_scan(q, k, v, ig, lf, state):
    bsz, nh, t, dh = q.shape
    nc = t // MLSTM_CHUNK

    def to_chunks(a):
        return jnp.moveaxis(a.reshape(bsz, nh, nc, MLSTM_CHUNK, *a.shape[3:]), 2, 0)

    causal = jnp.tril(jnp.ones((MLSTM_CHUNK, MLSTM_CHUNK), bool))

    def step(carry, inp):
        c_st, n_st, m_st = carry
        qc, kc, vc, ic, fc = inp
        b = jnp.cumsum(fc, axis=-1)
        dlog = b[..., :, None] - b[..., None, :] + ic[..., None, :]
        dlog = jnp.where(causal, dlog, -jnp.inf)
        m_inter = b + m_st[..., None]
        m_t = jnp.maximum(m_inter, jnp.max(dlog, axis=-1))
        s = jnp.einsum('bhtd,bhsd->bhts', qc, kc) * jnp.exp(dlog - m_t[..., None])
        dec = jnp.exp(m_inter - m_t)
        num = jnp.einsum('bhts,bhsd->bhtd', s, vc) + dec[..., None] * jnp.einsum('bhtd,bhde->bhte', qc, c_st)
        den = jnp.sum(s, -1) + dec * jnp.einsum('bhtd,bhd->bht', qc, n_st)
        h = num / jnp.maximum(jnp.abs(den), jnp.exp(-m_t))[..., None]
        b_last = b[..., -1]
        w_s = b_last[..., None] - b + ic
        m_new = jnp.maximum(b_last + m_st, jnp.max(w_s, axis=-1))
        a_s = jnp.exp(w_s - m_new[..., None])
        g_prev = jnp.exp(b_last + m_st - m_new)
        c_new = g_prev[..., None, None] * c_st + jnp.einsum('bhsd,bhse->bhde', a_s[..., None] * kc, vc)
        n_new = g_prev[..., None] * n_st + jnp.einsum('bhs,bhsd->bhd', a_s, kc)
        return (c_new, n_new, m_new), h

    state, hs = lax.scan(step, state, tuple(to_chunks(a) for a in (q, k, v, ig, lf)))
    return jnp.moveaxis(hs, 0, 2).reshape(bsz, nh, t, dh), state


def _mlstm_mixer(hc, hx, rows, w_in, b_in, conv_w, conv_b, hn_g, w_out, need_ctx):
    d = D_MODEL
    f32 = jnp.float32

    def prep(h, conv):
        bsz, n = h.shape[:2]
        p = h @ w_in + b_in
        qk = jax.nn.silu(conv(p[..., :2 * d]))

        def heads(a):
            return jnp.swapaxes(a.reshape(bsz, n, MLSTM_HEADS, MLSTM_DH), 1, 2).astype(f32)
        q = heads(qk[..., :d]) * (MLSTM_DH ** -0.5)
        k = heads(qk[..., d:])
        v = heads(p[..., 2 * d:3 * d])
        o = jax.nn.sigmoid(p[..., 3 * d:4 * d])
        g = jnp.transpose(p[..., 4 * d:].astype(f32).reshape(bsz, n, 4, MLSTM_HEADS), (2, 0, 3, 1))
        return q, k, v, o, g

    def bidir(q, k, v, g, st_f, st_b):
        flip = lambda a: jnp.flip(a, axis=2)
        h_f, st_f = _mlstm_scan(q, k, v, g[0], jax.nn.log_sigmoid(g[2]), st_f)
        h_b, st_b = _mlstm_scan(flip(q), flip(k), flip(v), flip(g[1]), flip(jax.nn.log_sigmoid(g[3])), st_b)
        return h_f + flip(h_b), st_f, st_b

    def finish(h, o):
        bsz, _, n, _ = h.shape
        hn = jnp.swapaxes(_head_norm(h, LN_EPS), 1, 2).reshape(bsz, n, d).astype(o.dtype)
        return (o * hn * hn_g) @ w_out

    bsz = hx.shape[0]
    zero = (jnp.zeros((bsz, MLSTM_HEADS, MLSTM_DH, MLSTM_DH), f32),
            jnp.zeros((bsz, MLSTM_HEADS, MLSTM_DH), f32),
            jnp.full((bsz, MLSTM_HEADS), M_INIT, f32))
    qc, kc, vc, oc, gc = prep(hc, lambda u: _seq_dwconv(u, conv_w, conv_b))
    h_c, st_f, st_b = bidir(qc, kc, vc, gc, zero, zero)
    qx, kx, vx, ox, gx = prep(hx, lambda u: _grid_dwconv(u, conv_w, conv_b, rows))
    h_x, _, _ = bidir(qx, kx, vx, gx, st_f, st_b)
    y_x = finish(h_x, ox)
    y_c = finish(h_c, oc) if need_ctx else None
    return y_c, y_x


def _rwkv7_scan(r, w, k, v, kk, kka, state):
    def step(s, inp):
        r_t, w_t, k_t, v_t, kk_t, kka_t = inp
        sa = jnp.einsum('bhvk,bhk->bhv', s, kk_t)
        s = s * w_t[:, :, None, :] - sa[..., None] * kka_t[:, :, None, :] + v_t[..., None] * k_t[:, :, None, :]
        return s, jnp.einsum('bhvk,bhk->bhv', s, r_t)
    state, ys = lax.scan(step, state, (r, w, k, v, kk, kka))
    return ys, state


def _rwkv7_mixer(hc, hx, rows, mu, w_rkv, w0, w1, w2, a0, a1, a2, g1, g2, k_k, k_a, r_k,
                 lnx_g, lnx_b, w_out, need_ctx):
    f32 = jnp.float32
    d = D_MODEL

    def prep(h, shifted):
        bsz, n, _ = h.shape
        xm = h[None] + (shifted - h)[None] * mu[:, None, None, :]
        r, k, v = jnp.einsum('nbtd,nde->nbte', xm[:3], w_rkv)
        wpre = w0[:, None, None, :] + jnp.einsum(
            'zbte,zed->zbtd', jnp.tanh(jnp.einsum('btd,zde->zbte', xm[3], w1)), w2)
        decay = jnp.exp(-jnp.exp(-jax.nn.softplus(-wpre.astype(f32)) - 0.5))
        a = jax.nn.sigmoid((a0[:, None, None, :] + jnp.einsum(
            'zbte,zed->zbtd', jnp.einsum('btd,zde->zbte', xm[4], a1), a2)).astype(f32))
        g = jax.nn.sigmoid(xm[5] @ g1) @ g2
        kk = (k * k_k).astype(f32).reshape(bsz, n, RWKV_HEADS, RWKV_N)
        kk = (kk / jnp.maximum(jnp.linalg.norm(kk, axis=-1, keepdims=True), 1e-12)).reshape(bsz, n, d)
        ktil = k.astype(f32)[None] * (1.0 + (a - 1.0) * k_a.astype(f32))
        return r, v, g, kk, decay, a, ktil

    def tm(t):
        return jnp.swapaxes(t, 0, 1).reshape(t.shape[1], t.shape[0], RWKV_HEADS, RWKV_N).astype(f32)

    def bidir(pp, st_f, st_b):
        r, v, g, kk, decay, a, ktil = pp
        ins_f = [tm(t) for t in (r, decay[0], ktil[0], v, kk, kk * a[0])]
        ins_b = [jnp.flip(tm(t), 0) for t in (r, decay[1], ktil[1], v, kk, kk * a[1])]
        y_f, st_f = _rwkv7_scan(*ins_f, st_f)
        y_b, st_b = _rwkv7_scan(*ins_b, st_b)
        return y_f + jnp.flip(y_b, 0), st_f, st_b

    def finish(y, pp):
        r, v, g, kk, decay, a, ktil = pp
        n, bsz = y.shape[:2]
        yn = jnp.swapaxes(_head_norm(y, RWKV_GN_EPS), 0, 1).reshape(bsz, n, d) * lnx_g + lnx_b
        kbar = 0.5 * (ktil[0] + ktil[1])
        bonus = jnp.sum((r * kbar * r_k).reshape(bsz, n, RWKV_HEADS, RWKV_N), -1, keepdims=True) \
            * v.reshape(bsz, n, RWKV_HEADS, RWKV_N)
        return ((yn + bonus.reshape(bsz, n, d)) * g).astype(v.dtype) @ w_out

    bsz = hx.shape[0]
    zero = jnp.zeros((bsz, RWKV_HEADS, RWKV_N, RWKV_N), f32)
    pc = prep(hc, _shift_seq(hc))
    y_c, st_f, st_b = bidir(pc, zero, zero)
    px = prep(hx, _qshift_grid(hx, rows))
    y_x, _, _ = bidir(px, st_f, st_b)
    out_x = finish(y_x, px)
    out_c = finish(y_c, pc) if need_ctx else None
    return out_c, out_x


def _peer(h, wq, keys, u_tab, v_tab):
    bsz, n, d = h.shape
    hk = PEER_DQ // 2
    kf = keys.astype(jnp.float32)

    def block(xb):
        nt = xb.shape[0]
        q = (xb @ wq).astype(jnp.float32).reshape(nt, PEER_HEADS, 2, hk)
        s = jnp.einsum('thpd,hpkd->thpk', q, kf)
        sv, si = lax.top_k(s, PEER_TOPK)
        cand = (sv[:, :, 0, :, None] + sv[:, :, 1, None, :]).reshape(nt, PEER_HEADS, PEER_TOPK ** 2)
        cidx = (si[:, :, 0, :, None] * N_KEYS + si[:, :, 1, None, :]).reshape(nt, PEER_HEADS, PEER_TOPK ** 2)
        fv, fi = lax.top_k(cand, PEER_TOPK)
        eidx = jnp.take_along_axis(cidx, fi, axis=-1).reshape(nt, PEER_HEADS * PEER_TOPK)
        gate = jax.nn.softmax(fv, axis=-1).reshape(nt, PEER_HEADS * PEER_TOPK)
        u = jnp.take(u_tab, eidx, axis=0)
        act = jax.nn.gelu(jnp.einsum('ted,td->te', u, xb), approximate=False)
        wgt = (gate * act.astype(jnp.float32)).astype(xb.dtype)
        return jnp.einsum('te,ted->td', wgt, jnp.take(v_tab, eidx, axis=0))

    out = lax.map(block, h.reshape(-1, PEER_BLOCK, d))
    return out.reshape(bsz, n, d)


def setup_inputs(seed: int = 0) -> dict:
    key = jax.random.key(seed)
    ks = iter(jax.random.split(key, 48))
    f32 = jnp.float32
    nrm = lambda shape, scale: jax.random.normal(next(ks), shape, f32) * scale
    d = D_MODEL
    na, nb = N_MLSTM_LAYERS, N_RWKV_LAYERS
    hm = MLSTM_HEADS

    x = nrm((BATCH, SEQ, d), 1.0)
    c = nrm((BATCH, d), 1.0)
    ctx = nrm((BATCH, CTX_LEN, d), 1.0)
    c_ctx = nrm((d,), 1.0)
    ada_w = nrm((DEPTH, d, N_MOD * d), 0.5 * d ** -0.5)
    ada_b = nrm((DEPTH, N_MOD * d), 0.02)
    ln_g = 1.0 + nrm((DEPTH, 2, d), 0.02)
    ln_b = nrm((DEPTH, 2, d), 0.02)

    col_scale = jnp.concatenate([jnp.ones((2 * d,), f32), jnp.full((d,), DN_BETA, f32),
                                 jnp.ones((d,), f32), jnp.full((4 * hm,), 0.5, f32)])
    ml_w_in = nrm((na, d, MLSTM_PROJ), d ** -0.5) * col_scale
    gate_bias = jnp.concatenate([jnp.zeros((2 * hm,), f32), jnp.tile(jnp.linspace(3.0, 6.0, hm), 2)])
    ml_b_in = nrm((na, MLSTM_PROJ), 0.02) + jnp.concatenate([jnp.zeros((4 * d,), f32), gate_bias])
    ml_conv_w = nrm((na, CONV_K, CONV_K, 2 * d), 1.0 / 3.0)
    ml_conv_b = nrm((na, 2 * d), 0.02)
    ml_hn_g = 1.0 + nrm((na, d), 0.02)
    ml_w_out = nrm((na, d, d), d ** -0.5 * DN_BETA)

    rw_mu = jax.random.uniform(next(ks), (nb, 6, d), f32)
    rw_w_rkv = nrm((nb, 3, d, d), d ** -0.5) * jnp.array([1.0, 1.0, DN_BETA], f32)[None, :, None, None]
    rw_w0 = jnp.broadcast_to(jnp.linspace(-6.0, 1.0, d), (nb, 2, d)) + nrm((nb, 2, d), 0.1)
    rw_w1 = nrm((nb, 2, d, DECAY_LORA), d ** -0.5)
    rw_w2 = nrm((nb, 2, DECAY_LORA, d), 0.1 * DECAY_LORA ** -0.5)
    rw_a0 = nrm((nb, 2, d), 0.1)
    rw_a1 = nrm((nb, 2, d, AAA_LORA), d ** -0.5)
    rw_a2 = nrm((nb, 2, AAA_LORA, d), 0.5 * AAA_LORA ** -0.5)
    rw_g1 = nrm((nb, d, GATE_LORA), d ** -0.5)
    rw_g2 = nrm((nb, GATE_LORA, d), GATE_LORA ** -0.5)
    rw_k_k = 0.85 + nrm((nb, d), 0.02)
    rw_k_a = 1.0 + nrm((nb, d), 0.02)
    rw_r_k = nrm((nb, d), 0.1)
    rw_lnx_g = 1.0 + nrm((nb, d), 0.02)
    rw_lnx_b = nrm((nb, d), 0.02)
    rw_w_out = nrm((nb, d, d), d ** -0.5 * DN_BETA)

    pk_wq = nrm((DEPTH, d, PEER_HEADS * PEER_DQ), d ** -0.5)
    pk_keys = nrm((DEPTH, PEER_HEADS, 2, N_KEYS, PEER_DQ // 2), (PEER_DQ // 2) ** -0.5)
    pk_u = nrm((DEPTH, N_EXPERTS, d), d ** -0.5)
    pk_v = nrm((DEPTH, N_EXPERTS, d), DN_BETA)
    return {"x": x, "c": c, "ctx": ctx, "c_ctx": c_ctx,
            "ada_w": ada_w, "ada_b": ada_b, "ln_g": ln_g, "ln_b": ln_b,
            "ml_w_in": ml_w_in, "ml_b_in": ml_b_in, "ml_conv_w": ml_conv_w, "ml_conv_b": ml_conv_b,
            "ml_hn_g": ml_hn_g, "ml_w_out": ml_w_out,
            "rw_mu": rw_mu, "rw_w_rkv": rw_w_rkv, "rw_w0": rw_w0, "rw_w1": rw_w1, "rw_w2": rw_w2,
            "rw_a0": rw_a0, "rw_a1": rw_a1, "rw_a2": rw_a2, "rw_g1": rw_g1, "rw_g2": rw_g2,
            "rw_k_k": rw_k_k, "rw_k_a": rw_k_a, "rw_r_k": rw_r_k, "rw_lnx_g": rw_lnx_g,
            "rw_lnx_b": rw_lnx_b, "rw_w_out": rw_w_out,
            "pk_wq": pk_wq, "pk_keys": pk_keys, "pk_u": pk_u, "pk_v": pk_v}


def reference(x, c, ctx, c_ctx, ada_w, ada_b, ln_g, ln_b,
              ml_w_in, ml_b_in, ml_conv_w, ml_conv_b, ml_hn_g, ml_w_out,
              rw_mu, rw_w_rkv, rw_w0, rw_w1, rw_w2, rw_a0, rw_a1, rw_a2, rw_g1, rw_g2,
              rw_k_k, rw_k_a, rw_r_k, rw_lnx_g, rw_lnx_b, rw_w_out,
              pk_wq, pk_keys, pk_u, pk_v):
    rows = x.shape[1] // GRID_W
    n_ctx = ctx.shape[1]
    s_lat = jax.nn.silu(c)
    s_ctx = jax.nn.silu(c_ctx)
    for i in range(DEPTH):
        last = i == DEPTH - 1
        j = i // N_MIXERS
        mx = jnp.split((s_lat @ ada_w[i] + ada_b[i])[:, None, :], N_MOD, axis=-1)
        mc = jnp.split((s_ctx @ ada_w[i] + ada_b[i])[None, None, :], N_MOD, axis=-1)
        hx = x * (1.0 + mx[1]) + mx[0]
        hc = ctx * (1.0 + mc[1]) + mc[0]
        if i % N_MIXERS == 0:
            yc, yx = _mlstm_mixer(hc, hx, rows, ml_w_in[j], ml_b_in[j], ml_conv_w[j], ml_conv_b[j],
                                  ml_hn_g[j], ml_w_out[j], not last)
        else:
            yc, yx = _rwkv7_mixer(hc, hx, rows, rw_mu[j], rw_w_rkv[j], rw_w0[j], rw_w1[j], rw_w2[j],
                                  rw_a0[j], rw_a1[j], rw_a2[j], rw_g1[j], rw_g2[j], rw_k_k[j],
                                  rw_k_a[j], rw_r_k[j], rw_lnx_g[j], rw_lnx_b[j], rw_w_out[j], not last)
        x = _layernorm(DN_ALPHA * x + mx[2] * yx, ln_g[i, 0], ln_b[i, 0])
        hx = x * (1.0 + mx[4]) + mx[3]
        if last:
            yx = _peer(hx, pk_wq[i], pk_keys[i], pk_u[i], pk_v[i])
        else:
            ctx = _layernorm(DN_ALPHA * ctx + mc[2] * yc, ln_g[i, 0], ln_b[i, 0])
            hc = ctx * (1.0 + mc[4]) + mc[3]
            y = _peer(jnp.concatenate([hc, hx], axis=1), pk_wq[i], pk_keys[i], pk_u[i], pk_v[i])
            ctx = _layernorm(DN_ALPHA * ctx + mc[5] * y[:, :n_ctx], ln_g[i, 1], ln_b[i, 1])
            yx = y[:, n_ctx:]
        x = _layernorm(DN_ALPHA * x + mx[5] * yx, ln_g[i, 1], ln_b[i, 1])
    return x
```

```python
import contextlib
import numpy as np
import concourse.bass as bass
import concourse.mybir as mybir
from concourse.alu_op_type import AluOpType as ALU
from concourse.bass_utils import run_bass_kernel_spmd

F32 = mybir.dt.float32
I32 = mybir.dt.int32
U32 = mybir.dt.uint32
AF = mybir.ActivationFunctionType


class KB:
    def __init__(self):
        self.nc = bass.Bass("TRN2", target_bir_lowering=False)
        nc = self.nc
        self.es = contextlib.ExitStack()
        self.es.enter_context(nc.cleanup_on_exit())
        self.engs = {"pe": nc.tensor, "dve": nc.vector, "act": nc.scalar,
                     "pool": nc.gpsimd, "sp": nc.sync}
        self.esem = {}
        self.ecnt = {}
        for e in self.engs:
            self.esem[e] = nc.alloc_semaphore(name=f"s_{e}")
            self.ecnt[e] = 0
        self.seen = {e: {} for e in self.engs}
        self.tr = {}
        self.dsem = {}
        self.n_inst = 0
        self._uid = 0

    def sb(self, name, shape, dt=F32):
        t = self.es.enter_context(self.nc.sbuf_tensor(name, list(shape), dt))
        return t

    def ps(self, name, shape, dt=F32):
        t = self.es.enter_context(self.nc.psum_tensor(name, list(shape), dt))
        return t

    def dram_in(self, name, shape, dt=F32):
        return self.nc.dram_tensor(name, list(shape), dt, kind="ExternalInput").ap()

    def dram_out(self, name, shape, dt=F32):
        return self.nc.dram_tensor(name, list(shape), dt, kind="ExternalOutput").ap()

    @staticmethod
    def _key(ap):
        t = getattr(ap, "tensor", ap)
        return t.name

    def _needs(self, reads, writes):
        needs = {}

        def need(sv):
            sem, val = sv
            k = sem.name if hasattr(sem, "name") else id(sem)
            if k not in needs or needs[k][1] < val:
                needs[k] = (sem, val)

        for ap in reads:
            st = self.tr.get(self._key(ap))
            if st and st[0]:
                need(st[0])
        for ap in writes:
            st = self.tr.get(self._key(ap))
            if st:
                if st[0]:
                    need(st[0])
                for sv in st[1].values():
                    need(sv)
        return needs

    def _emit_waits(self, e, needs, skip_sem=None):
        eng = self.engs[e]
        seen = self.seen[e]
        for k, (sem, val) in needs.items():
            if skip_sem is not None and sem is skip_sem:
                continue
            if seen.get(k, -1) >= val:
                continue
            eng.wait_ge(sem, val)
            seen[k] = val

    def _update(self, reads, writes, sv):
        sem, val = sv
        k = sem.name if hasattr(sem, "name") else id(sem)
        for ap in reads:
            st = self.tr.setdefault(self._key(ap), [None, {}])
            st[1][k] = sv
        for ap in writes:
            st = self.tr.setdefault(self._key(ap), [None, {}])
            st[0] = sv
            st[1] = {}

    def op(self, e, fn, reads=(), writes=(), same_ok=False):
        needs = self._needs(reads, writes)
        self._emit_waits(e, needs, skip_sem=self.esem[e] if same_ok else None)
        inst = fn()
        self.ecnt[e] += 1
        inst.then_inc(self.esem[e], 1)
        self._update(reads, writes, (self.esem[e], self.ecnt[e]))
        self.n_inst += 1
        return inst

    def dma(self, q, out, in_, fn=None, extra_reads=(), **kw):
        reads, writes = [in_] + list(extra_reads), [out]
        needs = self._needs(reads, writes)
        self._emit_waits(q, needs)
        sbt = None
        for ap in (out, in_):
            if "sbuf" in str(ap.space).lower() or "sb" == str(ap.space).lower():
                sbt = ap
        keyt = self._key(sbt if sbt is not None else out)
        if keyt not in self.dsem:
            self.dsem[keyt] = [self.nc.alloc_semaphore(name=f"d_{len(self.dsem)}"), 0]
        ds = self.dsem[keyt]
        if fn is None:
            inst = self.engs[q].dma_start(out=out, in_=in_, **kw)
        else:
            inst = fn()
        ds[1] += 16
        inst.then_inc(ds[0], 16)
        self._update(reads, writes, (ds[0], ds[1]))
        self.n_inst += 1
        return inst

    def finish(self):
        sp = self.engs["sp"]
        for e in self.engs:
            if self.ecnt[e] > 0 and e != "sp":
                sp.wait_ge(self.esem[e], self.ecnt[e])
        for k, (sem, cnt) in self.dsem.items():
            sp.wait_ge(sem, cnt)
        self.nc.all_engine_barrier()
        self.es.close()
        return self.nc


D = 1024
ALPHA = (2.0 * 4) ** 0.25
LN_EPS = 1e-5


def _consts(k, need_iota=False):
    ident_d = k.dram_in("ident", [128, 128])
    ident = k.sb("ident_sb", [128, 128])
    k.dma("sp", ident[:], ident_d[:, :])
    return ident


def _layernorm_tile(k, t, tmp, stats, mv, rstd, epst):
    nc = k.nc
    for j in range(2):
        k.op("dve", lambda j=j: nc.vector.bn_stats(out=stats[:, j * 6:(j + 1) * 6], in_=t[:, j * 512:(j + 1) * 512]),
             reads=[t], writes=[stats])
    k.op("dve", lambda: nc.vector.bn_aggr(out=mv[:, 0:2], in_=stats[:, 0:12]), reads=[stats], writes=[mv])
    k.op("act", lambda: nc.scalar.activation(out=rstd[:, 0:1], in_=mv[:, 1:2], func=AF.Sqrt, bias=epst[:, 0:1], scale=1.0),
         reads=[mv, epst], writes=[rstd])
    k.op("dve", lambda: nc.vector.reciprocal(out=rstd[:, 0:1], in_=rstd[:, 0:1]), reads=[rstd], writes=[rstd])
    k.op("dve", lambda: nc.vector.tensor_scalar(out=t[:], in0=t[:], scalar1=mv[:, 0:1], scalar2=rstd[:, 0:1],
                                                op0=ALU.subtract, op1=ALU.mult), reads=[t, mv, rstd], writes=[t])


def build_C2(NX, NCTX):
    k = KB()
    nc = k.nc
    TOK = NX + NCTX
    x1_d = k.dram_in("x1", [TOK, D])
    mod_d = k.dram_in("mod", [2, 3, 128, D])
    lnp_d = k.dram_in("lnp", [2, 128, D])
    wq_d = k.dram_in("wq", [128, 8, 2048])
    keysT_d = k.dram_in("keysT", [128, 16, 128])
    iota_d = k.dram_in("iota", [128, 256])
    u_d = k.dram_in("u_tab", [16384, D])
    v_d = k.dram_in("v_tab", [16384, D])
    out_d = k.dram_out("xout", [TOK, D])
    ident = _consts(k)

    wq = k.sb("wq_sb", [128, 8, 2048])
    keysT = k.sb("keysT_sb", [128, 16, 128])
    iota = k.sb("iota_sb", [128, 256])
    modt = [k.sb(f"mod{j}", [128, D]) for j in range(3)]
    lng = k.sb("lng", [128, D]); lnb = k.sb("lnb", [128, D])
    x1t = k.sb("x1t", [128, D]); h2 = k.sb("h2", [128, D]); acc = k.sb("acc", [128, D])
    junk = k.sb("junk", [128, D])
    NB = 4
    gb = [k.sb(f"gb{j}", [128, D]) for j in range(NB)]
    T = k.sb("T", [128, 8, 128]); qT = k.sb("qT", [128, 16, 128])
    R1 = k.sb("R1", [128, 16, 128]); R2 = k.sb("R2", [128, 16, 128]); R3 = k.sb("R3", [128, 8, 256])
    sv = k.sb("sv", [128, 16, 16]); si = k.sb("si", [128, 16, 16], U32); sif = k.sb("sif", [128, 16, 16])
    fv = k.sb("fv", [128, 8, 16]); fi = k.sb("fi", [128, 8, 16], U32); fif = k.sb("fif", [128, 8, 16])
    eidf = k.sb("eidf", [128, 128]); eid = k.sb("eid", [128, 128], U32)
    negm = k.sb("negm", [128, 8]); gs = k.sb("gs", [128, 8]); gate = k.sb("gate", [128, 8, 16])
    actv = k.sb("actv", [128, 128]); wgt = k.sb("wgt", [128, 128])
    stats = k.sb("stats", [128, 12]); mv = k.sb("mv", [128, 2]); rstd = k.sb("rstd", [128, 1])
    epst = k.sb("epst", [128, 1])
    pbank = [k.ps(f"pb{j}", [128, 4, 128]) for j in range(4)]

    k.op("pool", lambda: nc.gpsimd.memset(epst[:], LN_EPS), writes=[epst])
    k.op("pool", lambda: nc.gpsimd.memset(eid[:], 0), writes=[eid])
    for kc in range(8):
        k.dma("sp", wq[:, kc, :], wq_d[:, kc, :])
    k.dma("sp", keysT[:], keysT_d[:, :, :])
    k.dma("sp", iota[:], iota_d[:, :])
    k.dma("sp", lng[:], lnp_d[0]); k.dma("sp", lnb[:], lnp_d[1])

    tiles = [(i * 128, 128, 0) for i in range(NX // 128)]
    if NCTX:
        tiles.append((NX, NCTX, 1))
    cur_ty = None
    V = nc.vector
    for (r0, n, ty) in tiles:
        if ty != cur_ty:
            for j in range(3):
                k.dma("sp", modt[j][:], mod_d[ty, j])
            k.op("dve", lambda: V.tensor_scalar_add(out=modt[1][:], in0=modt[1][:], scalar1=1.0), reads=[modt[1]], writes=[modt[1]])
            cur_ty = ty
        k.dma("sp", x1t[:n, :], x1_d[r0:r0 + n, :])
        k.op("dve", lambda: V.tensor_tensor(out=h2[:], in0=x1t[:], in1=modt[1][:], op=ALU.mult), reads=[x1t, modt[1]], writes=[h2])
        k.op("dve", lambda: V.tensor_tensor(out=h2[:], in0=h2[:], in1=modt[0][:], op=ALU.add), reads=[h2, modt[0]], writes=[h2])
        for half in range(2):
            pb = pbank[half]
            for j in range(4):
                kc = half * 4 + j
                k.op("pe", lambda pb=pb, j=j, kc=kc: nc.tensor.transpose(out=pb[:, j, :], in_=h2[:, kc * 128:(kc + 1) * 128], identity=ident[:]),
                     reads=[h2, ident], writes=[pb], same_ok=True)
            k.op("act", lambda pb=pb, half=half: nc.scalar.copy(out=T[:, half * 4:(half + 1) * 4, :], in_=pb[:]), reads=[pb], writes=[T])
        for g4 in range(4):
            pb = pbank[g4]
            for j in range(4):
                hp = g4 * 4 + j
                for kc in range(8):
                    k.op("pe", lambda pb=pb, j=j, hp=hp, kc=kc: nc.tensor.matmul(pb[:, j, :], wq[:, kc, hp * 128:(hp + 1) * 128], T[:, kc, :],
                                                                              start=(kc == 0), stop=(kc == 7)),
                         reads=[wq, T], writes=[pb], same_ok=True)
            k.op("act", lambda pb=pb, g4=g4: nc.scalar.copy(out=qT[:, g4 * 4:(g4 + 1) * 4, :], in_=pb[:]), reads=[pb], writes=[qT])
        for g4 in range(4):
            pb = pbank[g4]
            for j in range(4):
                hp = g4 * 4 + j
                k.op("pe", lambda pb=pb, j=j, hp=hp: nc.tensor.matmul(pb[:, j, :], qT[:, hp, :], keysT[:, hp, :], start=True, stop=True),
                     reads=[qT, keysT], writes=[pb], same_ok=True)
            k.op("act", lambda pb=pb, g4=g4: nc.scalar.copy(out=R1[:, g4 * 4:(g4 + 1) * 4, :], in_=pb[:]), reads=[pb], writes=[R1])
        for hp in range(16):
            k.op("dve", lambda hp=hp: V.max(out=sv[:, hp, 0:8], in_=R1[:, hp, :]), reads=[R1], writes=[sv])
            k.op("dve", lambda hp=hp: V.max_index(out=si[:, hp, 0:8], in_max=sv[:, hp, 0:8], in_values=R1[:, hp, :]), reads=[R1, sv], writes=[si])
            k.op("dve", lambda hp=hp: V.match_replace(out=R2[:, hp, :], in_to_replace=sv[:, hp, 0:8], in_values=R1[:, hp, :], imm_value=-1e30),
                 reads=[R1, sv], writes=[R2])
            k.op("dve", lambda hp=hp: V.max(out=sv[:, hp, 8:16], in_=R2[:, hp, :]), reads=[R2], writes=[sv])
            k.op("dve", lambda hp=hp: V.max_index(out=si[:, hp, 8:16], in_max=sv[:, hp, 8:16], in_values=R2[:, hp, :]), reads=[R2, sv], writes=[si])
        k.op("dve", lambda: V.tensor_copy(out=sif[:], in_=si[:]), reads=[si], writes=[sif])
        sv4 = sv[:].rearrange("p (h two) k -> p h two k", two=2)
        sif4 = sif[:].rearrange("p (h two) k -> p h two k", two=2)
        cand = R1[:].rearrange("p (h a) (b j) -> p h (a b) j", a=2, b=8)
        cand_flat = R1[:].rearrange("p (h a) m -> p h (a m)", a=2)
        cand2_flat = R2[:].rearrange("p (h a) m -> p h (a m)", a=2)
        cidx = R3[:].rearrange("p h (i j) -> p h i j", i=16)
        k.op("dve", lambda: V.tensor_tensor(out=cand, in0=sv4[:, :, 0, :, None].broadcast_to([128, 8, 16, 16]),
                                            in1=sv4[:, :, 1, None, :].broadcast_to([128, 8, 16, 16]), op=ALU.add),
             reads=[sv], writes=[R1])
        k.op("dve", lambda: V.tensor_scalar_mul(out=sif4[:, :, 0, :], in0=sif4[:, :, 0, :], scalar1=128.0), reads=[sif], writes=[sif])
        k.op("dve", lambda: V.tensor_tensor(out=cidx, in0=sif4[:, :, 0, :, None].broadcast_to([128, 8, 16, 16]),
                                            in1=sif4[:, :, 1, None, :].broadcast_to([128, 8, 16, 16]), op=ALU.add),
             reads=[sif], writes=[R3])
        for h in range(8):
            k.op("dve", lambda h=h: V.max(out=fv[:, h, 0:8], in_=cand_flat[:, h, :]), reads=[R1], writes=[fv])
            k.op("dve", lambda h=h: V.max_index(out=fi[:, h, 0:8], in_max=fv[:, h, 0:8], in_values=cand_flat[:, h, :]), reads=[R1, fv], writes=[fi])
            k.op("dve", lambda h=h: V.match_replace(out=cand2_flat[:, h, :], in_to_replace=fv[:, h, 0:8], in_values=cand_flat[:, h, :], imm_value=-1e30),
                 reads=[R1, fv], writes=[R2])
            k.op("dve", lambda h=h: V.max(out=fv[:, h, 8:16], in_=cand2_flat[:, h, :]), reads=[R2], writes=[fv])
            k.op("dve", lambda h=h: V.max_index(out=fi[:, h, 8:16], in_max=fv[:, h, 8:16], in_values=cand2_flat[:, h, :]), reads=[R2, fv], writes=[fi])
        k.op("dve", lambda: V.tensor_copy(out=fif[:], in_=fi[:]), reads=[fi], writes=[fif])
        for h in range(8):
            for j in range(16):
                e = h * 16 + j
                k.op("dve", lambda h=h, j=j, e=e: V.scalar_tensor_tensor(out=junk[:, 0:256], in0=iota[:], scalar=fif[:, h, j:j + 1], in1=R3[:, h, :],
                                                                        op0=ALU.is_equal, op1=ALU.mult, accum_out=eidf[:, e:e + 1]),
                     reads=[iota, fif, R3], writes=[junk, eidf])
        k.op("dve", lambda: V.tensor_copy(out=eid[:], in_=eidf[:]), reads=[eidf], writes=[eid])
        k.op("dve", lambda: V.tensor_scalar_mul(out=negm[:], in0=fv[:, :, 0], scalar1=-1.0), reads=[fv], writes=[negm])
        for h in range(8):
            k.op("act", lambda h=h: nc.scalar.activation(out=gate[:, h, :], in_=fv[:, h, :], func=AF.Exp, bias=negm[:, h:h + 1], scale=1.0,
                                                         accum_out=gs[:, h:h + 1]), reads=[fv, negm], writes=[gate, gs])
        k.op("dve", lambda: V.reciprocal(out=gs[:], in_=gs[:]), reads=[gs], writes=[gs])
        k.op("dve", lambda: V.tensor_tensor(out=gate[:], in0=gate[:], in1=gs[:, :, None].broadcast_to([128, 8, 16]), op=ALU.mult),
             reads=[gate, gs], writes=[gate])
        for e in range(128):
            b = gb[e % NB]
            k.dma("pool", b[:], u_d[:, :], fn=lambda b=b, e=e: nc.gpsimd.indirect_dma_start(
                out=b[:], out_offset=None, in_=u_d[:, :], in_offset=bass.IndirectOffsetOnAxis(ap=eid[:, e:e + 1], axis=0)),
                extra_reads=[eid])
            k.op("dve", lambda b=b, e=e: V.scalar_tensor_tensor(out=junk[:], in0=b[:], scalar=1.0, in1=h2[:],
                                                                op0=ALU.mult, op1=ALU.mult, accum_out=actv[:, e:e + 1]),
                 reads=[b, h2], writes=[junk, actv])
        k.op("act", lambda: nc.scalar.activation(out=wgt[:], in_=actv[:], func=AF.Gelu), reads=[actv], writes=[wgt])
        k.op("dve", lambda: V.tensor_tensor(out=wgt[:], in0=wgt[:], in1=gate[:].rearrange("p h j -> p (h j)"), op=ALU.mult),
             reads=[wgt, gate], writes=[wgt])
        for e in range(128):
            b = gb[e % NB]
            k.dma("pool", b[:], v_d[:, :], fn=lambda b=b, e=e: nc.gpsimd.indirect_dma_start(
                out=b[:], out_offset=None, in_=v_d[:, :], in_offset=bass.IndirectOffsetOnAxis(ap=eid[:, e:e + 1], axis=0)),
                extra_reads=[eid])
            if e == 0:
                k.op("dve", lambda b=b, e=e: V.tensor_scalar_mul(out=acc[:], in0=b[:], scalar1=wgt[:, 0:1]), reads=[b, wgt], writes=[acc])
            else:
                k.op("dve", lambda b=b, e=e: V.scalar_tensor_tensor(out=acc[:], in0=b[:], scalar=wgt[:, e:e + 1], in1=acc[:],
                                                                    op0=ALU.mult, op1=ALU.add), reads=[b, wgt, acc], writes=[acc])
        k.op("dve", lambda: V.tensor_tensor(out=acc[:], in0=acc[:], in1=modt[2][:], op=ALU.mult), reads=[acc, modt[2]], writes=[acc])
        k.op("dve", lambda: V.scalar_tensor_tensor(out=acc[:], in0=x1t[:], scalar=ALPHA, in1=acc[:], op0=ALU.mult, op1=ALU.add),
             reads=[x1t, acc], writes=[acc])
        _layernorm_tile(k, acc, junk, stats, mv, rstd, epst)
        k.op("dve", lambda: V.tensor_tensor(out=acc[:], in0=acc[:], in1=lng[:], op=ALU.mult), reads=[acc, lng], writes=[acc])
        k.op("dve", lambda: V.tensor_tensor(out=junk[:], in0=acc[:], in1=lnb[:], op=ALU.add), reads=[acc, lnb], writes=[junk])
        k.dma("sp", out_d[r0:r0 + n, :], junk[:n, :])
    return k.finish()


def build_P0():
    k = KB(); nc = k.nc
    cT_d = k.dram_in("cT", [128, 8, 2])
    w_d = k.dram_in("w", [128, 8, 3072])
    b_d = k.dram_in("b", [2, 3072])
    out_d = k.dram_out("mod", [2, 3072])
    cT = k.sb("cT_sb", [128, 8, 2]); sT = k.sb("sT", [128, 8, 2])
    w = k.sb("w_sb", [128, 8, 3072]); b = k.sb("b_sb", [2, 3072]); o = k.sb("o_sb", [2, 3072])
    ps = [k.ps(f"ps{j}", [2, 512]) for j in range(2)]
    k.dma("sp", cT[:], cT_d[:, :, :]); k.dma("sp", b[:], b_d[:, :])
    for kc in range(8):
        k.dma("sp", w[:, kc, :], w_d[:, kc, :])
    k.op("act", lambda: nc.scalar.activation(out=sT[:], in_=cT[:], func=AF.Silu), reads=[cT], writes=[sT])
    for j in range(6):
        p = ps[j % 2]
        for kc in range(8):
            k.op("pe", lambda p=p, j=j, kc=kc: nc.tensor.matmul(p[:, :], sT[:, kc, :], w[:, kc, j * 512:(j + 1) * 512], start=(kc == 0), stop=(kc == 7)),
                 reads=[sT, w], writes=[p], same_ok=True)
        k.op("dve", lambda p=p, j=j: nc.vector.tensor_tensor(out=o[:, j * 512:(j + 1) * 512], in0=p[:, :], in1=b[:, j * 512:(j + 1) * 512], op=ALU.add),
             reads=[p, b], writes=[o])
    k.dma("sp", out_d[:, :], o[:])
    return k.finish()


def build_A_ml(NXc, NC):
    k = KB(); nc = k.nc; V = nc.vector
    NXE = NXc + 128
    R = NXc // 64
    NT = NXE + NC
    xT_d = k.dram_in("xT", [128, 8, NT])
    mod_d = k.dram_in("mod", [128, 2, 8, 2])
    w_d = k.dram_in("w_in", [128, 8, 4112])
    b_d = k.dram_in("b_in", [128, 33])
    cw_d = k.dram_in("conv_w", [128, 16, 9])
    cb_d = k.dram_in("conv_b", [128, 16])
    hm_d = k.dram_in("hmask", [128, 2])
    NO = NXc + NC
    q_d = k.dram_out("qk", [16, 128, NO])
    v_d = k.dram_out("v", [8, 128, NO])
    o_d = k.dram_out("o", [8, 128, NO])
    g_d = k.dram_out("g", [16, NO])

    hT = k.sb("hT", [128, 8, NT])
    mod = k.sb("mod_sb", [128, 2, 8, 2]); bsb = k.sb("b_sb", [128, 33]); cw = k.sb("cw", [128, 16, 9]); cb = k.sb("cb", [128, 16])
    hm = k.sb("hm", [128, 2])
    wbuf = [k.sb(f"wb{j}", [128, 8, 128]) for j in range(3)]
    pbuf = [k.sb(f"pbuf{j}", [128, NT]) for j in range(2)]
    cbuf = [k.sb(f"cbuf{j}", [128, NO]) for j in range(2)]
    obuf = [k.sb(f"obuf{j}", [128, NO]) for j in range(2)]
    psb = [k.ps(f"ps{j}", [128, 512]) for j in range(4)]
    for kc in range(8):
        k.dma("sp", hT[:, kc, :], xT_d[:, kc, :])
    k.dma("sp", mod[:], mod_d[:, :, :, :]); k.dma("sp", bsb[:], b_d[:, :]); k.dma("sp", cw[:], cw_d[:, :, :])
    k.dma("sp", cb[:], cb_d[:, :]); k.dma("sp", hm[:], hm_d[:, :])
    k.op("dve", lambda: V.tensor_scalar_add(out=mod[:, :, :, 1], in0=mod[:, :, :, 1], scalar1=1.0), reads=[mod], writes=[mod])
    for kc in range(8):
        k.op("dve", lambda kc=kc: V.tensor_scalar(out=hT[:, kc, 0:NXE], in0=hT[:, kc, 0:NXE], scalar1=mod[:, 0, kc, 1:2], scalar2=mod[:, 0, kc, 0:1],
                                                  op0=ALU.mult, op1=ALU.add), reads=[hT, mod], writes=[hT])
        k.op("dve", lambda kc=kc: V.tensor_scalar(out=hT[:, kc, NXE:NT], in0=hT[:, kc, NXE:NT], scalar1=mod[:, 1, kc, 1:2], scalar2=mod[:, 1, kc, 0:1],
                                                  op0=ALU.mult, op1=ALU.add), reads=[hT, mod], writes=[hT])
    blocks = []
    t0 = 0
    while t0 < NT:
        blocks.append((t0, min(512, NT - t0))); t0 += 512
    pi = 0
    for oc in range(33):
        M = 128 if oc < 32 else 16
        wb = wbuf[oc % 3]
        k.dma("sp", wb[:, :, 0:M], w_d[:, :, oc * 128:oc * 128 + M])
        pb = pbuf[oc % 2]
        for (b0, bn) in blocks:
            p = psb[pi % 4]; pi += 1
            for kc in range(8):
                k.op("pe", lambda p=p, wb=wb, kc=kc, b0=b0, bn=bn, M=M: nc.tensor.matmul(p[0:M, 0:bn], wb[:, kc, 0:M], hT[:, kc, b0:b0 + bn],
                                                                                      start=(kc == 0), stop=(kc == 7)),
                     reads=[wb, hT], writes=[p], same_ok=True)
            fn = AF.Sigmoid if 24 <= oc < 32 else AF.Identity
            k.op("act", lambda p=p, pb=pb, b0=b0, bn=bn, M=M, oc=oc, fn=fn: nc.scalar.activation(out=pb[0:M, b0:b0 + bn], in_=p[0:M, 0:bn], func=fn,
                                                                                           bias=bsb[0:M, oc:oc + 1], scale=1.0),
                 reads=[p, bsb], writes=[pb])
        if oc < 16:
            k.op("dve", lambda pb=pb: V.tensor_scalar_mul(out=pb[:, 0:64], in0=pb[:, 0:64], scalar1=hm[:, 0:1]), reads=[pb, hm], writes=[pb])
            k.op("dve", lambda pb=pb: V.tensor_scalar_mul(out=pb[:, NXE - 64:NXE], in0=pb[:, NXE - 64:NXE], scalar1=hm[:, 1:2]), reads=[pb, hm], writes=[pb])
            cbf = cbuf[oc % 2]
            pg = pb[:, 0:NXE].rearrange("p (r c) -> p r c", c=64)
            cg = cbf[:, 0:NXc].rearrange("p (r c) -> p r c", c=64)
            k.op("dve", lambda pg=pg, cg=cg, oc=oc: V.tensor_scalar(out=cg, in0=pg[:, 1:R + 1, :], scalar1=cw[:, oc, 4:5], scalar2=cb[:, oc:oc + 1],
                                                                 op0=ALU.mult, op1=ALU.add), reads=[pb, cw, cb], writes=[cbf])
            for dr in range(3):
                for dc in range(3):
                    if dr == 1 and dc == 1:
                        continue
                    c0, c1 = (1, 64) if dc == 0 else ((0, 63) if dc == 2 else (0, 64))
                    k.op("dve", lambda pg=pg, cg=cg, oc=oc, dr=dr, dc=dc, c0=c0, c1=c1: V.scalar_tensor_tensor(
                        out=cg[:, :, c0:c1], in0=pg[:, dr:dr + R, c0 + dc - 1:c1 + dc - 1], scalar=cw[:, oc, dr * 3 + dc:dr * 3 + dc + 1],
                        in1=cg[:, :, c0:c1], op0=ALU.mult, op1=ALU.add), reads=[pb, cw, cbf], writes=[cbf])
            k.op("dve", lambda pb=pb, cbf=cbf, oc=oc: V.tensor_scalar(out=cbf[:, NXc:NO], in0=pb[:, NXE:NT], scalar1=cw[:, oc, 4:5], scalar2=cb[:, oc:oc + 1],
                                                                    op0=ALU.mult, op1=ALU.add), reads=[pb, cw, cb], writes=[cbf])
            k.op("dve", lambda pb=pb, cbf=cbf, oc=oc: V.scalar_tensor_tensor(out=cbf[:, NXc + 1:NO], in0=pb[:, NXE:NT - 1], scalar=cw[:, oc, 3:4],
                                                                           in1=cbf[:, NXc + 1:NO], op0=ALU.mult, op1=ALU.add), reads=[pb, cw, cbf], writes=[cbf])
            k.op("dve", lambda pb=pb, cbf=cbf, oc=oc: V.scalar_tensor_tensor(out=cbf[:, NXc:NO - 1], in0=pb[:, NXE + 1:NT], scalar=cw[:, oc, 5:6],
                                                                           in1=cbf[:, NXc:NO - 1], op0=ALU.mult, op1=ALU.add), reads=[pb, cw, cbf], writes=[cbf])
            ob = obuf[oc % 2]
            k.op("act", lambda ob=ob, cbf=cbf: nc.scalar.activation(out=ob[:], in_=cbf[:], func=AF.Silu), reads=[cbf], writes=[ob])
            if oc < 8:
                k.op("dve", lambda ob=ob: V.tensor_scalar_mul(out=ob[:], in0=ob[:], scalar1=1.0 / 16.0), reads=[ob], writes=[ob])
            k.dma("sp", q_d[oc], ob[:])
        elif oc < 32:
            dst = v_d if oc < 24 else o_d
            j = oc - 16 if oc < 24 else oc - 24
            k.dma("sp", dst[j][:, 0:NXc], pb[:, 64:64 + NXc])
            k.dma("sp", dst[j][:, NXc:NO], pb[:, NXE:NT])
        else:
            k.dma("sp", g_d[:, 0:NXc], pb[0:16, 64:64 + NXc])
            k.dma("sp", g_d[:, NXc:NO], pb[0:16, NXE:NT])
    return k.finish()


def build_B_ml(NCH):
    k = KB(); nc = k.nc; V = nc.vector
    qT_d = k.dram_in("qT", [128, 2, NCH * 128])
    kT_d = k.dram_in("kT", [128, 2, NCH * 128])
    k_d = k.dram_in("k", [128, NCH, 256])
    v_d = k.dram_in("v", [128, NCH, 257])
    ig_d = k.dram_in("ig", [128, NCH]); fg_d = k.dram_in("fg", [128, NCH])
    tri_d = k.dram_in("tri", [128, 128]); mk_d = k.dram_in("maskT", [128, 128]); ones_d = k.dram_in("ones", [128, 128])
    h_d = k.dram_out("h", [128, NCH, 256])
    ident = _consts(k)
    tri = k.sb("tri_sb", [128, 128]); mk = k.sb("mk_sb", [128, 128]); ones = k.sb("ones_sb", [128, 128])
    ig = k.sb("ig_sb", [128, NCH]); LF = k.sb("LF", [128, NCH])
    k.dma("sp", tri[:], tri_d[:, :]); k.dma("sp", mk[:], mk_d[:, :]); k.dma("sp", ones[:], ones_d[:, :])
    k.dma("sp", ig[:], ig_d[:, :]); k.dma("sp", LF[:], fg_d[:, :])
    k.op("act", lambda: nc.scalar.activation(out=LF[:], in_=LF[:], func=AF.Exp, scale=-1.0), reads=[LF], writes=[LF])
    k.op("act", lambda: nc.scalar.activation(out=LF[:], in_=LF[:], func=AF.Ln, bias=1.0, scale=1.0), reads=[LF], writes=[LF])
    k.op("dve", lambda: V.tensor_scalar_mul(out=LF[:], in0=LF[:], scalar1=-1.0), reads=[LF], writes=[LF])
    NS = 3
    qb = [k.sb(f"qb{j}", [128, 2, 128]) for j in range(NS)]
    kb = [k.sb(f"kb{j}", [128, 2, 128]) for j in range(NS)]
    kt = [k.sb(f"kt{j}", [128, 256]) for j in range(NS)]
    vb = [k.sb(f"vb{j}", [128, 257]) for j in range(NS)]
    hb = [k.sb(f"hb{j}", [128, 256]) for j in range(2)]
    Cst = [k.sb(f"Cst{j}", [128, 257]) for j in range(2)]
    LFb = k.sb("LFb", [128, 128]); DT = k.sb("DT", [128, 128]); EB = k.sb("EB", [128, 128]); ST = k.sb("ST", [128, 128])
    qs = k.sb("qs", [128, 2, 128]); ka = k.sb("ka", [128, 256])
    wcol = k.sb("wcol", [128, 1]); acol = k.sb("acol", [128, 1]); Gc = k.sb("Gc", [128, 1]); den = k.sb("den", [128, 1])
    psA = k.ps("psA", [128, 128]); psB = k.ps("psB", [128, 128]); psC = k.ps("psC", [128, 2]); psS = k.ps("psS", [128, 128])
    psN = k.ps("psN", [128, 257]); psU = [k.ps(f"psU{j}", [128, 257]) for j in range(2)]
    for j in range(2):
        k.op("pool", lambda j=j: nc.gpsimd.memset(Cst[j][:], 0.0), writes=[Cst[j]])

    def load(c):
        s = c % NS
        k.dma("sp", qb[s][:], qT_d[:, :, c * 128:(c + 1) * 128])
        k.dma("sp", kb[s][:], kT_d[:, :, c * 128:(c + 1) * 128])
        k.dma("sp", kt[s][:], k_d[:, c, :])
        k.dma("sp", vb[s][:], v_d[:, c, :])
    load(0)
    if NCH > 1:
        load(1)
    for c in range(NCH):
        s = c % NS
        if c + 2 < NCH:
            load(c + 2)
        k.op("dve", lambda c=c: V.tensor_scalar_mul(out=LFb[:], in0=ones[:], scalar1=LF[:, c:c + 1]), reads=[ones, LF], writes=[LFb])
        k.op("pe", lambda: nc.tensor.matmul(psA[:, :], LFb[:], tri[:], start=True, stop=False), reads=[LFb, tri], writes=[psA], same_ok=True)
        k.op("pe", lambda: nc.tensor.matmul(psA[:, :], ident[:], mk[:], start=False, stop=True), reads=[ident, mk], writes=[psA], same_ok=True)
        k.op("pe", lambda: nc.tensor.matmul(psB[:, :], LFb[:], tri[:], start=True, stop=True), reads=[LFb, tri], writes=[psB], same_ok=True)
        k.op("pe", lambda c=c: nc.tensor.matmul(psC[:, 0:1], tri[:], LF[:, c:c + 1], start=True, stop=True), reads=[tri, LF], writes=[psC], same_ok=True)
        k.op("pe", lambda c=c: nc.tensor.matmul(psC[:, 1:2], ones[:], LF[:, c:c + 1], start=True, stop=True), reads=[ones, LF], writes=[psC], same_ok=True)
        k.op("dve", lambda c=c: V.tensor_tensor(out=wcol[:], in0=ig[:, c:c + 1], in1=psC[:, 0:1], op=ALU.subtract), reads=[ig, psC], writes=[wcol])
        k.op("act", lambda: nc.scalar.activation(out=DT[:], in_=psA[:, :], func=AF.Exp, bias=wcol[:, 0:1], scale=1.0), reads=[psA, wcol], writes=[DT])
        k.op("act", lambda: nc.scalar.activation(out=EB[:], in_=psB[:, :], func=AF.Exp), reads=[psB], writes=[EB])
        k.op("act", lambda: nc.scalar.activation(out=acol[:], in_=psC[:, 1:2], func=AF.Exp, bias=wcol[:, 0:1], scale=1.0), reads=[psC, wcol], writes=[acol])
        k.op("act", lambda: nc.scalar.activation(out=Gc[:], in_=psC[:, 1:2], func=AF.Exp), reads=[psC], writes=[Gc])
        for dc in range(2):
            k.op("pe", lambda s=s, dc=dc: nc.tensor.matmul(psS[:, :], kb[s][:, dc, :], qb[s][:, dc, :], start=(dc == 0), stop=(dc == 1)),
                 reads=[kb[s], qb[s]], writes=[psS], same_ok=True)
        k.op("dve", lambda: V.tensor_tensor(out=ST[:], in0=psS[:, :], in1=DT[:], op=ALU.mult), reads=[psS, DT], writes=[ST])
        k.op("dve", lambda s=s: V.tensor_tensor(out=qs[:], in0=qb[s][:], in1=EB[:, None, :].broadcast_to([128, 2, 128]), op=ALU.mult),
             reads=[qb[s], EB], writes=[qs])
        k.op("pe", lambda s=s: nc.tensor.matmul(psN[:, :], ST[:], vb[s][:], start=True, stop=False), reads=[ST, vb[s]], writes=[psN], same_ok=True)
        for dc in range(2):
            k.op("pe", lambda dc=dc: nc.tensor.matmul(psN[:, :], qs[:, dc, :], Cst[dc][:], start=False, stop=(dc == 1)),
                 reads=[qs, Cst[dc]], writes=[psN], same_ok=True)
        k.op("act", lambda: nc.scalar.activation(out=den[:], in_=psN[:, 256:257], func=AF.Abs), reads=[psN], writes=[den])
        k.op("dve", lambda: V.tensor_scalar_max(out=den[:], in0=den[:], scalar1=1.0), reads=[den], writes=[den])
        k.op("dve", lambda: V.reciprocal(out=den[:], in_=den[:]), reads=[den], writes=[den])
        hbb = hb[c % 2]
        k.op("dve", lambda hbb=hbb: V.tensor_scalar_mul(out=hbb[:], in0=psN[:, 0:256], scalar1=den[:, 0:1]), reads=[psN, den], writes=[hbb])
        k.dma("sp", h_d[:, c, :], hbb[:])
        k.op("pool", lambda s=s: nc.gpsimd.tensor_scalar(out=ka[:], in0=kt[s][:], scalar1=acol[:, 0:1], scalar2=None, op0=ALU.mult),
             reads=[kt[s], acol], writes=[ka])
        for dc in range(2):
            k.op("pe", lambda s=s, dc=dc: nc.tensor.matmul(psU[dc][:, :], ka[:, dc * 128:(dc + 1) * 128], vb[s][:], start=True, stop=True),
                 reads=[ka, vb[s]], writes=[psU[dc]], same_ok=True)
            k.op("dve", lambda dc=dc: V.scalar_tensor_tensor(out=Cst[dc][:], in0=Cst[dc][:], scalar=Gc[:, 0:1], in1=psU[dc][:, :], op0=ALU.mult, op1=ALU.add),
                 reads=[Cst[dc], Gc, psU[dc]], writes=[Cst[dc]])
    return k.finish()


def build_C1(kind, NX, NCTX):
    k = KB(); nc = k.nc; V = nc.vector
    TOK = NX + NCTX
    ml = kind == "ml"
    H, dh, eps = (4, 256, 1e-5) if ml else (16, 64, 64e-5)
    NPIECE = 3 if ml else 6
    NPRM = 1 if ml else 3
    xres_d = k.dram_in("xres", [TOK, D])
    pc_d = [k.dram_in(f"piece{j}", [TOK, D]) for j in range(NPIECE)]
    prm_d = k.dram_in("prm", [NPRM, 128, D])
    mod_d = k.dram_in("mod", [2, 128, D])
    lnp_d = k.dram_in("lnp", [2, 128, D])
    w_d = k.dram_in("w_out", [128, 8, D])
    out_d = k.dram_out("x1", [TOK, D])
    ident = _consts(k)
    w = k.sb("w_sb", [128, 8, D]); prm = [k.sb(f"prm{j}", [128, D]) for j in range(NPRM)]
    g1 = k.sb("g1", [128, D]); lng = k.sb("lng", [128, D]); lnb = k.sb("lnb", [128, D])
    xr = k.sb("xr", [128, D]); A = k.sb("A", [128, D]); B = k.sb("B", [128, D]); C = k.sb("C", [128, D])
    zT = k.sb("zT", [128, 8, 128]); t1 = k.sb("t1", [128, D]); junk = k.sb("junk", [128, D])
    sm = k.sb("sm", [128, H]); vs = k.sb("vs", [128, H]); bs = k.sb("bs", [128, H])
    stats = k.sb("stats", [128, 12]); mv = k.sb("mv", [128, 2]); rstd = k.sb("rstd", [128, 1])
    epst = k.sb("epst", [128, 1]); epsh = k.sb("epsh", [128, 1])
    pbank = [k.ps(f"pb{j}", [128, 512]) for j in range(4)]
    k.op("pool", lambda: nc.gpsimd.memset(epst[:], LN_EPS), writes=[epst])
    k.op("pool", lambda: nc.gpsimd.memset(epsh[:], eps), writes=[epsh])
    for kc in range(8):
        k.dma("sp", w[:, kc, :], w_d[:, kc, :])
    for j in range(NPRM):
        k.dma("sp", prm[j][:], prm_d[j])
    k.dma("sp", lng[:], lnp_d[0]); k.dma("sp", lnb[:], lnp_d[1])
    tiles = [(i * 128, 128, 0) for i in range(NX // 128)]
    if NCTX:
        tiles.append((NX, NCTX, 1))
    cur_ty = None
    hv = lambda t: t[:].rearrange("p (h e) -> p h e", h=H)
    bc = lambda s: s[:, :, None].broadcast_to([128, H, dh])
    for (r0, n, ty) in tiles:
        if ty != cur_ty:
            k.dma("sp", g1[:], mod_d[ty]); cur_ty = ty
        rs = slice(r0, r0 + n)
        k.dma("sp", xr[:n, :], xres_d[rs, :])
        k.dma("sp", A[:n, :], pc_d[0][rs, :]); k.dma("sp", B[:n, :], pc_d[1][rs, :])
        k.op("dve", lambda: V.tensor_tensor(out=A[:], in0=A[:], in1=B[:], op=ALU.add), reads=[A, B], writes=[A])
        k.op("dve", lambda: V.tensor_reduce(out=sm[:], in_=hv(A), axis=mybir.AxisListType.X, op=ALU.add), reads=[A], writes=[sm])
        k.op("dve", lambda: V.tensor_scalar_mul(out=sm[:], in0=sm[:], scalar1=1.0 / dh), reads=[sm], writes=[sm])
        k.op("dve", lambda: V.tensor_tensor(out=hv(A), in0=hv(A), in1=bc(sm), op=ALU.subtract), reads=[A, sm], writes=[A])
        k.op("dve", lambda: V.tensor_tensor(out=junk[:], in0=A[:], in1=A[:], op=ALU.mult), reads=[A], writes=[junk])
        k.op("dve", lambda: V.tensor_reduce(out=vs[:], in_=hv(junk), axis=mybir.AxisListType.X, op=ALU.add), reads=[junk], writes=[vs])
        k.op("act", lambda: nc.scalar.activation(out=vs[:], in_=vs[:], func=AF.Sqrt, bias=epsh[:, 0:1], scale=1.0 / dh), reads=[vs, epsh], writes=[vs])
        k.op("dve", lambda: V.reciprocal(out=vs[:], in_=vs[:]), reads=[vs], writes=[vs])
        k.op("dve", lambda: V.tensor_tensor(out=hv(A), in0=hv(A), in1=bc(vs), op=ALU.mult), reads=[A, vs], writes=[A])
        if ml:
            k.dma("sp", C[:n, :], pc_d[2][rs, :])
            k.op("dve", lambda: V.tensor_tensor(out=A[:], in0=A[:], in1=C[:], op=ALU.mult), reads=[A, C], writes=[A])
            k.op("dve", lambda: V.tensor_tensor(out=A[:], in0=A[:], in1=prm[0][:], op=ALU.mult), reads=[A, prm[0]], writes=[A])
        else:
            k.op("dve", lambda: V.tensor_tensor(out=A[:], in0=A[:], in1=prm[0][:], op=ALU.mult), reads=[A, prm[0]], writes=[A])
            k.op("dve", lambda: V.tensor_tensor(out=A[:], in0=A[:], in1=prm[1][:], op=ALU.add), reads=[A, prm[1]], writes=[A])
            k.dma("sp", B[:n, :], pc_d[2][rs, :]); k.dma("sp", C[:n, :], pc_d[3][rs, :])
            k.op("dve", lambda: V.tensor_tensor(out=B[:], in0=B[:], in1=C[:], op=ALU.mult), reads=[B, C], writes=[B])
            k.op("dve", lambda: V.tensor_tensor(out=B[:], in0=B[:], in1=prm[2][:], op=ALU.mult), reads=[B, prm[2]], writes=[B])
            k.op("dve", lambda: V.tensor_reduce(out=bs[:], in_=hv(B), axis=mybir.AxisListType.X, op=ALU.add), reads=[B], writes=[bs])
            k.dma("sp", C[:n, :], pc_d[4][rs, :])
            k.op("dve", lambda: V.tensor_tensor(out=hv(C), in0=hv(C), in1=bc(bs), op=ALU.mult), reads=[C, bs], writes=[C])
            k.op("dve", lambda: V.tensor_tensor(out=A[:], in0=A[:], in1=C[:], op=ALU.add), reads=[A, C], writes=[A])
            k.dma("sp", B[:n, :], pc_d[5][rs, :])
            k.op("dve", lambda: V.tensor_tensor(out=A[:], in0=A[:], in1=B[:], op=ALU.mult), reads=[A, B], writes=[A])
        for half in range(2):
            pb = pbank[half]
            for j in range(4):
                kc = half * 4 + j
                k.op("pe", lambda pb=pb, j=j, kc=kc: nc.tensor.transpose(out=pb[:, j * 128:(j + 1) * 128], in_=A[:, kc * 128:(kc + 1) * 128], identity=ident[:]),
                     reads=[A, ident], writes=[pb], same_ok=True)
            k.op("act", lambda pb=pb, half=half: nc.scalar.copy(out=zT[:, half * 4:(half + 1) * 4, :].rearrange("p a b -> p (a b)"), in_=pb[:, :]),
                 reads=[pb], writes=[zT])
        for half in range(2):
            pb = pbank[2 + half]
            for kc in range(8):
                k.op("pe", lambda pb=pb, kc=kc, half=half: nc.tensor.matmul(pb[:, :], zT[:, kc, :], w[:, kc, half * 512:(half + 1) * 512],
                                                                          start=(kc == 0), stop=(kc == 7)), reads=[zT, w], writes=[pb], same_ok=True)
            k.op("dve", lambda pb=pb, half=half: V.tensor_tensor(out=t1[:, half * 512:(half + 1) * 512], in0=pb[:, :], in1=g1[:, half * 512:(half + 1) * 512],
                                                                 op=ALU.mult), reads=[pb, g1], writes=[t1])
        k.op("dve", lambda: V.scalar_tensor_tensor(out=t1[:], in0=xr[:], scalar=ALPHA, in1=t1[:], op0=ALU.mult, op1=ALU.add), reads=[xr, t1], writes=[t1])
        _layernorm_tile(k, t1, junk, stats, mv, rstd, epst)
        k.op("dve", lambda: V.tensor_tensor(out=t1[:], in0=t1[:], in1=lng[:], op=ALU.mult), reads=[t1, lng], writes=[t1])
        k.op("dve", lambda: V.tensor_tensor(out=junk[:], in0=t1[:], in1=lnb[:], op=ALU.add), reads=[t1, lnb], writes=[junk])
        k.dma("sp", out_d[rs, :], junk[:n, :])
    return k.finish()


NCORES = 8
_PROGS = {}


def _run(key, builder, in_maps):
    if key not in _PROGS:
        _PROGS[key] = builder()
    res = run_bass_kernel_spmd(_PROGS[key], in_maps, core_ids=list(range(len(in_maps))))
    return res.results


def _f(a):
    return np.ascontiguousarray(a, dtype=np.float32)


def _bc(v):
    return _f(np.broadcast_to(np.asarray(v, np.float32), (128, v.shape[-1])))


def _kc_layout(w):
    return _f(w.reshape(8, 128, -1).transpose(1, 0, 2))


def _fm(vec):
    return _f(vec.reshape(-1, 128).T)


_IDENT = np.eye(128, dtype=np.float32)


def run_P0(c, c_ctx, ada_w, ada_b):
    depth = ada_w.shape[0]
    cc = np.stack([c.reshape(-1), c_ctx.reshape(-1)], axis=1)
    cT = _f(cc.reshape(8, 128, 2).transpose(1, 0, 2))
    in_maps = []
    for core in range(NCORES):
        i, half = core // 2, core % 2
        i = min(i, depth - 1)
        sl = slice(half * 3072, (half + 1) * 3072)
        in_maps.append({"cT": cT, "w": _kc_layout(ada_w[i][:, sl]), "b": _f(np.stack([ada_b[i][sl], ada_b[i][sl]]))})
    res = _run("P0", build_P0, in_maps)
    mods = np.zeros((depth, 2, 6144), np.float32)
    for core in range(2 * depth):
        i, half = core // 2, core % 2
        mods[i][:, half * 3072:(half + 1) * 3072] = res[core]["mod"]
    return mods.reshape(depth, 2, 6, 1024)


def run_C1(kind, x, ctx, pieces_x, pieces_c, prm, g1x, g1c, ln_g, ln_b, w_out):
    NX, NC = x.shape[0], ctx.shape[0]
    nxc, ncc = NX // NCORES, NC // NCORES
    common = {"prm": _f(np.stack([_bc(p) for p in prm])), "mod": _f(np.stack([_bc(g1x), _bc(g1c)])),
              "lnp": _f(np.stack([_bc(ln_g), _bc(ln_b)])), "w_out": _kc_layout(w_out), "ident": _IDENT}
    in_maps = []
    for c in range(NCORES):
        m = dict(common)
        m["xres"] = _f(np.concatenate([x[c * nxc:(c + 1) * nxc], ctx[c * ncc:(c + 1) * ncc]]))
        for j, (px, pc) in enumerate(zip(pieces_x, pieces_c)):
            m[f"piece{j}"] = _f(np.concatenate([px[c * nxc:(c + 1) * nxc], pc[c * ncc:(c + 1) * ncc]]))
        in_maps.append(m)
    res = _run(("C1", kind, nxc, ncc), lambda: build_C1(kind, nxc, ncc), in_maps)
    x1 = np.concatenate([r["x1"][:nxc] for r in res]); c1 = np.concatenate([r["x1"][nxc:] for r in res])
    return x1, c1


def run_C2(x1, c1, modx, modc, ln_g, ln_b, wq, keys, u_tab, v_tab):
    NX, NC = x1.shape[0], c1.shape[0]
    nxc, ncc = NX // NCORES, NC // NCORES
    common = {"mod": _f(np.stack([np.stack([_bc(m) for m in modx]), np.stack([_bc(m) for m in modc])])),
              "lnp": _f(np.stack([_bc(ln_g), _bc(ln_b)])), "wq": _kc_layout(wq),
              "keysT": _f(keys.reshape(16, 128, 128).transpose(2, 0, 1)),
              "iota": _f(np.broadcast_to(np.arange(256, dtype=np.float32), (128, 256))),
              "u_tab": _f(u_tab), "v_tab": _f(v_tab), "ident": _IDENT}
    in_maps = []
    for c in range(NCORES):
        m = dict(common)
        m["x1"] = _f(np.concatenate([x1[c * nxc:(c + 1) * nxc], c1[c * ncc:(c + 1) * ncc]]))
        in_maps.append(m)
    res = _run(("C2", nxc, ncc), lambda: build_C2(nxc, ncc), in_maps)
    x2 = np.concatenate([r["xout"][:nxc] for r in res]); c2 = np.concatenate([r["xout"][nxc:] for r in res])
    return x2, c2


def run_mlstm(x, ctx, modx, modc, w_in, b_in, conv_w, conv_b):
    NX, NC = x.shape[0], ctx.shape[0]
    nxc = NX // NCORES
    xpad = np.concatenate([np.zeros((64, D), np.float32), x, np.zeros((64, D), np.float32)])
    mod = np.zeros((128, 2, 8, 2), np.float32)
    for ty, m in enumerate((modx, modc)):
        mod[:, ty, :, 0] = _fm(m[0]); mod[:, ty, :, 1] = _fm(m[1])
    b33 = np.zeros((128, 33), np.float32)
    b33[:, :32] = _fm(b_in[:4096]); b33[:16, 32] = b_in[4096:]
    common = {"mod": mod, "w_in": _kc_layout(w_in), "b_in": b33,
              "conv_w": _f(conv_w.reshape(9, 16, 128).transpose(2, 1, 0)), "conv_b": _fm(conv_b)}
    in_maps = []
    for c in range(NCORES):
        win = np.concatenate([xpad[c * nxc:c * nxc + nxc + 128], ctx])
        m = dict(common)
        m["xT"] = _f(win.T.reshape(8, 128, -1).transpose(1, 0, 2))
        hm = np.ones((128, 2), np.float32)
        if c == 0:
            hm[:, 0] = 0
        if c == NCORES - 1:
            hm[:, 1] = 0
        m["hmask"] = hm
        in_maps.append(m)
    res = _run(("A_ml", nxc, NC), lambda: build_A_ml(nxc, NC), in_maps)

    def gather(name, nfeat):
        xs = np.concatenate([r[name].reshape(nfeat, -1)[:, :nxc] for r in res], axis=1)
        cs = res[0][name].reshape(nfeat, -1)[:, nxc:]
        return xs, cs
    qk_x, qk_c = gather("qk", 2048); v_x, v_c = gather("v", 1024); o_x, o_c = gather("o", 1024); g_x, g_c = gather("g", 16)
    T = NC + NX
    NCH = T // 128
    tri = _f(np.triu(np.ones((128, 128), np.float32)))
    maskT = _f(np.where(np.triu(np.ones((128, 128))) > 0, 0.0, -30000.0))
    consts = {"tri": tri, "maskT": maskT, "ones": np.ones((128, 128), np.float32), "ident": _IDENT}
    in_maps = []
    for core in range(NCORES):
        h, d = core % 4, core // 4

        def seqT(ax, ac):
            if d == 0:
                return np.concatenate([ac, ax], axis=1)
            return np.concatenate([ac[:, ::-1], ax[:, ::-1]], axis=1)
        hs = slice(h * 256, (h + 1) * 256)
        qT = seqT(qk_x[hs], qk_c[hs]); kT = seqT(qk_x[1024:][hs], qk_c[1024:][hs]); vT = seqT(v_x[hs], v_c[hs])
        ig = seqT(g_x[d * 4 + h][None], g_c[d * 4 + h][None])[0]
        fg = seqT(g_x[8 + d * 4 + h][None], g_c[8 + d * 4 + h][None])[0]
        m = dict(consts)
        m["qT"] = _f(qT.reshape(2, 128, T).transpose(1, 0, 2)); m["kT"] = _f(kT.reshape(2, 128, T).transpose(1, 0, 2))
        m["k"] = _f(kT.T.reshape(NCH, 128, 256).transpose(1, 0, 2))
        vext = np.concatenate([vT.T, np.ones((T, 1), np.float32)], axis=1)
        m["v"] = _f(vext.reshape(NCH, 128, 257).transpose(1, 0, 2))
        m["ig"] = _f(ig.reshape(NCH, 128).T); m["fg"] = _f(fg.reshape(NCH, 128).T)
        in_maps.append(m)
    res = _run(("B_ml", NCH), lambda: build_B_ml(NCH), in_maps)
    hf = np.zeros((T, D), np.float32); hb = np.zeros((T, D), np.float32)
    for core in range(NCORES):
        h, d = core % 4, core // 4
        hh = res[core]["h"].transpose(1, 0, 2).reshape(T, 256)
        if d == 0:
            hf[:, h * 256:(h + 1) * 256] = hh
        else:
            hb[:NC, h * 256:(h + 1) * 256] = hh[:NC][::-1]
            hb[NC:, h * 256:(h + 1) * 256] = hh[NC:][::-1]
    return (hf[NC:], hb[NC:], _f(o_x.T)), (hf[:NC], hb[:NC], _f(o_c.T))


def build_A_rw(NXc, NC):
    k = KB(); nc = k.nc; V = nc.vector
    NXE = NXc + 128
    NT = NXE + NC
    NO = NXc + NC
    BS = min(512, NXc)
    BW = max(BS, NC)
    xT_d = k.dram_in("xT", [128, 8, NT])
    mod_d = k.dram_in("mod", [128, 2, 8, 2])
    hm_d = k.dram_in("hmask", [128, 2])
    mu_d = k.dram_in("mu", [128, 6, 8])
    wrkv_d = k.dram_in("w_rkv", [3, 128, 8, D])
    w1_d = k.dram_in("w1", [4, 128, 8, 64])
    w2_d = k.dram_in("w2", [4, 64, D])
    g1_d = k.dram_in("g1", [128, 8, 160])
    g2a_d = k.dram_in("g2a", [128, D]); g2b_d = k.dram_in("g2b", [32, D])
    vec_d = k.dram_in("vecs", [128, 7, 8])
    bd_d = k.dram_in("bdones", [128, 128])
    names = ["r", "v", "kkneg", "b0", "b1", "ktil0", "ktil1", "logw0", "logw1", "kbar", "g"]
    outs = {n: k.dram_out(n, [8, 128, NO]) for n in names}

    mod = k.sb("mod_sb", [128, 2, 8, 2]); hm = k.sb("hm", [128, 2]); mu = k.sb("mu_sb", [128, 6, 8])
    w1 = [k.sb(f"w1_{j}", [128, 8, 64]) for j in range(4)]
    w2 = [k.sb(f"w2_{j}", [64, D]) for j in range(4)]
    g1 = k.sb("g1_sb", [128, 8, 160]); g2a = k.sb("g2a_sb", [128, D]); g2b = k.sb("g2b_sb", [32, D])
    vec = k.sb("vec_sb", [128, 7, 8]); bd = k.sb("bd_sb", [128, 128]); omka = k.sb("omka", [128, 8])
    hTb = k.sb("hTb", [128, 8, BW + 128]); sTb = k.sb("sTb", [128, 8, BW]); xm = k.sb("xm", [128, 8, BW])
    kT = k.sb("kT", [128, 8, BW]); kk = k.sb("kk", [128, 8, BW])
    th = [k.sb(f"th{j}", [64, BW]) for j in range(2)]
    gs0 = k.sb("gs0", [128, BW]); gs1 = k.sb("gs1", [32, BW])
    wbuf = [k.sb(f"wb{j}", [128, 8, 128]) for j in range(3)]
    ob = [k.sb(f"ob{j}", [128, BW]) for j in range(6)]
    tmp = [k.sb(f"tmp{j}", [128, BW]) for j in range(3)]
    psb = [k.ps(f"ps{j}", [128, 512]) for j in range(6)]
    cnt = {"ps": 0, "ob": 0, "wb": 0}

    def nps():
        cnt["ps"] += 1; return psb[cnt["ps"] % 6]

    def nob():
        cnt["ob"] += 1; return ob[cnt["ob"] % 6]

    k.dma("sp", mod[:], mod_d[:, :, :, :]); k.dma("sp", hm[:], hm_d[:, :]); k.dma("sp", mu[:], mu_d[:, :, :])
    for j in range(4):
        k.dma("sp", w1[j][:], w1_d[j]); k.dma("sp", w2[j][:], w2_d[j])
    k.dma("sp", g1[:], g1_d[:, :, :]); k.dma("sp", g2a[:], g2a_d[:, :]); k.dma("sp", g2b[:], g2b_d[:, :])
    k.dma("sp", vec[:], vec_d[:, :, :]); k.dma("sp", bd[:], bd_d[:, :])
    k.op("dve", lambda: V.tensor_scalar_add(out=mod[:, :, :, 1], in0=mod[:, :, :, 1], scalar1=1.0), reads=[mod], writes=[mod])
    k.op("dve", lambda: V.tensor_scalar(out=omka[:], in0=vec[:, 5, :], scalar1=-1.0, scalar2=1.0, op0=ALU.mult, op1=ALU.add), reads=[vec], writes=[omka])

    blocks = [(b0, BS, 0) for b0 in range(0, NXc, BS)]
    if NC:
        assert NC <= 512
        blocks.append((0, NC, 1))
    for (b0, bs, ty) in blocks:
        off = 64 if ty == 0 else 0
        wn = bs + 128 if ty == 0 else bs
        src0 = b0 if ty == 0 else NXE
        o0 = b0 if ty == 0 else NXc
        for kc in range(8):
            k.dma("sp", hTb[:, kc, 0:wn], xT_d[:, kc, src0:src0 + wn])
        for kc in range(8):
            k.op("dve", lambda kc=kc: V.tensor_scalar(out=hTb[:, kc, 0:wn], in0=hTb[:, kc, 0:wn], scalar1=mod[:, ty, kc, 1:2], scalar2=mod[:, ty, kc, 0:1],
                                                      op0=ALU.mult, op1=ALU.add), reads=[hTb, mod], writes=[hTb])
        k.op("pool", lambda: nc.gpsimd.memset(sTb[:], 0.0), writes=[sTb])
        if ty == 0:
            if b0 == 0:
                k.op("dve", lambda: V.tensor_scalar_mul(out=hTb[:, :, 0:64], in0=hTb[:, :, 0:64], scalar1=hm[:, 0:1]), reads=[hTb, hm], writes=[hTb])
            if b0 + bs == NXc:
                k.op("dve", lambda: V.tensor_scalar_mul(out=hTb[:, :, bs + 64:bs + 128], in0=hTb[:, :, bs + 64:bs + 128], scalar1=hm[:, 1:2]),
                     reads=[hTb, hm], writes=[hTb])
            hg = lambda kc0, kc1, lo: hTb[:, kc0:kc1, lo:lo + bs].rearrange("p k (r c) -> p k r c", c=64)
            sg = sTb[:, :, 0:bs].rearrange("p k (r c) -> p k r c", c=64)
            for kc in range(2):
                k.op("dve", lambda kc=kc: V.tensor_copy(out=sg[:, kc, :, 1:64], in_=hg(kc, kc + 1, 64)[:, 0, :, 0:63]), reads=[hTb], writes=[sTb])
                k.op("dve", lambda kc=kc: V.tensor_copy(out=sg[:, 2 + kc, :, 0:63], in_=hg(2 + kc, 3 + kc, 64)[:, 0, :, 1:64]), reads=[hTb], writes=[sTb])
            k.op("dve", lambda: V.tensor_copy(out=sTb[:, 4:6, 0:bs], in_=hTb[:, 4:6, 0:bs]), reads=[hTb], writes=[sTb])
            k.op("dve", lambda: V.tensor_copy(out=sTb[:, 6:8, 0:bs], in_=hTb[:, 6:8, 128:128 + bs]), reads=[hTb], writes=[sTb])
        else:
            k.op("dve", lambda: V.tensor_copy(out=sTb[:, 0:4, 1:bs], in_=hTb[:, 0:4, 0:bs - 1]), reads=[hTb], writes=[sTb])
            k.op("dve", lambda: V.tensor_copy(out=sTb[:, 4:8, 0:bs - 1], in_=hTb[:, 4:8, 1:bs]), reads=[hTb], writes=[sTb])
        hc = lambda kc: hTb[:, kc, off:off + bs]
        k.op("dve", lambda: V.tensor_tensor(out=sTb[:, :, 0:bs], in0=sTb[:, :, 0:bs], in1=hTb[:, :, off:off + bs], op=ALU.subtract), reads=[sTb, hTb], writes=[sTb])

        def mix(n):
            for kc in range(8):
                k.op("dve", lambda kc=kc: V.scalar_tensor_tensor(out=xm[:, kc, 0:bs], in0=sTb[:, kc, 0:bs], scalar=mu[:, n, kc:kc + 1], in1=hc(kc),
                                                                 op0=ALU.mult, op1=ALU.add), reads=[sTb, mu, hTb], writes=[xm])

        def proj(n, oc):
            cnt["wb"] += 1
            wb = wbuf[cnt["wb"] % 3]
            k.dma("sp", wb[:], wrkv_d[n][:, :, oc * 128:(oc + 1) * 128])
            p = nps()
            for kc in range(8):
                k.op("pe", lambda p=p, wb=wb, kc=kc: nc.tensor.matmul(p[:, 0:bs], wb[:, kc, :], xm[:, kc, 0:bs], start=(kc == 0), stop=(kc == 7)),
                     reads=[wb, xm], writes=[p], same_ok=True)
            return p

        def store(name, oc, t):
            k.dma("sp", outs[name][oc][:, o0:o0 + bs], t[:, 0:bs])

        mix(0)
        for oc in range(8):
            p = proj(0, oc); o = nob()
            k.op("act", lambda p=p, o=o: nc.scalar.copy(out=o[:, 0:bs], in_=p[:, 0:bs]), reads=[p], writes=[o])
            store("r", oc, o)
        mix(1)
        for oc in range(8):
            p = proj(1, oc)
            k.op("act", lambda p=p, oc=oc: nc.scalar.copy(out=kT[:, oc, 0:bs], in_=p[:, 0:bs]), reads=[p], writes=[kT])
            t0, t1 = tmp[0], tmp[1]
            k.op("dve", lambda oc=oc: V.tensor_scalar_mul(out=t0[:, 0:bs], in0=kT[:, oc, 0:bs], scalar1=vec[:, 4, oc:oc + 1]), reads=[kT, vec], writes=[t0])
            k.op("dve", lambda: V.tensor_tensor(out=t1[:, 0:bs], in0=t0[:, 0:bs], in1=t0[:, 0:bs], op=ALU.mult), reads=[t0], writes=[t1])
            p2 = nps()
            k.op("pe", lambda p2=p2: nc.tensor.matmul(p2[:, 0:bs], bd[:], t1[:, 0:bs], start=True, stop=True), reads=[bd, t1], writes=[p2], same_ok=True)
            k.op("act", lambda p2=p2: nc.scalar.activation(out=t1[:, 0:bs], in_=p2[:, 0:bs], func=AF.Sqrt), reads=[p2], writes=[t1])
            k.op("dve", lambda: V.tensor_scalar_max(out=t1[:, 0:bs], in0=t1[:, 0:bs], scalar1=1e-12), reads=[t1], writes=[t1])
            k.op("dve", lambda: V.reciprocal(out=t1[:, 0:bs], in_=t1[:, 0:bs]), reads=[t1], writes=[t1])
            k.op("dve", lambda oc=oc: V.tensor_tensor(out=kk[:, oc, 0:bs], in0=t0[:, 0:bs], in1=t1[:, 0:bs], op=ALU.mult), reads=[t0, t1], writes=[kk])
            o = nob()
            k.op("dve", lambda o=o, oc=oc: V.tensor_scalar_mul(out=o[:, 0:bs], in0=kk[:, oc, 0:bs], scalar1=-1.0), reads=[kk], writes=[o])
            store("kkneg", oc, o)
        mix(2)
        for oc in range(8):
            p = proj(2, oc); o = nob()
            k.op("act", lambda p=p, o=o: nc.scalar.copy(out=o[:, 0:bs], in_=p[:, 0:bs]), reads=[p], writes=[o])
            store("v", oc, o)

        def lora_in(j, dst, func):
            p = nps()
            for kc in range(8):
                k.op("pe", lambda p=p, kc=kc, j=j: nc.tensor.matmul(p[0:64, 0:bs], w1[j][:, kc, :], xm[:, kc, 0:bs], start=(kc == 0), stop=(kc == 7)),
                     reads=[w1[j], xm], writes=[p], same_ok=True)
            k.op("act", lambda p=p, dst=dst: nc.scalar.activation(out=dst[:, 0:bs], in_=p[0:64, 0:bs], func=func), reads=[p], writes=[dst])

        mix(3)
        for z in range(2):
            lora_in(z, th[z], AF.Tanh)
        for oc in range(8):
            for z in range(2):
                p = nps()
                k.op("pe", lambda p=p, z=z, oc=oc: nc.tensor.matmul(p[:, 0:bs], w2[z][:, oc * 128:(oc + 1) * 128], th[z][:, 0:bs], start=True, stop=True),
                     reads=[w2[z], th[z]], writes=[p], same_ok=True)
                o = nob()
                k.op("act", lambda p=p, o=o, z=z, oc=oc: nc.scalar.activation(out=o[:, 0:bs], in_=p[:, 0:bs], func=AF.Sigmoid, bias=vec[:, z, oc:oc + 1], scale=1.0),
                     reads=[p, vec], writes=[o])
                k.op("dve", lambda o=o: V.tensor_scalar_mul(out=o[:, 0:bs], in0=o[:, 0:bs], scalar1=-0.6065306597126334), reads=[o], writes=[o])
                store(f"logw{z}", oc, o)
        mix(4)
        for z in range(2):
            lora_in(2 + z, th[z], AF.Identity)
        for oc in range(8):
            kt_z = []
            for z in range(2):
                p = nps()
                k.op("pe", lambda p=p, z=z, oc=oc: nc.tensor.matmul(p[:, 0:bs], w2[2 + z][:, oc * 128:(oc + 1) * 128], th[z][:, 0:bs], start=True, stop=True),
                     reads=[w2[2 + z], th[z]], writes=[p], same_ok=True)
                asg = tmp[z]
                k.op("act", lambda p=p, asg=asg, z=z, oc=oc: nc.scalar.activation(out=asg[:, 0:bs], in_=p[:, 0:bs], func=AF.Sigmoid, bias=vec[:, 2 + z, oc:oc + 1], scale=1.0),
                     reads=[p, vec], writes=[asg])
                o = nob()
                k.op("dve", lambda o=o, asg=asg, oc=oc: V.tensor_tensor(out=o[:, 0:bs], in0=kk[:, oc, 0:bs], in1=asg[:, 0:bs], op=ALU.mult), reads=[kk, asg], writes=[o])
                store(f"b{z}", oc, o)
                k.op("dve", lambda asg=asg, oc=oc: V.tensor_scalar(out=asg[:, 0:bs], in0=asg[:, 0:bs], scalar1=vec[:, 5, oc:oc + 1], scalar2=omka[:, oc:oc + 1],
                                                                 op0=ALU.mult, op1=ALU.add), reads=[asg, vec, omka], writes=[asg])
                o2 = nob()
                k.op("dve", lambda o2=o2, asg=asg, oc=oc: V.tensor_tensor(out=o2[:, 0:bs], in0=asg[:, 0:bs], in1=kT[:, oc, 0:bs], op=ALU.mult), reads=[asg, kT], writes=[o2])
                store(f"ktil{z}", oc, o2)
                kt_z.append(o2)
            o3 = nob()
            k.op("dve", lambda o3=o3, a=kt_z[0], b=kt_z[1]: V.tensor_tensor(out=o3[:, 0:bs], in0=a[:, 0:bs], in1=b[:, 0:bs], op=ALU.add), reads=[kt_z[0], kt_z[1]], writes=[o3])
            k.op("dve", lambda o3=o3: V.tensor_scalar_mul(out=o3[:, 0:bs], in0=o3[:, 0:bs], scalar1=0.5), reads=[o3], writes=[o3])
            store("kbar", oc, o3)
        mix(5)
        for (m0, mn, dst) in ((0, 128, gs0), (128, 32, gs1)):
            p = nps()
            for kc in range(8):
                k.op("pe", lambda p=p, kc=kc, m0=m0, mn=mn: nc.tensor.matmul(p[0:mn, 0:bs], g1[:, kc, m0:m0 + mn], xm[:, kc, 0:bs], start=(kc == 0), stop=(kc == 7)),
                     reads=[g1, xm], writes=[p], same_ok=True)
            k.op("act", lambda p=p, dst=dst, mn=mn: nc.scalar.activation(out=dst[:, 0:bs], in_=p[0:mn, 0:bs], func=AF.Sigmoid), reads=[p], writes=[dst])
        for oc in range(8):
            p = nps()
            k.op("pe", lambda p=p, oc=oc: nc.tensor.matmul(p[:, 0:bs], g2a[:, oc * 128:(oc + 1) * 128], gs0[:, 0:bs], start=True, stop=False),
                 reads=[g2a, gs0], writes=[p], same_ok=True)
            k.op("pe", lambda p=p, oc=oc: nc.tensor.matmul(p[:, 0:bs], g2b[:, oc * 128:(oc + 1) * 128], gs1[:, 0:bs], start=False, stop=True),
                 reads=[g2b, gs1], writes=[p], same_ok=True)
            o = nob()
            k.op("act", lambda p=p, o=o: nc.scalar.copy(out=o[:, 0:bs], in_=p[:, 0:bs]), reads=[p], writes=[o])
            store("g", oc, o)
    return k.finish()


def build_B_rw(NCH):
    k = KB(); nc = k.nc; V = nc.vector
    NSC = 4
    T = NCH * 128
    tk_d = {n: k.dram_in(n, [128, NCH, NSC, 64]) for n in ("lw_t", "b_t", "k_t", "v_t")}
    ch_d = {n: k.dram_in(n, [64, NSC, T]) for n in ("r_c", "a_c", "b_c", "k_c")}
    tri_d = k.dram_in("tri", [128, 128]); tris_d = k.dram_in("tris", [128, 128])
    msu_d = k.dram_in("m_su", [128, 128]); msl_d = k.dram_in("m_sl", [128, 128]); miu_d = k.dram_in("m_iu", [128, 128])
    y_d = k.dram_out("y", [128, NCH, NSC, 64])
    ident = _consts(k)
    tri = k.sb("tri_sb", [128, 128]); tris = k.sb("tris_sb", [128, 128])
    msu = k.sb("msu", [128, 128]); msl = k.sb("msl", [128, 128]); miu = k.sb("miu", [128, 128])
    for t, d in ((tri, tri_d), (tris, tris_d), (msu, msu_d), (msl, msl_d), (miu, miu_d)):
        k.dma("sp", t[:], d[:, :])
    NS = 2
    tk = {n: [k.sb(f"{n}_s{j}", [128, NSC, 64]) for j in range(NS)] for n in tk_d}
    ch = {n: [k.sb(f"{n}_s{j}", [64, NSC, 128]) for j in range(NS)] for n in ch_d}
    Pinv = k.sb("Pinv", [128, NSC, 64]); PT = k.sb("PT", [64, NSC, 128]); PinvT = k.sb("PinvT", [64, NSC, 128]); Pm1T = k.sb("Pm1T", [64, NSC, 128])
    At = k.sb("At", [64, NSC, 128]); BtT = k.sb("BtT", [64, NSC, 128]); KtT = k.sb("KtT", [64, NSC, 128]); RtT = k.sb("RtT", [64, NSC, 128])
    Btok = k.sb("Btok", [128, NSC, 64]); Ktok = k.sb("Ktok", [128, NSC, 64])
    Nn = [k.sb(f"Nn{j}", [128, NSC, 128]) for j in range(2)]; NTt = [k.sb(f"NTt{j}", [128, NSC, 128]) for j in range(2)]
    X = k.sb("X", [128, NSC, 128]); XT = k.sb("XT", [128, NSC, 128])
    MakT = k.sb("MakT", [128, NSC, 128]); MrbT = k.sb("MrbT", [128, NSC, 128]); MrkT = k.sb("MrkT", [128, NSC, 128])
    W = k.sb("W", [128, NSC, 64]); U = k.sb("U", [128, NSC, 64]); Yb = [k.sb(f"Yb{j}", [128, NSC, 64]) for j in range(2)]
    Z = k.sb("Z", [64, NSC, 64]); Zt = k.sb("Zt", [64, NSC, 64])
    banks = [k.ps(f"bk{j}", [128, 512]) for j in range(8)]
    bi = [0]

    def bank():
        bi[0] += 1
        return banks[bi[0] % 8]
    k.op("pool", lambda: nc.gpsimd.memset(Z[:], 0.0), writes=[Z])

    def load(c):
        s = c % NS
        for n in tk_d:
            k.dma("sp", tk[n][s][:], tk_d[n][:, c, :, :])
        for n in ch_d:
            k.dma("sp", ch[n][s][:], ch_d[n][:, :, c * 128:(c + 1) * 128])
    load(0)
    bcm = lambda m: m[:, None, :].broadcast_to([128, NSC, 128])
    v3 = lambda b: b[:, :].rearrange("p (s t) -> p s t", s=NSC)
    v64 = lambda b: b[:, 0:NSC * 64].rearrange("p (s t) -> p s t", s=NSC)
    for c in range(NCH):
        s = c % NS
        if c + 1 < NCH:
            load(c + 1)
        LW, Bt_, Kt_, Vt = tk["lw_t"][s], tk["b_t"][s], tk["k_t"][s], tk["v_t"][s]
        Rc, Ac, Bc, Kc = ch["r_c"][s], ch["a_c"][s], ch["b_c"][s], ch["k_c"][s]
        pLP = bank()
        k.op("pe", lambda: nc.tensor.matmul(pLP[:, 0:256], tri[:], LW[:].rearrange("p s c -> p (s c)"), start=True, stop=True), reads=[tri, LW], writes=[pLP], same_ok=True)
        pLT = bank(); pL1 = bank()
        for sc in range(NSC):
            k.op("pe", lambda sc=sc: nc.tensor.matmul(pLT[0:64, sc * 128:(sc + 1) * 128], LW[:, sc, :], tri[:], start=True, stop=True), reads=[LW, tri], writes=[pLT], same_ok=True)
            k.op("pe", lambda sc=sc: nc.tensor.matmul(pL1[0:64, sc * 128:(sc + 1) * 128], LW[:, sc, :], tris[:], start=True, stop=True), reads=[LW, tris], writes=[pL1], same_ok=True)
        k.op("act", lambda: nc.scalar.activation(out=Pinv[:].rearrange("p s c -> p (s c)"), in_=pLP[:, 0:256], func=AF.Exp, scale=-1.0), reads=[pLP], writes=[Pinv])
        k.op("act", lambda: nc.scalar.activation(out=PT[:].rearrange("p s c -> p (s c)"), in_=pLT[0:64, :], func=AF.Exp), reads=[pLT], writes=[PT])
        k.op("act", lambda: nc.scalar.activation(out=PinvT[:].rearrange("p s c -> p (s c)"), in_=pLT[0:64, :], func=AF.Exp, scale=-1.0), reads=[pLT], writes=[PinvT])
        k.op("act", lambda: nc.scalar.activation(out=Pm1T[:].rearrange("p s c -> p (s c)"), in_=pL1[0:64, :], func=AF.Exp), reads=[pL1], writes=[Pm1T])
        k.op("dve", lambda: V.tensor_tensor(out=At[:], in0=Ac[:], in1=Pm1T[:], op=ALU.mult), reads=[Ac, Pm1T], writes=[At])
        k.op("dve", lambda: V.tensor_tensor(out=BtT[:], in0=Bc[:], in1=PinvT[:], op=ALU.mult), reads=[Bc, PinvT], writes=[BtT])
        k.op("dve", lambda: V.tensor_tensor(out=KtT[:], in0=Kc[:], in1=PinvT[:], op=ALU.mult), reads=[Kc, PinvT], writes=[KtT])
        k.op("dve", lambda: V.tensor_tensor(out=RtT[:], in0=Rc[:], in1=PT[:], op=ALU.mult), reads=[Rc, PT], writes=[RtT])
        k.op("pool", lambda: nc.gpsimd.tensor_tensor(out=Btok[:], in0=Bt_[:], in1=Pinv[:], op=ALU.mult), reads=[Bt_, Pinv], writes=[Btok])
        k.op("pool", lambda: nc.gpsimd.tensor_tensor(out=Ktok[:], in0=Kt_[:], in1=Pinv[:], op=ALU.mult), reads=[Kt_, Pinv], writes=[Ktok])
        for (L, Rr, dst, msk) in ((BtT, At, Nn[0], msu), (At, BtT, NTt[0], msl), (KtT, At, MakT, msu), (BtT, RtT, MrbT, miu), (KtT, RtT, MrkT, miu)):
            p = bank()
            for sc in range(NSC):
                k.op("pe", lambda p=p, L=L, Rr=Rr, sc=sc: nc.tensor.matmul(p[:, sc * 128:(sc + 1) * 128], L[:, sc, :], Rr[:, sc, :], start=True, stop=True),
                     reads=[L, Rr], writes=[p], same_ok=True)
            k.op("dve", lambda p=p, dst=dst, msk=msk: V.tensor_tensor(out=dst[:], in0=v3(p), in1=bcm(msk), op=ALU.mult), reads=[p, msk], writes=[dst])
        k.op("dve", lambda: V.tensor_tensor(out=X[:], in0=Nn[0][:], in1=bcm(ident), op=ALU.add), reads=[Nn[0], ident], writes=[X])
        k.op("dve", lambda: V.tensor_tensor(out=XT[:], in0=NTt[0][:], in1=bcm(ident), op=ALU.add), reads=[NTt[0], ident], writes=[XT])
        cur = 0
        for it in range(6):
            nxt = 1 - cur
            last = it == 5
            pN2 = bank()
            for sc in range(NSC):
                k.op("pe", lambda sc=sc, cur=cur, pN2=pN2: nc.tensor.matmul(pN2[:, sc * 128:(sc + 1) * 128], NTt[cur][:, sc, :], Nn[cur][:, sc, :], start=True, stop=True),
                     reads=[NTt[cur], Nn[cur]], writes=[pN2], same_ok=True)
            k.op("act", lambda pN2=pN2, nxt=nxt: nc.scalar.copy(out=Nn[nxt][:], in_=v3(pN2)), reads=[pN2], writes=[Nn[nxt]])
            if not last:
                pT2 = bank()
                for sc in range(NSC):
                    k.op("pe", lambda sc=sc, cur=cur, pT2=pT2: nc.tensor.matmul(pT2[:, sc * 128:(sc + 1) * 128], Nn[cur][:, sc, :], NTt[cur][:, sc, :], start=True, stop=True),
                         reads=[Nn[cur], NTt[cur]], writes=[pT2], same_ok=True)
                k.op("act", lambda pT2=pT2, nxt=nxt: nc.scalar.copy(out=NTt[nxt][:], in_=v3(pT2)), reads=[pT2], writes=[NTt[nxt]])
            pX = bank()
            for sc in range(NSC):
                k.op("pe", lambda sc=sc, nxt=nxt, pX=pX: nc.tensor.matmul(pX[:, sc * 128:(sc + 1) * 128], XT[:, sc, :], Nn[nxt][:, sc, :], start=True, stop=True),
                     reads=[XT, Nn[nxt]], writes=[pX], same_ok=True)
            if not last:
                pXT = bank()
                for sc in range(NSC):
                    k.op("pe", lambda sc=sc, nxt=nxt, pXT=pXT: nc.tensor.matmul(pXT[:, sc * 128:(sc + 1) * 128], X[:, sc, :], NTt[nxt][:, sc, :], start=True, stop=True),
                         reads=[X, NTt[nxt]], writes=[pXT], same_ok=True)
            k.op("dve", lambda pX=pX: V.tensor_tensor(out=X[:], in0=X[:], in1=v3(pX), op=ALU.add), reads=[X, pX], writes=[X])
            if not last:
                k.op("dve", lambda pXT=pXT: V.tensor_tensor(out=XT[:], in0=XT[:], in1=v3(pXT), op=ALU.add), reads=[XT, pXT], writes=[XT])
            cur = nxt
        pW = bank()
        for sc in range(NSC):
            k.op("pe", lambda sc=sc: nc.tensor.matmul(pW[:, sc * 64:(sc + 1) * 64], At[:, sc, :], Z[:, sc, :], start=True, stop=False), reads=[At, Z], writes=[pW], same_ok=True)
            k.op("pe", lambda sc=sc: nc.tensor.matmul(pW[:, sc * 64:(sc + 1) * 64], MakT[:, sc, :], Vt[:, sc, :], start=False, stop=True), reads=[MakT, Vt], writes=[pW], same_ok=True)
        k.op("act", lambda: nc.scalar.copy(out=W[:], in_=v64(pW)), reads=[pW], writes=[W])
        pU = bank()
        for sc in range(NSC):
            k.op("pe", lambda sc=sc: nc.tensor.matmul(pU[:, sc * 64:(sc + 1) * 64], X[:, sc, :], W[:, sc, :], start=True, stop=True), reads=[X, W], writes=[pU], same_ok=True)
        k.op("act", lambda: nc.scalar.copy(out=U[:], in_=v64(pU)), reads=[pU], writes=[U])
        pY = bank()
        for sc in range(NSC):
            k.op("pe", lambda sc=sc: nc.tensor.matmul(pY[:, sc * 64:(sc + 1) * 64], RtT[:, sc, :], Z[:, sc, :], start=True, stop=False), reads=[RtT, Z], writes=[pY], same_ok=True)
            k.op("pe", lambda sc=sc: nc.tensor.matmul(pY[:, sc * 64:(sc + 1) * 64], MrbT[:, sc, :], U[:, sc, :], start=False, stop=False), reads=[MrbT, U], writes=[pY], same_ok=True)
            k.op("pe", lambda sc=sc: nc.tensor.matmul(pY[:, sc * 64:(sc + 1) * 64], MrkT[:, sc, :], Vt[:, sc, :], start=False, stop=True), reads=[MrkT, Vt], writes=[pY], same_ok=True)
        yb = Yb[c % 2]
        k.op("act", lambda yb=yb: nc.scalar.copy(out=yb[:], in_=v64(pY)), reads=[pY], writes=[yb])
        k.dma("sp", y_d[:, c, :, :], yb[:])
        pZ = bank()
        for sc in range(NSC):
            k.op("pe", lambda sc=sc: nc.tensor.matmul(pZ[0:64, sc * 64:(sc + 1) * 64], Btok[:, sc, :], U[:, sc, :], start=True, stop=False), reads=[Btok, U], writes=[pZ], same_ok=True)
            k.op("pe", lambda sc=sc: nc.tensor.matmul(pZ[0:64, sc * 64:(sc + 1) * 64], Ktok[:, sc, :], Vt[:, sc, :], start=False, stop=True), reads=[Ktok, Vt], writes=[pZ], same_ok=True)
        k.op("dve", lambda: V.tensor_tensor(out=Zt[:], in0=Z[:], in1=pZ[0:64, 0:NSC * 64].rearrange("p (s t) -> p s t", s=NSC), op=ALU.add), reads=[Z, pZ], writes=[Zt])
        k.op("dve", lambda: V.tensor_tensor(out=Z[:], in0=Zt[:], in1=PT[:, :, 127:128].broadcast_to([64, NSC, 64]), op=ALU.mult), reads=[Zt, PT], writes=[Z])
    return k.finish()


def run_rwkv(x, ctx, modx, modc, P):
    NX, NC = x.shape[0], ctx.shape[0]
    nxc = NX // NCORES
    xpad = np.concatenate([np.zeros((64, D), np.float32), x, np.zeros((64, D), np.float32)])
    mod = np.zeros((128, 2, 8, 2), np.float32)
    for ty, m in enumerate((modx, modc)):
        mod[:, ty, :, 0] = _fm(m[0]); mod[:, ty, :, 1] = _fm(m[1])
    vecs = np.zeros((128, 7, 8), np.float32)
    for j, v in enumerate((P["w0"][0], P["w0"][1], P["a0"][0], P["a0"][1], P["k_k"], P["k_a"])):
        vecs[:, j, :] = _fm(v)
    bd = np.zeros((128, 128), np.float32); bd[:64, :64] = 1; bd[64:, 64:] = 1
    common = {"mod": mod, "mu": _f(np.stack([_fm(P["mu"][n]) for n in range(6)], axis=1)),
              "w_rkv": _f(np.stack([_kc_layout(P["w_rkv"][n]) for n in range(3)])),
              "w1": _f(np.stack([_kc_layout(P["w1"][0]), _kc_layout(P["w1"][1]), _kc_layout(P["a1"][0]), _kc_layout(P["a1"][1])])),
              "w2": _f(np.stack([P["w2"][0], P["w2"][1], P["a2"][0], P["a2"][1]])),
              "g1": _kc_layout(P["g1"]), "g2a": _f(P["g2"][:128]), "g2b": _f(P["g2"][128:]), "vecs": vecs, "bdones": bd}
    in_maps = []
    for c in range(NCORES):
        win = np.concatenate([xpad[c * nxc:c * nxc + nxc + 128], ctx])
        m = dict(common)
        m["xT"] = _f(win.T.reshape(8, 128, -1).transpose(1, 0, 2))
        hm = np.ones((128, 2), np.float32)
        if c == 0:
            hm[:, 0] = 0
        if c == NCORES - 1:
            hm[:, 1] = 0
        m["hmask"] = hm
        in_maps.append(m)
    res = _run(("A_rw", nxc, NC), lambda: build_A_rw(nxc, NC), in_maps)
    fmx, fmc = {}, {}
    for n in ["r", "v", "kkneg", "b0", "b1", "ktil0", "ktil1", "logw0", "logw1", "kbar", "g"]:
        fmx[n] = np.concatenate([r[n].reshape(1024, -1)[:, :nxc] for r in res], axis=1)
        fmc[n] = res[0][n].reshape(1024, -1)[:, nxc:]
    T = NC + NX
    NCH = T // 128
    iu = np.triu(np.ones((128, 128), np.float32)); su = np.triu(np.ones((128, 128), np.float32), 1)
    consts = {"tri": _f(iu), "tris": _f(su), "m_su": _f(su), "m_sl": _f(su.T), "m_iu": _f(iu), "ident": _IDENT}
    in_maps = []
    for core in range(NCORES):
        tkm = {n: [] for n in ("lw_t", "b_t", "k_t", "v_t")}
        chm = {n: [] for n in ("r_c", "a_c", "b_c", "k_c")}
        for j in range(4):
            sid = core * 4 + j
            hd, z = sid // 2, sid % 2
            hs = slice(hd * 64, (hd + 1) * 64)

            def seq(n):
                ax, ac = fmx[n][hs], fmc[n][hs]
                if z == 0:
                    return np.concatenate([ac, ax], axis=1)
                return np.concatenate([ac[:, ::-1], ax[:, ::-1]], axis=1)
            tkm["lw_t"].append(seq(f"logw{z}").T); tkm["b_t"].append(seq(f"b{z}").T); tkm["k_t"].append(seq(f"ktil{z}").T); tkm["v_t"].append(seq("v").T)
            chm["r_c"].append(seq("r")); chm["a_c"].append(seq("kkneg")); chm["b_c"].append(seq(f"b{z}")); chm["k_c"].append(seq(f"ktil{z}"))
        m = dict(consts)
        for n, lst in tkm.items():
            m[n] = _f(np.stack(lst).reshape(4, NCH, 128, 64).transpose(2, 1, 0, 3))
        for n, lst in chm.items():
            m[n] = _f(np.stack(lst).transpose(1, 0, 2))
        in_maps.append(m)
    res = _run(("B_rw", NCH), lambda: build_B_rw(NCH), in_maps)
    yf = np.zeros((T, D), np.float32); yb = np.zeros((T, D), np.float32)
    for core in range(NCORES):
        yy = res[core]["y"]
        for j in range(4):
            sid = core * 4 + j
            hd, z = sid // 2, sid % 2
            ys = yy[:, :, j, :].transpose(1, 0, 2).reshape(T, 64)
            if z == 0:
                yf[:, hd * 64:(hd + 1) * 64] = ys
            else:
                yb[:NC, hd * 64:(hd + 1) * 64] = ys[:NC][::-1]
                yb[NC:, hd * 64:(hd + 1) * 64] = ys[NC:][::-1]
    px = [yf[NC:], yb[NC:]] + [_f(fmx[n].T) for n in ("r", "kbar", "v", "g")]
    pc = [yf[:NC], yb[:NC]] + [_f(fmc[n].T) for n in ("r", "kbar", "v", "g")]
    return px, pc


def kernel(x, c, ctx, c_ctx, ada_w, ada_b, ln_g, ln_b,
           ml_w_in, ml_b_in, ml_conv_w, ml_conv_b, ml_hn_g, ml_w_out,
           rw_mu, rw_w_rkv, rw_w0, rw_w1, rw_w2, rw_a0, rw_a1, rw_a2, rw_g1, rw_g2,
           rw_k_k, rw_k_a, rw_r_k, rw_lnx_g, rw_lnx_b, rw_w_out,
           pk_wq, pk_keys, pk_u, pk_v):
    A = lambda a: np.asarray(a, dtype=np.float32)
    xs = A(x)[0]; cs = A(ctx)[0]
    depth = ada_w.shape[0]
    mods = run_P0(A(c), A(c_ctx), A(ada_w), A(ada_b))
    for i in range(depth):
        j = i // 2
        modx, modc = mods[i, 0], mods[i, 1]
        if i % 2 == 0:
            px, pc = run_mlstm(xs, cs, modx, modc, A(ml_w_in[j]), A(ml_b_in[j]), A(ml_conv_w[j]), A(ml_conv_b[j]))
            x1, c1 = run_C1("ml", xs, cs, list(px), list(pc), [A(ml_hn_g[j])], modx[2], modc[2], A(ln_g[i, 0]), A(ln_b[i, 0]), A(ml_w_out[j]))
        else:
            P = {"mu": A(rw_mu[j]), "w_rkv": A(rw_w_rkv[j]), "w0": A(rw_w0[j]), "w1": A(rw_w1[j]), "w2": A(rw_w2[j]),
                 "a0": A(rw_a0[j]), "a1": A(rw_a1[j]), "a2": A(rw_a2[j]), "g1": A(rw_g1[j]), "g2": A(rw_g2[j]),
                 "k_k": A(rw_k_k[j]), "k_a": A(rw_k_a[j])}
            px, pc = run_rwkv(xs, cs, modx, modc, P)
            x1, c1 = run_C1("rw", xs, cs, px, pc, [A(rw_lnx_g[j]), A(rw_lnx_b[j]), A(rw_r_k[j])], modx[2], modc[2],
                            A(ln_g[i, 0]), A(ln_b[i, 0]), A(rw_w_out[j]))
        xs, cs = run_C2(x1, c1, modx[3:6], modc[3:6], A(ln_g[i, 1]), A(ln_b[i, 1]), A(pk_wq[i]), A(pk_keys[i]), A(pk_u[i]), A(pk_v[i]))
    return np.ascontiguousarray(xs[None].astype(np.float32))
```

```python
import contextlib
import numpy as np
import concourse.bass as bass
import concourse.mybir as mybir
from concourse.alu_op_type import AluOpType as ALU
from concourse.bass_utils import run_bass_kernel_spmd

F32 = mybir.dt.float32
I32 = mybir.dt.int32
U32 = mybir.dt.uint32
AF = mybir.ActivationFunctionType


class KB:
    def __init__(self):
        self.nc = bass.Bass("TRN2", target_bir_lowering=False)
        nc = self.nc
        self.es = contextlib.ExitStack()
        self.es.enter_context(nc.cleanup_on_exit())
        self.engs = {"pe": nc.tensor, "dve": nc.vector, "act": nc.scalar,
                     "pool": nc.gpsimd, "sp": nc.sync}
        self.esem = {}
        self.ecnt = {}
        for e in self.engs:
            self.esem[e] = nc.alloc_semaphore(name=f"s_{e}")
            self.ecnt[e] = 0
        self.seen = {e: {} for e in self.engs}
        self.tr = {}
        self.dsem = {}
        self.n_inst = 0
        self._uid = 0

    def sb(self, name, shape, dt=F32):
        t = self.es.enter_context(self.nc.sbuf_tensor(name, list(shape), dt))
        return t

    def ps(self, name, shape, dt=F32):
        t = self.es.enter_context(self.nc.psum_tensor(name, list(shape), dt))
        return t

    def dram_in(self, name, shape, dt=F32):
        return self.nc.dram_tensor(name, list(shape), dt, kind="ExternalInput").ap()

    def dram_out(self, name, shape, dt=F32):
        return self.nc.dram_tensor(name, list(shape), dt, kind="ExternalOutput").ap()

    @staticmethod
    def _key(ap):
        t = getattr(ap, "tensor", ap)
        return t.name

    def _needs(self, reads, writes):
        needs = {}

        def need(sv):
            sem, val = sv
            k = sem.name if hasattr(sem, "name") else id(sem)
            if k not in needs or needs[k][1] < val:
                needs[k] = (sem, val)

        for ap in reads:
            st = self.tr.get(self._key(ap))
            if st and st[0]:
                need(st[0])
        for ap in writes:
            st = self.tr.get(self._key(ap))
            if st:
                if st[0]:
                    need(st[0])
                for sv in st[1].values():
                    need(sv)
        return needs

    def _emit_waits(self, e, needs, skip_sem=None):
        eng = self.engs[e]
        seen = self.seen[e]
        for k, (sem, val) in needs.items():
            if skip_sem is not None and sem is skip_sem:
                continue
            if seen.get(k, -1) >= val:
                continue
            eng.wait_ge(sem, val)
            seen[k] = val

    def _update(self, reads, writes, sv):
        sem, val = sv
        k = sem.name if hasattr(sem, "name") else id(sem)
        for ap in reads:
            st = self.tr.setdefault(self._key(ap), [None, {}])
            st[1][k] = sv
        for ap in writes:
            st = self.tr.setdefault(self._key(ap), [None, {}])
            st[0] = sv
            st[1] = {}

    def op(self, e, fn, reads=(), writes=(), same_ok=False):
        needs = self._needs(reads, writes)
        self._emit_waits(e, needs, skip_sem=self.esem[e] if same_ok else None)
        inst = fn()
        self.ecnt[e] += 1
        inst.then_inc(self.esem[e], 1)
        self._update(reads, writes, (self.esem[e], self.ecnt[e]))
        self.n_inst += 1
        return inst

    def dma(self, q, out, in_, fn=None, extra_reads=(), **kw):
        reads, writes = [in_] + list(extra_reads), [out]
        needs = self._needs(reads, writes)
        self._emit_waits(q, needs)
        sbt = None
        for ap in (out, in_):
            if "sbuf" in str(ap.space).lower() or "sb" == str(ap.space).lower():
                sbt = ap
        keyt = self._key(sbt if sbt is not None else out)
        if keyt not in self.dsem:
            self.dsem[keyt] = [self.nc.alloc_semaphore(name=f"d_{len(self.dsem)}"), 0]
        ds = self.dsem[keyt]
        if fn is None:
            inst = self.engs[q].dma_start(out=out, in_=in_, **kw)
        else:
            inst = fn()
        ds[1] += 16
        inst.then_inc(ds[0], 16)
        self._update(reads, writes, (ds[0], ds[1]))
        self.n_inst += 1
        return inst

    def finish(self):
        sp = self.engs["sp"]
        for e in self.engs:
            if self.ecnt[e] > 0 and e != "sp":
                sp.wait_ge(self.esem[e], self.ecnt[e])
        for k, (sem, cnt) in self.dsem.items():
            sp.wait_ge(sem, cnt)
        self.nc.all_engine_barrier()
        self.es.close()
        return self.nc


D = 1024
ALPHA = (2.0 * 4) ** 0.25
LN_EPS = 1e-5


def _consts(k, need_iota=False):
    ident_d = k.dram_in("ident", [128, 128])
    ident = k.sb("ident_sb", [128, 128])
    k.dma("sp", ident[:], ident_d[:, :])
    return ident


def _layernorm_tile(k, t, tmp, stats, mv, rstd, epst):
    nc = k.nc
    for j in range(2):
        k.op("dve", lambda j=j: nc.vector.bn_stats(out=stats[:, j * 6:(j + 1) * 6], in_=t[:, j * 512:(j + 1) * 512]),
             reads=[t], writes=[stats])
    k.op("dve", lambda: nc.vector.bn_aggr(out=mv[:, 0:2], in_=stats[:, 0:12]), reads=[stats], writes=[mv])
    k.op("act", lambda: nc.scalar.activation(out=rstd[:, 0:1], in_=mv[:, 1:2], func=AF.Sqrt, bias=epst[:, 0:1], scale=1.0),
         reads=[mv, epst], writes=[rstd])
    k.op("dve", lambda: nc.vector.reciprocal(out=rstd[:, 0:1], in_=rstd[:, 0:1]), reads=[rstd], writes=[rstd])
    k.op("dve", lambda: nc.vector.tensor_scalar(out=t[:], in0=t[:], scalar1=mv[:, 0:1], scalar2=rstd[:, 0:1],
                                                op0=ALU.subtract, op1=ALU.mult), reads=[t, mv, rstd], writes=[t])


def build_C2(NX, NCTX):
    k = KB()
    nc = k.nc
    TOK = NX + NCTX
    x1_d = k.dram_in("x1", [TOK, D])
    mod_d = k.dram_in("mod", [2, 3, 128, D])
    lnp_d = k.dram_in("lnp", [2, 128, D])
    wq_d = k.dram_in("wq", [128, 8, 2048])
    keysT_d = k.dram_in("keysT", [128, 16, 128])
    iota_d = k.dram_in("iota", [128, 256])
    u_d = k.dram_in("u_tab", [16384, D])
    v_d = k.dram_in("v_tab", [16384, D])
    out_d = k.dram_out("xout", [TOK, D])
    ident = _consts(k)

    wq = k.sb("wq_sb", [128, 8, 2048])
    keysT = k.sb("keysT_sb", [128, 16, 128])
    iota = k.sb("iota_sb", [128, 256])
    modt = [k.sb(f"mod{j}", [128, D]) for j in range(3)]
    lng = k.sb("lng", [128, D]); lnb = k.sb("lnb", [128, D])
    x1t = k.sb("x1t", [128, D]); h2 = k.sb("h2", [128, D]); acc = k.sb("acc", [128, D])
    junk = k.sb("junk", [128, D])
    NB = 8
    gb = [k.sb(f"gb{j}", [128, D]) for j in range(NB)]
    T = k.sb("T", [128, 8, 128]); qT = k.sb("qT", [128, 16, 128])
    R1 = k.sb("R1", [128, 16, 128]); R2 = k.sb("R2", [128, 16, 128]); R3 = k.sb("R3", [128, 8, 256])
    sv = k.sb("sv", [128, 16, 16]); si = k.sb("si", [128, 16, 16], U32); sif = k.sb("sif", [128, 16, 16])
    fv = k.sb("fv", [128, 8, 16]); fi = k.sb("fi", [128, 8, 16], U32); fif = k.sb("fif", [128, 8, 16])
    eidf = k.sb("eidf", [128, 128]); eid = k.sb("eid", [128, 128], U32)
    negm = k.sb("negm", [128, 8]); gs = k.sb("gs", [128, 8]); gate = k.sb("gate", [128, 8, 16])
    actv = k.sb("actv", [128, 128]); wgt = k.sb("wgt", [128, 128])
    stats = k.sb("stats", [128, 12]); mv = k.sb("mv", [128, 2]); rstd = k.sb("rstd", [128, 1])
    epst = k.sb("epst", [128, 1])
    pbank = [k.ps(f"pb{j}", [128, 4, 128]) for j in range(4)]

    k.op("pool", lambda: nc.gpsimd.memset(epst[:], LN_EPS), writes=[epst])
    k.op("pool", lambda: nc.gpsimd.memset(eid[:], 0), writes=[eid])
    for kc in range(8):
        k.dma("sp", wq[:, kc, :], wq_d[:, kc, :])
    k.dma("sp", keysT[:], keysT_d[:, :, :])
    k.dma("sp", iota[:], iota_d[:, :])
    k.dma("sp", lng[:], lnp_d[0]); k.dma("sp", lnb[:], lnp_d[1])

    tiles = [(i * 128, 128, 0) for i in range(NX // 128)]
    if NCTX:
        tiles.append((NX, NCTX, 1))
    cur_ty = None
    V = nc.vector
    for (r0, n, ty) in tiles:
        if ty != cur_ty:
            for j in range(3):
                k.dma("sp", modt[j][:], mod_d[ty, j])
            k.op("dve", lambda: V.tensor_scalar_add(out=modt[1][:], in0=modt[1][:], scalar1=1.0), reads=[modt[1]], writes=[modt[1]])
            cur_ty = ty
        k.dma("sp", x1t[:n, :], x1_d[r0:r0 + n, :])
        k.op("dve", lambda: V.tensor_tensor(out=h2[:], in0=x1t[:], in1=modt[1][:], op=ALU.mult), reads=[x1t, modt[1]], writes=[h2])
        k.op("dve", lambda: V.tensor_tensor(out=h2[:], in0=h2[:], in1=modt[0][:], op=ALU.add), reads=[h2, modt[0]], writes=[h2])
        for half in range(2):
            pb = pbank[half]
            for j in range(4):
                kc = half * 4 + j
                k.op("pe", lambda pb=pb, j=j, kc=kc: nc.tensor.transpose(out=pb[:, j, :], in_=h2[:, kc * 128:(kc + 1) * 128], identity=ident[:]),
                     reads=[h2, ident], writes=[pb], same_ok=True)
            k.op("act", lambda pb=pb, half=half: nc.scalar.copy(out=T[:, half * 4:(half + 1) * 4, :], in_=pb[:]), reads=[pb], writes=[T])
        for g4 in range(4):
            pb = pbank[g4]
            for j in range(4):
                hp = g4 * 4 + j
                for kc in range(8):
                    k.op("pe", lambda pb=pb, j=j, hp=hp, kc=kc: nc.tensor.matmul(pb[:, j, :], wq[:, kc, hp * 128:(hp + 1) * 128], T[:, kc, :],
                                                                              start=(kc == 0), stop=(kc == 7)),
                         reads=[wq, T], writes=[pb], same_ok=True)
            k.op("act", lambda pb=pb, g4=g4: nc.scalar.copy(out=qT[:, g4 * 4:(g4 + 1) * 4, :], in_=pb[:]), reads=[pb], writes=[qT])
        for g4 in range(4):
            pb = pbank[g4]
            for j in range(4):
                hp = g4 * 4 + j
                k.op("pe", lambda pb=pb, j=j, hp=hp: nc.tensor.matmul(pb[:, j, :], qT[:, hp, :], keysT[:, hp, :], start=True, stop=True),
                     reads=[qT, keysT], writes=[pb], same_ok=True)
            k.op("act", lambda pb=pb, g4=g4: nc.scalar.copy(out=R1[:, g4 * 4:(g4 + 1) * 4, :], in_=pb[:]), reads=[pb], writes=[R1])
        for hp in range(16):
            k.op("dve", lambda hp=hp: V.max(out=sv[:, hp, 0:8], in_=R1[:, hp, :]), reads=[R1], writes=[sv])
            k.op("dve", lambda hp=hp: V.max_index(out=si[:, hp, 0:8], in_max=sv[:, hp, 0:8], in_values=R1[:, hp, :]), reads=[R1, sv], writes=[si])
            k.op("dve", lambda hp=hp: V.match_replace(out=R2[:, hp, :], in_to_replace=sv[:, hp, 0:8], in_values=R1[:, hp, :], imm_value=-1e30),
                 reads=[R1, sv], writes=[R2])
            k.op("dve", lambda hp=hp: V.max(out=sv[:, hp, 8:16], in_=R2[:, hp, :]), reads=[R2], writes=[sv])
            k.op("dve", lambda hp=hp: V.max_index(out=si[:, hp, 8:16], in_max=sv[:, hp, 8:16], in_values=R2[:, hp, :]), reads=[R2, sv], writes=[si])
        k.op("dve", lambda: V.tensor_copy(out=sif[:], in_=si[:]), reads=[si], writes=[sif])
        sv4 = sv[:].rearrange("p (h two) k -> p h two k", two=2)
        sif4 = sif[:].rearrange("p (h two) k -> p h two k", two=2)
        cand = R1[:].rearrange("p (h a) (b j) -> p h (a b) j", a=2, b=8)
        cand_flat = R1[:].rearrange("p (h a) m -> p h (a m)", a=2)
        cand2_flat = R2[:].rearrange("p (h a) m -> p h (a m)", a=2)
        cidx = R3[:].rearrange("p h (i j) -> p h i j", i=16)
        k.op("dve", lambda: V.tensor_tensor(out=cand, in0=sv4[:, :, 0, :, None].broadcast_to([128, 8, 16, 16]),
                                            in1=sv4[:, :, 1, None, :].broadcast_to([128, 8, 16, 16]), op=ALU.add),
             reads=[sv], writes=[R1])
        k.op("dve", lambda: V.tensor_scalar_mul(out=sif4[:, :, 0, :], in0=sif4[:, :, 0, :], scalar1=128.0), reads=[sif], writes=[sif])
        k.op("dve", lambda: V.tensor_tensor(out=cidx, in0=sif4[:, :, 0, :, None].broadcast_to([128, 8, 16, 16]),
                                            in1=sif4[:, :, 1, None, :].broadcast_to([128, 8, 16, 16]), op=ALU.add),
             reads=[sif], writes=[R3])
        for h in range(8):
            k.op("dve", lambda h=h: V.max(out=fv[:, h, 0:8], in_=cand_flat[:, h, :]), reads=[R1], writes=[fv])
            k.op("dve", lambda h=h: V.max_index(out=fi[:, h, 0:8], in_max=fv[:, h, 0:8], in_values=cand_flat[:, h, :]), reads=[R1, fv], writes=[fi])
            k.op("dve", lambda h=h: V.match_replace(out=cand2_flat[:, h, :], in_to_replace=fv[:, h, 0:8], in_values=cand_flat[:, h, :], imm_value=-1e30),
                 reads=[R1, fv], writes=[R2])
            k.op("dve", lambda h=h: V.max(out=fv[:, h, 8:16], in_=cand2_flat[:, h, :]), reads=[R2], writes=[fv])
            k.op("dve", lambda h=h: V.max_index(out=fi[:, h, 8:16], in_max=fv[:, h, 8:16], in_values=cand2_flat[:, h, :]), reads=[R2, fv], writes=[fi])
        k.op("dve", lambda: V.tensor_copy(out=fif[:], in_=fi[:]), reads=[fi], writes=[fif])
        for h in range(8):
            for j in range(16):
                e = h * 16 + j
                k.op("dve", lambda h=h, j=j, e=e: V.scalar_tensor_tensor(out=junk[:, 0:256], in0=iota[:], scalar=fif[:, h, j:j + 1], in1=R3[:, h, :],
                                                                        op0=ALU.is_equal, op1=ALU.mult, accum_out=eidf[:, e:e + 1]),
                     reads=[iota, fif, R3], writes=[junk, eidf])
        k.op("dve", lambda: V.tensor_copy(out=eid[:], in_=eidf[:]), reads=[eidf], writes=[eid])
        k.op("dve", lambda: V.tensor_scalar_mul(out=negm[:], in0=fv[:, :, 0], scalar1=-1.0), reads=[fv], writes=[negm])
        for h in range(8):
            k.op("act", lambda h=h: nc.scalar.activation(out=gate[:, h, :], in_=fv[:, h, :], func=AF.Exp, bias=negm[:, h:h + 1], scale=1.0,
                                                         accum_out=gs[:, h:h + 1]), reads=[fv, negm], writes=[gate, gs])
        k.op("dve", lambda: V.reciprocal(out=gs[:], in_=gs[:]), reads=[gs], writes=[gs])
        k.op("dve", lambda: V.tensor_tensor(out=gate[:], in0=gate[:], in1=gs[:, :, None].broadcast_to([128, 8, 16]), op=ALU.mult),
             reads=[gate, gs], writes=[gate])
        for e in range(128):
            b = gb[e % NB]
            k.dma("pool", b[:], u_d[:, :], fn=lambda b=b, e=e: nc.gpsimd.indirect_dma_start(
                out=b[:], out_offset=None, in_=u_d[:, :], in_offset=bass.IndirectOffsetOnAxis(ap=eid[:, e:e + 1], axis=0)),
                extra_reads=[eid])
            k.op("dve", lambda b=b, e=e: V.scalar_tensor_tensor(out=junk[:], in0=b[:], scalar=1.0, in1=h2[:],
                                                                op0=ALU.mult, op1=ALU.mult, accum_out=actv[:, e:e + 1]),
                 reads=[b, h2], writes=[junk, actv])
        k.op("act", lambda: nc.scalar.activation(out=wgt[:], in_=actv[:], func=AF.Gelu), reads=[actv], writes=[wgt])
        k.op("dve", lambda: V.tensor_tensor(out=wgt[:], in0=wgt[:], in1=gate[:].rearrange("p h j -> p (h j)"), op=ALU.mult),
             reads=[wgt, gate], writes=[wgt])
        for e in range(128):
            b = gb[e % NB]
            k.dma("pool", b[:], v_d[:, :], fn=lambda b=b, e=e: nc.gpsimd.indirect_dma_start(
                out=b[:], out_offset=None, in_=v_d[:, :], in_offset=bass.IndirectOffsetOnAxis(ap=eid[:, e:e + 1], axis=0)),
                extra_reads=[eid])
            if e == 0:
                k.op("dve", lambda b=b, e=e: V.tensor_scalar_mul(out=acc[:], in0=b[:], scalar1=wgt[:, 0:1]), reads=[b, wgt], writes=[acc])
            else:
                k.op("dve", lambda b=b, e=e: V.scalar_tensor_tensor(out=acc[:], in0=b[:], scalar=wgt[:, e:e + 1], in1=acc[:],
                                                                    op0=ALU.mult, op1=ALU.add), reads=[b, wgt, acc], writes=[acc])
        k.op("dve", lambda: V.tensor_tensor(out=acc[:], in0=acc[:], in1=modt[2][:], op=ALU.mult), reads=[acc, modt[2]], writes=[acc])
        k.op("dve", lambda: V.scalar_tensor_tensor(out=acc[:], in0=x1t[:], scalar=ALPHA, in1=acc[:], op0=ALU.mult, op1=ALU.add),
             reads=[x1t, acc], writes=[acc])
        _layernorm_tile(k, acc, junk, stats, mv, rstd, epst)
        k.op("dve", lambda: V.tensor_tensor(out=acc[:], in0=acc[:], in1=lng[:], op=ALU.mult), reads=[acc, lng], writes=[acc])
        k.op("dve", lambda: V.tensor_tensor(out=junk[:], in0=acc[:], in1=lnb[:], op=ALU.add), reads=[acc, lnb], writes=[junk])
        k.dma("sp", out_d[r0:r0 + n, :], junk[:n, :])
    return k.finish()


def build_P0():
    k = KB(); nc = k.nc
    cT_d = k.dram_in("cT", [128, 8, 2])
    w_d = k.dram_in("w", [128, 8, 3072])
    b_d = k.dram_in("b", [2, 3072])
    out_d = k.dram_out("mod", [2, 3072])
    cT = k.sb("cT_sb", [128, 8, 2]); sT = k.sb("sT", [128, 8, 2])
    w = k.sb("w_sb", [128, 8, 3072]); b = k.sb("b_sb", [2, 3072]); o = k.sb("o_sb", [2, 3072])
    ps = [k.ps(f"ps{j}", [2, 512]) for j in range(2)]
    k.dma("sp", cT[:], cT_d[:, :, :]); k.dma("sp", b[:], b_d[:, :])
    for kc in range(8):
        k.dma("sp", w[:, kc, :], w_d[:, kc, :])
    k.op("act", lambda: nc.scalar.activation(out=sT[:], in_=cT[:], func=AF.Silu), reads=[cT], writes=[sT])
    for j in range(6):
        p = ps[j % 2]
        for kc in range(8):
            k.op("pe", lambda p=p, j=j, kc=kc: nc.tensor.matmul(p[:, :], sT[:, kc, :], w[:, kc, j * 512:(j + 1) * 512], start=(kc == 0), stop=(kc == 7)),
                 reads=[sT, w], writes=[p], same_ok=True)
        k.op("dve", lambda p=p, j=j: nc.vector.tensor_tensor(out=o[:, j * 512:(j + 1) * 512], in0=p[:, :], in1=b[:, j * 512:(j + 1) * 512], op=ALU.add),
             reads=[p, b], writes=[o])
    k.dma("sp", out_d[:, :], o[:])
    return k.finish()


def build_A_ml(NXc, NC):
    k = KB(); nc = k.nc; V = nc.vector
    NXE = NXc + 128
    R = NXc // 64
    NT = NXE + NC
    xT_d = k.dram_in("xT", [128, 8, NT])
    mod_d = k.dram_in("mod", [128, 2, 8, 2])
    w_d = k.dram_in("w_in", [128, 8, 4112])
    b_d = k.dram_in("b_in", [128, 33])
    cw_d = k.dram_in("conv_w", [128, 16, 9])
    cb_d = k.dram_in("conv_b", [128, 16])
    hm_d = k.dram_in("hmask", [128, 2])
    NO = NXc + NC
    q_d = k.dram_out("qk", [16, 128, NO])
    v_d = k.dram_out("v", [8, 128, NO])
    o_d = k.dram_out("o", [8, 128, NO])
    g_d = k.dram_out("g", [16, NO])

    hT = k.sb("hT", [128, 8, NT])
    mod = k.sb("mod_sb", [128, 2, 8, 2]); bsb = k.sb("b_sb", [128, 33]); cw = k.sb("cw", [128, 16, 9]); cb = k.sb("cb", [128, 16])
    hm = k.sb("hm", [128, 2])
    wbuf = [k.sb(f"wb{j}", [128, 8, 128]) for j in range(3)]
    pbuf = [k.sb(f"pbuf{j}", [128, NT]) for j in range(2)]
    cbuf = [k.sb(f"cbuf{j}", [128, NO]) for j in range(2)]
    obuf = [k.sb(f"obuf{j}", [128, NO]) for j in range(2)]
    psb = [k.ps(f"ps{j}", [128, 512]) for j in range(4)]
    for kc in range(8):
        k.dma("sp", hT[:, kc, :], xT_d[:, kc, :])
    k.dma("sp", mod[:], mod_d[:, :, :, :]); k.dma("sp", bsb[:], b_d[:, :]); k.dma("sp", cw[:], cw_d[:, :, :])
    k.dma("sp", cb[:], cb_d[:, :]); k.dma("sp", hm[:], hm_d[:, :])
    k.op("dve", lambda: V.tensor_scalar_add(out=mod[:, :, :, 1], in0=mod[:, :, :, 1], scalar1=1.0), reads=[mod], writes=[mod])
    for kc in range(8):
        k.op("dve", lambda kc=kc: V.tensor_scalar(out=hT[:, kc, 0:NXE], in0=hT[:, kc, 0:NXE], scalar1=mod[:, 0, kc, 1:2], scalar2=mod[:, 0, kc, 0:1],
                                                  op0=ALU.mult, op1=ALU.add), reads=[hT, mod], writes=[hT])
        k.op("dve", lambda kc=kc: V.tensor_scalar(out=hT[:, kc, NXE:NT], in0=hT[:, kc, NXE:NT], scalar1=mod[:, 1, kc, 1:2], scalar2=mod[:, 1, kc, 0:1],
                                                  op0=ALU.mult, op1=ALU.add), reads=[hT, mod], writes=[hT])
    blocks = []
    t0 = 0
    while t0 < NT:
        blocks.append((t0, min(512, NT - t0))); t0 += 512
    pi = 0
    for oc in range(33):
        M = 128 if oc < 32 else 16
        wb = wbuf[oc % 3]
        k.dma("sp", wb[:, :, 0:M], w_d[:, :, oc * 128:oc * 128 + M])
        pb = pbuf[oc % 2]
        for (b0, bn) in blocks:
            p = psb[pi % 4]; pi += 1
            for kc in range(8):
                k.op("pe", lambda p=p, wb=wb, kc=kc, b0=b0, bn=bn, M=M: nc.tensor.matmul(p[0:M, 0:bn], wb[:, kc, 0:M], hT[:, kc, b0:b0 + bn],
                                                                                      start=(kc == 0), stop=(kc == 7)),
                     reads=[wb, hT], writes=[p], same_ok=True)
            fn = AF.Sigmoid if 24 <= oc < 32 else AF.Identity
            k.op("act", lambda p=p, pb=pb, b0=b0, bn=bn, M=M, oc=oc, fn=fn: nc.scalar.activation(out=pb[0:M, b0:b0 + bn], in_=p[0:M, 0:bn], func=fn,
                                                                                           bias=bsb[0:M, oc:oc + 1], scale=1.0),
                 reads=[p, bsb], writes=[pb])
        if oc < 16:
            k.op("dve", lambda pb=pb: V.tensor_scalar_mul(out=pb[:, 0:64], in0=pb[:, 0:64], scalar1=hm[:, 0:1]), reads=[pb, hm], writes=[pb])
            k.op("dve", lambda pb=pb: V.tensor_scalar_mul(out=pb[:, NXE - 64:NXE], in0=pb[:, NXE - 64:NXE], scalar1=hm[:, 1:2]), reads=[pb, hm], writes=[pb])
            cbf = cbuf[oc % 2]
            pg = pb[:, 0:NXE].rearrange("p (r c) -> p r c", c=64)
            cg = cbf[:, 0:NXc].rearrange("p (r c) -> p r c", c=64)
            k.op("dve", lambda pg=pg, cg=cg, oc=oc: V.tensor_scalar(out=cg, in0=pg[:, 1:R + 1, :], scalar1=cw[:, oc, 4:5], scalar2=cb[:, oc:oc + 1],
                                                                 op0=ALU.mult, op1=ALU.add), reads=[pb, cw, cb], writes=[cbf])
            for dr in range(3):
                for dc in range(3):
                    if dr == 1 and dc == 1:
                        continue
                    c0, c1 = (1, 64) if dc == 0 else ((0, 63) if dc == 2 else (0, 64))
                    k.op("dve", lambda pg=pg, cg=cg, oc=oc, dr=dr, dc=dc, c0=c0, c1=c1: V.scalar_tensor_tensor(
                        out=cg[:, :, c0:c1], in0=pg[:, dr:dr + R, c0 + dc - 1:c1 + dc - 1], scalar=cw[:, oc, dr * 3 + dc:dr * 3 + dc + 1],
                        in1=cg[:, :, c0:c1], op0=ALU.mult, op1=ALU.add), reads=[pb, cw, cbf], writes=[cbf])
            k.op("dve", lambda pb=pb, cbf=cbf, oc=oc: V.tensor_scalar(out=cbf[:, NXc:NO], in0=pb[:, NXE:NT], scalar1=cw[:, oc, 4:5], scalar2=cb[:, oc:oc + 1],
                                                                    op0=ALU.mult, op1=ALU.add), reads=[pb, cw, cb], writes=[cbf])
            k.op("dve", lambda pb=pb, cbf=cbf, oc=oc: V.scalar_tensor_tensor(out=cbf[:, NXc + 1:NO], in0=pb[:, NXE:NT - 1], scalar=cw[:, oc, 3:4],
                                                                           in1=cbf[:, NXc + 1:NO], op0=ALU.mult, op1=ALU.add), reads=[pb, cw, cbf], writes=[cbf])
            k.op("dve", lambda pb=pb, cbf=cbf, oc=oc: V.scalar_tensor_tensor(out=cbf[:, NXc:NO - 1], in0=pb[:, NXE + 1:NT], scalar=cw[:, oc, 5:6],
                                                                           in1=cbf[:, NXc:NO - 1], op0=ALU.mult, op1=ALU.add), reads=[pb, cw, cbf], writes=[cbf])
            ob = obuf[oc % 2]
            k.op("act", lambda ob=ob, cbf=cbf: nc.scalar.activation(out=ob[:], in_=cbf[:], func=AF.Silu), reads=[cbf], writes=[ob])
            if oc < 8:
                k.op("dve", lambda ob=ob: V.tensor_scalar_mul(out=ob[:], in0=ob[:], scalar1=1.0 / 16.0), reads=[ob], writes=[ob])
            k.dma("sp", q_d[oc], ob[:])
        elif oc < 32:
            dst = v_d if oc < 24 else o_d
            j = oc - 16 if oc < 24 else oc - 24
            k.dma("sp", dst[j][:, 0:NXc], pb[:, 64:64 + NXc])
            k.dma("sp", dst[j][:, NXc:NO], pb[:, NXE:NT])
        else:
            k.dma("sp", g_d[:, 0:NXc], pb[0:16, 64:64 + NXc])
            k.dma("sp", g_d[:, NXc:NO], pb[0:16, NXE:NT])
    return k.finish()


def build_B_ml(NCH):
    k = KB(); nc = k.nc; V = nc.vector
    qT_d = k.dram_in("qT", [128, 2, NCH * 128])
    kT_d = k.dram_in("kT", [128, 2, NCH * 128])
    k_d = k.dram_in("k", [128, NCH, 256])
    v_d = k.dram_in("v", [128, NCH, 257])
    ig_d = k.dram_in("ig", [128, NCH]); fg_d = k.dram_in("fg", [128, NCH])
    tri_d = k.dram_in("tri", [128, 128]); mk_d = k.dram_in("maskT", [128, 128]); ones_d = k.dram_in("ones", [128, 128])
    h_d = k.dram_out("h", [128, NCH, 256])
    ident = _consts(k)
    tri = k.sb("tri_sb", [128, 128]); mk = k.sb("mk_sb", [128, 128]); ones = k.sb("ones_sb", [128, 128])
    ig = k.sb("ig_sb", [128, NCH]); LF = k.sb("LF", [128, NCH])
    k.dma("sp", tri[:], tri_d[:, :]); k.dma("sp", mk[:], mk_d[:, :]); k.dma("sp", ones[:], ones_d[:, :])
    k.dma("sp", ig[:], ig_d[:, :]); k.dma("sp", LF[:], fg_d[:, :])
    k.op("act", lambda: nc.scalar.activation(out=LF[:], in_=LF[:], func=AF.Exp, scale=-1.0), reads=[LF], writes=[LF])
    k.op("act", lambda: nc.scalar.activation(out=LF[:], in_=LF[:], func=AF.Ln, bias=1.0, scale=1.0), reads=[LF], writes=[LF])
    k.op("dve", lambda: V.tensor_scalar_mul(out=LF[:], in0=LF[:], scalar1=-1.0), reads=[LF], writes=[LF])
    NS = 3
    qb = [k.sb(f"qb{j}", [128, 2, 128]) for j in range(NS)]
    kb = [k.sb(f"kb{j}", [128, 2, 128]) for j in range(NS)]
    kt = [k.sb(f"kt{j}", [128, 256]) for j in range(NS)]
    vb = [k.sb(f"vb{j}", [128, 257]) for j in range(NS)]
    hb = [k.sb(f"hb{j}", [128, 256]) for j in range(2)]
    Cst = [k.sb(f"Cst{j}", [128, 257]) for j in range(2)]
    LFb = k.sb("LFb", [128, 128]); DT = k.sb("DT", [128, 128]); EB = k.sb("EB", [128, 128]); ST = k.sb("ST", [128, 128])
    qs = k.sb("qs", [128, 2, 128]); ka = k.sb("ka", [128, 256])
    wcol = k.sb("wcol", [128, 1]); acol = k.sb("acol", [128, 1]); Gc = k.sb("Gc", [128, 1]); den = k.sb("den", [128, 1])
    psA = k.ps("psA", [128, 128]); psB = k.ps("psB", [128, 128]); psC = k.ps("psC", [128, 2]); psS = k.ps("psS", [128, 128])
    psN = k.ps("psN", [128, 257]); psU = [k.ps(f"psU{j}", [128, 257]) for j in range(2)]
    for j in range(2):
        k.op("pool", lambda j=j: nc.gpsimd.memset(Cst[j][:], 0.0), writes=[Cst[j]])

    def load(c):
        s = c % NS
        k.dma("sp", qb[s][:], qT_d[:, :, c * 128:(c + 1) * 128])
        k.dma("sp", kb[s][:], kT_d[:, :, c * 128:(c + 1) * 128])
        k.dma("sp", kt[s][:], k_d[:, c, :])
        k.dma("sp", vb[s][:], v_d[:, c, :])
    load(0)
    if NCH > 1:
        load(1)
    for c in range(NCH):
        s = c % NS
        if c + 2 < NCH:
            load(c + 2)
        k.op("dve", lambda c=c: V.tensor_scalar_mul(out=LFb[:], in0=ones[:], scalar1=LF[:, c:c + 1]), reads=[ones, LF], writes=[LFb])
        k.op("pe", lambda: nc.tensor.matmul(psA[:, :], LFb[:], tri[:], start=True, stop=False), reads=[LFb, tri], writes=[psA], same_ok=True)
        k.op("pe", lambda: nc.tensor.matmul(psA[:, :], ident[:], mk[:], start=False, stop=True), reads=[ident, mk], writes=[psA], same_ok=True)
        k.op("pe", lambda: nc.tensor.matmul(psB[:, :], LFb[:], tri[:], start=True, stop=True), reads=[LFb, tri], writes=[psB], same_ok=True)
        k.op("pe", lambda c=c: nc.tensor.matmul(psC[:, 0:1], tri[:], LF[:, c:c + 1], start=True, stop=True), reads=[tri, LF], writes=[psC], same_ok=True)
        k.op("pe", lambda c=c: nc.tensor.matmul(psC[:, 1:2], ones[:], LF[:, c:c + 1], start=True, stop=True), reads=[ones, LF], writes=[psC], same_ok=True)
        k.op("dve", lambda c=c: V.tensor_tensor(out=wcol[:], in0=ig[:, c:c + 1], in1=psC[:, 0:1], op=ALU.subtract), reads=[ig, psC], writes=[wcol])
        k.op("act", lambda: nc.scalar.activation(out=DT[:], in_=psA[:, :], func=AF.Exp, bias=wcol[:, 0:1], scale=1.0), reads=[psA, wcol], writes=[DT])
        k.op("act", lambda: nc.scalar.activation(out=EB[:], in_=psB[:, :], func=AF.Exp), reads=[psB], writes=[EB])
        k.op("act", lambda: nc.scalar.activation(out=acol[:], in_=psC[:, 1:2], func=AF.Exp, bias=wcol[:, 0:1], scale=1.0), reads=[psC, wcol], writes=[acol])
        k.op("act", lambda: nc.scalar.activation(out=Gc[:], in_=psC[:, 1:2], func=AF.Exp), reads=[psC], writes=[Gc])
        for dc in range(2):
            k.op("pe", lambda s=s, dc=dc: nc.tensor.matmul(psS[:, :], kb[s][:, dc, :], qb[s][:, dc, :], start=(dc == 0), stop=(dc == 1)),
                 reads=[kb[s], qb[s]], writes=[psS], same_ok=True)
        k.op("dve", lambda: V.tensor_tensor(out=ST[:], in0=psS[:, :], in1=DT[:], op=ALU.mult), reads=[psS, DT], writes=[ST])
        k.op("dve", lambda s=s: V.tensor_tensor(out=qs[:], in0=qb[s][:], in1=EB[:, None, :].broadcast_to([128, 2, 128]), op=ALU.mult),
             reads=[qb[s], EB], writes=[qs])
        k.op("pe", lambda s=s: nc.tensor.matmul(psN[:, :], ST[:], vb[s][:], start=True, stop=False), reads=[ST, vb[s]], writes=[psN], same_ok=True)
        for dc in range(2):
            k.op("pe", lambda dc=dc: nc.tensor.matmul(psN[:, :], qs[:, dc, :], Cst[dc][:], start=False, stop=(dc == 1)),
                 reads=[qs, Cst[dc]], writes=[psN], same_ok=True)
        k.op("act", lambda: nc.scalar.activation(out=den[:], in_=psN[:, 256:257], func=AF.Abs), reads=[psN], writes=[den])
        k.op("dve", lambda: V.tensor_scalar_max(out=den[:], in0=den[:], scalar1=1.0), reads=[den], writes=[den])
        k.op("dve", lambda: V.reciprocal(out=den[:], in_=den[:]), reads=[den], writes=[den])
        hbb = hb[c % 2]
        k.op("dve", lambda hbb=hbb: V.tensor_scalar_mul(out=hbb[:], in0=psN[:, 0:256], scalar1=den[:, 0:1]), reads=[psN, den], writes=[hbb])
        k.dma("sp", h_d[:, c, :], hbb[:])
        k.op("pool", lambda s=s: nc.gpsimd.tensor_scalar(out=ka[:], in0=kt[s][:], scalar1=acol[:, 0:1], scalar2=None, op0=ALU.mult),
             reads=[kt[s], acol], writes=[ka])
        for dc in range(2):
            k.op("pe", lambda s=s, dc=dc: nc.tensor.matmul(psU[dc][:, :], ka[:, dc * 128:(dc + 1) * 128], vb[s][:], start=True, stop=True),
                 reads=[ka, vb[s]], writes=[psU[dc]], same_ok=True)
            k.op("dve", lambda dc=dc: V.scalar_tensor_tensor(out=Cst[dc][:], in0=Cst[dc][:], scalar=Gc[:, 0:1], in1=psU[dc][:, :], op0=ALU.mult, op1=ALU.add),
                 reads=[Cst[dc], Gc, psU[dc]], writes=[Cst[dc]])
    return k.finish()


def build_C1(kind, NX, NCTX):
    k = KB(); nc = k.nc; V = nc.vector
    TOK = NX + NCTX
    ml = kind == "ml"
    H, dh, eps = (4, 256, 1e-5) if ml else (16, 64, 64e-5)
    NPIECE = 3 if ml else 6
    NPRM = 1 if ml else 3
    xres_d = k.dram_in("xres", [TOK, D])
    pc_d = [k.dram_in(f"piece{j}", [TOK, D]) for j in range(NPIECE)]
    prm_d = k.dram_in("prm", [NPRM, 128, D])
    mod_d = k.dram_in("mod", [2, 128, D])
    lnp_d = k.dram_in("lnp", [2, 128, D])
    w_d = k.dram_in("w_out", [128, 8, D])
    out_d = k.dram_out("x1", [TOK, D])
    ident = _consts(k)
    w = k.sb("w_sb", [128, 8, D]); prm = [k.sb(f"prm{j}", [128, D]) for j in range(NPRM)]
    g1 = k.sb("g1", [128, D]); lng = k.sb("lng", [128, D]); lnb = k.sb("lnb", [128, D])
    xr = k.sb("xr", [128, D]); A = k.sb("A", [128, D]); B = k.sb("B", [128, D]); C = k.sb("C", [128, D])
    zT = k.sb("zT", [128, 8, 128]); t1 = k.sb("t1", [128, D]); junk = k.sb("junk", [128, D])
    sm = k.sb("sm", [128, H]); vs = k.sb("vs", [128, H]); bs = k.sb("bs", [128, H])
    stats = k.sb("stats", [128, 12]); mv = k.sb("mv", [128, 2]); rstd = k.sb("rstd", [128, 1])
    epst = k.sb("epst", [128, 1]); epsh = k.sb("epsh", [128, 1])
    pbank = [k.ps(f"pb{j}", [128, 512]) for j in range(4)]
    k.op("pool", lambda: nc.gpsimd.memset(epst[:], LN_EPS), writes=[epst])
    k.op("pool", lambda: nc.gpsimd.memset(epsh[:], eps), writes=[epsh])
    for kc in range(8):
        k.dma("sp", w[:, kc, :], w_d[:, kc, :])
    for j in range(NPRM):
        k.dma("sp", prm[j][:], prm_d[j])
    k.dma("sp", lng[:], lnp_d[0]); k.dma("sp", lnb[:], lnp_d[1])
    tiles = [(i * 128, 128, 0) for i in range(NX // 128)]
    if NCTX:
        tiles.append((NX, NCTX, 1))
    cur_ty = None
    hv = lambda t: t[:].rearrange("p (h e) -> p h e", h=H)
    bc = lambda s: s[:, :, None].broadcast_to([128, H, dh])
    for (r0, n, ty) in tiles:
        if ty != cur_ty:
            k.dma("sp", g1[:], mod_d[ty]); cur_ty = ty
        rs = slice(r0, r0 + n)
        k.dma("sp", xr[:n, :], xres_d[rs, :])
        k.dma("sp", A[:n, :], pc_d[0][rs, :]); k.dma("sp", B[:n, :], pc_d[1][rs, :])
        k.op("dve", lambda: V.tensor_tensor(out=A[:], in0=A[:], in1=B[:], op=ALU.add), reads=[A, B], writes=[A])
        k.op("dve", lambda: V.tensor_reduce(out=sm[:], in_=hv(A), axis=mybir.AxisListType.X, op=ALU.add), reads=[A], writes=[sm])
        k.op("dve", lambda: V.tensor_scalar_mul(out=sm[:], in0=sm[:], scalar1=1.0 / dh), reads=[sm], writes=[sm])
        k.op("dve", lambda: V.tensor_tensor(out=hv(A), in0=hv(A), in1=bc(sm), op=ALU.subtract), reads=[A, sm], writes=[A])
        k.op("dve", lambda: V.tensor_tensor(out=junk[:], in0=A[:], in1=A[:], op=ALU.mult), reads=[A], writes=[junk])
        k.op("dve", lambda: V.tensor_reduce(out=vs[:], in_=hv(junk), axis=mybir.AxisListType.X, op=ALU.add), reads=[junk], writes=[vs])
        k.op("act", lambda: nc.scalar.activation(out=vs[:], in_=vs[:], func=AF.Sqrt, bias=epsh[:, 0:1], scale=1.0 / dh), reads=[vs, epsh], writes=[vs])
        k.op("dve", lambda: V.reciprocal(out=vs[:], in_=vs[:]), reads=[vs], writes=[vs])
        k.op("dve", lambda: V.tensor_tensor(out=hv(A), in0=hv(A), in1=bc(vs), op=ALU.mult), reads=[A, vs], writes=[A])
        if ml:
            k.dma("sp", C[:n, :], pc_d[2][rs, :])
            k.op("dve", lambda: V.tensor_tensor(out=A[:], in0=A[:], in1=C[:], op=ALU.mult), reads=[A, C], writes=[A])
            k.op("dve", lambda: V.tensor_tensor(out=A[:], in0=A[:], in1=prm[0][:], op=ALU.mult), reads=[A, prm[0]], writes=[A])
        else:
            k.op("dve", lambda: V.tensor_tensor(out=A[:], in0=A[:], in1=prm[0][:], op=ALU.mult), reads=[A, prm[0]], writes=[A])
            k.op("dve", lambda: V.tensor_tensor(out=A[:], in0=A[:], in1=prm[1][:], op=ALU.add), reads=[A, prm[1]], writes=[A])
            k.dma("sp", B[:n, :], pc_d[2][rs, :]); k.dma("sp", C[:n, :], pc_d[3][rs, :])
            k.op("dve", lambda: V.tensor_tensor(out=B[:], in0=B[:], in1=C[:], op=ALU.mult), reads=[B, C], writes=[B])
            k.op("dve", lambda: V.tensor_tensor(out=B[:], in0=B[:], in1=prm[2][:], op=ALU.mult), reads=[B, prm[2]], writes=[B])
            k.op("dve", lambda: V.tensor_reduce(out=bs[:], in_=hv(B), axis=mybir.AxisListType.X, op=ALU.add), reads=[B], writes=[bs])
            k.dma("sp", C[:n, :], pc_d[4][rs, :])
            k.op("dve", lambda: V.tensor_tensor(out=hv(C), in0=hv(C), in1=bc(bs), op=ALU.mult), reads=[C, bs], writes=[C])
            k.op("dve", lambda: V.tensor_tensor(out=A[:], in0=A[:], in1=C[:], op=ALU.add), reads=[A, C], writes=[A])
            k.dma("sp", B[:n, :], pc_d[5][rs, :])
            k.op("dve", lambda: V.tensor_tensor(out=A[:], in0=A[:], in1=B[:], op=ALU.mult), reads=[A, B], writes=[A])
        for half in range(2):
            pb = pbank[half]
            for j in range(4):
                kc = half * 4 + j
                k.op("pe", lambda pb=pb, j=j, kc=kc: nc.tensor.transpose(out=pb[:, j * 128:(j + 1) * 128], in_=A[:, kc * 128:(kc + 1) * 128], identity=ident[:]),
                     reads=[A, ident], writes=[pb], same_ok=True)
            k.op("act", lambda pb=pb, half=half: nc.scalar.copy(out=zT[:, half * 4:(half + 1) * 4, :].rearrange("p a b -> p (a b)"), in_=pb[:, :]),
                 reads=[pb], writes=[zT])
        for half in range(2):
            pb = pbank[2 + half]
            for kc in range(8):
                k.op("pe", lambda pb=pb, kc=kc, half=half: nc.tensor.matmul(pb[:, :], zT[:, kc, :], w[:, kc, half * 512:(half + 1) * 512],
                                                                          start=(kc == 0), stop=(kc == 7)), reads=[zT, w], writes=[pb], same_ok=True)
            k.op("dve", lambda pb=pb, half=half: V.tensor_tensor(out=t1[:, half * 512:(half + 1) * 512], in0=pb[:, :], in1=g1[:, half * 512:(half + 1) * 512],
                                                                 op=ALU.mult), reads=[pb, g1], writes=[t1])
        k.op("dve", lambda: V.scalar_tensor_tensor(out=t1[:], in0=xr[:], scalar=ALPHA, in1=t1[:], op0=ALU.mult, op1=ALU.add), reads=[xr, t1], writes=[t1])
        _layernorm_tile(k, t1, junk, stats, mv, rstd, epst)
        k.op("dve", lambda: V.tensor_tensor(out=t1[:], in0=t1[:], in1=lng[:], op=ALU.mult), reads=[t1, lng], writes=[t1])
        k.op("dve", lambda: V.tensor_tensor(out=junk[:], in0=t1[:], in1=lnb[:], op=ALU.add), reads=[t1, lnb], writes=[junk])
        k.dma("sp", out_d[rs, :], junk[:n, :])
    return k.finish()


NCORES = 8
_PROGS = {}


def _run(key, builder, in_maps):
    if key not in _PROGS:
        _PROGS[key] = builder()
    res = run_bass_kernel_spmd(_PROGS[key], in_maps, core_ids=list(range(len(in_maps))))
    return res.results


def _f(a):
    return np.ascontiguousarray(a, dtype=np.float32)


def _bc(v):
    return _f(np.broadcast_to(np.asarray(v, np.float32), (128, v.shape[-1])))


def _kc_layout(w):
    return _f(w.reshape(8, 128, -1).transpose(1, 0, 2))


def _fm(vec):
    return _f(vec.reshape(-1, 128).T)


_IDENT = np.eye(128, dtype=np.float32)


def run_P0(c, c_ctx, ada_w, ada_b):
    depth = ada_w.shape[0]
    cc = np.stack([c.reshape(-1), c_ctx.reshape(-1)], axis=1)
    cT = _f(cc.reshape(8, 128, 2).transpose(1, 0, 2))
    in_maps = []
    for core in range(NCORES):
        i, half = core // 2, core % 2
        i = min(i, depth - 1)
        sl = slice(half * 3072, (half + 1) * 3072)
        in_maps.append({"cT": cT, "w": _kc_layout(ada_w[i][:, sl]), "b": _f(np.stack([ada_b[i][sl], ada_b[i][sl]]))})
    res = _run("P0", build_P0, in_maps)
    mods = np.zeros((depth, 2, 6144), np.float32)
    for core in range(2 * depth):
        i, half = core // 2, core % 2
        mods[i][:, half * 3072:(half + 1) * 3072] = res[core]["mod"]
    return mods.reshape(depth, 2, 6, 1024)


def run_C1(kind, x, ctx, pieces_x, pieces_c, prm, g1x, g1c, ln_g, ln_b, w_out):
    NX, NC = x.shape[0], ctx.shape[0]
    nxc, ncc = NX // NCORES, NC // NCORES
    common = {"prm": _f(np.stack([_bc(p) for p in prm])), "mod": _f(np.stack([_bc(g1x), _bc(g1c)])),
              "lnp": _f(np.stack([_bc(ln_g), _bc(ln_b)])), "w_out": _kc_layout(w_out), "ident": _IDENT}
    in_maps = []
    for c in range(NCORES):
        m = dict(common)
        m["xres"] = _f(np.concatenate([x[c * nxc:(c + 1) * nxc], ctx[c * ncc:(c + 1) * ncc]]))
        for j, (px, pc) in enumerate(zip(pieces_x, pieces_c)):
            m[f"piece{j}"] = _f(np.concatenate([px[c * nxc:(c + 1) * nxc], pc[c * ncc:(c + 1) * ncc]]))
        in_maps.append(m)
    res = _run(("C1", kind, nxc, ncc), lambda: build_C1(kind, nxc, ncc), in_maps)
    x1 = np.concatenate([r["x1"][:nxc] for r in res]); c1 = np.concatenate([r["x1"][nxc:] for r in res])
    return x1, c1


def run_C2(x1, c1, modx, modc, ln_g, ln_b, wq, keys, u_tab, v_tab):
    NX, NC = x1.shape[0], c1.shape[0]
    nxc, ncc = NX // NCORES, NC // NCORES
    common = {"mod": _f(np.stack([np.stack([_bc(m) for m in modx]), np.stack([_bc(m) for m in modc])])),
              "lnp": _f(np.stack([_bc(ln_g), _bc(ln_b)])), "wq": _kc_layout(wq),
              "keysT": _f(keys.reshape(16, 128, 128).transpose(2, 0, 1)),
              "iota": _f(np.broadcast_to(np.arange(256, dtype=np.float32), (128, 256))),
              "u_tab": _f(u_tab), "v_tab": _f(v_tab), "ident": _IDENT}
    in_maps = []
    for c in range(NCORES):
        m = dict(common)
        m["x1"] = _f(np.concatenate([x1[c * nxc:(c + 1) * nxc], c1[c * ncc:(c + 1) * ncc]]))
        in_maps.append(m)
    res = _run(("C2", nxc, ncc), lambda: build_C2(nxc, ncc), in_maps)
    x2 = np.concatenate([r["xout"][:nxc] for r in res]); c2 = np.concatenate([r["xout"][nxc:] for r in res])
    return x2, c2


def run_mlstm(x, ctx, modx, modc, w_in, b_in, conv_w, conv_b):
    NX, NC = x.shape[0], ctx.shape[0]
    nxc = NX // NCORES
    xpad = np.concatenate([np.zeros((64, D), np.float32), x, np.zeros((64, D), np.float32)])
    mod = np.zeros((128, 2, 8, 2), np.float32)
    for ty, m in enumerate((modx, modc)):
        mod[:, ty, :, 0] = _fm(m[0]); mod[:, ty, :, 1] = _fm(m[1])
    b33 = np.zeros((128, 33), np.float32)
    b33[:, :32] = _fm(b_in[:4096]); b33[:16, 32] = b_in[4096:]
    common = {"mod": mod, "w_in": _kc_layout(w_in), "b_in": b33,
              "conv_w": _f(conv_w.reshape(9, 16, 128).transpose(2, 1, 0)), "conv_b": _fm(conv_b)}
    in_maps = []
    for c in range(NCORES):
        win = np.concatenate([xpad[c * nxc:c * nxc + nxc + 128], ctx])
        m = dict(common)
        m["xT"] = _f(win.T.reshape(8, 128, -1).transpose(1, 0, 2))
        hm = np.ones((128, 2), np.float32)
        if c == 0:
            hm[:, 0] = 0
        if c == NCORES - 1:
            hm[:, 1] = 0
        m["hmask"] = hm
        in_maps.append(m)
    res = _run(("A_ml", nxc, NC), lambda: build_A_ml(nxc, NC), in_maps)

    def gather(name, nfeat):
        xs = np.concatenate([r[name].reshape(nfeat, -1)[:, :nxc] for r in res], axis=1)
        cs = res[0][name].reshape(nfeat, -1)[:, nxc:]
        return xs, cs
    qk_x, qk_c = gather("qk", 2048); v_x, v_c = gather("v", 1024); o_x, o_c = gather("o", 1024); g_x, g_c = gather("g", 16)
    T = NC + NX
    NCH = T // 128
    tri = _f(np.triu(np.ones((128, 128), np.float32)))
    maskT = _f(np.where(np.triu(np.ones((128, 128))) > 0, 0.0, -30000.0))
    consts = {"tri": tri, "maskT": maskT, "ones": np.ones((128, 128), np.float32), "ident": _IDENT}
    in_maps = []
    for core in range(NCORES):
        h, d = core % 4, core // 4

        def seqT(ax, ac):
            if d == 0:
                return np.concatenate([ac, ax], axis=1)
            return np.concatenate([ac[:, ::-1], ax[:, ::-1]], axis=1)
        hs = slice(h * 256, (h + 1) * 256)
        qT = seqT(qk_x[hs], qk_c[hs]); kT = seqT(qk_x[1024:][hs], qk_c[1024:][hs]); vT = seqT(v_x[hs], v_c[hs])
        ig = seqT(g_x[d * 4 + h][None], g_c[d * 4 + h][None])[0]
        fg = seqT(g_x[8 + d * 4 + h][None], g_c[8 + d * 4 + h][None])[0]
        m = dict(consts)
        m["qT"] = _f(qT.reshape(2, 128, T).transpose(1, 0, 2)); m["kT"] = _f(kT.reshape(2, 128, T).transpose(1, 0, 2))
        m["k"] = _f(kT.T.reshape(NCH, 128, 256).transpose(1, 0, 2))
        vext = np.concatenate([vT.T, np.ones((T, 1), np.float32)], axis=1)
        m["v"] = _f(vext.reshape(NCH, 128, 257).transpose(1, 0, 2))
        m["ig"] = _f(ig.reshape(NCH, 128).T); m["fg"] = _f(fg.reshape(NCH, 128).T)
        in_maps.append(m)
    res = _run(("B_ml", NCH), lambda: build_B_ml(NCH), in_maps)
    hf = np.zeros((T, D), np.float32); hb = np.zeros((T, D), np.float32)
    for core in range(NCORES):
        h, d = core % 4, core // 4
        hh = res[core]["h"].transpose(1, 0, 2).reshape(T, 256)
        if d == 0:
            hf[:, h * 256:(h + 1) * 256] = hh
        else:
            hb[:NC, h * 256:(h + 1) * 256] = hh[:NC][::-1]
            hb[NC:, h * 256:(h + 1) * 256] = hh[NC:][::-1]
    return (hf[NC:], hb[NC:], _f(o_x.T)), (hf[:NC], hb[:NC], _f(o_c.T))


def build_A_rw(NXc, NC):
    k = KB(); nc = k.nc; V = nc.vector
    NXE = NXc + 128
    NT = NXE + NC
    NO = NXc + NC
    BS = min(512, NXc)
    BW = max(BS, NC)
    xT_d = k.dram_in("xT", [128, 8, NT])
    mod_d = k.dram_in("mod", [128, 2, 8, 2])
    hm_d = k.dram_in("hmask", [128, 2])
    mu_d = k.dram_in("mu", [128, 6, 8])
    wrkv_d = k.dram_in("w_rkv", [3, 128, 8, D])
    w1_d = k.dram_in("w1", [4, 128, 8, 64])
    w2_d = k.dram_in("w2", [4, 64, D])
    g1_d = k.dram_in("g1", [128, 8, 160])
    g2a_d = k.dram_in("g2a", [128, D]); g2b_d = k.dram_in("g2b", [32, D])
    vec_d = k.dram_in("vecs", [128, 7, 8])
    bd_d = k.dram_in("bdones", [128, 128])
    names = ["r", "v", "kkneg", "b0", "b1", "ktil0", "ktil1", "logw0", "logw1", "kbar", "g"]
    outs = {n: k.dram_out(n, [8, 128, NO]) for n in names}

    mod = k.sb("mod_sb", [128, 2, 8, 2]); hm = k.sb("hm", [128, 2]); mu = k.sb("mu_sb", [128, 6, 8])
    w1 = [k.sb(f"w1_{j}", [128, 8, 64]) for j in range(4)]
    w2 = [k.sb(f"w2_{j}", [64, D]) for j in range(4)]
    g1 = k.sb("g1_sb", [128, 8, 160]); g2a = k.sb("g2a_sb", [128, D]); g2b = k.sb("g2b_sb", [32, D])
    vec = k.sb("vec_sb", [128, 7, 8]); bd = k.sb("bd_sb", [128, 128]); omka = k.sb("omka", [128, 8])
    hTb = k.sb("hTb", [128, 8, BW + 128]); sTb = k.sb("sTb", [128, 8, BW]); xm = k.sb("xm", [128, 8, BW])
    kT = k.sb("kT", [128, 8, BW]); kk = k.sb("kk", [128, 8, BW])
    th = [k.sb(f"th{j}", [64, BW]) for j in range(2)]
    gs0 = k.sb("gs0", [128, BW]); gs1 = k.sb("gs1", [32, BW])
    wbuf = [k.sb(f"wb{j}", [128, 8, 128]) for j in range(3)]
    ob = [k.sb(f"ob{j}", [128, BW]) for j in range(6)]
    tmp = [k.sb(f"tmp{j}", [128, BW]) for j in range(3)]
    psb = [k.ps(f"ps{j}", [128, 512]) for j in range(6)]
    cnt = {"ps": 0, "ob": 0, "wb": 0}

    def nps():
        cnt["ps"] += 1; return psb[cnt["ps"] % 6]

    def nob():
        cnt["ob"] += 1; return ob[cnt["ob"] % 6]

    k.dma("sp", mod[:], mod_d[:, :, :, :]); k.dma("sp", hm[:], hm_d[:, :]); k.dma("sp", mu[:], mu_d[:, :, :])
    for j in range(4):
        k.dma("sp", w1[j][:], w1_d[j]); k.dma("sp", w2[j][:], w2_d[j])
    k.dma("sp", g1[:], g1_d[:, :, :]); k.dma("sp", g2a[:], g2a_d[:, :]); k.dma("sp", g2b[:], g2b_d[:, :])
    k.dma("sp", vec[:], vec_d[:, :, :]); k.dma("sp", bd[:], bd_d[:, :])
    k.op("dve", lambda: V.tensor_scalar_add(out=mod[:, :, :, 1], in0=mod[:, :, :, 1], scalar1=1.0), reads=[mod], writes=[mod])
    k.op("dve", lambda: V.tensor_scalar(out=omka[:], in0=vec[:, 5, :], scalar1=-1.0, scalar2=1.0, op0=ALU.mult, op1=ALU.add), reads=[vec], writes=[omka])

    blocks = [(b0, BS, 0) for b0 in range(0, NXc, BS)]
    if NC:
        assert NC <= 512
        blocks.append((0, NC, 1))
    for (b0, bs, ty) in blocks:
        off = 64 if ty == 0 else 0
        wn = bs + 128 if ty == 0 else bs
        src0 = b0 if ty == 0 else NXE
        o0 = b0 if ty == 0 else NXc
        for kc in range(8):
            k.dma("sp", hTb[:, kc, 0:wn], xT_d[:, kc, src0:src0 + wn])
        for kc in range(8):
            k.op("dve", lambda kc=kc: V.tensor_scalar(out=hTb[:, kc, 0:wn], in0=hTb[:, kc, 0:wn], scalar1=mod[:, ty, kc, 1:2], scalar2=mod[:, ty, kc, 0:1],
                                                      op0=ALU.mult, op1=ALU.add), reads=[hTb, mod], writes=[hTb])
        k.op("pool", lambda: nc.gpsimd.memset(sTb[:], 0.0), writes=[sTb])
        if ty == 0:
            if b0 == 0:
                k.op("dve", lambda: V.tensor_scalar_mul(out=hTb[:, :, 0:64], in0=hTb[:, :, 0:64], scalar1=hm[:, 0:1]), reads=[hTb, hm], writes=[hTb])
            if b0 + bs == NXc:
                k.op("dve", lambda: V.tensor_scalar_mul(out=hTb[:, :, bs + 64:bs + 128], in0=hTb[:, :, bs + 64:bs + 128], scalar1=hm[:, 1:2]),
                     reads=[hTb, hm], writes=[hTb])
            hg = lambda kc0, kc1, lo: hTb[:, kc0:kc1, lo:lo + bs].rearrange("p k (r c) -> p k r c", c=64)
            sg = sTb[:, :, 0:bs].rearrange("p k (r c) -> p k r c", c=64)
            for kc in range(2):
                k.op("dve", lambda kc=kc: V.tensor_copy(out=sg[:, kc, :, 1:64], in_=hg(kc, kc + 1, 64)[:, 0, :, 0:63]), reads=[hTb], writes=[sTb])
                k.op("dve", lambda kc=kc: V.tensor_copy(out=sg[:, 2 + kc, :, 0:63], in_=hg(2 + kc, 3 + kc, 64)[:, 0, :, 1:64]), reads=[hTb], writes=[sTb])
            k.op("dve", lambda: V.tensor_copy(out=sTb[:, 4:6, 0:bs], in_=hTb[:, 4:6, 0:bs]), reads=[hTb], writes=[sTb])
            k.op("dve", lambda: V.tensor_copy(out=sTb[:, 6:8, 0:bs], in_=hTb[:, 6:8, 128:128 + bs]), reads=[hTb], writes=[sTb])
        else:
            k.op("dve", lambda: V.tensor_copy(out=sTb[:, 0:4, 1:bs], in_=hTb[:, 0:4, 0:bs - 1]), reads=[hTb], writes=[sTb])
            k.op("dve", lambda: V.tensor_copy(out=sTb[:, 4:8, 0:bs - 1], in_=hTb[:, 4:8, 1:bs]), reads=[hTb], writes=[sTb])
        hc = lambda kc: hTb[:, kc, off:off + bs]
        k.op("dve", lambda: V.tensor_tensor(out=sTb[:, :, 0:bs], in0=sTb[:, :, 0:bs], in1=hTb[:, :, off:off + bs], op=ALU.subtract), reads=[sTb, hTb], writes=[sTb])

        def mix(n):
            for kc in range(8):
                k.op("dve", lambda kc=kc: V.scalar_tensor_tensor(out=xm[:, kc, 0:bs], in0=sTb[:, kc, 0:bs], scalar=mu[:, n, kc:kc + 1], in1=hc(kc),
                                                                 op0=ALU.mult, op1=ALU.add), reads=[sTb, mu, hTb], writes=[xm])

        def proj(n, oc):
            cnt["wb"] += 1
            wb = wbuf[cnt["wb"] % 3]
            k.dma("sp", wb[:], wrkv_d[n][:, :, oc * 128:(oc + 1) * 128])
            p = nps()
            for kc in range(8):
                k.op("pe", lambda p=p, wb=wb, kc=kc: nc.tensor.matmul(p[:, 0:bs], wb[:, kc, :], xm[:, kc, 0:bs], start=(kc == 0), stop=(kc == 7)),
                     reads=[wb, xm], writes=[p], same_ok=True)
            return p

        def store(name, oc, t):
            k.dma("sp", outs[name][oc][:, o0:o0 + bs], t[:, 0:bs])

        mix(0)
        for oc in range(8):
            p = proj(0, oc); o = nob()
            k.op("act", lambda p=p, o=o: nc.scalar.copy(out=o[:, 0:bs], in_=p[:, 0:bs]), reads=[p], writes=[o])
            store("r", oc, o)
        mix(1)
        for oc in range(8):
            p = proj(1, oc)
            k.op("act", lambda p=p, oc=oc: nc.scalar.copy(out=kT[:, oc, 0:bs], in_=p[:, 0:bs]), reads=[p], writes=[kT])
            t0, t1 = tmp[0], tmp[1]
            k.op("dve", lambda oc=oc: V.tensor_scalar_mul(out=t0[:, 0:bs], in0=kT[:, oc, 0:bs], scalar1=vec[:, 4, oc:oc + 1]), reads=[kT, vec], writes=[t0])
            k.op("dve", lambda: V.tensor_tensor(out=t1[:, 0:bs], in0=t0[:, 0:bs], in1=t0[:, 0:bs], op=ALU.mult), reads=[t0], writes=[t1])
            p2 = nps()
            k.op("pe", lambda p2=p2: nc.tensor.matmul(p2[:, 0:bs], bd[:], t1[:, 0:bs], start=True, stop=True), reads=[bd, t1], writes=[p2], same_ok=True)
            k.op("act", lambda p2=p2: nc.scalar.activation(out=t1[:, 0:bs], in_=p2[:, 0:bs], func=AF.Sqrt), reads=[p2], writes=[t1])
            k.op("dve", lambda: V.tensor_scalar_max(out=t1[:, 0:bs], in0=t1[:, 0:bs], scalar1=1e-12), reads=[t1], writes=[t1])
            k.op("dve", lambda: V.reciprocal(out=t1[:, 0:bs], in_=t1[:, 0:bs]), reads=[t1], writes=[t1])
            k.op("dve", lambda oc=oc: V.tensor_tensor(out=kk[:, oc, 0:bs], in0=t0[:, 0:bs], in1=t1[:, 0:bs], op=ALU.mult), reads=[t0, t1], writes=[kk])
            o = nob()
            k.op("dve", lambda o=o, oc=oc: V.tensor_scalar_mul(out=o[:, 0:bs], in0=kk[:, oc, 0:bs], scalar1=-1.0), reads=[kk], writes=[o])
            store("kkneg", oc, o)
        mix(2)
        for oc in range(8):
            p = proj(2, oc); o = nob()
            k.op("act", lambda p=p, o=o: nc.scalar.copy(out=o[:, 0:bs], in_=p[:, 0:bs]), reads=[p], writes=[o])
            store("v", oc, o)

        def lora_in(j, dst, func):
            p = nps()
            for kc in range(8):
                k.op("pe", lambda p=p, kc=kc, j=j: nc.tensor.matmul(p[0:64, 0:bs], w1[j][:, kc, :], xm[:, kc, 0:bs], start=(kc == 0), stop=(kc == 7)),
                     reads=[w1[j], xm], writes=[p], same_ok=True)
            k.op("act", lambda p=p, dst=dst: nc.scalar.activation(out=dst[:, 0:bs], in_=p[0:64, 0:bs], func=func), reads=[p], writes=[dst])

        mix(3)
        for z in range(2):
            lora_in(z, th[z], AF.Tanh)
        for oc in range(8):
            for z in range(2):
                p = nps()
                k.op("pe", lambda p=p, z=z, oc=oc: nc.tensor.matmul(p[:, 0:bs], w2[z][:, oc * 128:(oc + 1) * 128], th[z][:, 0:bs], start=True, stop=True),
                     reads=[w2[z], th[z]], writes=[p], same_ok=True)
                o = nob()
                k.op("act", lambda p=p, o=o, z=z, oc=oc: nc.scalar.activation(out=o[:, 0:bs], in_=p[:, 0:bs], func=AF.Sigmoid, bias=vec[:, z, oc:oc + 1], scale=1.0),
                     reads=[p, vec], writes=[o])
                k.op("dve", lambda o=o: V.tensor_scalar_mul(out=o[:, 0:bs], in0=o[:, 0:bs], scalar1=-0.6065306597126334), reads=[o], writes=[o])
                store(f"logw{z}", oc, o)
        mix(4)
        for z in range(2):
            lora_in(2 + z, th[z], AF.Identity)
        for oc in range(8):
            kt_z = []
            for z in range(2):
                p = nps()
                k.op("pe", lambda p=p, z=z, oc=oc: nc.tensor.matmul(p[:, 0:bs], w2[2 + z][:, oc * 128:(oc + 1) * 128], th[z][:, 0:bs], start=True, stop=True),
                     reads=[w2[2 + z], th[z]], writes=[p], same_ok=True)
                asg = tmp[z]
                k.op("act", lambda p=p, asg=asg, z=z, oc=oc: nc.scalar.activation(out=asg[:, 0:bs], in_=p[:, 0:bs], func=AF.Sigmoid, bias=vec[:, 2 + z, oc:oc + 1], scale=1.0),
                     reads=[p, vec], writes=[asg])
                o = nob()
                k.op("dve", lambda o=o, asg=asg, oc=oc: V.tensor_tensor(out=o[:, 0:bs], in0=kk[:, oc, 0:bs], in1=asg[:, 0:bs], op=ALU.mult), reads=[kk, asg], writes=[o])
                store(f"b{z}", oc, o)
                k.op("dve", lambda asg=asg, oc=oc: V.tensor_scalar(out=asg[:, 0:bs], in0=asg[:, 0:bs], scalar1=vec[:, 5, oc:oc + 1], scalar2=omka[:, oc:oc + 1],
                                                                 op0=ALU.mult, op1=ALU.add), reads=[asg, vec, omka], writes=[asg])
                o2 = nob()
                k.op("dve", lambda o2=o2, asg=asg, oc=oc: V.tensor_tensor(out=o2[:, 0:bs], in0=asg[:, 0:bs], in1=kT[:, oc, 0:bs], op=ALU.mult), reads=[asg, kT], writes=[o2])
                store(f"ktil{z}", oc, o2)
                kt_z.append(o2)
            o3 = nob()
            k.op("dve", lambda o3=o3, a=kt_z[0], b=kt_z[1]: V.tensor_tensor(out=o3[:, 0:bs], in0=a[:, 0:bs], in1=b[:, 0:bs], op=ALU.add), reads=[kt_z[0], kt_z[1]], writes=[o3])
            k.op("dve", lambda o3=o3: V.tensor_scalar_mul(out=o3[:, 0:bs], in0=o3[:, 0:bs], scalar1=0.5), reads=[o3], writes=[o3])
            store("kbar", oc, o3)
        mix(5)
        for (m0, mn, dst) in ((0, 128, gs0), (128, 32, gs1)):
            p = nps()
            for kc in range(8):
                k.op("pe", lambda p=p, kc=kc, m0=m0, mn=mn: nc.tensor.matmul(p[0:mn, 0:bs], g1[:, kc, m0:m0 + mn], xm[:, kc, 0:bs], start=(kc == 0), stop=(kc == 7)),
                     reads=[g1, xm], writes=[p], same_ok=True)
            k.op("act", lambda p=p, dst=dst, mn=mn: nc.scalar.activation(out=dst[:, 0:bs], in_=p[0:mn, 0:bs], func=AF.Sigmoid), reads=[p], writes=[dst])
        for oc in range(8):
            p = nps()
            k.op("pe", lambda p=p, oc=oc: nc.tensor.matmul(p[:, 0:bs], g2a[:, oc * 128:(oc + 1) * 128], gs0[:, 0:bs], start=True, stop=False),
                 reads=[g2a, gs0], writes=[p], same_ok=True)
            k.op("pe", lambda p=p, oc=oc: nc.tensor.matmul(p[:, 0:bs], g2b[:, oc * 128:(oc + 1) * 128], gs1[:, 0:bs], start=False, stop=True),
                 reads=[g2b, gs1], writes=[p], same_ok=True)
            o = nob()
            k.op("act", lambda p=p, o=o: nc.scalar.copy(out=o[:, 0:bs], in_=p[:, 0:bs]), reads=[p], writes=[o])
            store("g", oc, o)
    return k.finish()


def build_B_rw(NCH):
    k = KB(); nc = k.nc; V = nc.vector
    NSC = 4
    T = NCH * 128
    tk_d = {n: k.dram_in(n, [128, NCH, NSC, 64]) for n in ("lw_t", "b_t", "k_t", "v_t")}
    ch_d = {n: k.dram_in(n, [64, NSC, T]) for n in ("r_c", "a_c", "b_c", "k_c")}
    tri_d = k.dram_in("tri", [128, 128]); tris_d = k.dram_in("tris", [128, 128])
    msu_d = k.dram_in("m_su", [128, 128]); msl_d = k.dram_in("m_sl", [128, 128]); miu_d = k.dram_in("m_iu", [128, 128])
    y_d = k.dram_out("y", [128, NCH, NSC, 64])
    ident = _consts(k)
    tri = k.sb("tri_sb", [128, 128]); tris = k.sb("tris_sb", [128, 128])
    msu = k.sb("msu", [128, 128]); msl = k.sb("msl", [128, 128]); miu = k.sb("miu", [128, 128])
    for t, d in ((tri, tri_d), (tris, tris_d), (msu, msu_d), (msl, msl_d), (miu, miu_d)):
        k.dma("sp", t[:], d[:, :])
    NS = 2
    tk = {n: [k.sb(f"{n}_s{j}", [128, NSC, 64]) for j in range(NS)] for n in tk_d}
    ch = {n: [k.sb(f"{n}_s{j}", [64, NSC, 128]) for j in range(NS)] for n in ch_d}
    Pinv = k.sb("Pinv", [128, NSC, 64]); PT = k.sb("PT", [64, NSC, 128]); PinvT = k.sb("PinvT", [64, NSC, 128]); Pm1T = k.sb("Pm1T", [64, NSC, 128])
    At = k.sb("At", [64, NSC, 128]); BtT = k.sb("BtT", [64, NSC, 128]); KtT = k.sb("KtT", [64, NSC, 128]); RtT = k.sb("RtT", [64, NSC, 128])
    Btok = k.sb("Btok", [128, NSC, 64]); Ktok = k.sb("Ktok", [128, NSC, 64])
    Nn = [k.sb(f"Nn{j}", [128, NSC, 128]) for j in range(2)]; NTt = [k.sb(f"NTt{j}", [128, NSC, 128]) for j in range(2)]
    X = k.sb("X", [128, NSC, 128]); XT = k.sb("XT", [128, NSC, 128])
    MakT = k.sb("MakT", [128, NSC, 128]); MrbT = k.sb("MrbT", [128, NSC, 128]); MrkT = k.sb("MrkT", [128, NSC, 128])
    W = k.sb("W", [128, NSC, 64]); U = k.sb("U", [128, NSC, 64]); Yb = [k.sb(f"Yb{j}", [128, NSC, 64]) for j in range(2)]
    Z = k.sb("Z", [64, NSC, 64]); Zt = k.sb("Zt", [64, NSC, 64])
    banks = [k.ps(f"bk{j}", [128, 512]) for j in range(8)]
    bi = [0]

    def bank():
        bi[0] += 1
        return banks[bi[0] % 8]
    k.op("pool", lambda: nc.gpsimd.memset(Z[:], 0.0), writes=[Z])

    def load(c):
        s = c % NS
        for n in tk_d:
            k.dma("sp", tk[n][s][:], tk_d[n][:, c, :, :])
        for n in ch_d:
            k.dma("sp", ch[n][s][:], ch_d[n][:, :, c * 128:(c + 1) * 128])
    load(0)
    bcm = lambda m: m[:, None, :].broadcast_to([128, NSC, 128])
    v3 = lambda b: b[:, :].rearrange("p (s t) -> p s t", s=NSC)
    v64 = lambda b: b[:, 0:NSC * 64].rearrange("p (s t) -> p s t", s=NSC)
    for c in range(NCH):
        s = c % NS
        if c + 1 < NCH:
            load(c + 1)
        LW, Bt_, Kt_, Vt = tk["lw_t"][s], tk["b_t"][s], tk["k_t"][s], tk["v_t"][s]
        Rc, Ac, Bc, Kc = ch["r_c"][s], ch["a_c"][s], ch["b_c"][s], ch["k_c"][s]
        pLP = bank()
        k.op("pe", lambda: nc.tensor.matmul(pLP[:, 0:256], tri[:], LW[:].rearrange("p s c -> p (s c)"), start=True, stop=True), reads=[tri, LW], writes=[pLP], same_ok=True)
        pLT = bank(); pL1 = bank()
        for sc in range(NSC):
            k.op("pe", lambda sc=sc: nc.tensor.matmul(pLT[0:64, sc * 128:(sc + 1) * 128], LW[:, sc, :], tri[:], start=True, stop=True), reads=[LW, tri], writes=[pLT], same_ok=True)
            k.op("pe", lambda sc=sc: nc.tensor.matmul(pL1[0:64, sc * 128:(sc + 1) * 128], LW[:, sc, :], tris[:], start=True, stop=True), reads=[LW, tris], writes=[pL1], same_ok=True)
        k.op("act", lambda: nc.scalar.activation(out=Pinv[:].rearrange("p s c -> p (s c)"), in_=pLP[:, 0:256], func=AF.Exp, scale=-1.0), reads=[pLP], writes=[Pinv])
        k.op("act", lambda: nc.scalar.activation(out=PT[:].rearrange("p s c -> p (s c)"), in_=pLT[0:64, :], func=AF.Exp), reads=[pLT], writes=[PT])
        k.op("act", lambda: nc.scalar.activation(out=PinvT[:].rearrange("p s c -> p (s c)"), in_=pLT[0:64, :], func=AF.Exp, scale=-1.0), reads=[pLT], writes=[PinvT])
        k.op("act", lambda: nc.scalar.activation(out=Pm1T[:].rearrange("p s c -> p (s c)"), in_=pL1[0:64, :], func=AF.Exp), reads=[pL1], writes=[Pm1T])
        k.op("dve", lambda: V.tensor_tensor(out=At[:], in0=Ac[:], in1=Pm1T[:], op=ALU.mult), reads=[Ac, Pm1T], writes=[At])
        k.op("dve", lambda: V.tensor_tensor(out=BtT[:], in0=Bc[:], in1=PinvT[:], op=ALU.mult), reads=[Bc, PinvT], writes=[BtT])
        k.op("dve", lambda: V.tensor_tensor(out=KtT[:], in0=Kc[:], in1=PinvT[:], op=ALU.mult), reads=[Kc, PinvT], writes=[KtT])
        k.op("dve", lambda: V.tensor_tensor(out=RtT[:], in0=Rc[:], in1=PT[:], op=ALU.mult), reads=[Rc, PT], writes=[RtT])
        k.op("pool", lambda: nc.gpsimd.tensor_tensor(out=Btok[:], in0=Bt_[:], in1=Pinv[:], op=ALU.mult), reads=[Bt_, Pinv], writes=[Btok])
        k.op("pool", lambda: nc.gpsimd.tensor_tensor(out=Ktok[:], in0=Kt_[:], in1=Pinv[:], op=ALU.mult), reads=[Kt_, Pinv], writes=[Ktok])
        for (L, Rr, dst, msk) in ((BtT, At, Nn[0], msu), (At, BtT, NTt[0], msl), (KtT, At, MakT, msu), (BtT, RtT, MrbT, miu), (KtT, RtT, MrkT, miu)):
            p = bank()
            for sc in range(NSC):
                k.op("pe", lambda p=p, L=L, Rr=Rr, sc=sc: nc.tensor.matmul(p[:, sc * 128:(sc + 1) * 128], L[:, sc, :], Rr[:, sc, :], start=True, stop=True),
                     reads=[L, Rr], writes=[p], same_ok=True)
            k.op("dve", lambda p=p, dst=dst, msk=msk: V.tensor_tensor(out=dst[:], in0=v3(p), in1=bcm(msk), op=ALU.mult), reads=[p, msk], writes=[dst])
        k.op("dve", lambda: V.tensor_tensor(out=X[:], in0=Nn[0][:], in1=bcm(ident), op=ALU.add), reads=[Nn[0], ident], writes=[X])
        k.op("dve", lambda: V.tensor_tensor(out=XT[:], in0=NTt[0][:], in1=bcm(ident), op=ALU.add), reads=[NTt[0], ident], writes=[XT])
        cur = 0
        for it in range(6):
            nxt = 1 - cur
            last = it == 5
            pN2 = bank()
            for sc in range(NSC):
                k.op("pe", lambda sc=sc, cur=cur, pN2=pN2: nc.tensor.matmul(pN2[:, sc * 128:(sc + 1) * 128], NTt[cur][:, sc, :], Nn[cur][:, sc, :], start=True, stop=True),
                     reads=[NTt[cur], Nn[cur]], writes=[pN2], same_ok=True)
            k.op("act", lambda pN2=pN2, nxt=nxt: nc.scalar.copy(out=Nn[nxt][:], in_=v3(pN2)), reads=[pN2], writes=[Nn[nxt]])
            if not last:
                pT2 = bank()
                for sc in range(NSC):
                    k.op("pe", lambda sc=sc, cur=cur, pT2=pT2: nc.tensor.matmul(pT2[:, sc * 128:(sc + 1) * 128], Nn[cur][:, sc, :], NTt[cur][:, sc, :], start=True, stop=True),
                         reads=[Nn[cur], NTt[cur]], writes=[pT2], same_ok=True)
                k.op("act", lambda pT2=pT2, nxt=nxt: nc.scalar.copy(out=NTt[nxt][:], in_=v3(pT2)), reads=[pT2], writes=[NTt[nxt]])
            pX = bank()
            for sc in range(NSC):
                k.op("pe", lambda sc=sc, nxt=nxt, pX=pX: nc.tensor.matmul(pX[:, sc * 128:(sc + 1) * 128], XT[:, sc, :], Nn[nxt][:, sc, :], start=True, stop=True),
                     reads=[XT, Nn[nxt]], writes=[pX], same_ok=True)
            if not last:
                pXT = bank()
                for sc in range(NSC):
                    k.op("pe", lambda sc=sc, nxt=nxt, pXT=pXT: nc.tensor.matmul(pXT[:, sc * 128:(sc + 1) * 128], X[:, sc, :], NTt[nxt][:, sc, :], start=True, stop=True),
                         reads=[X, NTt[nxt]], writes=[pXT], same_ok=True)
            k.op("dve", lambda pX=pX: V.tensor_tensor(out=X[:], in0=X[:], in1=v3(pX), op=ALU.add), reads=[X, pX], writes=[X])
            if not last:
                k.op("dve", lambda pXT=pXT: V.tensor_tensor(out=XT[:], in0=XT[:], in1=v3(pXT), op=ALU.add), reads=[XT, pXT], writes=[XT])
            cur = nxt
        pW = bank()
        for sc in range(NSC):
            k.op("pe", lambda sc=sc: nc.tensor.matmul(pW[:, sc * 64:(sc + 1) * 64], At[:, sc, :], Z[:, sc, :], start=True, stop=False), reads=[At, Z], writes=[pW], same_ok=True)
            k.op("pe", lambda sc=sc: nc.tensor.matmul(pW[:, sc * 64:(sc + 1) * 64], MakT[:, sc, :], Vt[:, sc, :], start=False, stop=True), reads=[MakT, Vt], writes=[pW], same_ok=True)
        k.op("act", lambda: nc.scalar.copy(out=W[:], in_=v64(pW)), reads=[pW], writes=[W])
        pU = bank()
        for sc in range(NSC):
            k.op("pe", lambda sc=sc: nc.tensor.matmul(pU[:, sc * 64:(sc + 1) * 64], X[:, sc, :], W[:, sc, :], start=True, stop=True), reads=[X, W], writes=[pU], same_ok=True)
        k.op("act", lambda: nc.scalar.copy(out=U[:], in_=v64(pU)), reads=[pU], writes=[U])
        pY = bank()
        for sc in range(NSC):
            k.op("pe", lambda sc=sc: nc.tensor.matmul(pY[:, sc * 64:(sc + 1) * 64], RtT[:, sc, :], Z[:, sc, :], start=True, stop=False), reads=[RtT, Z], writes=[pY], same_ok=True)
            k.op("pe", lambda sc=sc: nc.tensor.matmul(pY[:, sc * 64:(sc + 1) * 64], MrbT[:, sc, :], U[:, sc, :], start=False, stop=False), reads=[MrbT, U], writes=[pY], same_ok=True)
            k.op("pe", lambda sc=sc: nc.tensor.matmul(pY[:, sc * 64:(sc + 1) * 64], MrkT[:, sc, :], Vt[:, sc, :], start=False, stop=True), reads=[MrkT, Vt], writes=[pY], same_ok=True)
        yb = Yb[c % 2]
        k.op("act", lambda yb=yb: nc.scalar.copy(out=yb[:], in_=v64(pY)), reads=[pY], writes=[yb])
        k.dma("sp", y_d[:, c, :, :], yb[:])
        pZ = bank()
        for sc in range(NSC):
            k.op("pe", lambda sc=sc: nc.tensor.matmul(pZ[0:64, sc * 64:(sc + 1) * 64], Btok[:, sc, :], U[:, sc, :], start=True, stop=False), reads=[Btok, U], writes=[pZ], same_ok=True)
            k.op("pe", lambda sc=sc: nc.tensor.matmul(pZ[0:64, sc * 64:(sc + 1) * 64], Ktok[:, sc, :], Vt[:, sc, :], start=False, stop=True), reads=[Ktok, Vt], writes=[pZ], same_ok=True)
        k.op("dve", lambda: V.tensor_tensor(out=Zt[:], in0=Z[:], in1=pZ[0:64, 0:NSC * 64].rearrange("p (s t) -> p s t", s=NSC), op=ALU.add), reads=[Z, pZ], writes=[Zt])
        k.op("dve", lambda: V.tensor_tensor(out=Z[:], in0=Zt[:], in1=PT[:, :, 127:128].broadcast_to([64, NSC, 64]), op=ALU.mult), reads=[Zt, PT], writes=[Z])
    return k.finish()


def run_rwkv(x, ctx, modx, modc, P):
    NX, NC = x.shape[0], ctx.shape[0]
    nxc = NX // NCORES
    xpad = np.concatenate([np.zeros((64, D), np.float32), x, np.zeros((64, D), np.float32)])
    mod = np.zeros((128, 2, 8, 2), np.float32)
    for ty, m in enumerate((modx, modc)):
        mod[:, ty, :, 0] = _fm(m[0]); mod[:, ty, :, 1] = _fm(m[1])
    vecs = np.zeros((128, 7, 8), np.float32)
    for j, v in enumerate((P["w0"][0], P["w0"][1], P["a0"][0], P["a0"][1], P["k_k"], P["k_a"])):
        vecs[:, j, :] = _fm(v)
    bd = np.zeros((128, 128), np.float32); bd[:64, :64] = 1; bd[64:, 64:] = 1
    common = {"mod": mod, "mu": _f(np.stack([_fm(P["mu"][n]) for n in range(6)], axis=1)),
              "w_rkv": _f(np.stack([_kc_layout(P["w_rkv"][n]) for n in range(3)])),
              "w1": _f(np.stack([_kc_layout(P["w1"][0]), _kc_layout(P["w1"][1]), _kc_layout(P["a1"][0]), _kc_layout(P["a1"][1])])),
              "w2": _f(np.stack([P["w2"][0], P["w2"][1], P["a2"][0], P["a2"][1]])),
              "g1": _kc_layout(P["g1"]), "g2a": _f(P["g2"][:128]), "g2b": _f(P["g2"][128:]), "vecs": vecs, "bdones": bd}
    in_maps = []
    for c in range(NCORES):
        win = np.concatenate([xpad[c * nxc:c * nxc + nxc + 128], ctx])
        m = dict(common)
        m["xT"] = _f(win.T.reshape(8, 128, -1).transpose(1, 0, 2))
        hm = np.ones((128, 2), np.float32)
        if c == 0:
            hm[:, 0] = 0
        if c == NCORES - 1:
            hm[:, 1] = 0
        m["hmask"] = hm
        in_maps.append(m)
    res = _run(("A_rw", nxc, NC), lambda: build_A_rw(nxc, NC), in_maps)
    fmx, fmc = {}, {}
    for n in ["r", "v", "kkneg", "b0", "b1", "ktil0", "ktil1", "logw0", "logw1", "kbar", "g"]:
        fmx[n] = np.concatenate([r[n].reshape(1024, -1)[:, :nxc] for r in res], axis=1)
        fmc[n] = res[0][n].reshape(1024, -1)[:, nxc:]
    T = NC + NX
    NCH = T // 128
    iu = np.triu(np.ones((128, 128), np.float32)); su = np.triu(np.ones((128, 128), np.float32), 1)
    consts = {"tri": _f(iu), "tris": _f(su), "m_su": _f(su), "m_sl": _f(su.T), "m_iu": _f(iu), "ident": _IDENT}
    in_maps = []
    for core in range(NCORES):
        tkm = {n: [] for n in ("lw_t", "b_t", "k_t", "v_t")}
        chm = {n: [] for n in ("r_c", "a_c", "b_c", "k_c")}
        for j in range(4):
            sid = core * 4 + j
            hd, z = sid // 2, sid % 2
            hs = slice(hd * 64, (hd + 1) * 64)

            def seq(n):
                ax, ac = fmx[n][hs], fmc[n][hs]
                if z == 0:
                    return np.concatenate([ac, ax], axis=1)
                return np.concatenate([ac[:, ::-1], ax[:, ::-1]], axis=1)
            tkm["lw_t"].append(seq(f"logw{z}").T); tkm["b_t"].append(seq(f"b{z}").T); tkm["k_t"].append(seq(f"ktil{z}").T); tkm["v_t"].append(seq("v").T)
            chm["r_c"].append(seq("r")); chm["a_c"].append(seq("kkneg")); chm["b_c"].append(seq(f"b{z}")); chm["k_c"].append(seq(f"ktil{z}"))
        m = dict(consts)
        for n, lst in tkm.items():
            m[n] = _f(np.stack(lst).reshape(4, NCH, 128, 64).transpose(2, 1, 0, 3))
        for n, lst in chm.items():
            m[n] = _f(np.stack(lst).transpose(1, 0, 2))
        in_maps.append(m)
    res = _run(("B_rw", NCH), lambda: build_B_rw(NCH), in_maps)
    yf = np.zeros((T, D), np.float32); yb = np.zeros((T, D), np.float32)
    for core in range(NCORES):
        yy = res[core]["y"]
        for j in range(4):
            sid = core * 4 + j
            hd, z = sid // 2, sid % 2
            ys = yy[:, :, j, :].transpose(1, 0, 2).reshape(T, 64)
            if z == 0:
                yf[:, hd * 64:(hd + 1) * 64] = ys
            else:
                yb[:NC, hd * 64:(hd + 1) * 64] = ys[:NC][::-1]
                yb[NC:, hd * 64:(hd + 1) * 64] = ys[NC:][::-1]
    px = [yf[NC:], yb[NC:]] + [_f(fmx[n].T) for n in ("r", "kbar", "v", "g")]
    pc = [yf[:NC], yb[:NC]] + [_f(fmc[n].T) for n in ("r", "kbar", "v", "g")]
    return px, pc


def kernel(x, c, ctx, c_ctx, ada_w, ada_b, ln_g, ln_b,
           ml_w_in, ml_b_in, ml_conv_w, ml_conv_b, ml_hn_g, ml_w_out,
           rw_mu, rw_w_rkv, rw_w0, rw_w1, rw_w2, rw_a0, rw_a1, rw_a2, rw_g1, rw_g2,
           rw_k_k, rw_k_a, rw_r_k, rw_lnx_g, rw_lnx_b, rw_w_out,
           pk_wq, pk_keys, pk_u, pk_v):
    A = lambda a: np.asarray(a, dtype=np.float32)
    xs = A(x)[0]; cs = A(ctx)[0]
    depth = ada_w.shape[0]
    mods = run_P0(A(c), A(c_ctx), A(ada_w), A(ada_b))
    for i in range(depth):
        j = i // 2
        modx, modc = mods[i, 0], mods[i, 1]
        if i % 2 == 0:
            px, pc = run_mlstm(xs, cs, modx, modc, A(ml_w_in[j]), A(ml_b_in[j]), A(ml_conv_w[j]), A(ml_conv_b[j]))
            x1, c1 = run_C1("ml", xs, cs, list(px), list(pc), [A(ml_hn_g[j])], modx[2], modc[2], A(ln_g[i, 0]), A(ln_b[i, 0]), A(ml_w_out[j]))
        else:
            P = {"mu": A(rw_mu[j]), "w_rkv": A(rw_w_rkv[j]), "w0": A(rw_w0[j]), "w1": A(rw_w1[j]), "w2": A(rw_w2[j]),
                 "a0": A(rw_a0[j]), "a1": A(rw_a1[j]), "a2": A(rw_a2[j]), "g1": A(rw_g1[j]), "g2": A(rw_g2[j]),
                 "k_k": A(rw_k_k[j]), "k_a": A(rw_k_a[j])}
            px, pc = run_rwkv(xs, cs, modx, modc, P)
            x1, c1 = run_C1("rw", xs, cs, px, pc, [A(rw_lnx_g[j]), A(rw_lnx_b[j]), A(rw_r_k[j])], modx[2], modc[2],
                            A(ln_g[i, 0]), A(ln_b[i, 0]), A(rw_w_out[j]))
        xs, cs = run_C2(x1, c1, modx[3:6], modc[3:6], A(ln_g[i, 1]), A(ln_b[i, 1]), A(pk_wq[i]), A(pk_keys[i]), A(pk_u[i]), A(pk_v[i]))
    return np.ascontiguousarray(xs[None].astype(np.float32))
```

```python
import contextlib
import numpy as np
import concourse.bass as bass
import concourse.mybir as mybir
from concourse.alu_op_type import AluOpType as ALU
from concourse.bass_utils import run_bass_kernel_spmd

F32 = mybir.dt.float32
I32 = mybir.dt.int32
U32 = mybir.dt.uint32
AF = mybir.ActivationFunctionType


class KB:
    def __init__(self):
        self.nc = bass.Bass("TRN2", target_bir_lowering=False)
        nc = self.nc
        self.es = contextlib.ExitStack()
        self.es.enter_context(nc.cleanup_on_exit())
        self.engs = {"pe": nc.tensor, "dve": nc.vector, "act": nc.scalar,
                     "pool": nc.gpsimd, "sp": nc.sync}
        self.esem = {}
        self.ecnt = {}
        for e in self.engs:
            self.esem[e] = nc.alloc_semaphore(name=f"s_{e}")
            self.ecnt[e] = 0
        self.seen = {e: {} for e in self.engs}
        self.tr = {}
        self.dsem = {}
        self.n_inst = 0
        self._uid = 0

    def sb(self, name, shape, dt=F32):
        t = self.es.enter_context(self.nc.sbuf_tensor(name, list(shape), dt))
        return t

    def ps(self, name, shape, dt=F32):
        t = self.es.enter_context(self.nc.psum_tensor(name, list(shape), dt))
        return t

    def dram_in(self, name, shape, dt=F32):
        return self.nc.dram_tensor(name, list(shape), dt, kind="ExternalInput").ap()

    def dram_out(self, name, shape, dt=F32):
        return self.nc.dram_tensor(name, list(shape), dt, kind="ExternalOutput").ap()

    @staticmethod
    def _key(ap):
        t = getattr(ap, "tensor", ap)
        return t.name

    def _needs(self, reads, writes):
        needs = {}

        def need(sv):
            sem, val = sv
            k = sem.name if hasattr(sem, "name") else id(sem)
            if k not in needs or needs[k][1] < val:
                needs[k] = (sem, val)

        for ap in reads:
            st = self.tr.get(self._key(ap))
            if st and st[0]:
                need(st[0])
        for ap in writes:
            st = self.tr.get(self._key(ap))
            if st:
                if st[0]:
                    need(st[0])
                for sv in st[1].values():
                    need(sv)
        return needs

    def _emit_waits(self, e, needs, skip_sem=None):
        eng = self.engs[e]
        seen = self.seen[e]
        for k, (sem, val) in needs.items():
            if skip_sem is not None and sem is skip_sem:
                continue
            if seen.get(k, -1) >= val:
                continue
            eng.wait_ge(sem, val)
            seen[k] = val

    def _update(self, reads, writes, sv):
        sem, val = sv
        k = sem.name if hasattr(sem, "name") else id(sem)
        for ap in reads:
            st = self.tr.setdefault(self._key(ap), [None, {}])
            st[1][k] = sv
        for ap in writes:
            st = self.tr.setdefault(self._key(ap), [None, {}])
            st[0] = sv
            st[1] = {}

    def op(self, e, fn, reads=(), writes=(), same_ok=False):
        needs = self._needs(reads, writes)
        self._emit_waits(e, needs, skip_sem=self.esem[e] if same_ok else None)
        inst = fn()
        self.ecnt[e] += 1
        inst.then_inc(self.esem[e], 1)
        self._update(reads, writes, (self.esem[e], self.ecnt[e]))
        self.n_inst += 1
        return inst

    def dma(self, q, out, in_, fn=None, extra_reads=(), **kw):
        reads, writes = [in_] + list(extra_reads), [out]
        needs = self._needs(reads, writes)
        self._emit_waits(q, needs)
        sbt = None
        for ap in (out, in_):
            if "sbuf" in str(ap.space).lower() or "sb" == str(ap.space).lower():
                sbt = ap
        keyt = self._key(sbt if sbt is not None else out)
        if keyt not in self.dsem:
            self.dsem[keyt] = [self.nc.alloc_semaphore(name=f"d_{len(self.dsem)}"), 0]
        ds = self.dsem[keyt]
        if fn is None:
            inst = self.engs[q].dma_start(out=out, in_=in_, **kw)
        else:
            inst = fn()
        ds[1] += 16
        inst.then_inc(ds[0], 16)
        self._update(reads, writes, (ds[0], ds[1]))
        self.n_inst += 1
        return inst

    def finish(self):
        sp = self.engs["sp"]
        for e in self.engs:
            if self.ecnt[e] > 0 and e != "sp":
                sp.wait_ge(self.esem[e], self.ecnt[e])
        for k, (sem, cnt) in self.dsem.items():
            sp.wait_ge(sem, cnt)
        self.nc.all_engine_barrier()
        self.es.close()
        return self.nc


D = 1024
ALPHA = (2.0 * 4) ** 0.25
LN_EPS = 1e-5


def _consts(k, need_iota=False):
    ident_d = k.dram_in("ident", [128, 128])
    ident = k.sb("ident_sb", [128, 128])
    k.dma("sp", ident[:], ident_d[:, :])
    return ident


def _layernorm_tile(k, t, tmp, stats, mv, rstd, epst):
    nc = k.nc
    for j in range(2):
        k.op("dve", lambda j=j: nc.vector.bn_stats(out=stats[:, j * 6:(j + 1) * 6], in_=t[:, j * 512:(j + 1) * 512]),
             reads=[t], writes=[stats])
    k.op("dve", lambda: nc.vector.bn_aggr(out=mv[:, 0:2], in_=stats[:, 0:12]), reads=[stats], writes=[mv])
    k.op("act", lambda: nc.scalar.activation(out=rstd[:, 0:1], in_=mv[:, 1:2], func=AF.Sqrt, bias=epst[:, 0:1], scale=1.0),
         reads=[mv, epst], writes=[rstd])
    k.op("dve", lambda: nc.vector.reciprocal(out=rstd[:, 0:1], in_=rstd[:, 0:1]), reads=[rstd], writes=[rstd])
    k.op("dve", lambda: nc.vector.tensor_scalar(out=t[:], in0=t[:], scalar1=mv[:, 0:1], scalar2=rstd[:, 0:1],
                                                op0=ALU.subtract, op1=ALU.mult), reads=[t, mv, rstd], writes=[t])


def build_C2(NX, NCTX):
    k = KB()
    nc = k.nc
    TOK = NX + NCTX
    x1_d = k.dram_in("x1", [TOK, D])
    mod_d = k.dram_in("mod", [2, 3, 128, D])
    lnp_d = k.dram_in("lnp", [2, 128, D])
    wq_d = k.dram_in("wq", [128, 8, 2048])
    keysT_d = k.dram_in("keysT", [128, 16, 128])
    iota_d = k.dram_in("iota", [128, 256])
    u_d = k.dram_in("u_tab", [16384, D])
    v_d = k.dram_in("v_tab", [16384, D])
    out_d = k.dram_out("xout", [TOK, D])
    ident = _consts(k)

    wq = k.sb("wq_sb", [128, 8, 2048])
    keysT = k.sb("keysT_sb", [128, 16, 128])
    iota = k.sb("iota_sb", [128, 256])
    modt = [k.sb(f"mod{j}", [128, D]) for j in range(3)]
    lng = k.sb("lng", [128, D]); lnb = k.sb("lnb", [128, D])
    x1t = k.sb("x1t", [128, D]); h2 = k.sb("h2", [128, D]); acc = k.sb("acc", [128, D])
    junk = k.sb("junk", [128, D])
    NB = 12
    gb = [k.sb(f"gb{j}", [128, D]) for j in range(NB)]
    T = k.sb("T", [128, 8, 128]); qT = k.sb("qT", [128, 16, 128])
    R1 = k.sb("R1", [128, 16, 128]); R2 = k.sb("R2", [128, 16, 128]); R3 = k.sb("R3", [128, 8, 256])
    sv = k.sb("sv", [128, 16, 16]); si = k.sb("si", [128, 16, 16], U32); sif = k.sb("sif", [128, 16, 16])
    fv = k.sb("fv", [128, 8, 16]); fi = k.sb("fi", [128, 8, 16], U32); fif = k.sb("fif", [128, 8, 16])
    eidf = k.sb("eidf", [128, 128]); eid = k.sb("eid", [128, 128], U32)
    negm = k.sb("negm", [128, 8]); gs = k.sb("gs", [128, 8]); gate = k.sb("gate", [128, 8, 16])
    actv = k.sb("actv", [128, 128]); wgt = k.sb("wgt", [128, 128])
    stats = k.sb("stats", [128, 12]); mv = k.sb("mv", [128, 2]); rstd = k.sb("rstd", [128, 1])
    epst = k.sb("epst", [128, 1])
    pbank = [k.ps(f"pb{j}", [128, 4, 128]) for j in range(4)]

    k.op("pool", lambda: nc.gpsimd.memset(epst[:], LN_EPS), writes=[epst])
    k.op("pool", lambda: nc.gpsimd.memset(eid[:], 0), writes=[eid])
    for kc in range(8):
        k.dma("sp", wq[:, kc, :], wq_d[:, kc, :])
    k.dma("sp", keysT[:], keysT_d[:, :, :])
    k.dma("sp", iota[:], iota_d[:, :])
    k.dma("sp", lng[:], lnp_d[0]); k.dma("sp", lnb[:], lnp_d[1])

    tiles = [(i * 128, 128, 0) for i in range(NX // 128)]
    if NCTX:
        tiles.append((NX, NCTX, 1))
    cur_ty = None
    V = nc.vector
    for (r0, n, ty) in tiles:
        if ty != cur_ty:
            for j in range(3):
                k.dma("sp", modt[j][:], mod_d[ty, j])
            k.op("dve", lambda: V.tensor_scalar_add(out=modt[1][:], in0=modt[1][:], scalar1=1.0), reads=[modt[1]], writes=[modt[1]])
            cur_ty = ty
        k.dma("sp", x1t[:n, :], x1_d[r0:r0 + n, :])
        k.op("dve", lambda: V.tensor_tensor(out=h2[:], in0=x1t[:], in1=modt[1][:], op=ALU.mult), reads=[x1t, modt[1]], writes=[h2])
        k.op("dve", lambda: V.tensor_tensor(out=h2[:], in0=h2[:], in1=modt[0][:], op=ALU.add), reads=[h2, modt[0]], writes=[h2])
        for half in range(2):
            pb = pbank[half]
            for j in range(4):
                kc = half * 4 + j
                k.op("pe", lambda pb=pb, j=j, kc=kc: nc.tensor.transpose(out=pb[:, j, :], in_=h2[:, kc * 128:(kc + 1) * 128], identity=ident[:]),
                     reads=[h2, ident], writes=[pb], same_ok=True)
            k.op("act", lambda pb=pb, half=half: nc.scalar.copy(out=T[:, half * 4:(half + 1) * 4, :], in_=pb[:]), reads=[pb], writes=[T])
        for g4 in range(4):
            pb = pbank[g4]
            for j in range(4):
                hp = g4 * 4 + j
                for kc in range(8):
                    k.op("pe", lambda pb=pb, j=j, hp=hp, kc=kc: nc.tensor.matmul(pb[:, j, :], wq[:, kc, hp * 128:(hp + 1) * 128], T[:, kc, :],
                                                                              start=(kc == 0), stop=(kc == 7)),
                         reads=[wq, T], writes=[pb], same_ok=True)
            k.op("act", lambda pb=pb, g4=g4: nc.scalar.copy(out=qT[:, g4 * 4:(g4 + 1) * 4, :], in_=pb[:]), reads=[pb], writes=[qT])
        for g4 in range(4):
            pb = pbank[g4]
            for j in range(4):
                hp = g4 * 4 + j
                k.op("pe", lambda pb=pb, j=j, hp=hp: nc.tensor.matmul(pb[:, j, :], qT[:, hp, :], keysT[:, hp, :], start=True, stop=True),
                     reads=[qT, keysT], writes=[pb], same_ok=True)
            k.op("act", lambda pb=pb, g4=g4: nc.scalar.copy(out=R1[:, g4 * 4:(g4 + 1) * 4, :], in_=pb[:]), reads=[pb], writes=[R1])
        for hp in range(16):
            k.op("dve", lambda hp=hp: V.max(out=sv[:, hp, 0:8], in_=R1[:, hp, :]), reads=[R1], writes=[sv])
            k.op("dve", lambda hp=hp: V.max_index(out=si[:, hp, 0:8], in_max=sv[:, hp, 0:8], in_values=R1[:, hp, :]), reads=[R1, sv], writes=[si])
            k.op("dve", lambda hp=hp: V.match_replace(out=R2[:, hp, :], in_to_replace=sv[:, hp, 0:8], in_values=R1[:, hp, :], imm_value=-1e30),
                 reads=[R1, sv], writes=[R2])
            k.op("dve", lambda hp=hp: V.max(out=sv[:, hp, 8:16], in_=R2[:, hp, :]), reads=[R2], writes=[sv])
            k.op("dve", lambda hp=hp: V.max_index(out=si[:, hp, 8:16], in_max=sv[:, hp, 8:16], in_values=R2[:, hp, :]), reads=[R2, sv], writes=[si])
        k.op("dve", lambda: V.tensor_copy(out=sif[:], in_=si[:]), reads=[si], writes=[sif])
        sv4 = sv[:].rearrange("p (h two) k -> p h two k", two=2)
        sif4 = sif[:].rearrange("p (h two) k -> p h two k", two=2)
        cand = R1[:].rearrange("p (h a) (b j) -> p h (a b) j", a=2, b=8)
        cand_flat = R1[:].rearrange("p (h a) m -> p h (a m)", a=2)
        cand2_flat = R2[:].rearrange("p (h a) m -> p h (a m)", a=2)
        cidx = R3[:].rearrange("p h (i j) -> p h i j", i=16)
        k.op("dve", lambda: V.tensor_tensor(out=cand, in0=sv4[:, :, 0, :, None].broadcast_to([128, 8, 16, 16]),
                                            in1=sv4[:, :, 1, None, :].broadcast_to([128, 8, 16, 16]), op=ALU.add),
             reads=[sv], writes=[R1])
        k.op("dve", lambda: V.tensor_scalar_mul(out=sif4[:, :, 0, :], in0=sif4[:, :, 0, :], scalar1=128.0), reads=[sif], writes=[sif])
        k.op("dve", lambda: V.tensor_tensor(out=cidx, in0=sif4[:, :, 0, :, None].broadcast_to([128, 8, 16, 16]),
                                            in1=sif4[:, :, 1, None, :].broadcast_to([128, 8, 16, 16]), op=ALU.add),
             reads=[sif], writes=[R3])
        for h in range(8):
            k.op("dve", lambda h=h: V.max(out=fv[:, h, 0:8], in_=cand_flat[:, h, :]), reads=[R1], writes=[fv])
            k.op("dve", lambda h=h: V.max_index(out=fi[:, h, 0:8], in_max=fv[:, h, 0:8], in_values=cand_flat[:, h, :]), reads=[R1, fv], writes=[fi])
            k.op("dve", lambda h=h: V.match_replace(out=cand2_flat[:, h, :], in_to_replace=fv[:, h, 0:8], in_values=cand_flat[:, h, :], imm_value=-1e30),
                 reads=[R1, fv], writes=[R2])
            k.op("dve", lambda h=h: V.max(out=fv[:, h, 8:16], in_=cand2_flat[:, h, :]), reads=[R2], writes=[fv])
            k.op("dve", lambda h=h: V.max_index(out=fi[:, h, 8:16], in_max=fv[:, h, 8:16], in_values=cand2_flat[:, h, :]), reads=[R2, fv], writes=[fi])
        k.op("dve", lambda: V.tensor_copy(out=fif[:], in_=fi[:]), reads=[fi], writes=[fif])
        for h in range(8):
            for j in range(16):
                e = h * 16 + j
                k.op("dve", lambda h=h, j=j, e=e: V.scalar_tensor_tensor(out=junk[:, 0:256], in0=iota[:], scalar=fif[:, h, j:j + 1], in1=R3[:, h, :],
                                                                        op0=ALU.is_equal, op1=ALU.mult, accum_out=eidf[:, e:e + 1]),
                     reads=[iota, fif, R3], writes=[junk, eidf])
        k.op("dve", lambda: V.tensor_copy(out=eid[:], in_=eidf[:]), reads=[eidf], writes=[eid])
        k.op("dve", lambda: V.tensor_scalar_mul(out=negm[:], in0=fv[:, :, 0], scalar1=-1.0), reads=[fv], writes=[negm])
        for h in range(8):
            k.op("act", lambda h=h: nc.scalar.activation(out=gate[:, h, :], in_=fv[:, h, :], func=AF.Exp, bias=negm[:, h:h + 1], scale=1.0,
                                                         accum_out=gs[:, h:h + 1]), reads=[fv, negm], writes=[gate, gs])
        k.op("dve", lambda: V.reciprocal(out=gs[:], in_=gs[:]), reads=[gs], writes=[gs])
        k.op("dve", lambda: V.tensor_tensor(out=gate[:], in0=gate[:], in1=gs[:, :, None].broadcast_to([128, 8, 16]), op=ALU.mult),
             reads=[gate, gs], writes=[gate])
        for e in range(128):
            b = gb[e % NB]
            k.dma("pool", b[:], u_d[:, :], fn=lambda b=b, e=e: nc.gpsimd.indirect_dma_start(
                out=b[:], out_offset=None, in_=u_d[:, :], in_offset=bass.IndirectOffsetOnAxis(ap=eid[:, e:e + 1], axis=0)),
                extra_reads=[eid])
            k.op("dve", lambda b=b, e=e: V.scalar_tensor_tensor(out=junk[:], in0=b[:], scalar=1.0, in1=h2[:],
                                                                op0=ALU.mult, op1=ALU.mult, accum_out=actv[:, e:e + 1]),
                 reads=[b, h2], writes=[junk, actv])
        k.op("act", lambda: nc.scalar.activation(out=wgt[:], in_=actv[:], func=AF.Gelu), reads=[actv], writes=[wgt])
        k.op("dve", lambda: V.tensor_tensor(out=wgt[:], in0=wgt[:], in1=gate[:].rearrange("p h j -> p (h j)"), op=ALU.mult),
             reads=[wgt, gate], writes=[wgt])
        for e in range(128):
            b = gb[e % NB]
            k.dma("pool", b[:], v_d[:, :], fn=lambda b=b, e=e: nc.gpsimd.indirect_dma_start(
                out=b[:], out_offset=None, in_=v_d[:, :], in_offset=bass.IndirectOffsetOnAxis(ap=eid[:, e:e + 1], axis=0)),
                extra_reads=[eid])
            if e == 0:
                k.op("dve", lambda b=b, e=e: V.tensor_scalar_mul(out=acc[:], in0=b[:], scalar1=wgt[:, 0:1]), reads=[b, wgt], writes=[acc])
            else:
                k.op("dve", lambda b=b, e=e: V.scalar_tensor_tensor(out=acc[:], in0=b[:], scalar=wgt[:, e:e + 1], in1=acc[:],
                                                                    op0=ALU.mult, op1=ALU.add), reads=[b, wgt, acc], writes=[acc])
        k.op("dve", lambda: V.tensor_tensor(out=acc[:], in0=acc[:], in1=modt[2][:], op=ALU.mult), reads=[acc, modt[2]], writes=[acc])
        k.op("dve", lambda: V.scalar_tensor_tensor(out=acc[:], in0=x1t[:], scalar=ALPHA, in1=acc[:], op0=ALU.mult, op1=ALU.add),
             reads=[x1t, acc], writes=[acc])
        _layernorm_tile(k, acc, junk, stats, mv, rstd, epst)
        k.op("dve", lambda: V.tensor_tensor(out=acc[:], in0=acc[:], in1=lng[:], op=ALU.mult), reads=[acc, lng], writes=[acc])
        k.op("dve", lambda: V.tensor_tensor(out=junk[:], in0=acc[:], in1=lnb[:], op=ALU.add), reads=[acc, lnb], writes=[junk])
        k.dma("sp", out_d[r0:r0 + n, :], junk[:n, :])
    return k.finish()


def build_P0():
    k = KB(); nc = k.nc
    cT_d = k.dram_in("cT", [128, 8, 2])
    w_d = k.dram_in("w", [128, 8, 3072])
    b_d = k.dram_in("b", [2, 3072])
    out_d = k.dram_out("mod", [2, 3072])
    cT = k.sb("cT_sb", [128, 8, 2]); sT = k.sb("sT", [128, 8, 2])
    w = k.sb("w_sb", [128, 8, 3072]); b = k.sb("b_sb", [2, 3072]); o = k.sb("o_sb", [2, 3072])
    ps = [k.ps(f"ps{j}", [2, 512]) for j in range(2)]
    k.dma("sp", cT[:], cT_d[:, :, :]); k.dma("sp", b[:], b_d[:, :])
    for kc in range(8):
        k.dma("sp", w[:, kc, :], w_d[:, kc, :])
    k.op("act", lambda: nc.scalar.activation(out=sT[:], in_=cT[:], func=AF.Silu), reads=[cT], writes=[sT])
    for j in range(6):
        p = ps[j % 2]
        for kc in range(8):
            k.op("pe", lambda p=p, j=j, kc=kc: nc.tensor.matmul(p[:, :], sT[:, kc, :], w[:, kc, j * 512:(j + 1) * 512], start=(kc == 0), stop=(kc == 7)),
                 reads=[sT, w], writes=[p], same_ok=True)
        k.op("dve", lambda p=p, j=j: nc.vector.tensor_tensor(out=o[:, j * 512:(j + 1) * 512], in0=p[:, :], in1=b[:, j * 512:(j + 1) * 512], op=ALU.add),
             reads=[p, b], writes=[o])
    k.dma("sp", out_d[:, :], o[:])
    return k.finish()


def build_A_ml(NXc, NC):
    k = KB(); nc = k.nc; V = nc.vector
    NXE = NXc + 128
    R = NXc // 64
    NT = NXE + NC
    xT_d = k.dram_in("xT", [128, 8, NT])
    mod_d = k.dram_in("mod", [128, 2, 8, 2])
    w_d = k.dram_in("w_in", [128, 8, 4112])
    b_d = k.dram_in("b_in", [128, 33])
    cw_d = k.dram_in("conv_w", [128, 16, 9])
    cb_d = k.dram_in("conv_b", [128, 16])
    hm_d = k.dram_in("hmask", [128, 2])
    NO = NXc + NC
    q_d = k.dram_out("qk", [16, 128, NO])
    v_d = k.dram_out("v", [8, 128, NO])
    o_d = k.dram_out("o", [8, 128, NO])
    g_d = k.dram_out("g", [16, NO])

    hT = k.sb("hT", [128, 8, NT])
    mod = k.sb("mod_sb", [128, 2, 8, 2]); bsb = k.sb("b_sb", [128, 33]); cw = k.sb("cw", [128, 16, 9]); cb = k.sb("cb", [128, 16])
    hm = k.sb("hm", [128, 2])
    wbuf = [k.sb(f"wb{j}", [128, 8, 128]) for j in range(3)]
    pbuf = [k.sb(f"pbuf{j}", [128, NT]) for j in range(2)]
    cbuf = [k.sb(f"cbuf{j}", [128, NO]) for j in range(2)]
    obuf = [k.sb(f"obuf{j}", [128, NO]) for j in range(2)]
    psb = [k.ps(f"ps{j}", [128, 512]) for j in range(4)]
    for kc in range(8):
        k.dma("sp", hT[:, kc, :], xT_d[:, kc, :])
    k.dma("sp", mod[:], mod_d[:, :, :, :]); k.dma("sp", bsb[:], b_d[:, :]); k.dma("sp", cw[:], cw_d[:, :, :])
    k.dma("sp", cb[:], cb_d[:, :]); k.dma("sp", hm[:], hm_d[:, :])
    k.op("dve", lambda: V.tensor_scalar_add(out=mod[:, :, :, 1], in0=mod[:, :, :, 1], scalar1=1.0), reads=[mod], writes=[mod])
    for kc in range(8):
        k.op("dve", lambda kc=kc: V.tensor_scalar(out=hT[:, kc, 0:NXE], in0=hT[:, kc, 0:NXE], scalar1=mod[:, 0, kc, 1:2], scalar2=mod[:, 0, kc, 0:1],
                                                  op0=ALU.mult, op1=ALU.add), reads=[hT, mod], writes=[hT])
        k.op("dve", lambda kc=kc: V.tensor_scalar(out=hT[:, kc, NXE:NT], in0=hT[:, kc, NXE:NT], scalar1=mod[:, 1, kc, 1:2], scalar2=mod[:, 1, kc, 0:1],
                                                  op0=ALU.mult, op1=ALU.add), reads=[hT, mod], writes=[hT])
    blocks = []
    t0 = 0
    while t0 < NT:
        blocks.append((t0, min(512, NT - t0))); t0 += 512
    pi = 0
    for oc in range(33):
        M = 128 if oc < 32 else 16
        wb = wbuf[oc % 3]
        k.dma("sp", wb[:, :, 0:M], w_d[:, :, oc * 128:oc * 128 + M])
        pb = pbuf[oc % 2]
        for (b0, bn) in blocks:
            p = psb[pi % 4]; pi += 1
            for kc in range(8):
                k.op("pe", lambda p=p, wb=wb, kc=kc, b0=b0, bn=bn, M=M: nc.tensor.matmul(p[0:M, 0:bn], wb[:, kc, 0:M], hT[:, kc, b0:b0 + bn],
                                                                                      start=(kc == 0), stop=(kc == 7)),
                     reads=[wb, hT], writes=[p], same_ok=True)
            fn = AF.Sigmoid if 24 <= oc < 32 else AF.Identity
            k.op("act", lambda p=p, pb=pb, b0=b0, bn=bn, M=M, oc=oc, fn=fn: nc.scalar.activation(out=pb[0:M, b0:b0 + bn], in_=p[0:M, 0:bn], func=fn,
                                                                                           bias=bsb[0:M, oc:oc + 1], scale=1.0),
                 reads=[p, bsb], writes=[pb])
        if oc < 16:
            k.op("dve", lambda pb=pb: V.tensor_scalar_mul(out=pb[:, 0:64], in0=pb[:, 0:64], scalar1=hm[:, 0:1]), reads=[pb, hm], writes=[pb])
            k.op("dve", lambda pb=pb: V.tensor_scalar_mul(out=pb[:, NXE - 64:NXE], in0=pb[:, NXE - 64:NXE], scalar1=hm[:, 1:2]), reads=[pb, hm], writes=[pb])
            cbf = cbuf[oc % 2]
            pg = pb[:, 0:NXE].rearrange("p (r c) -> p r c", c=64)
            cg = cbf[:, 0:NXc].rearrange("p (r c) -> p r c", c=64)
            k.op("dve", lambda pg=pg, cg=cg, oc=oc: V.tensor_scalar(out=cg, in0=pg[:, 1:R + 1, :], scalar1=cw[:, oc, 4:5], scalar2=cb[:, oc:oc + 1],
                                                                 op0=ALU.mult, op1=ALU.add), reads=[pb, cw, cb], writes=[cbf])
            for dr in range(3):
                for dc in range(3):
                    if dr == 1 and dc == 1:
                        continue
                    c0, c1 = (1, 64) if dc == 0 else ((0, 63) if dc == 2 else (0, 64))
                    k.op("dve", lambda pg=pg, cg=cg, oc=oc, dr=dr, dc=dc, c0=c0, c1=c1: V.scalar_tensor_tensor(
                        out=cg[:, :, c0:c1], in0=pg[:, dr:dr + R, c0 + dc - 1:c1 + dc - 1], scalar=cw[:, oc, dr * 3 + dc:dr * 3 + dc + 1],
                        in1=cg[:, :, c0:c1], op0=ALU.mult, op1=ALU.add), reads=[pb, cw, cbf], writes=[cbf])
            k.op("dve", lambda pb=pb, cbf=cbf, oc=oc: V.tensor_scalar(out=cbf[:, NXc:NO], in0=pb[:, NXE:NT], scalar1=cw[:, oc, 4:5], scalar2=cb[:, oc:oc + 1],
                                                                    op0=ALU.mult, op1=ALU.add), reads=[pb, cw, cb], writes=[cbf])
            k.op("dve", lambda pb=pb, cbf=cbf, oc=oc: V.scalar_tensor_tensor(out=cbf[:, NXc + 1:NO], in0=pb[:, NXE:NT - 1], scalar=cw[:, oc, 3:4],
                                                                           in1=cbf[:, NXc + 1:NO], op0=ALU.mult, op1=ALU.add), reads=[pb, cw, cbf], writes=[cbf])
            k.op("dve", lambda pb=pb, cbf=cbf, oc=oc: V.scalar_tensor_tensor(out=cbf[:, NXc:NO - 1], in0=pb[:, NXE + 1:NT], scalar=cw[:, oc, 5:6],
                                                                           in1=cbf[:, NXc:NO - 1], op0=ALU.mult, op1=ALU.add), reads=[pb, cw, cbf], writes=[cbf])
            ob = obuf[oc % 2]
            k.op("act", lambda ob=ob, cbf=cbf: nc.scalar.activation(out=ob[:], in_=cbf[:], func=AF.Silu), reads=[cbf], writes=[ob])
            if oc < 8:
                k.op("dve", lambda ob=ob: V.tensor_scalar_mul(out=ob[:], in0=ob[:], scalar1=1.0 / 16.0), reads=[ob], writes=[ob])
            k.dma("sp", q_d[oc], ob[:])
        elif oc < 32:
            dst = v_d if oc < 24 else o_d
            j = oc - 16 if oc < 24 else oc - 24
            k.dma("sp", dst[j][:, 0:NXc], pb[:, 64:64 + NXc])
            k.dma("sp", dst[j][:, NXc:NO], pb[:, NXE:NT])
        else:
            k.dma("sp", g_d[:, 0:NXc], pb[0:16, 64:64 + NXc])
            k.dma("sp", g_d[:, NXc:NO], pb[0:16, NXE:NT])
    return k.finish()


def build_B_ml(NCH):
    k = KB(); nc = k.nc; V = nc.vector
    qT_d = k.dram_in("qT", [128, 2, NCH * 128])
    kT_d = k.dram_in("kT", [128, 2, NCH * 128])
    k_d = k.dram_in("k", [128, NCH, 256])
    v_d = k.dram_in("v", [128, NCH, 257])
    ig_d = k.dram_in("ig", [128, NCH]); fg_d = k.dram_in("fg", [128, NCH])
    tri_d = k.dram_in("tri", [128, 128]); mk_d = k.dram_in("maskT", [128, 128]); ones_d = k.dram_in("ones", [128, 128])
    h_d = k.dram_out("h", [128, NCH, 256])
    ident = _consts(k)
    tri = k.sb("tri_sb", [128, 128]); mk = k.sb("mk_sb", [128, 128]); ones = k.sb("ones_sb", [128, 128])
    ig = k.sb("ig_sb", [128, NCH]); LF = k.sb("LF", [128, NCH])
    k.dma("sp", tri[:], tri_d[:, :]); k.dma("sp", mk[:], mk_d[:, :]); k.dma("sp", ones[:], ones_d[:, :])
    k.dma("sp", ig[:], ig_d[:, :]); k.dma("sp", LF[:], fg_d[:, :])
    k.op("act", lambda: nc.scalar.activation(out=LF[:], in_=LF[:], func=AF.Exp, scale=-1.0), reads=[LF], writes=[LF])
    k.op("act", lambda: nc.scalar.activation(out=LF[:], in_=LF[:], func=AF.Ln, bias=1.0, scale=1.0), reads=[LF], writes=[LF])
    k.op("dve", lambda: V.tensor_scalar_mul(out=LF[:], in0=LF[:], scalar1=-1.0), reads=[LF], writes=[LF])
    NS = 3
    qb = [k.sb(f"qb{j}", [128, 2, 128]) for j in range(NS)]
    kb = [k.sb(f"kb{j}", [128, 2, 128]) for j in range(NS)]
    kt = [k.sb(f"kt{j}", [128, 256]) for j in range(NS)]
    vb = [k.sb(f"vb{j}", [128, 257]) for j in range(NS)]
    hb = [k.sb(f"hb{j}", [128, 256]) for j in range(2)]
    Cst = [k.sb(f"Cst{j}", [128, 257]) for j in range(2)]
    LFb = k.sb("LFb", [128, 128]); DT = k.sb("DT", [128, 128]); EB = k.sb("EB", [128, 128]); ST = k.sb("ST", [128, 128])
    qs = k.sb("qs", [128, 2, 128]); ka = k.sb("ka", [128, 256])
    wcol = k.sb("wcol", [128, 1]); acol = k.sb("acol", [128, 1]); Gc = k.sb("Gc", [128, 1]); den = k.sb("den", [128, 1])
    psA = k.ps("psA", [128, 128]); psB = k.ps("psB", [128, 128]); psC = k.ps("psC", [128, 2]); psS = k.ps("psS", [128, 128])
    psN = k.ps("psN", [128, 257]); psU = [k.ps(f"psU{j}", [128, 257]) for j in range(2)]
    for j in range(2):
        k.op("pool", lambda j=j: nc.gpsimd.memset(Cst[j][:], 0.0), writes=[Cst[j]])

    def load(c):
        s = c % NS
        k.dma("sp", qb[s][:], qT_d[:, :, c * 128:(c + 1) * 128])
        k.dma("sp", kb[s][:], kT_d[:, :, c * 128:(c + 1) * 128])
        k.dma("sp", kt[s][:], k_d[:, c, :])
        k.dma("sp", vb[s][:], v_d[:, c, :])
    load(0)
    if NCH > 1:
        load(1)
    for c in range(NCH):
        s = c % NS
        if c + 2 < NCH:
            load(c + 2)
        k.op("dve", lambda c=c: V.tensor_scalar_mul(out=LFb[:], in0=ones[:], scalar1=LF[:, c:c + 1]), reads=[ones, LF], writes=[LFb])
        k.op("pe", lambda: nc.tensor.matmul(psA[:, :], LFb[:], tri[:], start=True, stop=False), reads=[LFb, tri], writes=[psA], same_ok=True)
        k.op("pe", lambda: nc.tensor.matmul(psA[:, :], ident[:], mk[:], start=False, stop=True), reads=[ident, mk], writes=[psA], same_ok=True)
        k.op("pe", lambda: nc.tensor.matmul(psB[:, :], LFb[:], tri[:], start=True, stop=True), reads=[LFb, tri], writes=[psB], same_ok=True)
        k.op("pe", lambda c=c: nc.tensor.matmul(psC[:, 0:1], tri[:], LF[:, c:c + 1], start=True, stop=True), reads=[tri, LF], writes=[psC], same_ok=True)
        k.op("pe", lambda c=c: nc.tensor.matmul(psC[:, 1:2], ones[:], LF[:, c:c + 1], start=True, stop=True), reads=[ones, LF], writes=[psC], same_ok=True)
        k.op("dve", lambda c=c: V.tensor_tensor(out=wcol[:], in0=ig[:, c:c + 1], in1=psC[:, 0:1], op=ALU.subtract), reads=[ig, psC], writes=[wcol])
        k.op("act", lambda: nc.scalar.activation(out=DT[:], in_=psA[:, :], func=AF.Exp, bias=wcol[:, 0:1], scale=1.0), reads=[psA, wcol], writes=[DT])
        k.op("act", lambda: nc.scalar.activation(out=EB[:], in_=psB[:, :], func=AF.Exp), reads=[psB], writes=[EB])
        k.op("act", lambda: nc.scalar.activation(out=acol[:], in_=psC[:, 1:2], func=AF.Exp, bias=wcol[:, 0:1], scale=1.0), reads=[psC, wcol], writes=[acol])
        k.op("act", lambda: nc.scalar.activation(out=Gc[:], in_=psC[:, 1:2], func=AF.Exp), reads=[psC], writes=[Gc])
        for dc in range(2):
            k.op("pe", lambda s=s, dc=dc: nc.tensor.matmul(psS[:, :], kb[s][:, dc, :], qb[s][:, dc, :], start=(dc == 0), stop=(dc == 1)),
                 reads=[kb[s], qb[s]], writes=[psS], same_ok=True)
        k.op("dve", lambda: V.tensor_tensor(out=ST[:], in0=psS[:, :], in1=DT[:], op=ALU.mult), reads=[psS, DT], writes=[ST])
        k.op("dve", lambda s=s: V.tensor_tensor(out=qs[:], in0=qb[s][:], in1=EB[:, None, :].broadcast_to([128, 2, 128]), op=ALU.mult),
             reads=[qb[s], EB], writes=[qs])
        k.op("pe", lambda s=s: nc.tensor.matmul(psN[:, :], ST[:], vb[s][:], start=True, stop=False), reads=[ST, vb[s]], writes=[psN], same_ok=True)
        for dc in range(2):
            k.op("pe", lambda dc=dc: nc.tensor.matmul(psN[:, :], qs[:, dc, :], Cst[dc][:], start=False, stop=(dc == 1)),
                 reads=[qs, Cst[dc]], writes=[psN], same_ok=True)
        k.op("act", lambda: nc.scalar.activation(out=den[:], in_=psN[:, 256:257], func=AF.Abs), reads=[psN], writes=[den])
        k.op("dve", lambda: V.tensor_scalar_max(out=den[:], in0=den[:], scalar1=1.0), reads=[den], writes=[den])
        k.op("dve", lambda: V.reciprocal(out=den[:], in_=den[:]), reads=[den], writes=[den])
        hbb = hb[c % 2]
        k.op("dve", lambda hbb=hbb: V.tensor_scalar_mul(out=hbb[:], in0=psN[:, 0:256], scalar1=den[:, 0:1]), reads=[psN, den], writes=[hbb])
        k.dma("sp", h_d[:, c, :], hbb[:])
        k.op("pool", lambda s=s: nc.gpsimd.tensor_scalar(out=ka[:], in0=kt[s][:], scalar1=acol[:, 0:1], scalar2=None, op0=ALU.mult),
             reads=[kt[s], acol], writes=[ka])
        for dc in range(2):
            k.op("pe", lambda s=s, dc=dc: nc.tensor.matmul(psU[dc][:, :], ka[:, dc * 128:(dc + 1) * 128], vb[s][:], start=True, stop=True),
                 reads=[ka, vb[s]], writes=[psU[dc]], same_ok=True)
            k.op("dve", lambda dc=dc: V.scalar_tensor_tensor(out=Cst[dc][:], in0=Cst[dc][:], scalar=Gc[:, 0:1], in1=psU[dc][:, :], op0=ALU.mult, op1=ALU.add),
                 reads=[Cst[dc], Gc, psU[dc]], writes=[Cst[dc]])
    return k.finish()


def build_C1(kind, NX, NCTX):
    k = KB(); nc = k.nc; V = nc.vector
    TOK = NX + NCTX
    ml = kind == "ml"
    H, dh, eps = (4, 256, 1e-5) if ml else (16, 64, 64e-5)
    NPIECE = 3 if ml else 6
    NPRM = 1 if ml else 3
    xres_d = k.dram_in("xres", [TOK, D])
    pc_d = [k.dram_in(f"piece{j}", [TOK, D]) for j in range(NPIECE)]
    prm_d = k.dram_in("prm", [NPRM, 128, D])
    mod_d = k.dram_in("mod", [2, 128, D])
    lnp_d = k.dram_in("lnp", [2, 128, D])
    w_d = k.dram_in("w_out", [128, 8, D])
    out_d = k.dram_out("x1", [TOK, D])
    ident = _consts(k)
    w = k.sb("w_sb", [128, 8, D]); prm = [k.sb(f"prm{j}", [128, D]) for j in range(NPRM)]
    g1 = k.sb("g1", [128, D]); lng = k.sb("lng", [128, D]); lnb = k.sb("lnb", [128, D])
    xr = k.sb("xr", [128, D]); A = k.sb("A", [128, D]); B = k.sb("B", [128, D]); C = k.sb("C", [128, D])
    zT = k.sb("zT", [128, 8, 128]); t1 = k.sb("t1", [128, D]); junk = k.sb("junk", [128, D])
    sm = k.sb("sm", [128, H]); vs = k.sb("vs", [128, H]); bs = k.sb("bs", [128, H])
    stats = k.sb("stats", [128, 12]); mv = k.sb("mv", [128, 2]); rstd = k.sb("rstd", [128, 1])
    epst = k.sb("epst", [128, 1]); epsh = k.sb("epsh", [128, 1])
    pbank = [k.ps(f"pb{j}", [128, 512]) for j in range(4)]
    k.op("pool", lambda: nc.gpsimd.memset(epst[:], LN_EPS), writes=[epst])
    k.op("pool", lambda: nc.gpsimd.memset(epsh[:], eps), writes=[epsh])
    for kc in range(8):
        k.dma("sp", w[:, kc, :], w_d[:, kc, :])
    for j in range(NPRM):
        k.dma("sp", prm[j][:], prm_d[j])
    k.dma("sp", lng[:], lnp_d[0]); k.dma("sp", lnb[:], lnp_d[1])
    tiles = [(i * 128, 128, 0) for i in range(NX // 128)]
    if NCTX:
        tiles.append((NX, NCTX, 1))
    cur_ty = None
    hv = lambda t: t[:].rearrange("p (h e) -> p h e", h=H)
    bc = lambda s: s[:, :, None].broadcast_to([128, H, dh])
    for (r0, n, ty) in tiles:
        if ty != cur_ty:
            k.dma("sp", g1[:], mod_d[ty]); cur_ty = ty
        rs = slice(r0, r0 + n)
        k.dma("sp", xr[:n, :], xres_d[rs, :])
        k.dma("sp", A[:n, :], pc_d[0][rs, :]); k.dma("sp", B[:n, :], pc_d[1][rs, :])
        k.op("dve", lambda: V.tensor_tensor(out=A[:], in0=A[:], in1=B[:], op=ALU.add), reads=[A, B], writes=[A])
        k.op("dve", lambda: V.tensor_reduce(out=sm[:], in_=hv(A), axis=mybir.AxisListType.X, op=ALU.add), reads=[A], writes=[sm])
        k.op("dve", lambda: V.tensor_scalar_mul(out=sm[:], in0=sm[:], scalar1=1.0 / dh), reads=[sm], writes=[sm])
        k.op("dve", lambda: V.tensor_tensor(out=hv(A), in0=hv(A), in1=bc(sm), op=ALU.subtract), reads=[A, sm], writes=[A])
        k.op("dve", lambda: V.tensor_tensor(out=junk[:], in0=A[:], in1=A[:], op=ALU.mult), reads=[A], writes=[junk])
        k.op("dve", lambda: V.tensor_reduce(out=vs[:], in_=hv(junk), axis=mybir.AxisListType.X, op=ALU.add), reads=[junk], writes=[vs])
        k.op("act", lambda: nc.scalar.activation(out=vs[:], in_=vs[:], func=AF.Sqrt, bias=epsh[:, 0:1], scale=1.0 / dh), reads=[vs, epsh], writes=[vs])
        k.op("dve", lambda: V.reciprocal(out=vs[:], in_=vs[:]), reads=[vs], writes=[vs])
        k.op("dve", lambda: V.tensor_tensor(out=hv(A), in0=hv(A), in1=bc(vs), op=ALU.mult), reads=[A, vs], writes=[A])
        if ml:
            k.dma("sp", C[:n, :], pc_d[2][rs, :])
            k.op("dve", lambda: V.tensor_tensor(out=A[:], in0=A[:], in1=C[:], op=ALU.mult), reads=[A, C], writes=[A])
            k.op("dve", lambda: V.tensor_tensor(out=A[:], in0=A[:], in1=prm[0][:], op=ALU.mult), reads=[A, prm[0]], writes=[A])
        else:
            k.op("dve", lambda: V.tensor_tensor(out=A[:], in0=A[:], in1=prm[0][:], op=ALU.mult), reads=[A, prm[0]], writes=[A])
            k.op("dve", lambda: V.tensor_tensor(out=A[:], in0=A[:], in1=prm[1][:], op=ALU.add), reads=[A, prm[1]], writes=[A])
            k.dma("sp", B[:n, :], pc_d[2][rs, :]); k.dma("sp", C[:n, :], pc_d[3][rs, :])
            k.op("dve", lambda: V.tensor_tensor(out=B[:], in0=B[:], in1=C[:], op=ALU.mult), reads=[B, C], writes=[B])
            k.op("dve", lambda: V.tensor_tensor(out=B[:], in0=B[:], in1=prm[2][:], op=ALU.mult), reads=[B, prm[2]], writes=[B])
            k.op("dve", lambda: V.tensor_reduce(out=bs[:], in_=hv(B), axis=mybir.AxisListType.X, op=ALU.add), reads=[B], writes=[bs])
            k.dma("sp", C[:n, :], pc_d[4][rs, :])
            k.op("dve", lambda: V.tensor_tensor(out=hv(C), in0=hv(C), in1=bc(bs), op=ALU.mult), reads=[C, bs], writes=[C])
            k.op("dve", lambda: V.tensor_tensor(out=A[:], in0=A[:], in1=C[:], op=ALU.add), reads=[A, C], writes=[A])
            k.dma("sp", B[:n, :], pc_d[5][rs, :])
            k.op("dve", lambda: V.tensor_tensor(out=A[:], in0=A[:], in1=B[:], op=ALU.mult), reads=[A, B], writes=[A])
        for half in range(2):
            pb = pbank[half]
            for j in range(4):
                kc = half * 4 + j
                k.op("pe", lambda pb=pb, j=j, kc=kc: nc.tensor.transpose(out=pb[:, j * 128:(j + 1) * 128], in_=A[:, kc * 128:(kc + 1) * 128], identity=ident[:]),
                     reads=[A, ident], writes=[pb], same_ok=True)
            k.op("act", lambda pb=pb, half=half: nc.scalar.copy(out=zT[:, half * 4:(half + 1) * 4, :].rearrange("p a b -> p (a b)"), in_=pb[:, :]),
                 reads=[pb], writes=[zT])
        for half in range(2):
            pb = pbank[2 + half]
            for kc in range(8):
                k.op("pe", lambda pb=pb, kc=kc, half=half: nc.tensor.matmul(pb[:, :], zT[:, kc, :], w[:, kc, half * 512:(half + 1) * 512],
                                                                          start=(kc == 0), stop=(kc == 7)), reads=[zT, w], writes=[pb], same_ok=True)
            k.op("dve", lambda pb=pb, half=half: V.tensor_tensor(out=t1[:, half * 512:(half + 1) * 512], in0=pb[:, :], in1=g1[:, half * 512:(half + 1) * 512],
                                                                 op=ALU.mult), reads=[pb, g1], writes=[t1])
        k.op("dve", lambda: V.scalar_tensor_tensor(out=t1[:], in0=xr[:], scalar=ALPHA, in1=t1[:], op0=ALU.mult, op1=ALU.add), reads=[xr, t1], writes=[t1])
        _layernorm_tile(k, t1, junk, stats, mv, rstd, epst)
        k.op("dve", lambda: V.tensor_tensor(out=t1[:], in0=t1[:], in1=lng[:], op=ALU.mult), reads=[t1, lng], writes=[t1])
        k.op("dve", lambda: V.tensor_tensor(out=junk[:], in0=t1[:], in1=lnb[:], op=ALU.add), reads=[t1, lnb], writes=[junk])
        k.dma("sp", out_d[rs, :], junk[:n, :])
    return k.finish()


NCORES = 8
_PROGS = {}


def _run(key, builder, in_maps):
    if key not in _PROGS:
        _PROGS[key] = builder()
    res = run_bass_kernel_spmd(_PROGS[key], in_maps, core_ids=list(range(len(in_maps))))
    return res.results


def _f(a):
    return np.ascontiguousarray(a, dtype=np.float32)


def _bc(v):
    return _f(np.broadcast_to(np.asarray(v, np.float32), (128, v.shape[-1])))


def _kc_layout(w):
    return _f(w.reshape(8, 128, -1).transpose(1, 0, 2))


def _fm(vec):
    return _f(vec.reshape(-1, 128).T)


_IDENT = np.eye(128, dtype=np.float32)


def run_P0(c, c_ctx, ada_w, ada_b):
    depth = ada_w.shape[0]
    cc = np.stack([c.reshape(-1), c_ctx.reshape(-1)], axis=1)
    cT = _f(cc.reshape(8, 128, 2).transpose(1, 0, 2))
    in_maps = []
    for core in range(NCORES):
        i, half = core // 2, core % 2
        i = min(i, depth - 1)
        sl = slice(half * 3072, (half + 1) * 3072)
        in_maps.append({"cT": cT, "w": _kc_layout(ada_w[i][:, sl]), "b": _f(np.stack([ada_b[i][sl], ada_b[i][sl]]))})
    res = _run("P0", build_P0, in_maps)
    mods = np.zeros((depth, 2, 6144), np.float32)
    for core in range(2 * depth):
        i, half = core // 2, core % 2
        mods[i][:, half * 3072:(half + 1) * 3072] = res[core]["mod"]
    return mods.reshape(depth, 2, 6, 1024)


def run_C1(kind, x, ctx, pieces_x, pieces_c, prm, g1x, g1c, ln_g, ln_b, w_out):
    NX, NC = x.shape[0], ctx.shape[0]
    nxc, ncc = NX // NCORES, NC // NCORES
    common = {"prm": _f(np.stack([_bc(p) for p in prm])), "mod": _f(np.stack([_bc(g1x), _bc(g1c)])),
              "lnp": _f(np.stack([_bc(ln_g), _bc(ln_b)])), "w_out": _kc_layout(w_out), "ident": _IDENT}
    in_maps = []
    for c in range(NCORES):
        m = dict(common)
        m["xres"] = _f(np.concatenate([x[c * nxc:(c + 1) * nxc], ctx[c * ncc:(c + 1) * ncc]]))
        for j, (px, pc) in enumerate(zip(pieces_x, pieces_c)):
            m[f"piece{j}"] = _f(np.concatenate([px[c * nxc:(c + 1) * nxc], pc[c * ncc:(c + 1) * ncc]]))
        in_maps.append(m)
    res = _run(("C1", kind, nxc, ncc), lambda: build_C1(kind, nxc, ncc), in_maps)
    x1 = np.concatenate([r["x1"][:nxc] for r in res]); c1 = np.concatenate([r["x1"][nxc:] for r in res])
    return x1, c1


def run_C2(x1, c1, modx, modc, ln_g, ln_b, wq, keys, u_tab, v_tab):
    NX, NC = x1.shape[0], c1.shape[0]
    nxc, ncc = NX // NCORES, NC // NCORES
    common = {"mod": _f(np.stack([np.stack([_bc(m) for m in modx]), np.stack([_bc(m) for m in modc])])),
              "lnp": _f(np.stack([_bc(ln_g), _bc(ln_b)])), "wq": _kc_layout(wq),
              "keysT": _f(keys.reshape(16, 128, 128).transpose(2, 0, 1)),
              "iota": _f(np.broadcast_to(np.arange(256, dtype=np.float32), (128, 256))),
              "u_tab": _f(u_tab), "v_tab": _f(v_tab), "ident": _IDENT}
    in_maps = []
    for c in range(NCORES):
        m = dict(common)
        m["x1"] = _f(np.concatenate([x1[c * nxc:(c + 1) * nxc], c1[c * ncc:(c + 1) * ncc]]))
        in_maps.append(m)
    res = _run(("C2", nxc, ncc), lambda: build_C2(nxc, ncc), in_maps)
    x2 = np.concatenate([r["xout"][:nxc] for r in res]); c2 = np.concatenate([r["xout"][nxc:] for r in res])
    return x2, c2


def run_mlstm(x, ctx, modx, modc, w_in, b_in, conv_w, conv_b):
    NX, NC = x.shape[0], ctx.shape[0]
    nxc = NX // NCORES
    xpad = np.concatenate([np.zeros((64, D), np.float32), x, np.zeros((64, D), np.float32)])
    mod = np.zeros((128, 2, 8, 2), np.float32)
    for ty, m in enumerate((modx, modc)):
        mod[:, ty, :, 0] = _fm(m[0]); mod[:, ty, :, 1] = _fm(m[1])
    b33 = np.zeros((128, 33), np.float32)
    b33[:, :32] = _fm(b_in[:4096]); b33[:16, 32] = b_in[4096:]
    common = {"mod": mod, "w_in": _kc_layout(w_in), "b_in": b33,
              "conv_w": _f(conv_w.reshape(9, 16, 128).transpose(2, 1, 0)), "conv_b": _fm(conv_b)}
    in_maps = []
    for c in range(NCORES):
        win = np.concatenate([xpad[c * nxc:c * nxc + nxc + 128], ctx])
        m = dict(common)
        m["xT"] = _f(win.T.reshape(8, 128, -1).transpose(1, 0, 2))
        hm = np.ones((128, 2), np.float32)
        if c == 0:
            hm[:, 0] = 0
        if c == NCORES - 1:
            hm[:, 1] = 0
        m["hmask"] = hm
        in_maps.append(m)
    res = _run(("A_ml", nxc, NC), lambda: build_A_ml(nxc, NC), in_maps)

    def gather(name, nfeat):
        xs = np.concatenate([r[name].reshape(nfeat, -1)[:, :nxc] for r in res], axis=1)
        cs = res[0][name].reshape(nfeat, -1)[:, nxc:]
        return xs, cs
    qk_x, qk_c = gather("qk", 2048); v_x, v_c = gather("v", 1024); o_x, o_c = gather("o", 1024); g_x, g_c = gather("g", 16)
    T = NC + NX
    NCH = T // 128
    tri = _f(np.triu(np.ones((128, 128), np.float32)))
    maskT = _f(np.where(np.triu(np.ones((128, 128))) > 0, 0.0, -30000.0))
    consts = {"tri": tri, "maskT": maskT, "ones": np.ones((128, 128), np.float32), "ident": _IDENT}
    in_maps = []
    for core in range(NCORES):
        h, d = core % 4, core // 4

        def seqT(ax, ac):
            if d == 0:
                return np.concatenate([ac, ax], axis=1)
            return np.concatenate([ac[:, ::-1], ax[:, ::-1]], axis=1)
        hs = slice(h * 256, (h + 1) * 256)
        qT = seqT(qk_x[hs], qk_c[hs]); kT = seqT(qk_x[1024:][hs], qk_c[1024:][hs]); vT = seqT(v_x[hs], v_c[hs])
        ig = seqT(g_x[d * 4 + h][None], g_c[d * 4 + h][None])[0]
        fg = seqT(g_x[8 + d * 4 + h][None], g_c[8 + d * 4 + h][None])[0]
        m = dict(consts)
        m["qT"] = _f(qT.reshape(2, 128, T).transpose(1, 0, 2)); m["kT"] = _f(kT.reshape(2, 128, T).transpose(1, 0, 2))
        m["k"] = _f(kT.T.reshape(NCH, 128, 256).transpose(1, 0, 2))
        vext = np.concatenate([vT.T, np.ones((T, 1), np.float32)], axis=1)
        m["v"] = _f(vext.reshape(NCH, 128, 257).transpose(1, 0, 2))
        m["ig"] = _f(ig.reshape(NCH, 128).T); m["fg"] = _f(fg.reshape(NCH, 128).T)
        in_maps.append(m)
    res = _run(("B_ml", NCH), lambda: build_B_ml(NCH), in_maps)
    hf = np.zeros((T, D), np.float32); hb = np.zeros((T, D), np.float32)
    for core in range(NCORES):
        h, d = core % 4, core // 4
        hh = res[core]["h"].transpose(1, 0, 2).reshape(T, 256)
        if d == 0:
            hf[:, h * 256:(h + 1) * 256] = hh
        else:
            hb[:NC, h * 256:(h + 1) * 256] = hh[:NC][::-1]
            hb[NC:, h * 256:(h + 1) * 256] = hh[NC:][::-1]
    return (hf[NC:], hb[NC:], _f(o_x.T)), (hf[:NC], hb[:NC], _f(o_c.T))


def build_A_rw(NXc, NC):
    k = KB(); nc = k.nc; V = nc.vector
    NXE = NXc + 128
    NT = NXE + NC
    NO = NXc + NC
    BS = min(512, NXc)
    BW = max(BS, NC)
    xT_d = k.dram_in("xT", [128, 8, NT])
    mod_d = k.dram_in("mod", [128, 2, 8, 2])
    hm_d = k.dram_in("hmask", [128, 2])
    mu_d = k.dram_in("mu", [128, 6, 8])
    wrkv_d = k.dram_in("w_rkv", [3, 128, 8, D])
    w1_d = k.dram_in("w1", [4, 128, 8, 64])
    w2_d = k.dram_in("w2", [4, 64, D])
    g1_d = k.dram_in("g1", [128, 8, 160])
    g2a_d = k.dram_in("g2a", [128, D]); g2b_d = k.dram_in("g2b", [32, D])
    vec_d = k.dram_in("vecs", [128, 7, 8])
    bd_d = k.dram_in("bdones", [128, 128])
    names = ["r", "v", "kkneg", "b0", "b1", "ktil0", "ktil1", "logw0", "logw1", "kbar", "g"]
    outs = {n: k.dram_out(n, [8, 128, NO]) for n in names}

    mod = k.sb("mod_sb", [128, 2, 8, 2]); hm = k.sb("hm", [128, 2]); mu = k.sb("mu_sb", [128, 6, 8])
    w1 = [k.sb(f"w1_{j}", [128, 8, 64]) for j in range(4)]
    w2 = [k.sb(f"w2_{j}", [64, D]) for j in range(4)]
    g1 = k.sb("g1_sb", [128, 8, 160]); g2a = k.sb("g2a_sb", [128, D]); g2b = k.sb("g2b_sb", [32, D])
    vec = k.sb("vec_sb", [128, 7, 8]); bd = k.sb("bd_sb", [128, 128]); omka = k.sb("omka", [128, 8])
    hTb = k.sb("hTb", [128, 8, BW + 128]); sTb = k.sb("sTb", [128, 8, BW]); xm = k.sb("xm", [128, 8, BW])
    kT = k.sb("kT", [128, 8, BW]); kk = k.sb("kk", [128, 8, BW])
    th = [k.sb(f"th{j}", [64, BW]) for j in range(2)]
    gs0 = k.sb("gs0", [128, BW]); gs1 = k.sb("gs1", [32, BW])
    wbuf = [k.sb(f"wb{j}", [128, 8, 128]) for j in range(3)]
    ob = [k.sb(f"ob{j}", [128, BW]) for j in range(6)]
    tmp = [k.sb(f"tmp{j}", [128, BW]) for j in range(3)]
    psb = [k.ps(f"ps{j}", [128, 512]) for j in range(6)]
    cnt = {"ps": 0, "ob": 0, "wb": 0}

    def nps():
        cnt["ps"] += 1; return psb[cnt["ps"] % 6]

    def nob():
        cnt["ob"] += 1; return ob[cnt["ob"] % 6]

    k.dma("sp", mod[:], mod_d[:, :, :, :]); k.dma("sp", hm[:], hm_d[:, :]); k.dma("sp", mu[:], mu_d[:, :, :])
    for j in range(4):
        k.dma("sp", w1[j][:], w1_d[j]); k.dma("sp", w2[j][:], w2_d[j])
    k.dma("sp", g1[:], g1_d[:, :, :]); k.dma("sp", g2a[:], g2a_d[:, :]); k.dma("sp", g2b[:], g2b_d[:, :])
    k.dma("sp", vec[:], vec_d[:, :, :]); k.dma("sp", bd[:], bd_d[:, :])
    k.op("dve", lambda: V.tensor_scalar_add(out=mod[:, :, :, 1], in0=mod[:, :, :, 1], scalar1=1.0), reads=[mod], writes=[mod])
    k.op("dve", lambda: V.tensor_scalar(out=omka[:], in0=vec[:, 5, :], scalar1=-1.0, scalar2=1.0, op0=ALU.mult, op1=ALU.add), reads=[vec], writes=[omka])

    blocks = [(b0, BS, 0) for b0 in range(0, NXc, BS)]
    if NC:
        assert NC <= 512
        blocks.append((0, NC, 1))
    for (b0, bs, ty) in blocks:
        off = 64 if ty == 0 else 0
        wn = bs + 128 if ty == 0 else bs
        src0 = b0 if ty == 0 else NXE
        o0 = b0 if ty == 0 else NXc
        for kc in range(8):
            k.dma("sp", hTb[:, kc, 0:wn], xT_d[:, kc, src0:src0 + wn])
        for kc in range(8):
            k.op("dve", lambda kc=kc: V.tensor_scalar(out=hTb[:, kc, 0:wn], in0=hTb[:, kc, 0:wn], scalar1=mod[:, ty, kc, 1:2], scalar2=mod[:, ty, kc, 0:1],
                                                      op0=ALU.mult, op1=ALU.add), reads=[hTb, mod], writes=[hTb])
        k.op("pool", lambda: nc.gpsimd.memset(sTb[:], 0.0), writes=[sTb])
        if ty == 0:
            if b0 == 0:
                k.op("dve", lambda: V.tensor_scalar_mul(out=hTb[:, :, 0:64], in0=hTb[:, :, 0:64], scalar1=hm[:, 0:1]), reads=[hTb, hm], writes=[hTb])
            if b0 + bs == NXc:
                k.op("dve", lambda: V.tensor_scalar_mul(out=hTb[:, :, bs + 64:bs + 128], in0=hTb[:, :, bs + 64:bs + 128], scalar1=hm[:, 1:2]),
                     reads=[hTb, hm], writes=[hTb])
            hg = lambda kc0, kc1, lo: hTb[:, kc0:kc1, lo:lo + bs].rearrange("p k (r c) -> p k r c", c=64)
            sg = sTb[:, :, 0:bs].rearrange("p k (r c) -> p k r c", c=64)
            for kc in range(2):
                k.op("dve", lambda kc=kc: V.tensor_copy(out=sg[:, kc, :, 1:64], in_=hg(kc, kc + 1, 64)[:, 0, :, 0:63]), reads=[hTb], writes=[sTb])
                k.op("dve", lambda kc=kc: V.tensor_copy(out=sg[:, 2 + kc, :, 0:63], in_=hg(2 + kc, 3 + kc, 64)[:, 0, :, 1:64]), reads=[hTb], writes=[sTb])
            k.op("dve", lambda: V.tensor_copy(out=sTb[:, 4:6, 0:bs], in_=hTb[:, 4:6, 0:bs]), reads=[hTb], writes=[sTb])
            k.op("dve", lambda: V.tensor_copy(out=sTb[:, 6:8, 0:bs], in_=hTb[:, 6:8, 128:128 + bs]), reads=[hTb], writes=[sTb])
        else:
            k.op("dve", lambda: V.tensor_copy(out=sTb[:, 0:4, 1:bs], in_=hTb[:, 0:4, 0:bs - 1]), reads=[hTb], writes=[sTb])
            k.op("dve", lambda: V.tensor_copy(out=sTb[:, 4:8, 0:bs - 1], in_=hTb[:, 4:8, 1:bs]), reads=[hTb], writes=[sTb])
        hc = lambda kc: hTb[:, kc, off:off + bs]
        k.op("dve", lambda: V.tensor_tensor(out=sTb[:, :, 0:bs], in0=sTb[:, :, 0:bs], in1=hTb[:, :, off:off + bs], op=ALU.subtract), reads=[sTb, hTb], writes=[sTb])

        def mix(n):
            for kc in range(8):
                k.op("dve", lambda kc=kc: V.scalar_tensor_tensor(out=xm[:, kc, 0:bs], in0=sTb[:, kc, 0:bs], scalar=mu[:, n, kc:kc + 1], in1=hc(kc),
                                                                 op0=ALU.mult, op1=ALU.add), reads=[sTb, mu, hTb], writes=[xm])

        def proj(n, oc):
            cnt["wb"] += 1
            wb = wbuf[cnt["wb"] % 3]
            k.dma("sp", wb[:], wrkv_d[n][:, :, oc * 128:(oc + 1) * 128])
            p = nps()
            for kc in range(8):
                k.op("pe", lambda p=p, wb=wb, kc=kc: nc.tensor.matmul(p[:, 0:bs], wb[:, kc, :], xm[:, kc, 0:bs], start=(kc == 0), stop=(kc == 7)),
                     reads=[wb, xm], writes=[p], same_ok=True)
            return p

        def store(name, oc, t):
            k.dma("sp", outs[name][oc][:, o0:o0 + bs], t[:, 0:bs])

        mix(0)
        for oc in range(8):
            p = proj(0, oc); o = nob()
            k.op("act", lambda p=p, o=o: nc.scalar.copy(out=o[:, 0:bs], in_=p[:, 0:bs]), reads=[p], writes=[o])
            store("r", oc, o)
        mix(1)
        for oc in range(8):
            p = proj(1, oc)
            k.op("act", lambda p=p, oc=oc: nc.scalar.copy(out=kT[:, oc, 0:bs], in_=p[:, 0:bs]), reads=[p], writes=[kT])
            t0, t1 = tmp[0], tmp[1]
            k.op("dve", lambda oc=oc: V.tensor_scalar_mul(out=t0[:, 0:bs], in0=kT[:, oc, 0:bs], scalar1=vec[:, 4, oc:oc + 1]), reads=[kT, vec], writes=[t0])
            k.op("dve", lambda: V.tensor_tensor(out=t1[:, 0:bs], in0=t0[:, 0:bs], in1=t0[:, 0:bs], op=ALU.mult), reads=[t0], writes=[t1])
            p2 = nps()
            k.op("pe", lambda p2=p2: nc.tensor.matmul(p2[:, 0:bs], bd[:], t1[:, 0:bs], start=True, stop=True), reads=[bd, t1], writes=[p2], same_ok=True)
            k.op("act", lambda p2=p2: nc.scalar.activation(out=t1[:, 0:bs], in_=p2[:, 0:bs], func=AF.Sqrt), reads=[p2], writes=[t1])
            k.op("dve", lambda: V.tensor_scalar_max(out=t1[:, 0:bs], in0=t1[:, 0:bs], scalar1=1e-12), reads=[t1], writes=[t1])
            k.op("dve", lambda: V.reciprocal(out=t1[:, 0:bs], in_=t1[:, 0:bs]), reads=[t1], writes=[t1])
            k.op("dve", lambda oc=oc: V.tensor_tensor(out=kk[:, oc, 0:bs], in0=t0[:, 0:bs], in1=t1[:, 0:bs], op=ALU.mult), reads=[t0, t1], writes=[kk])
            o = nob()
            k.op("dve", lambda o=o, oc=oc: V.tensor_scalar_mul(out=o[:, 0:bs], in0=kk[:, oc, 0:bs], scalar1=-1.0), reads=[kk], writes=[o])
            store("kkneg", oc, o)
        mix(2)
        for oc in range(8):
            p = proj(2, oc); o = nob()
            k.op("act", lambda p=p, o=o: nc.scalar.copy(out=o[:, 0:bs], in_=p[:, 0:bs]), reads=[p], writes=[o])
            store("v", oc, o)

        def lora_in(j, dst, func):
            p = nps()
            for kc in range(8):
                k.op("pe", lambda p=p, kc=kc, j=j: nc.tensor.matmul(p[0:64, 0:bs], w1[j][:, kc, :], xm[:, kc, 0:bs], start=(kc == 0), stop=(kc == 7)),
                     reads=[w1[j], xm], writes=[p], same_ok=True)
            k.op("act", lambda p=p, dst=dst: nc.scalar.activation(out=dst[:, 0:bs], in_=p[0:64, 0:bs], func=func), reads=[p], writes=[dst])

        mix(3)
        for z in range(2):
            lora_in(z, th[z], AF.Tanh)
        for oc in range(8):
            for z in range(2):
                p = nps()
                k.op("pe", lambda p=p, z=z, oc=oc: nc.tensor.matmul(p[:, 0:bs], w2[z][:, oc * 128:(oc + 1) * 128], th[z][:, 0:bs], start=True, stop=True),
                     reads=[w2[z], th[z]], writes=[p], same_ok=True)
                o = nob()
                k.op("act", lambda p=p, o=o, z=z, oc=oc: nc.scalar.activation(out=o[:, 0:bs], in_=p[:, 0:bs], func=AF.Sigmoid, bias=vec[:, z, oc:oc + 1], scale=1.0),
                     reads=[p, vec], writes=[o])
                k.op("dve", lambda o=o: V.tensor_scalar_mul(out=o[:, 0:bs], in0=o[:, 0:bs], scalar1=-0.6065306597126334), reads=[o], writes=[o])
                store(f"logw{z}", oc, o)
        mix(4)
        for z in range(2):
            lora_in(2 + z, th[z], AF.Identity)
        for oc in range(8):
            kt_z = []
            for z in range(2):
                p = nps()
                k.op("pe", lambda p=p, z=z, oc=oc: nc.tensor.matmul(p[:, 0:bs], w2[2 + z][:, oc * 128:(oc + 1) * 128], th[z][:, 0:bs], start=True, stop=True),
                     reads=[w2[2 + z], th[z]], writes=[p], same_ok=True)
                asg = tmp[z]
                k.op("act", lambda p=p, asg=asg, z=z, oc=oc: nc.scalar.activation(out=asg[:, 0:bs], in_=p[:, 0:bs], func=AF.Sigmoid, bias=vec[:, 2 + z, oc:oc + 1], scale=1.0),
                     reads=[p, vec], writes=[asg])
                o = nob()
                k.op("dve", lambda o=o, asg=asg, oc=oc: V.tensor_tensor(out=o[:, 0:bs], in0=kk[:, oc, 0:bs], in1=asg[:, 0:bs], op=ALU.mult), reads=[kk, asg], writes=[o])
                store(f"b{z}", oc, o)
                k.op("dve", lambda asg=asg, oc=oc: V.tensor_scalar(out=asg[:, 0:bs], in0=asg[:, 0:bs], scalar1=vec[:, 5, oc:oc + 1], scalar2=omka[:, oc:oc + 1],
                                                                 op0=ALU.mult, op1=ALU.add), reads=[asg, vec, omka], writes=[asg])
                o2 = nob()
                k.op("dve", lambda o2=o2, asg=asg, oc=oc: V.tensor_tensor(out=o2[:, 0:bs], in0=asg[:, 0:bs], in1=kT[:, oc, 0:bs], op=ALU.mult), reads=[asg, kT], writes=[o2])
                store(f"ktil{z}", oc, o2)
                kt_z.append(o2)
            o3 = nob()
            k.op("dve", lambda o3=o3, a=kt_z[0], b=kt_z[1]: V.tensor_tensor(out=o3[:, 0:bs], in0=a[:, 0:bs], in1=b[:, 0:bs], op=ALU.add), reads=[kt_z[0], kt_z[1]], writes=[o3])
            k.op("dve", lambda o3=o3: V.tensor_scalar_mul(out=o3[:, 0:bs], in0=o3[:, 0:bs], scalar1=0.5), reads=[o3], writes=[o3])
            store("kbar", oc, o3)
        mix(5)
        for (m0, mn, dst) in ((0, 128, gs0), (128, 32, gs1)):
            p = nps()
            for kc in range(8):
                k.op("pe", lambda p=p, kc=kc, m0=m0, mn=mn: nc.tensor.matmul(p[0:mn, 0:bs], g1[:, kc, m0:m0 + mn], xm[:, kc, 0:bs], start=(kc == 0), stop=(kc == 7)),
                     reads=[g1, xm], writes=[p], same_ok=True)
            k.op("act", lambda p=p, dst=dst, mn=mn: nc.scalar.activation(out=dst[:, 0:bs], in_=p[0:mn, 0:bs], func=AF.Sigmoid), reads=[p], writes=[dst])
        for oc in range(8):
            p = nps()
            k.op("pe", lambda p=p, oc=oc: nc.tensor.matmul(p[:, 0:bs], g2a[:, oc * 128:(oc + 1) * 128], gs0[:, 0:bs], start=True, stop=False),
                 reads=[g2a, gs0], writes=[p], same_ok=True)
            k.op("pe", lambda p=p, oc=oc: nc.tensor.matmul(p[:, 0:bs], g2b[:, oc * 128:(oc + 1) * 128], gs1[:, 0:bs], start=False, stop=True),
                 reads=[g2b, gs1], writes=[p], same_ok=True)
            o = nob()
            k.op("act", lambda p=p, o=o: nc.scalar.copy(out=o[:, 0:bs], in_=p[:, 0:bs]), reads=[p], writes=[o])
            store("g", oc, o)
    return k.finish()


def build_B_rw(NCH):
    k = KB(); nc = k.nc; V = nc.vector
    NSC = 4
    T = NCH * 128
    tk_d = {n: k.dram_in(n, [128, NCH, NSC, 64]) for n in ("lw_t", "b_t", "k_t", "v_t")}
    ch_d = {n: k.dram_in(n, [64, NSC, T]) for n in ("r_c", "a_c", "b_c", "k_c")}
    tri_d = k.dram_in("tri", [128, 128]); tris_d = k.dram_in("tris", [128, 128])
    msu_d = k.dram_in("m_su", [128, 128]); msl_d = k.dram_in("m_sl", [128, 128]); miu_d = k.dram_in("m_iu", [128, 128])
    y_d = k.dram_out("y", [128, NCH, NSC, 64])
    ident = _consts(k)
    tri = k.sb("tri_sb", [128, 128]); tris = k.sb("tris_sb", [128, 128])
    msu = k.sb("msu", [128, 128]); msl = k.sb("msl", [128, 128]); miu = k.sb("miu", [128, 128])
    for t, d in ((tri, tri_d), (tris, tris_d), (msu, msu_d), (msl, msl_d), (miu, miu_d)):
        k.dma("sp", t[:], d[:, :])
    NS = 2
    tk = {n: [k.sb(f"{n}_s{j}", [128, NSC, 64]) for j in range(NS)] for n in tk_d}
    ch = {n: [k.sb(f"{n}_s{j}", [64, NSC, 128]) for j in range(NS)] for n in ch_d}
    Pinv = k.sb("Pinv", [128, NSC, 64]); PT = k.sb("PT", [64, NSC, 128]); PinvT = k.sb("PinvT", [64, NSC, 128]); Pm1T = k.sb("Pm1T", [64, NSC, 128])
    At = k.sb("At", [64, NSC, 128]); BtT = k.sb("BtT", [64, NSC, 128]); KtT = k.sb("KtT", [64, NSC, 128]); RtT = k.sb("RtT", [64, NSC, 128])
    Btok = k.sb("Btok", [128, NSC, 64]); Ktok = k.sb("Ktok", [128, NSC, 64])
    Nn = [k.sb(f"Nn{j}", [128, NSC, 128]) for j in range(2)]; NTt = [k.sb(f"NTt{j}", [128, NSC, 128]) for j in range(2)]
    X = k.sb("X", [128, NSC, 128]); XT = k.sb("XT", [128, NSC, 128])
    MakT = k.sb("MakT", [128, NSC, 128]); MrbT = k.sb("MrbT", [128, NSC, 128]); MrkT = k.sb("MrkT", [128, NSC, 128])
    W = k.sb("W", [128, NSC, 64]); U = k.sb("U", [128, NSC, 64]); Yb = [k.sb(f"Yb{j}", [128, NSC, 64]) for j in range(2)]
    Z = k.sb("Z", [64, NSC, 64]); Zt = k.sb("Zt", [64, NSC, 64])
    banks = [k.ps(f"bk{j}", [128, 512]) for j in range(8)]
    bi = [0]

    def bank():
        bi[0] += 1
        return banks[bi[0] % 8]
    k.op("pool", lambda: nc.gpsimd.memset(Z[:], 0.0), writes=[Z])

    def load(c):
        s = c % NS
        for n in tk_d:
            k.dma("sp", tk[n][s][:], tk_d[n][:, c, :, :])
        for n in ch_d:
            k.dma("sp", ch[n][s][:], ch_d[n][:, :, c * 128:(c + 1) * 128])
    load(0)
    bcm = lambda m: m[:, None, :].broadcast_to([128, NSC, 128])
    v3 = lambda b: b[:, :].rearrange("p (s t) -> p s t", s=NSC)
    v64 = lambda b: b[:, 0:NSC * 64].rearrange("p (s t) -> p s t", s=NSC)
    for c in range(NCH):
        s = c % NS
        if c + 1 < NCH:
            load(c + 1)
        LW, Bt_, Kt_, Vt = tk["lw_t"][s], tk["b_t"][s], tk["k_t"][s], tk["v_t"][s]
        Rc, Ac, Bc, Kc = ch["r_c"][s], ch["a_c"][s], ch["b_c"][s], ch["k_c"][s]
        pLP = bank()
        k.op("pe", lambda: nc.tensor.matmul(pLP[:, 0:256], tri[:], LW[:].rearrange("p s c -> p (s c)"), start=True, stop=True), reads=[tri, LW], writes=[pLP], same_ok=True)
        pLT = bank(); pL1 = bank()
        for sc in range(NSC):
            k.op("pe", lambda sc=sc: nc.tensor.matmul(pLT[0:64, sc * 128:(sc + 1) * 128], LW[:, sc, :], tri[:], start=True, stop=True), reads=[LW, tri], writes=[pLT], same_ok=True)
            k.op("pe", lambda sc=sc: nc.tensor.matmul(pL1[0:64, sc * 128:(sc + 1) * 128], LW[:, sc, :], tris[:], start=True, stop=True), reads=[LW, tris], writes=[pL1], same_ok=True)
        k.op("act", lambda: nc.scalar.activation(out=Pinv[:].rearrange("p s c -> p (s c)"), in_=pLP[:, 0:256], func=AF.Exp, scale=-1.0), reads=[pLP], writes=[Pinv])
        k.op("act", lambda: nc.scalar.activation(out=PT[:].rearrange("p s c -> p (s c)"), in_=pLT[0:64, :], func=AF.Exp), reads=[pLT], writes=[PT])
        k.op("act", lambda: nc.scalar.activation(out=PinvT[:].rearrange("p s c -> p (s c)"), in_=pLT[0:64, :], func=AF.Exp, scale=-1.0), reads=[pLT], writes=[PinvT])
        k.op("act", lambda: nc.scalar.activation(out=Pm1T[:].rearrange("p s c -> p (s c)"), in_=pL1[0:64, :], func=AF.Exp), reads=[pL1], writes=[Pm1T])
        k.op("dve", lambda: V.tensor_tensor(out=At[:], in0=Ac[:], in1=Pm1T[:], op=ALU.mult), reads=[Ac, Pm1T], writes=[At])
        k.op("dve", lambda: V.tensor_tensor(out=BtT[:], in0=Bc[:], in1=PinvT[:], op=ALU.mult), reads=[Bc, PinvT], writes=[BtT])
        k.op("dve", lambda: V.tensor_tensor(out=KtT[:], in0=Kc[:], in1=PinvT[:], op=ALU.mult), reads=[Kc, PinvT], writes=[KtT])
        k.op("dve", lambda: V.tensor_tensor(out=RtT[:], in0=Rc[:], in1=PT[:], op=ALU.mult), reads=[Rc, PT], writes=[RtT])
        k.op("pool", lambda: nc.gpsimd.tensor_tensor(out=Btok[:], in0=Bt_[:], in1=Pinv[:], op=ALU.mult), reads=[Bt_, Pinv], writes=[Btok])
        k.op("pool", lambda: nc.gpsimd.tensor_tensor(out=Ktok[:], in0=Kt_[:], in1=Pinv[:], op=ALU.mult), reads=[Kt_, Pinv], writes=[Ktok])
        for (L, Rr, dst, msk) in ((BtT, At, Nn[0], msu), (At, BtT, NTt[0], msl), (KtT, At, MakT, msu), (BtT, RtT, MrbT, miu), (KtT, RtT, MrkT, miu)):
            p = bank()
            for sc in range(NSC):
                k.op("pe", lambda p=p, L=L, Rr=Rr, sc=sc: nc.tensor.matmul(p[:, sc * 128:(sc + 1) * 128], L[:, sc, :], Rr[:, sc, :], start=True, stop=True),
                     reads=[L, Rr], writes=[p], same_ok=True)
            k.op("dve", lambda p=p, dst=dst, msk=msk: V.tensor_tensor(out=dst[:], in0=v3(p), in1=bcm(msk), op=ALU.mult), reads=[p, msk], writes=[dst])
        k.op("dve", lambda: V.tensor_tensor(out=X[:], in0=Nn[0][:], in1=bcm(ident), op=ALU.add), reads=[Nn[0], ident], writes=[X])
        k.op("dve", lambda: V.tensor_tensor(out=XT[:], in0=NTt[0][:], in1=bcm(ident), op=ALU.add), reads=[NTt[0], ident], writes=[XT])
        cur = 0
        for it in range(6):
            nxt = 1 - cur
            last = it == 5
            pN2 = bank()
            for sc in range(NSC):
                k.op("pe", lambda sc=sc, cur=cur, pN2=pN2: nc.tensor.matmul(pN2[:, sc * 128:(sc + 1) * 128], NTt[cur][:, sc, :], Nn[cur][:, sc, :], start=True, stop=True),
                     reads=[NTt[cur], Nn[cur]], writes=[pN2], same_ok=True)
            k.op("act", lambda pN2=pN2, nxt=nxt: nc.scalar.copy(out=Nn[nxt][:], in_=v3(pN2)), reads=[pN2], writes=[Nn[nxt]])
            if not last:
                pT2 = bank()
                for sc in range(NSC):
                    k.op("pe", lambda sc=sc, cur=cur, pT2=pT2: nc.tensor.matmul(pT2[:, sc * 128:(sc + 1) * 128], Nn[cur][:, sc, :], NTt[cur][:, sc, :], start=True, stop=True),
                         reads=[Nn[cur], NTt[cur]], writes=[pT2], same_ok=True)
                k.op("act", lambda pT2=pT2, nxt=nxt: nc.scalar.copy(out=NTt[nxt][:], in_=v3(pT2)), reads=[pT2], writes=[NTt[nxt]])
            pX = bank()
            for sc in range(NSC):
                k.op("pe", lambda sc=sc, nxt=nxt, pX=pX: nc.tensor.matmul(pX[:, sc * 128:(sc + 1) * 128], XT[:, sc, :], Nn[nxt][:, sc, :], start=True, stop=True),
                     reads=[XT, Nn[nxt]], writes=[pX], same_ok=True)
            if not last:
                pXT = bank()
                for sc in range(NSC):
                    k.op("pe", lambda sc=sc, nxt=nxt, pXT=pXT: nc.tensor.matmul(pXT[:, sc * 128:(sc + 1) * 128], X[:, sc, :], NTt[nxt][:, sc, :], start=True, stop=True),
                         reads=[X, NTt[nxt]], writes=[pXT], same_ok=True)
            k.op("dve", lambda pX=pX: V.tensor_tensor(out=X[:], in0=X[:], in1=v3(pX), op=ALU.add), reads=[X, pX], writes=[X])
            if not last:
                k.op("dve", lambda pXT=pXT: V.tensor_tensor(out=XT[:], in0=XT[:], in1=v3(pXT), op=ALU.add), reads=[XT, pXT], writes=[XT])
            cur = nxt
        pW = bank()
        for sc in range(NSC):
            k.op("pe", lambda sc=sc: nc.tensor.matmul(pW[:, sc * 64:(sc + 1) * 64], At[:, sc, :], Z[:, sc, :], start=True, stop=False), reads=[At, Z], writes=[pW], same_ok=True)
            k.op("pe", lambda sc=sc: nc.tensor.matmul(pW[:, sc * 64:(sc + 1) * 64], MakT[:, sc, :], Vt[:, sc, :], start=False, stop=True), reads=[MakT, Vt], writes=[pW], same_ok=True)
        k.op("act", lambda: nc.scalar.copy(out=W[:], in_=v64(pW)), reads=[pW], writes=[W])
        pU = bank()
        for sc in range(NSC):
            k.op("pe", lambda sc=sc: nc.tensor.matmul(pU[:, sc * 64:(sc + 1) * 64], X[:, sc, :], W[:, sc, :], start=True, stop=True), reads=[X, W], writes=[pU], same_ok=True)
        k.op("act", lambda: nc.scalar.copy(out=U[:], in_=v64(pU)), reads=[pU], writes=[U])
        pY = bank()
        for sc in range(NSC):
            k.op("pe", lambda sc=sc: nc.tensor.matmul(pY[:, sc * 64:(sc + 1) * 64], RtT[:, sc, :], Z[:, sc, :], start=True, stop=False), reads=[RtT, Z], writes=[pY], same_ok=True)
            k.op("pe", lambda sc=sc: nc.tensor.matmul(pY[:, sc * 64:(sc + 1) * 64], MrbT[:, sc, :], U[:, sc, :], start=False, stop=False), reads=[MrbT, U], writes=[pY], same_ok=True)
            k.op("pe", lambda sc=sc: nc.tensor.matmul(pY[:, sc * 64:(sc + 1) * 64], MrkT[:, sc, :], Vt[:, sc, :], start=False, stop=True), reads=[MrkT, Vt], writes=[pY], same_ok=True)
        yb = Yb[c % 2]
        k.op("act", lambda yb=yb: nc.scalar.copy(out=yb[:], in_=v64(pY)), reads=[pY], writes=[yb])
        k.dma("sp", y_d[:, c, :, :], yb[:])
        pZ = bank()
        for sc in range(NSC):
            k.op("pe", lambda sc=sc: nc.tensor.matmul(pZ[0:64, sc * 64:(sc + 1) * 64], Btok[:, sc, :], U[:, sc, :], start=True, stop=False), reads=[Btok, U], writes=[pZ], same_ok=True)
            k.op("pe", lambda sc=sc: nc.tensor.matmul(pZ[0:64, sc * 64:(sc + 1) * 64], Ktok[:, sc, :], Vt[:, sc, :], start=False, stop=True), reads=[Ktok, Vt], writes=[pZ], same_ok=True)
        k.op("dve", lambda: V.tensor_tensor(out=Zt[:], in0=Z[:], in1=pZ[0:64, 0:NSC * 64].rearrange("p (s t) -> p s t", s=NSC), op=ALU.add), reads=[Z, pZ], writes=[Zt])
        k.op("dve", lambda: V.tensor_tensor(out=Z[:], in0=Zt[:], in1=PT[:, :, 127:128].broadcast_to([64, NSC, 64]), op=ALU.mult), reads=[Zt, PT], writes=[Z])
    return k.finish()


def run_rwkv(x, ctx, modx, modc, P):
    NX, NC = x.shape[0], ctx.shape[0]
    nxc = NX // NCORES
    xpad = np.concatenate([np.zeros((64, D), np.float32), x, np.zeros((64, D), np.float32)])
    mod = np.zeros((128, 2, 8, 2), np.float32)
    for ty, m in enumerate((modx, modc)):
        mod[:, ty, :, 0] = _fm(m[0]); mod[:, ty, :, 1] = _fm(m[1])
    vecs = np.zeros((128, 7, 8), np.float32)
    for j, v in enumerate((P["w0"][0], P["w0"][1], P["a0"][0], P["a0"][1], P["k_k"], P["k_a"])):
        vecs[:, j, :] = _fm(v)
    bd = np.zeros((128, 128), np.float32); bd[:64, :64] = 1; bd[64:, 64:] = 1
    common = {"mod": mod, "mu": _f(np.stack([_fm(P["mu"][n]) for n in range(6)], axis=1)),
              "w_rkv": _f(np.stack([_kc_layout(P["w_rkv"][n]) for n in range(3)])),
              "w1": _f(np.stack([_kc_layout(P["w1"][0]), _kc_layout(P["w1"][1]), _kc_layout(P["a1"][0]), _kc_layout(P["a1"][1])])),
              "w2": _f(np.stack([P["w2"][0], P["w2"][1], P["a2"][0], P["a2"][1]])),
              "g1": _kc_layout(P["g1"]), "g2a": _f(P["g2"][:128]), "g2b": _f(P["g2"][128:]), "vecs": vecs, "bdones": bd}
    in_maps = []
    for c in range(NCORES):
        win = np.concatenate([xpad[c * nxc:c * nxc + nxc + 128], ctx])
        m = dict(common)
        m["xT"] = _f(win.T.reshape(8, 128, -1).transpose(1, 0, 2))
        hm = np.ones((128, 2), np.float32)
        if c == 0:
            hm[:, 0] = 0
        if c == NCORES - 1:
            hm[:, 1] = 0
        m["hmask"] = hm
        in_maps.append(m)
    res = _run(("A_rw", nxc, NC), lambda: build_A_rw(nxc, NC), in_maps)
    fmx, fmc = {}, {}
    for n in ["r", "v", "kkneg", "b0", "b1", "ktil0", "ktil1", "logw0", "logw1", "kbar", "g"]:
        fmx[n] = np.concatenate([r[n].reshape(1024, -1)[:, :nxc] for r in res], axis=1)
        fmc[n] = res[0][n].reshape(1024, -1)[:, nxc:]
    T = NC + NX
    NCH = T // 128
    iu = np.triu(np.ones((128, 128), np.float32)); su = np.triu(np.ones((128, 128), np.float32), 1)
    consts = {"tri": _f(iu), "tris": _f(su), "m_su": _f(su), "m_sl": _f(su.T), "m_iu": _f(iu), "ident": _IDENT}
    in_maps = []
    for core in range(NCORES):
        tkm = {n: [] for n in ("lw_t", "b_t", "k_t", "v_t")}
        chm = {n: [] for n in ("r_c", "a_c", "b_c", "k_c")}
        for j in range(4):
            sid = core * 4 + j
            hd, z = sid // 2, sid % 2
            hs = slice(hd * 64, (hd + 1) * 64)

            def seq(n):
                ax, ac = fmx[n][hs], fmc[n][hs]
                if z == 0:
                    return np.concatenate([ac, ax], axis=1)
                return np.concatenate([ac[:, ::-1], ax[:, ::-1]], axis=1)
            tkm["lw_t"].append(seq(f"logw{z}").T); tkm["b_t"].append(seq(f"b{z}").T); tkm["k_t"].append(seq(f"ktil{z}").T); tkm["v_t"].append(seq("v").T)
            chm["r_c"].append(seq("r")); chm["a_c"].append(seq("kkneg")); chm["b_c"].append(seq(f"b{z}")); chm["k_c"].append(seq(f"ktil{z}"))
        m = dict(consts)
        for n, lst in tkm.items():
            m[n] = _f(np.stack(lst).reshape(4, NCH, 128, 64).transpose(2, 1, 0, 3))
        for n, lst in chm.items():
            m[n] = _f(np.stack(lst).transpose(1, 0, 2))
        in_maps.append(m)
    res = _run(("B_rw", NCH), lambda: build_B_rw(NCH), in_maps)
    yf = np.zeros((T, D), np.float32); yb = np.zeros((T, D), np.float32)
    for core in range(NCORES):
        yy = res[core]["y"]
        for j in range(4):
            sid = core * 4 + j
            hd, z = sid // 2, sid % 2
            ys = yy[:, :, j, :].transpose(1, 0, 2).reshape(T, 64)
            if z == 0:
                yf[:, hd * 64:(hd + 1) * 64] = ys
            else:
                yb[:NC, hd * 64:(hd + 1) * 64] = ys[:NC][::-1]
                yb[NC:, hd * 64:(hd + 1) * 64] = ys[NC:][::-1]
    px = [yf[NC:], yb[NC:]] + [_f(fmx[n].T) for n in ("r", "kbar", "v", "g")]
    pc = [yf[:NC], yb[:NC]] + [_f(fmc[n].T) for n in ("r", "kbar", "v", "g")]
    return px, pc


def kernel(x, c, ctx, c_ctx, ada_w, ada_b, ln_g, ln_b,
           ml_w_in, ml_b_in, ml_conv_w, ml_conv_b, ml_hn_g, ml_w_out,
           rw_mu, rw_w_rkv, rw_w0, rw_w1, rw_w2, rw_a0, rw_a1, rw_a2, rw_g1, rw_g2,
           rw_k_k, rw_k_a, rw_r_k, rw_lnx_g, rw_lnx_b, rw_w_out,
           pk_wq, pk_keys, pk_u, pk_v):
    A = lambda a: np.asarray(a, dtype=np.float32)
    xs = A(x)[0]; cs = A(ctx)[0]
    depth = ada_w.shape[0]
    mods = run_P0(A(c), A(c_ctx), A(ada_w), A(ada_b))
    for i in range(depth):
        j = i // 2
        modx, modc = mods[i, 0], mods[i, 1]
        if i % 2 == 0:
            px, pc = run_mlstm(xs, cs, modx, modc, A(ml_w_in[j]), A(ml_b_in[j]), A(ml_conv_w[j]), A(ml_conv_b[j]))
            x1, c1 = run_C1("ml", xs, cs, list(px), list(pc), [A(ml_hn_g[j])], modx[2], modc[2], A(ln_g[i, 0]), A(ln_b[i, 0]), A(ml_w_out[j]))
        else:
            P = {"mu": A(rw_mu[j]), "w_rkv": A(rw_w_rkv[j]), "w0": A(rw_w0[j]), "w1": A(rw_w1[j]), "w2": A(rw_w2[j]),
                 "a0": A(rw_a0[j]), "a1": A(rw_a1[j]), "a2": A(rw_a2[j]), "g1": A(rw_g1[j]), "g2": A(rw_g2[j]),
                 "k_k": A(rw_k_k[j]), "k_a": A(rw_k_a[j])}
            px, pc = run_rwkv(xs, cs, modx, modc, P)
            x1, c1 = run_C1("rw", xs, cs, px, pc, [A(rw_lnx_g[j]), A(rw_lnx_b[j]), A(rw_r_k[j])], modx[2], modc[2],
                            A(ln_g[i, 0]), A(ln_b[i, 0]), A(rw_w_out[j]))
        xs, cs = run_C2(x1, c1, modx[3:6], modc[3:6], A(ln_g[i, 1]), A(ln_b[i, 1]), A(pk_wq[i]), A(pk_keys[i]), A(pk_u[i]), A(pk_v[i]))
    return np.ascontiguousarray(xs[None].astype(np.float32))
```

```python
import contextlib
import numpy as np
import concourse.bass as bass
import concourse.mybir as mybir
from concourse.alu_op_type import AluOpType as ALU
from concourse.bass_utils import run_bass_kernel_spmd

F32 = mybir.dt.float32
I32 = mybir.dt.int32
U32 = mybir.dt.uint32
AF = mybir.ActivationFunctionType


class KB:
    def __init__(self):
        self.nc = bass.Bass("TRN2", target_bir_lowering=False)
        nc = self.nc
        self.es = contextlib.ExitStack()
        self.es.enter_context(nc.cleanup_on_exit())
        self.engs = {"pe": nc.tensor, "dve": nc.vector, "act": nc.scalar,
                     "pool": nc.gpsimd, "sp": nc.sync}
        self.esem = {}
        self.ecnt = {}
        for e in self.engs:
            self.esem[e] = nc.alloc_semaphore(name=f"s_{e}")
            self.ecnt[e] = 0
        self.seen = {e: {} for e in self.engs}
        self.tr = {}
        self.dsem = {}
        self.n_inst = 0
        self._uid = 0
        self.rec = None

    def sb(self, name, shape, dt=F32):
        t = self.es.enter_context(self.nc.sbuf_tensor(name, list(shape), dt))
        return t

    def ps(self, name, shape, dt=F32):
        t = self.es.enter_context(self.nc.psum_tensor(name, list(shape), dt))
        return t

    def dram_in(self, name, shape, dt=F32):
        return self.nc.dram_tensor(name, list(shape), dt, kind="ExternalInput").ap()

    def dram_out(self, name, shape, dt=F32):
        return self.nc.dram_tensor(name, list(shape), dt, kind="ExternalOutput").ap()

    @staticmethod
    def _key(ap):
        t = getattr(ap, "tensor", ap)
        return t.name

    def _needs(self, reads, writes):
        needs = {}

        def need(sv):
            sem, val = sv
            k = sem.name if hasattr(sem, "name") else id(sem)
            if k not in needs or needs[k][1] < val:
                needs[k] = (sem, val)

        for ap in reads:
            st = self.tr.get(self._key(ap))
            if st and st[0]:
                need(st[0])
        for ap in writes:
            st = self.tr.get(self._key(ap))
            if st:
                if st[0]:
                    need(st[0])
                for sv in st[1].values():
                    need(sv)
        return needs

    def _emit_waits(self, e, needs, skip_sem=None):
        eng = self.engs[e]
        seen = self.seen[e]
        for k, (sem, val) in needs.items():
            if skip_sem is not None and sem is skip_sem:
                continue
            if seen.get(k, -1) >= val:
                continue
            eng.wait_ge(sem, val)
            seen[k] = val

    def _update(self, reads, writes, sv):
        sem, val = sv
        k = sem.name if hasattr(sem, "name") else id(sem)
        for ap in reads:
            st = self.tr.setdefault(self._key(ap), [None, {}])
            st[1][k] = sv
        for ap in writes:
            st = self.tr.setdefault(self._key(ap), [None, {}])
            st[0] = sv
            st[1] = {}

    def op(self, e, fn, reads=(), writes=(), same_ok=False):
        if self.rec is not None:
            self.rec.append(("op", (e, fn), dict(reads=reads, writes=writes, same_ok=same_ok)))
            return None
        needs = self._needs(reads, writes)
        self._emit_waits(e, needs, skip_sem=self.esem[e] if same_ok else None)
        inst = fn()
        self.ecnt[e] += 1
        inst.then_inc(self.esem[e], 1)
        self._update(reads, writes, (self.esem[e], self.ecnt[e]))
        self.n_inst += 1
        return inst

    def dma(self, q, out, in_, fn=None, extra_reads=(), **kw):
        if self.rec is not None:
            self.rec.append(("dma", (q, out, in_), dict(fn=fn, extra_reads=extra_reads, **kw)))
            return None
        reads, writes = [in_] + list(extra_reads), [out]
        needs = self._needs(reads, writes)
        self._emit_waits(q, needs)
        sbt = None
        for ap in (out, in_):
            if "sbuf" in str(ap.space).lower() or "sb" == str(ap.space).lower():
                sbt = ap
        keyt = self._key(sbt if sbt is not None else out)
        if keyt not in self.dsem:
            self.dsem[keyt] = [self.nc.alloc_semaphore(name=f"d_{len(self.dsem)}"), 0]
        ds = self.dsem[keyt]
        if fn is None:
            inst = self.engs[q].dma_start(out=out, in_=in_, **kw)
        else:
            inst = fn()
        ds[1] += 16
        inst.then_inc(ds[0], 16)
        self._update(reads, writes, (ds[0], ds[1]))
        self.n_inst += 1
        return inst

    def replay(self, lst, n):
        for _ in range(min(n, len(lst))):
            kind, a, kw = lst.pop(0)
            if kind == "op":
                self.op(*a, **kw)
            else:
                self.dma(*a, **kw)

    def finish(self):
        sp = self.engs["sp"]
        for e in self.engs:
            if self.ecnt[e] > 0 and e != "sp":
                sp.wait_ge(self.esem[e], self.ecnt[e])
        for k, (sem, cnt) in self.dsem.items():
            sp.wait_ge(sem, cnt)
        self.nc.all_engine_barrier()
        self.es.close()
        return self.nc


D = 1024
ALPHA = (2.0 * 4) ** 0.25
LN_EPS = 1e-5


def _consts(k, need_iota=False):
    ident_d = k.dram_in("ident", [128, 128])
    ident = k.sb("ident_sb", [128, 128])
    k.dma("sp", ident[:], ident_d[:, :])
    return ident


def _layernorm_tile(k, t, tmp, stats, mv, rstd, epst):
    nc = k.nc
    for j in range(2):
        k.op("dve", lambda j=j: nc.vector.bn_stats(out=stats[:, j * 6:(j + 1) * 6], in_=t[:, j * 512:(j + 1) * 512]),
             reads=[t], writes=[stats])
    k.op("dve", lambda: nc.vector.bn_aggr(out=mv[:, 0:2], in_=stats[:, 0:12]), reads=[stats], writes=[mv])
    k.op("act", lambda: nc.scalar.activation(out=rstd[:, 0:1], in_=mv[:, 1:2], func=AF.Sqrt, bias=epst[:, 0:1], scale=1.0),
         reads=[mv, epst], writes=[rstd])
    k.op("dve", lambda: nc.vector.reciprocal(out=rstd[:, 0:1], in_=rstd[:, 0:1]), reads=[rstd], writes=[rstd])
    k.op("dve", lambda: nc.vector.tensor_scalar(out=t[:], in0=t[:], scalar1=mv[:, 0:1], scalar2=rstd[:, 0:1],
                                                op0=ALU.subtract, op1=ALU.mult), reads=[t, mv, rstd], writes=[t])


def build_C2(NX, NCTX):
    k = KB()
    nc = k.nc
    TOK = NX + NCTX
    x1_d = k.dram_in("x1", [TOK, D])
    mod_d = k.dram_in("mod", [2, 3, 128, D])
    lnp_d = k.dram_in("lnp", [2, 128, D])
    wq_d = k.dram_in("wq", [128, 8, 2048])
    keysT_d = k.dram_in("keysT", [128, 16, 128])
    iota_d = k.dram_in("iota", [128, 256])
    u_d = k.dram_in("u_tab", [16384, D])
    v_d = k.dram_in("v_tab", [16384, D])
    out_d = k.dram_out("xout", [TOK, D])
    ident = _consts(k)

    wq = k.sb("wq_sb", [128, 8, 2048])
    keysT = k.sb("keysT_sb", [128, 16, 128])
    iota = k.sb("iota_sb", [128, 256])
    modts = [[k.sb(f"mod{t}_{j}", [128, D]) for j in range(3)] for t in range(2)]
    lng = k.sb("lng", [128, D]); lnb = k.sb("lnb", [128, D])
    x1ts = [k.sb(f"x1t{q}", [128, D]) for q in range(2)]; h2s = [k.sb(f"h2{q}", [128, D]) for q in range(2)]; acc = k.sb("acc", [128, D])
    junk = k.sb("junk", [128, D]); junk2 = k.sb("junk2", [128, 256])
    NB = 8
    gb = [k.sb(f"gb{j}", [128, D]) for j in range(NB)]
    T = k.sb("T", [128, 8, 128]); qT = k.sb("qT", [128, 16, 128])
    R1 = k.sb("R1", [128, 16, 128]); R2 = k.sb("R2", [128, 16, 128]); R3 = k.sb("R3", [128, 8, 256])
    sv = k.sb("sv", [128, 16, 16]); si = k.sb("si", [128, 16, 16], U32); sif = k.sb("sif", [128, 16, 16])
    fv = k.sb("fv", [128, 8, 16]); fi = k.sb("fi", [128, 8, 16], U32); fif = k.sb("fif", [128, 8, 16])
    eidf = k.sb("eidf", [128, 128]); eids = [k.sb(f"eid{q}", [128, 128], U32) for q in range(2)]
    negm = k.sb("negm", [128, 8]); gs = k.sb("gs", [128, 8]); gates = [k.sb(f"gate{q}", [128, 8, 16]) for q in range(2)]
    actv = k.sb("actv", [128, 128]); wgt = k.sb("wgt", [128, 128])
    stats = k.sb("stats", [128, 12]); mv = k.sb("mv", [128, 2]); rstd = k.sb("rstd", [128, 1])
    epst = k.sb("epst", [128, 1])
    pbank = [k.ps(f"pb{j}", [128, 4, 128]) for j in range(4)]

    k.op("pool", lambda: nc.gpsimd.memset(epst[:], LN_EPS), writes=[epst])
    for q in range(2):
        k.op("pool", lambda q=q: nc.gpsimd.memset(eids[q][:], 0), writes=[eids[q]])
        k.op("pool", lambda q=q: nc.gpsimd.memset(x1ts[q][:], 0.0), writes=[x1ts[q]])
    for kc in range(8):
        k.dma("sp", wq[:, kc, :], wq_d[:, kc, :])
    k.dma("sp", keysT[:], keysT_d[:, :, :])
    k.dma("sp", iota[:], iota_d[:, :])
    k.dma("sp", lng[:], lnp_d[0]); k.dma("sp", lnb[:], lnp_d[1])

    tiles = [(i * 128, 128, 0) for i in range(NX // 128)]
    if NCTX:
        tiles.append((NX, NCTX, 1))
    V = nc.vector
    for t_ in range(2 if NCTX else 1):
        for j in range(3):
            k.dma("sp", modts[t_][j][:], mod_d[t_, j])
        k.op("dve", lambda t_=t_: V.tensor_scalar_add(out=modts[t_][1][:], in0=modts[t_][1][:], scalar1=1.0), reads=[modts[t_][1]], writes=[modts[t_][1]])

    def emit_select(ti, r0, n, ty, x1t, h2, eid, gate, modt):
            k.dma("sp", x1t[:n, :], x1_d[r0:r0 + n, :])
            k.op("dve", lambda: V.tensor_tensor(out=h2[:], in0=x1t[:], in1=modt[1][:], op=ALU.mult), reads=[x1t, modt[1]], writes=[h2])
            k.op("dve", lambda: V.tensor_tensor(out=h2[:], in0=h2[:], in1=modt[0][:], op=ALU.add), reads=[h2, modt[0]], writes=[h2])
            for half in range(2):
                pb = pbank[half]
                for j in range(4):
                    kc = half * 4 + j
                    k.op("pe", lambda pb=pb, j=j, kc=kc: nc.tensor.transpose(out=pb[:, j, :], in_=h2[:, kc * 128:(kc + 1) * 128], identity=ident[:]),
                         reads=[h2, ident], writes=[pb], same_ok=True)
                k.op("act", lambda pb=pb, half=half: nc.scalar.copy(out=T[:, half * 4:(half + 1) * 4, :], in_=pb[:]), reads=[pb], writes=[T])
            for g4 in range(4):
                pb = pbank[g4]
                for j in range(4):
                    hp = g4 * 4 + j
                    for kc in range(8):
                        k.op("pe", lambda pb=pb, j=j, hp=hp, kc=kc: nc.tensor.matmul(pb[:, j, :], wq[:, kc, hp * 128:(hp + 1) * 128], T[:, kc, :],
                                                                                  start=(kc == 0), stop=(kc == 7)),
                             reads=[wq, T], writes=[pb], same_ok=True)
                k.op("act", lambda pb=pb, g4=g4: nc.scalar.copy(out=qT[:, g4 * 4:(g4 + 1) * 4, :], in_=pb[:]), reads=[pb], writes=[qT])
            for g4 in range(4):
                pb = pbank[g4]
                for j in range(4):
                    hp = g4 * 4 + j
                    k.op("pe", lambda pb=pb, j=j, hp=hp: nc.tensor.matmul(pb[:, j, :], qT[:, hp, :], keysT[:, hp, :], start=True, stop=True),
                         reads=[qT, keysT], writes=[pb], same_ok=True)
                k.op("act", lambda pb=pb, g4=g4: nc.scalar.copy(out=R1[:, g4 * 4:(g4 + 1) * 4, :], in_=pb[:]), reads=[pb], writes=[R1])
            for hp in range(16):
                k.op("dve", lambda hp=hp: V.max(out=sv[:, hp, 0:8], in_=R1[:, hp, :]), reads=[R1], writes=[sv])
                k.op("dve", lambda hp=hp: V.max_index(out=si[:, hp, 0:8], in_max=sv[:, hp, 0:8], in_values=R1[:, hp, :]), reads=[R1, sv], writes=[si])
                k.op("dve", lambda hp=hp: V.match_replace(out=R2[:, hp, :], in_to_replace=sv[:, hp, 0:8], in_values=R1[:, hp, :], imm_value=-1e30),
                     reads=[R1, sv], writes=[R2])
                k.op("dve", lambda hp=hp: V.max(out=sv[:, hp, 8:16], in_=R2[:, hp, :]), reads=[R2], writes=[sv])
                k.op("dve", lambda hp=hp: V.max_index(out=si[:, hp, 8:16], in_max=sv[:, hp, 8:16], in_values=R2[:, hp, :]), reads=[R2, sv], writes=[si])
            k.op("dve", lambda: V.tensor_copy(out=sif[:], in_=si[:]), reads=[si], writes=[sif])
            sv4 = sv[:].rearrange("p (h two) k -> p h two k", two=2)
            sif4 = sif[:].rearrange("p (h two) k -> p h two k", two=2)
            cand = R1[:].rearrange("p (h a) (b j) -> p h (a b) j", a=2, b=8)
            cand_flat = R1[:].rearrange("p (h a) m -> p h (a m)", a=2)
            cand2_flat = R2[:].rearrange("p (h a) m -> p h (a m)", a=2)
            cidx = R3[:].rearrange("p h (i j) -> p h i j", i=16)
            k.op("dve", lambda: V.tensor_tensor(out=cand, in0=sv4[:, :, 0, :, None].broadcast_to([128, 8, 16, 16]),
                                                in1=sv4[:, :, 1, None, :].broadcast_to([128, 8, 16, 16]), op=ALU.add),
                 reads=[sv], writes=[R1])
            k.op("dve", lambda: V.tensor_scalar_mul(out=sif4[:, :, 0, :], in0=sif4[:, :, 0, :], scalar1=128.0), reads=[sif], writes=[sif])
            k.op("dve", lambda: V.tensor_tensor(out=cidx, in0=sif4[:, :, 0, :, None].broadcast_to([128, 8, 16, 16]),
                                                in1=sif4[:, :, 1, None, :].broadcast_to([128, 8, 16, 16]), op=ALU.add),
                 reads=[sif], writes=[R3])
            for h in range(8):
                k.op("dve", lambda h=h: V.max(out=fv[:, h, 0:8], in_=cand_flat[:, h, :]), reads=[R1], writes=[fv])
                k.op("dve", lambda h=h: V.max_index(out=fi[:, h, 0:8], in_max=fv[:, h, 0:8], in_values=cand_flat[:, h, :]), reads=[R1, fv], writes=[fi])
                k.op("dve", lambda h=h: V.match_replace(out=cand2_flat[:, h, :], in_to_replace=fv[:, h, 0:8], in_values=cand_flat[:, h, :], imm_value=-1e30),
                     reads=[R1, fv], writes=[R2])
                k.op("dve", lambda h=h: V.max(out=fv[:, h, 8:16], in_=cand2_flat[:, h, :]), reads=[R2], writes=[fv])
                k.op("dve", lambda h=h: V.max_index(out=fi[:, h, 8:16], in_max=fv[:, h, 8:16], in_values=cand2_flat[:, h, :]), reads=[R2, fv], writes=[fi])
            k.op("dve", lambda: V.tensor_copy(out=fif[:], in_=fi[:]), reads=[fi], writes=[fif])
            for h in range(8):
                for j in range(16):
                    e = h * 16 + j
                    k.op("dve", lambda h=h, j=j, e=e: V.scalar_tensor_tensor(out=junk2[:, 0:256], in0=iota[:], scalar=fif[:, h, j:j + 1], in1=R3[:, h, :],
                                                                            op0=ALU.is_equal, op1=ALU.mult, accum_out=eidf[:, e:e + 1]),
                         reads=[iota, fif, R3], writes=[junk2, eidf])
            k.op("dve", lambda: V.tensor_copy(out=eid[:], in_=eidf[:]), reads=[eidf], writes=[eid])
            k.op("dve", lambda: V.tensor_scalar_mul(out=negm[:], in0=fv[:, :, 0], scalar1=-1.0), reads=[fv], writes=[negm])
            for h in range(8):
                k.op("act", lambda h=h: nc.scalar.activation(out=gate[:, h, :], in_=fv[:, h, :], func=AF.Exp, bias=negm[:, h:h + 1], scale=1.0,
                                                             accum_out=gs[:, h:h + 1]), reads=[fv, negm], writes=[gate, gs])
            k.op("dve", lambda: V.reciprocal(out=gs[:], in_=gs[:]), reads=[gs], writes=[gs])
            k.op("dve", lambda: V.tensor_tensor(out=gate[:], in0=gate[:], in1=gs[:, :, None].broadcast_to([128, 8, 16]), op=ALU.mult),
                 reads=[gate, gs], writes=[gate])

    def emit_gather(ti, r0, n, ty, x1t, h2, eid, gate, modt, pending):
            for e in range(128):
                b = gb[e % NB]
                k.dma("pool", b[:], u_d[:, :], fn=lambda b=b, e=e: nc.gpsimd.indirect_dma_start(
                    out=b[:], out_offset=None, in_=u_d[:, :], in_offset=bass.IndirectOffsetOnAxis(ap=eid[:, e:e + 1], axis=0)),
                    extra_reads=[eid])
                k.op("dve", lambda b=b, e=e: V.scalar_tensor_tensor(out=junk[:], in0=b[:], scalar=1.0, in1=h2[:],
                                                                    op0=ALU.mult, op1=ALU.mult, accum_out=actv[:, e:e + 1]),
                     reads=[b, h2], writes=[junk, actv])
                k.replay(pending, 2)
            k.op("act", lambda: nc.scalar.activation(out=wgt[:], in_=actv[:], func=AF.Gelu), reads=[actv], writes=[wgt])
            k.op("dve", lambda: V.tensor_tensor(out=wgt[:], in0=wgt[:], in1=gate[:].rearrange("p h j -> p (h j)"), op=ALU.mult),
                 reads=[wgt, gate], writes=[wgt])
            for e in range(128):
                b = gb[e % NB]
                k.dma("pool", b[:], v_d[:, :], fn=lambda b=b, e=e: nc.gpsimd.indirect_dma_start(
                    out=b[:], out_offset=None, in_=v_d[:, :], in_offset=bass.IndirectOffsetOnAxis(ap=eid[:, e:e + 1], axis=0)),
                    extra_reads=[eid])
                if e == 0:
                    k.op("dve", lambda b=b, e=e: V.tensor_scalar_mul(out=acc[:], in0=b[:], scalar1=wgt[:, 0:1]), reads=[b, wgt], writes=[acc])
                else:
                    k.op("dve", lambda b=b, e=e: V.scalar_tensor_tensor(out=acc[:], in0=b[:], scalar=wgt[:, e:e + 1], in1=acc[:],
                                                                        op0=ALU.mult, op1=ALU.add), reads=[b, wgt, acc], writes=[acc])
                k.replay(pending, 3)
            k.op("dve", lambda: V.tensor_tensor(out=acc[:], in0=acc[:], in1=modt[2][:], op=ALU.mult), reads=[acc, modt[2]], writes=[acc])
            k.op("dve", lambda: V.scalar_tensor_tensor(out=acc[:], in0=x1t[:], scalar=ALPHA, in1=acc[:], op0=ALU.mult, op1=ALU.add),
                 reads=[x1t, acc], writes=[acc])
            _layernorm_tile(k, acc, junk, stats, mv, rstd, epst)
            k.op("dve", lambda: V.tensor_tensor(out=acc[:], in0=acc[:], in1=lng[:], op=ALU.mult), reads=[acc, lng], writes=[acc])
            k.op("dve", lambda: V.tensor_tensor(out=junk[:], in0=acc[:], in1=lnb[:], op=ALU.add), reads=[acc, lnb], writes=[junk])
            k.dma("sp", out_d[r0:r0 + n, :], junk[:n, :])

    bufs = lambda ti, ty: (x1ts[ti % 2], h2s[ti % 2], eids[ti % 2], gates[ti % 2], modts[ty])
    for ti, (r0, n, ty) in enumerate(tiles):
        if ti == 0:
            emit_select(ti, r0, n, ty, *bufs(ti, ty))
        pending = []
        if ti + 1 < len(tiles):
            r1, n1, ty1 = tiles[ti + 1]
            k.rec = pending
            emit_select(ti + 1, r1, n1, ty1, *bufs(ti + 1, ty1))
            k.rec = None
        emit_gather(ti, r0, n, ty, *bufs(ti, ty), pending)
        k.replay(pending, len(pending))
    return k.finish()
def build_P0():
    k = KB(); nc = k.nc
    cT_d = k.dram_in("cT", [128, 8, 2])
    w_d = k.dram_in("w", [128, 8, 3072])
    b_d = k.dram_in("b", [2, 3072])
    out_d = k.dram_out("mod", [2, 3072])
    cT = k.sb("cT_sb", [128, 8, 2]); sT = k.sb("sT", [128, 8, 2])
    w = k.sb("w_sb", [128, 8, 3072]); b = k.sb("b_sb", [2, 3072]); o = k.sb("o_sb", [2, 3072])
    ps = [k.ps(f"ps{j}", [2, 512]) for j in range(2)]
    k.dma("sp", cT[:], cT_d[:, :, :]); k.dma("sp", b[:], b_d[:, :])
    for kc in range(8):
        k.dma("sp", w[:, kc, :], w_d[:, kc, :])
    k.op("act", lambda: nc.scalar.activation(out=sT[:], in_=cT[:], func=AF.Silu), reads=[cT], writes=[sT])
    for j in range(6):
        p = ps[j % 2]
        for kc in range(8):
            k.op("pe", lambda p=p, j=j, kc=kc: nc.tensor.matmul(p[:, :], sT[:, kc, :], w[:, kc, j * 512:(j + 1) * 512], start=(kc == 0), stop=(kc == 7)),
                 reads=[sT, w], writes=[p], same_ok=True)
        k.op("dve", lambda p=p, j=j: nc.vector.tensor_tensor(out=o[:, j * 512:(j + 1) * 512], in0=p[:, :], in1=b[:, j * 512:(j + 1) * 512], op=ALU.add),
             reads=[p, b], writes=[o])
    k.dma("sp", out_d[:, :], o[:])
    return k.finish()


def build_A_ml(NXc, NC):
    k = KB(); nc = k.nc; V = nc.vector
    NXE = NXc + 128
    R = NXc // 64
    NT = NXE + NC
    xT_d = k.dram_in("xT", [128, 8, NT])
    mod_d = k.dram_in("mod", [128, 2, 8, 2])
    w_d = k.dram_in("w_in", [128, 8, 4112])
    b_d = k.dram_in("b_in", [128, 33])
    cw_d = k.dram_in("conv_w", [128, 16, 9])
    cb_d = k.dram_in("conv_b", [128, 16])
    hm_d = k.dram_in("hmask", [128, 2])
    NO = NXc + NC
    q_d = k.dram_out("qk", [16, 128, NO])
    v_d = k.dram_out("v", [8, 128, NO])
    o_d = k.dram_out("o", [8, 128, NO])
    g_d = k.dram_out("g", [16, NO])

    hT = k.sb("hT", [128, 8, NT])
    mod = k.sb("mod_sb", [128, 2, 8, 2]); bsb = k.sb("b_sb", [128, 33]); cw = k.sb("cw", [128, 16, 9]); cb = k.sb("cb", [128, 16])
    hm = k.sb("hm", [128, 2])
    wbuf = [k.sb(f"wb{j}", [128, 8, 128]) for j in range(3)]
    pbuf = [k.sb(f"pbuf{j}", [128, NT]) for j in range(2)]
    cbuf = [k.sb(f"cbuf{j}", [128, NO]) for j in range(2)]
    obuf = [k.sb(f"obuf{j}", [128, NO]) for j in range(2)]
    psb = [k.ps(f"ps{j}", [128, 512]) for j in range(4)]
    for kc in range(8):
        k.dma("sp", hT[:, kc, :], xT_d[:, kc, :])
    k.dma("sp", mod[:], mod_d[:, :, :, :]); k.dma("sp", bsb[:], b_d[:, :]); k.dma("sp", cw[:], cw_d[:, :, :])
    k.dma("sp", cb[:], cb_d[:, :]); k.dma("sp", hm[:], hm_d[:, :])
    k.op("dve", lambda: V.tensor_scalar_add(out=mod[:, :, :, 1], in0=mod[:, :, :, 1], scalar1=1.0), reads=[mod], writes=[mod])
    for kc in range(8):
        k.op("dve", lambda kc=kc: V.tensor_scalar(out=hT[:, kc, 0:NXE], in0=hT[:, kc, 0:NXE], scalar1=mod[:, 0, kc, 1:2], scalar2=mod[:, 0, kc, 0:1],
                                                  op0=ALU.mult, op1=ALU.add), reads=[hT, mod], writes=[hT])
        k.op("dve", lambda kc=kc: V.tensor_scalar(out=hT[:, kc, NXE:NT], in0=hT[:, kc, NXE:NT], scalar1=mod[:, 1, kc, 1:2], scalar2=mod[:, 1, kc, 0:1],
                                                  op0=ALU.mult, op1=ALU.add), reads=[hT, mod], writes=[hT])
    blocks = []
    t0 = 0
    while t0 < NT:
        blocks.append((t0, min(512, NT - t0))); t0 += 512
    pi = 0
    for oc in range(33):
        M = 128 if oc < 32 else 16
        wb = wbuf[oc % 3]
        k.dma("sp", wb[:, :, 0:M], w_d[:, :, oc * 128:oc * 128 + M])
        pb = pbuf[oc % 2]
        for (b0, bn) in blocks:
            p = psb[pi % 4]; pi += 1
            for kc in range(8):
                k.op("pe", lambda p=p, wb=wb, kc=kc, b0=b0, bn=bn, M=M: nc.tensor.matmul(p[0:M, 0:bn], wb[:, kc, 0:M], hT[:, kc, b0:b0 + bn],
                                                                                      start=(kc == 0), stop=(kc == 7)),
                     reads=[wb, hT], writes=[p], same_ok=True)
            fn = AF.Sigmoid if 24 <= oc < 32 else AF.Identity
            k.op("act", lambda p=p, pb=pb, b0=b0, bn=bn, M=M, oc=oc, fn=fn: nc.scalar.activation(out=pb[0:M, b0:b0 + bn], in_=p[0:M, 0:bn], func=fn,
                                                                                           bias=bsb[0:M, oc:oc + 1], scale=1.0),
                 reads=[p, bsb], writes=[pb])
        if oc < 16:
            k.op("dve", lambda pb=pb: V.tensor_scalar_mul(out=pb[:, 0:64], in0=pb[:, 0:64], scalar1=hm[:, 0:1]), reads=[pb, hm], writes=[pb])
            k.op("dve", lambda pb=pb: V.tensor_scalar_mul(out=pb[:, NXE - 64:NXE], in0=pb[:, NXE - 64:NXE], scalar1=hm[:, 1:2]), reads=[pb, hm], writes=[pb])
            cbf = cbuf[oc % 2]
            pg = pb[:, 0:NXE].rearrange("p (r c) -> p r c", c=64)
            cg = cbf[:, 0:NXc].rearrange("p (r c) -> p r c", c=64)
            k.op("dve", lambda pg=pg, cg=cg, oc=oc: V.tensor_scalar(out=cg, in0=pg[:, 1:R + 1, :], scalar1=cw[:, oc, 4:5], scalar2=cb[:, oc:oc + 1],
                                                                 op0=ALU.mult, op1=ALU.add), reads=[pb, cw, cb], writes=[cbf])
            for dr in range(3):
                for dc in range(3):
                    if dr == 1 and dc == 1:
                        continue
                    c0, c1 = (1, 64) if dc == 0 else ((0, 63) if dc == 2 else (0, 64))
                    k.op("dve", lambda pg=pg, cg=cg, oc=oc, dr=dr, dc=dc, c0=c0, c1=c1: V.scalar_tensor_tensor(
                        out=cg[:, :, c0:c1], in0=pg[:, dr:dr + R, c0 + dc - 1:c1 + dc - 1], scalar=cw[:, oc, dr * 3 + dc:dr * 3 + dc + 1],
                        in1=cg[:, :, c0:c1], op0=ALU.mult, op1=ALU.add), reads=[pb, cw, cbf], writes=[cbf])
            k.op("dve", lambda pb=pb, cbf=cbf, oc=oc: V.tensor_scalar(out=cbf[:, NXc:NO], in0=pb[:, NXE:NT], scalar1=cw[:, oc, 4:5], scalar2=cb[:, oc:oc + 1],
                                                                    op0=ALU.mult, op1=ALU.add), reads=[pb, cw, cb], writes=[cbf])
            k.op("dve", lambda pb=pb, cbf=cbf, oc=oc: V.scalar_tensor_tensor(out=cbf[:, NXc + 1:NO], in0=pb[:, NXE:NT - 1], scalar=cw[:, oc, 3:4],
                                                                           in1=cbf[:, NXc + 1:NO], op0=ALU.mult, op1=ALU.add), reads=[pb, cw, cbf], writes=[cbf])
            k.op("dve", lambda pb=pb, cbf=cbf, oc=oc: V.scalar_tensor_tensor(out=cbf[:, NXc:NO - 1], in0=pb[:, NXE + 1:NT], scalar=cw[:, oc, 5:6],
                                                                           in1=cbf[:, NXc:NO - 1], op0=ALU.mult, op1=ALU.add), reads=[pb, cw, cbf], writes=[cbf])
            ob = obuf[oc % 2]
            k.op("act", lambda ob=ob, cbf=cbf: nc.scalar.activation(out=ob[:], in_=cbf[:], func=AF.Silu), reads=[cbf], writes=[ob])
            if oc < 8:
                k.op("dve", lambda ob=ob: V.tensor_scalar_mul(out=ob[:], in0=ob[:], scalar1=1.0 / 16.0), reads=[ob], writes=[ob])
            k.dma("sp", q_d[oc], ob[:])
        elif oc < 32:
            dst = v_d if oc < 24 else o_d
            j = oc - 16 if oc < 24 else oc - 24
            k.dma("sp", dst[j][:, 0:NXc], pb[:, 64:64 + NXc])
            k.dma("sp", dst[j][:, NXc:NO], pb[:, NXE:NT])
        else:
            k.dma("sp", g_d[:, 0:NXc], pb[0:16, 64:64 + NXc])
            k.dma("sp", g_d[:, NXc:NO], pb[0:16, NXE:NT])
    return k.finish()


def build_B_ml(NCH):
    k = KB(); nc = k.nc; V = nc.vector
    qT_d = k.dram_in("qT", [128, 2, NCH * 128])
    kT_d = k.dram_in("kT", [128, 2, NCH * 128])
    k_d = k.dram_in("k", [128, NCH, 256])
    v_d = k.dram_in("v", [128, NCH, 257])
    ig_d = k.dram_in("ig", [128, NCH]); fg_d = k.dram_in("fg", [128, NCH])
    tri_d = k.dram_in("tri", [128, 128]); mk_d = k.dram_in("maskT", [128, 128]); ones_d = k.dram_in("ones", [128, 128])
    h_d = k.dram_out("h", [128, NCH, 256])
    ident = _consts(k)
    tri = k.sb("tri_sb", [128, 128]); mk = k.sb("mk_sb", [128, 128]); ones = k.sb("ones_sb", [128, 128])
    ig = k.sb("ig_sb", [128, NCH]); LF = k.sb("LF", [128, NCH])
    k.dma("sp", tri[:], tri_d[:, :]); k.dma("sp", mk[:], mk_d[:, :]); k.dma("sp", ones[:], ones_d[:, :])
    k.dma("sp", ig[:], ig_d[:, :]); k.dma("sp", LF[:], fg_d[:, :])
    k.op("act", lambda: nc.scalar.activation(out=LF[:], in_=LF[:], func=AF.Exp, scale=-1.0), reads=[LF], writes=[LF])
    k.op("act", lambda: nc.scalar.activation(out=LF[:], in_=LF[:], func=AF.Ln, bias=1.0, scale=1.0), reads=[LF], writes=[LF])
    k.op("dve", lambda: V.tensor_scalar_mul(out=LF[:], in0=LF[:], scalar1=-1.0), reads=[LF], writes=[LF])
    NS = 3
    qb = [k.sb(f"qb{j}", [128, 2, 128]) for j in range(NS)]
    kb = [k.sb(f"kb{j}", [128, 2, 128]) for j in range(NS)]
    kt = [k.sb(f"kt{j}", [128, 256]) for j in range(NS)]
    vb = [k.sb(f"vb{j}", [128, 257]) for j in range(NS)]
    hb = [k.sb(f"hb{j}", [128, 256]) for j in range(2)]
    Cst = [k.sb(f"Cst{j}", [128, 257]) for j in range(2)]
    LFb = k.sb("LFb", [128, 128]); DT = k.sb("DT", [128, 128]); EB = k.sb("EB", [128, 128]); ST = k.sb("ST", [128, 128])
    qs = k.sb("qs", [128, 2, 128]); ka = k.sb("ka", [128, 256])
    wcol = k.sb("wcol", [128, 1]); acol = k.sb("acol", [128, 1]); Gc = k.sb("Gc", [128, 1]); den = k.sb("den", [128, 1])
    psA = k.ps("psA", [128, 128]); psB = k.ps("psB", [128, 128]); psC = k.ps("psC", [128, 2]); psS = k.ps("psS", [128, 128])
    psN = k.ps("psN", [128, 257]); psU = [k.ps(f"psU{j}", [128, 257]) for j in range(2)]
    for j in range(2):
        k.op("pool", lambda j=j: nc.gpsimd.memset(Cst[j][:], 0.0), writes=[Cst[j]])

    def load(c):
        s = c % NS
        k.dma("sp", qb[s][:], qT_d[:, :, c * 128:(c + 1) * 128])
        k.dma("sp", kb[s][:], kT_d[:, :, c * 128:(c + 1) * 128])
        k.dma("sp", kt[s][:], k_d[:, c, :])
        k.dma("sp", vb[s][:], v_d[:, c, :])
    load(0)
    if NCH > 1:
        load(1)
    for c in range(NCH):
        s = c % NS
        if c + 2 < NCH:
            load(c + 2)
        k.op("dve", lambda c=c: V.tensor_scalar_mul(out=LFb[:], in0=ones[:], scalar1=LF[:, c:c + 1]), reads=[ones, LF], writes=[LFb])
        k.op("pe", lambda: nc.tensor.matmul(psA[:, :], LFb[:], tri[:], start=True, stop=False), reads=[LFb, tri], writes=[psA], same_ok=True)
        k.op("pe", lambda: nc.tensor.matmul(psA[:, :], ident[:], mk[:], start=False, stop=True), reads=[ident, mk], writes=[psA], same_ok=True)
        k.op("pe", lambda: nc.tensor.matmul(psB[:, :], LFb[:], tri[:], start=True, stop=True), reads=[LFb, tri], writes=[psB], same_ok=True)
        k.op("pe", lambda c=c: nc.tensor.matmul(psC[:, 0:1], tri[:], LF[:, c:c + 1], start=True, stop=True), reads=[tri, LF], writes=[psC], same_ok=True)
        k.op("pe", lambda c=c: nc.tensor.matmul(psC[:, 1:2], ones[:], LF[:, c:c + 1], start=True, stop=True), reads=[ones, LF], writes=[psC], same_ok=True)
        k.op("dve", lambda c=c: V.tensor_tensor(out=wcol[:], in0=ig[:, c:c + 1], in1=psC[:, 0:1], op=ALU.subtract), reads=[ig, psC], writes=[wcol])
        k.op("act", lambda: nc.scalar.activation(out=DT[:], in_=psA[:, :], func=AF.Exp, bias=wcol[:, 0:1], scale=1.0), reads=[psA, wcol], writes=[DT])
        k.op("act", lambda: nc.scalar.activation(out=EB[:], in_=psB[:, :], func=AF.Exp), reads=[psB], writes=[EB])
        k.op("act", lambda: nc.scalar.activation(out=acol[:], in_=psC[:, 1:2], func=AF.Exp, bias=wcol[:, 0:1], scale=1.0), reads=[psC, wcol], writes=[acol])
        k.op("act", lambda: nc.scalar.activation(out=Gc[:], in_=psC[:, 1:2], func=AF.Exp), reads=[psC], writes=[Gc])
        for dc in range(2):
            k.op("pe", lambda s=s, dc=dc: nc.tensor.matmul(psS[:, :], kb[s][:, dc, :], qb[s][:, dc, :], start=(dc == 0), stop=(dc == 1)),
                 reads=[kb[s], qb[s]], writes=[psS], same_ok=True)
        k.op("dve", lambda: V.tensor_tensor(out=ST[:], in0=psS[:, :], in1=DT[:], op=ALU.mult), reads=[psS, DT], writes=[ST])
        k.op("dve", lambda s=s: V.tensor_tensor(out=qs[:], in0=qb[s][:], in1=EB[:, None, :].broadcast_to([128, 2, 128]), op=ALU.mult),
             reads=[qb[s], EB], writes=[qs])
        k.op("pe", lambda s=s: nc.tensor.matmul(psN[:, :], ST[:], vb[s][:], start=True, stop=False), reads=[ST, vb[s]], writes=[psN], same_ok=True)
        for dc in range(2):
            k.op("pe", lambda dc=dc: nc.tensor.matmul(psN[:, :], qs[:, dc, :], Cst[dc][:], start=False, stop=(dc == 1)),
                 reads=[qs, Cst[dc]], writes=[psN], same_ok=True)
        k.op("act", lambda: nc.scalar.activation(out=den[:], in_=psN[:, 256:257], func=AF.Abs), reads=[psN], writes=[den])
        k.op("dve", lambda: V.tensor_scalar_max(out=den[:], in0=den[:], scalar1=1.0), reads=[den], writes=[den])
        k.op("dve", lambda: V.reciprocal(out=den[:], in_=den[:]), reads=[den], writes=[den])
        hbb = hb[c % 2]
        k.op("dve", lambda hbb=hbb: V.tensor_scalar_mul(out=hbb[:], in0=psN[:, 0:256], scalar1=den[:, 0:1]), reads=[psN, den], writes=[hbb])
        k.dma("sp", h_d[:, c, :], hbb[:])
        k.op("pool", lambda s=s: nc.gpsimd.tensor_scalar(out=ka[:], in0=kt[s][:], scalar1=acol[:, 0:1], scalar2=None, op0=ALU.mult),
             reads=[kt[s], acol], writes=[ka])
        for dc in range(2):
            k.op("pe", lambda s=s, dc=dc: nc.tensor.matmul(psU[dc][:, :], ka[:, dc * 128:(dc + 1) * 128], vb[s][:], start=True, stop=True),
                 reads=[ka, vb[s]], writes=[psU[dc]], same_ok=True)
            k.op("dve", lambda dc=dc: V.scalar_tensor_tensor(out=Cst[dc][:], in0=Cst[dc][:], scalar=Gc[:, 0:1], in1=psU[dc][:, :], op0=ALU.mult, op1=ALU.add),
                 reads=[Cst[dc], Gc, psU[dc]], writes=[Cst[dc]])
    return k.finish()


def build_C1(kind, NX, NCTX):
    k = KB(); nc = k.nc; V = nc.vector
    TOK = NX + NCTX
    ml = kind == "ml"
    H, dh, eps = (4, 256, 1e-5) if ml else (16, 64, 64e-5)
    NPIECE = 3 if ml else 6
    NPRM = 1 if ml else 3
    xres_d = k.dram_in("xres", [TOK, D])
    pc_d = [k.dram_in(f"piece{j}", [TOK, D]) for j in range(NPIECE)]
    prm_d = k.dram_in("prm", [NPRM, 128, D])
    mod_d = k.dram_in("mod", [2, 128, D])
    lnp_d = k.dram_in("lnp", [2, 128, D])
    w_d = k.dram_in("w_out", [128, 8, D])
    out_d = k.dram_out("x1", [TOK, D])
    ident = _consts(k)
    w = k.sb("w_sb", [128, 8, D]); prm = [k.sb(f"prm{j}", [128, D]) for j in range(NPRM)]
    g1 = k.sb("g1", [128, D]); lng = k.sb("lng", [128, D]); lnb = k.sb("lnb", [128, D])
    xr = k.sb("xr", [128, D]); A = k.sb("A", [128, D]); B = k.sb("B", [128, D]); C = k.sb("C", [128, D])
    zT = k.sb("zT", [128, 8, 128]); t1 = k.sb("t1", [128, D]); junk = k.sb("junk", [128, D])
    sm = k.sb("sm", [128, H]); vs = k.sb("vs", [128, H]); bs = k.sb("bs", [128, H])
    stats = k.sb("stats", [128, 12]); mv = k.sb("mv", [128, 2]); rstd = k.sb("rstd", [128, 1])
    epst = k.sb("epst", [128, 1]); epsh = k.sb("epsh", [128, 1])
    pbank = [k.ps(f"pb{j}", [128, 512]) for j in range(4)]
    k.op("pool", lambda: nc.gpsimd.memset(epst[:], LN_EPS), writes=[epst])
    k.op("pool", lambda: nc.gpsimd.memset(epsh[:], eps), writes=[epsh])
    for kc in range(8):
        k.dma("sp", w[:, kc, :], w_d[:, kc, :])
    for j in range(NPRM):
        k.dma("sp", prm[j][:], prm_d[j])
    k.dma("sp", lng[:], lnp_d[0]); k.dma("sp", lnb[:], lnp_d[1])
    tiles = [(i * 128, 128, 0) for i in range(NX // 128)]
    if NCTX:
        tiles.append((NX, NCTX, 1))
    cur_ty = None
    hv = lambda t: t[:].rearrange("p (h e) -> p h e", h=H)
    bc = lambda s: s[:, :, None].broadcast_to([128, H, dh])
    for (r0, n, ty) in tiles:
        if ty != cur_ty:
            k.dma("sp", g1[:], mod_d[ty]); cur_ty = ty
        rs = slice(r0, r0 + n)
        k.dma("sp", xr[:n, :], xres_d[rs, :])
        k.dma("sp", A[:n, :], pc_d[0][rs, :]); k.dma("sp", B[:n, :], pc_d[1][rs, :])
        k.op("dve", lambda: V.tensor_tensor(out=A[:], in0=A[:], in1=B[:], op=ALU.add), reads=[A, B], writes=[A])
        k.op("dve", lambda: V.tensor_reduce(out=sm[:], in_=hv(A), axis=mybir.AxisListType.X, op=ALU.add), reads=[A], writes=[sm])
        k.op("dve", lambda: V.tensor_scalar_mul(out=sm[:], in0=sm[:], scalar1=1.0 / dh), reads=[sm], writes=[sm])
        k.op("dve", lambda: V.tensor_tensor(out=hv(A), in0=hv(A), in1=bc(sm), op=ALU.subtract), reads=[A, sm], writes=[A])
        k.op("dve", lambda: V.tensor_tensor(out=junk[:], in0=A[:], in1=A[:], op=ALU.mult), reads=[A], writes=[junk])
        k.op("dve", lambda: V.tensor_reduce(out=vs[:], in_=hv(junk), axis=mybir.AxisListType.X, op=ALU.add), reads=[junk], writes=[vs])
        k.op("act", lambda: nc.scalar.activation(out=vs[:], in_=vs[:], func=AF.Sqrt, bias=epsh[:, 0:1], scale=1.0 / dh), reads=[vs, epsh], writes=[vs])
        k.op("dve", lambda: V.reciprocal(out=vs[:], in_=vs[:]), reads=[vs], writes=[vs])
        k.op("dve", lambda: V.tensor_tensor(out=hv(A), in0=hv(A), in1=bc(vs), op=ALU.mult), reads=[A, vs], writes=[A])
        if ml:
            k.dma("sp", C[:n, :], pc_d[2][rs, :])
            k.op("dve", lambda: V.tensor_tensor(out=A[:], in0=A[:], in1=C[:], op=ALU.mult), reads=[A, C], writes=[A])
            k.op("dve", lambda: V.tensor_tensor(out=A[:], in0=A[:], in1=prm[0][:], op=ALU.mult), reads=[A, prm[0]], writes=[A])
        else:
            k.op("dve", lambda: V.tensor_tensor(out=A[:], in0=A[:], in1=prm[0][:], op=ALU.mult), reads=[A, prm[0]], writes=[A])
            k.op("dve", lambda: V.tensor_tensor(out=A[:], in0=A[:], in1=prm[1][:], op=ALU.add), reads=[A, prm[1]], writes=[A])
            k.dma("sp", B[:n, :], pc_d[2][rs, :]); k.dma("sp", C[:n, :], pc_d[3][rs, :])
            k.op("dve", lambda: V.tensor_tensor(out=B[:], in0=B[:], in1=C[:], op=ALU.mult), reads=[B, C], writes=[B])
            k.op("dve", lambda: V.tensor_tensor(out=B[:], in0=B[:], in1=prm[2][:], op=ALU.mult), reads=[B, prm[2]], writes=[B])
            k.op("dve", lambda: V.tensor_reduce(out=bs[:], in_=hv(B), axis=mybir.AxisListType.X, op=ALU.add), reads=[B], writes=[bs])
            k.dma("sp", C[:n, :], pc_d[4][rs, :])
            k.op("dve", lambda: V.tensor_tensor(out=hv(C), in0=hv(C), in1=bc(bs), op=ALU.mult), reads=[C, bs], writes=[C])
            k.op("dve", lambda: V.tensor_tensor(out=A[:], in0=A[:], in1=C[:], op=ALU.add), reads=[A, C], writes=[A])
            k.dma("sp", B[:n, :], pc_d[5][rs, :])
            k.op("dve", lambda: V.tensor_tensor(out=A[:], in0=A[:], in1=B[:], op=ALU.mult), reads=[A, B], writes=[A])
        for half in range(2):
            pb = pbank[half]
            for j in range(4):
                kc = half * 4 + j
                k.op("pe", lambda pb=pb, j=j, kc=kc: nc.tensor.transpose(out=pb[:, j * 128:(j + 1) * 128], in_=A[:, kc * 128:(kc + 1) * 128], identity=ident[:]),
                     reads=[A, ident], writes=[pb], same_ok=True)
            k.op("act", lambda pb=pb, half=half: nc.scalar.copy(out=zT[:, half * 4:(half + 1) * 4, :].rearrange("p a b -> p (a b)"), in_=pb[:, :]),
                 reads=[pb], writes=[zT])
        for half in range(2):
            pb = pbank[2 + half]
            for kc in range(8):
                k.op("pe", lambda pb=pb, kc=kc, half=half: nc.tensor.matmul(pb[:, :], zT[:, kc, :], w[:, kc, half * 512:(half + 1) * 512],
                                                                          start=(kc == 0), stop=(kc == 7)), reads=[zT, w], writes=[pb], same_ok=True)
            k.op("dve", lambda pb=pb, half=half: V.tensor_tensor(out=t1[:, half * 512:(half + 1) * 512], in0=pb[:, :], in1=g1[:, half * 512:(half + 1) * 512],
                                                                 op=ALU.mult), reads=[pb, g1], writes=[t1])
        k.op("dve", lambda: V.scalar_tensor_tensor(out=t1[:], in0=xr[:], scalar=ALPHA, in1=t1[:], op0=ALU.mult, op1=ALU.add), reads=[xr, t1], writes=[t1])
        _layernorm_tile(k, t1, junk, stats, mv, rstd, epst)
        k.op("dve", lambda: V.tensor_tensor(out=t1[:], in0=t1[:], in1=lng[:], op=ALU.mult), reads=[t1, lng], writes=[t1])
        k.op("dve", lambda: V.tensor_tensor(out=junk[:], in0=t1[:], in1=lnb[:], op=ALU.add), reads=[t1, lnb], writes=[junk])
        k.dma("sp", out_d[rs, :], junk[:n, :])
    return k.finish()


NCORES = 8
_PROGS = {}


def _run(key, builder, in_maps):
    if key not in _PROGS:
        _PROGS[key] = builder()
    res = run_bass_kernel_spmd(_PROGS[key], in_maps, core_ids=list(range(len(in_maps))))
    return res.results


def _f(a):
    return np.ascontiguousarray(a, dtype=np.float32)


def _bc(v):
    return _f(np.broadcast_to(np.asarray(v, np.float32), (128, v.shape[-1])))


def _kc_layout(w):
    return _f(w.reshape(8, 128, -1).transpose(1, 0, 2))


def _fm(vec):
    return _f(vec.reshape(-1, 128).T)


_IDENT = np.eye(128, dtype=np.float32)


def run_P0(c, c_ctx, ada_w, ada_b):
    depth = ada_w.shape[0]
    cc = np.stack([c.reshape(-1), c_ctx.reshape(-1)], axis=1)
    cT = _f(cc.reshape(8, 128, 2).transpose(1, 0, 2))
    in_maps = []
    for core in range(NCORES):
        i, half = core // 2, core % 2
        i = min(i, depth - 1)
        sl = slice(half * 3072, (half + 1) * 3072)
        in_maps.append({"cT": cT, "w": _kc_layout(ada_w[i][:, sl]), "b": _f(np.stack([ada_b[i][sl], ada_b[i][sl]]))})
    res = _run("P0", build_P0, in_maps)
    mods = np.zeros((depth, 2, 6144), np.float32)
    for core in range(2 * depth):
        i, half = core // 2, core % 2
        mods[i][:, half * 3072:(half + 1) * 3072] = res[core]["mod"]
    return mods.reshape(depth, 2, 6, 1024)


def run_C1(kind, x, ctx, pieces_x, pieces_c, prm, g1x, g1c, ln_g, ln_b, w_out):
    NX, NC = x.shape[0], ctx.shape[0]
    nxc, ncc = NX // NCORES, NC // NCORES
    common = {"prm": _f(np.stack([_bc(p) for p in prm])), "mod": _f(np.stack([_bc(g1x), _bc(g1c)])),
              "lnp": _f(np.stack([_bc(ln_g), _bc(ln_b)])), "w_out": _kc_layout(w_out), "ident": _IDENT}
    in_maps = []
    for c in range(NCORES):
        m = dict(common)
        m["xres"] = _f(np.concatenate([x[c * nxc:(c + 1) * nxc], ctx[c * ncc:(c + 1) * ncc]]))
        for j, (px, pc) in enumerate(zip(pieces_x, pieces_c)):
            m[f"piece{j}"] = _f(np.concatenate([px[c * nxc:(c + 1) * nxc], pc[c * ncc:(c + 1) * ncc]]))
        in_maps.append(m)
    res = _run(("C1", kind, nxc, ncc), lambda: build_C1(kind, nxc, ncc), in_maps)
    x1 = np.concatenate([r["x1"][:nxc] for r in res]); c1 = np.concatenate([r["x1"][nxc:] for r in res])
    return x1, c1


def run_C2(x1, c1, modx, modc, ln_g, ln_b, wq, keys, u_tab, v_tab):
    NX, NC = x1.shape[0], c1.shape[0]
    nxc, ncc = NX // NCORES, NC // NCORES
    common = {"mod": _f(np.stack([np.stack([_bc(m) for m in modx]), np.stack([_bc(m) for m in modc])])),
              "lnp": _f(np.stack([_bc(ln_g), _bc(ln_b)])), "wq": _kc_layout(wq),
              "keysT": _f(keys.reshape(16, 128, 128).transpose(2, 0, 1)),
              "iota": _f(np.broadcast_to(np.arange(256, dtype=np.float32), (128, 256))),
              "u_tab": _f(u_tab), "v_tab": _f(v_tab), "ident": _IDENT}
    in_maps = []
    for c in range(NCORES):
        m = dict(common)
        m["x1"] = _f(np.concatenate([x1[c * nxc:(c + 1) * nxc], c1[c * ncc:(c + 1) * ncc]]))
        in_maps.append(m)
    res = _run(("C2", nxc, ncc), lambda: build_C2(nxc, ncc), in_maps)
    x2 = np.concatenate([r["xout"][:nxc] for r in res]); c2 = np.concatenate([r["xout"][nxc:] for r in res])
    return x2, c2


def run_mlstm(x, ctx, modx, modc, w_in, b_in, conv_w, conv_b):
    NX, NC = x.shape[0], ctx.shape[0]
    nxc = NX // NCORES
    xpad = np.concatenate([np.zeros((64, D), np.float32), x, np.zeros((64, D), np.float32)])
    mod = np.zeros((128, 2, 8, 2), np.float32)
    for ty, m in enumerate((modx, modc)):
        mod[:, ty, :, 0] = _fm(m[0]); mod[:, ty, :, 1] = _fm(m[1])
    b33 = np.zeros((128, 33), np.float32)
    b33[:, :32] = _fm(b_in[:4096]); b33[:16, 32] = b_in[4096:]
    common = {"mod": mod, "w_in": _kc_layout(w_in), "b_in": b33,
              "conv_w": _f(conv_w.reshape(9, 16, 128).transpose(2, 1, 0)), "conv_b": _fm(conv_b)}
    in_maps = []
    for c in range(NCORES):
        win = np.concatenate([xpad[c * nxc:c * nxc + nxc + 128], ctx])
        m = dict(common)
        m["xT"] = _f(win.T.reshape(8, 128, -1).transpose(1, 0, 2))
        hm = np.ones((128, 2), np.float32)
        if c == 0:
            hm[:, 0] = 0
        if c == NCORES - 1:
            hm[:, 1] = 0
        m["hmask"] = hm
        in_maps.append(m)
    res = _run(("A_ml", nxc, NC), lambda: build_A_ml(nxc, NC), in_maps)

    def gather(name, nfeat):
        xs = np.concatenate([r[name].reshape(nfeat, -1)[:, :nxc] for r in res], axis=1)
        cs = res[0][name].reshape(nfeat, -1)[:, nxc:]
        return xs, cs
    qk_x, qk_c = gather("qk", 2048); v_x, v_c = gather("v", 1024); o_x, o_c = gather("o", 1024); g_x, g_c = gather("g", 16)
    T = NC + NX
    NCH = T // 128
    tri = _f(np.triu(np.ones((128, 128), np.float32)))
    maskT = _f(np.where(np.triu(np.ones((128, 128))) > 0, 0.0, -30000.0))
    consts = {"tri": tri, "maskT": maskT, "ones": np.ones((128, 128), np.float32), "ident": _IDENT}
    in_maps = []
    for core in range(NCORES):
        h, d = core % 4, core // 4

        def seqT(ax, ac):
            if d == 0:
                return np.concatenate([ac, ax], axis=1)
            return np.concatenate([ac[:, ::-1], ax[:, ::-1]], axis=1)
        hs = slice(h * 256, (h + 1) * 256)
        qT = seqT(qk_x[hs], qk_c[hs]); kT = seqT(qk_x[1024:][hs], qk_c[1024:][hs]); vT = seqT(v_x[hs], v_c[hs])
        ig = seqT(g_x[d * 4 + h][None], g_c[d * 4 + h][None])[0]
        fg = seqT(g_x[8 + d * 4 + h][None], g_c[8 + d * 4 + h][None])[0]
        m = dict(consts)
        m["qT"] = _f(qT.reshape(2, 128, T).transpose(1, 0, 2)); m["kT"] = _f(kT.reshape(2, 128, T).transpose(1, 0, 2))
        m["k"] = _f(kT.T.reshape(NCH, 128, 256).transpose(1, 0, 2))
        vext = np.concatenate([vT.T, np.ones((T, 1), np.float32)], axis=1)
        m["v"] = _f(vext.reshape(NCH, 128, 257).transpose(1, 0, 2))
        m["ig"] = _f(ig.reshape(NCH, 128).T); m["fg"] = _f(fg.reshape(NCH, 128).T)
        in_maps.append(m)
    res = _run(("B_ml", NCH), lambda: build_B_ml(NCH), in_maps)
    hf = np.zeros((T, D), np.float32); hb = np.zeros((T, D), np.float32)
    for core in range(NCORES):
        h, d = core % 4, core // 4
        hh = res[core]["h"].transpose(1, 0, 2).reshape(T, 256)
        if d == 0:
            hf[:, h * 256:(h + 1) * 256] = hh
        else:
            hb[:NC, h * 256:(h + 1) * 256] = hh[:NC][::-1]
            hb[NC:, h * 256:(h + 1) * 256] = hh[NC:][::-1]
    return (hf[NC:], hb[NC:], _f(o_x.T)), (hf[:NC], hb[:NC], _f(o_c.T))


def build_A_rw(NXc, NC):
    k = KB(); nc = k.nc; V = nc.vector
    NXE = NXc + 128
    NT = NXE + NC
    NO = NXc + NC
    BS = min(512, NXc)
    BW = max(BS, NC)
    xT_d = k.dram_in("xT", [128, 8, NT])
    mod_d = k.dram_in("mod", [128, 2, 8, 2])
    hm_d = k.dram_in("hmask", [128, 2])
    mu_d = k.dram_in("mu", [128, 6, 8])
    wrkv_d = k.dram_in("w_rkv", [3, 128, 8, D])
    w1_d = k.dram_in("w1", [4, 128, 8, 64])
    w2_d = k.dram_in("w2", [4, 64, D])
    g1_d = k.dram_in("g1", [128, 8, 160])
    g2a_d = k.dram_in("g2a", [128, D]); g2b_d = k.dram_in("g2b", [32, D])
    vec_d = k.dram_in("vecs", [128, 7, 8])
    bd_d = k.dram_in("bdones", [128, 128])
    names = ["r", "v", "kkneg", "b0", "b1", "ktil0", "ktil1", "logw0", "logw1", "kbar", "g"]
    outs = {n: k.dram_out(n, [8, 128, NO]) for n in names}

    mod = k.sb("mod_sb", [128, 2, 8, 2]); hm = k.sb("hm", [128, 2]); mu = k.sb("mu_sb", [128, 6, 8])
    w1 = [k.sb(f"w1_{j}", [128, 8, 64]) for j in range(4)]
    w2 = [k.sb(f"w2_{j}", [64, D]) for j in range(4)]
    g1 = k.sb("g1_sb", [128, 8, 160]); g2a = k.sb("g2a_sb", [128, D]); g2b = k.sb("g2b_sb", [32, D])
    vec = k.sb("vec_sb", [128, 7, 8]); bd = k.sb("bd_sb", [128, 128]); omka = k.sb("omka", [128, 8])
    hTb = k.sb("hTb", [128, 8, BW + 128]); sTb = k.sb("sTb", [128, 8, BW]); xm = k.sb("xm", [128, 8, BW])
    kT = k.sb("kT", [128, 8, BW]); kk = k.sb("kk", [128, 8, BW])
    th = [k.sb(f"th{j}", [64, BW]) for j in range(2)]
    gs0 = k.sb("gs0", [128, BW]); gs1 = k.sb("gs1", [32, BW])
    wbuf = [k.sb(f"wb{j}", [128, 8, 128]) for j in range(3)]
    ob = [k.sb(f"ob{j}", [128, BW]) for j in range(6)]
    tmp = [k.sb(f"tmp{j}", [128, BW]) for j in range(3)]
    psb = [k.ps(f"ps{j}", [128, 512]) for j in range(6)]
    cnt = {"ps": 0, "ob": 0, "wb": 0}

    def nps():
        cnt["ps"] += 1; return psb[cnt["ps"] % 6]

    def nob():
        cnt["ob"] += 1; return ob[cnt["ob"] % 6]

    k.dma("sp", mod[:], mod_d[:, :, :, :]); k.dma("sp", hm[:], hm_d[:, :]); k.dma("sp", mu[:], mu_d[:, :, :])
    for j in range(4):
        k.dma("sp", w1[j][:], w1_d[j]); k.dma("sp", w2[j][:], w2_d[j])
    k.dma("sp", g1[:], g1_d[:, :, :]); k.dma("sp", g2a[:], g2a_d[:, :]); k.dma("sp", g2b[:], g2b_d[:, :])
    k.dma("sp", vec[:], vec_d[:, :, :]); k.dma("sp", bd[:], bd_d[:, :])
    k.op("dve", lambda: V.tensor_scalar_add(out=mod[:, :, :, 1], in0=mod[:, :, :, 1], scalar1=1.0), reads=[mod], writes=[mod])
    k.op("dve", lambda: V.tensor_scalar(out=omka[:], in0=vec[:, 5, :], scalar1=-1.0, scalar2=1.0, op0=ALU.mult, op1=ALU.add), reads=[vec], writes=[omka])

    blocks = [(b0, BS, 0) for b0 in range(0, NXc, BS)]
    if NC:
        assert NC <= 512
        blocks.append((0, NC, 1))
    for (b0, bs, ty) in blocks:
        off = 64 if ty == 0 else 0
        wn = bs + 128 if ty == 0 else bs
        src0 = b0 if ty == 0 else NXE
        o0 = b0 if ty == 0 else NXc
        for kc in range(8):
            k.dma("sp", hTb[:, kc, 0:wn], xT_d[:, kc, src0:src0 + wn])
        for kc in range(8):
            k.op("dve", lambda kc=kc: V.tensor_scalar(out=hTb[:, kc, 0:wn], in0=hTb[:, kc, 0:wn], scalar1=mod[:, ty, kc, 1:2], scalar2=mod[:, ty, kc, 0:1],
                                                      op0=ALU.mult, op1=ALU.add), reads=[hTb, mod], writes=[hTb])
        k.op("pool", lambda: nc.gpsimd.memset(sTb[:], 0.0), writes=[sTb])
        if ty == 0:
            if b0 == 0:
                k.op("dve", lambda: V.tensor_scalar_mul(out=hTb[:, :, 0:64], in0=hTb[:, :, 0:64], scalar1=hm[:, 0:1]), reads=[hTb, hm], writes=[hTb])
            if b0 + bs == NXc:
                k.op("dve", lambda: V.tensor_scalar_mul(out=hTb[:, :, bs + 64:bs + 128], in0=hTb[:, :, bs + 64:bs + 128], scalar1=hm[:, 1:2]),
                     reads=[hTb, hm], writes=[hTb])
            hg = lambda kc0, kc1, lo: hTb[:, kc0:kc1, lo:lo + bs].rearrange("p k (r c) -> p k r c", c=64)
            sg = sTb[:, :, 0:bs].rearrange("p k (r c) -> p k r c", c=64)
            for kc in range(2):
                k.op("dve", lambda kc=kc: V.tensor_copy(out=sg[:, kc, :, 1:64], in_=hg(kc, kc + 1, 64)[:, 0, :, 0:63]), reads=[hTb], writes=[sTb])
                k.op("dve", lambda kc=kc: V.tensor_copy(out=sg[:, 2 + kc, :, 0:63], in_=hg(2 + kc, 3 + kc, 64)[:, 0, :, 1:64]), reads=[hTb], writes=[sTb])
            k.op("dve", lambda: V.tensor_copy(out=sTb[:, 4:6, 0:bs], in_=hTb[:, 4:6, 0:bs]), reads=[hTb], writes=[sTb])
            k.op("dve", lambda: V.tensor_copy(out=sTb[:, 6:8, 0:bs], in_=hTb[:, 6:8, 128:128 + bs]), reads=[hTb], writes=[sTb])
        else:
            k.op("dve", lambda: V.tensor_copy(out=sTb[:, 0:4, 1:bs], in_=hTb[:, 0:4, 0:bs - 1]), reads=[hTb], writes=[sTb])
            k.op("dve", lambda: V.tensor_copy(out=sTb[:, 4:8, 0:bs - 1], in_=hTb[:, 4:8, 1:bs]), reads=[hTb], writes=[sTb])
        hc = lambda kc: hTb[:, kc, off:off + bs]
        k.op("dve", lambda: V.tensor_tensor(out=sTb[:, :, 0:bs], in0=sTb[:, :, 0:bs], in1=hTb[:, :, off:off + bs], op=ALU.subtract), reads=[sTb, hTb], writes=[sTb])

        def mix(n):
            for kc in range(8):
                k.op("dve", lambda kc=kc: V.scalar_tensor_tensor(out=xm[:, kc, 0:bs], in0=sTb[:, kc, 0:bs], scalar=mu[:, n, kc:kc + 1], in1=hc(kc),
                                                                 op0=ALU.mult, op1=ALU.add), reads=[sTb, mu, hTb], writes=[xm])

        def proj(n, oc):
            cnt["wb"] += 1
            wb = wbuf[cnt["wb"] % 3]
            k.dma("sp", wb[:], wrkv_d[n][:, :, oc * 128:(oc + 1) * 128])
            p = nps()
            for kc in range(8):
                k.op("pe", lambda p=p, wb=wb, kc=kc: nc.tensor.matmul(p[:, 0:bs], wb[:, kc, :], xm[:, kc, 0:bs], start=(kc == 0), stop=(kc == 7)),
                     reads=[wb, xm], writes=[p], same_ok=True)
            return p

        def store(name, oc, t):
            k.dma("sp", outs[name][oc][:, o0:o0 + bs], t[:, 0:bs])

        mix(0)
        for oc in range(8):
            p = proj(0, oc); o = nob()
            k.op("act", lambda p=p, o=o: nc.scalar.copy(out=o[:, 0:bs], in_=p[:, 0:bs]), reads=[p], writes=[o])
            store("r", oc, o)
        mix(1)
        for oc in range(8):
            p = proj(1, oc)
            k.op("act", lambda p=p, oc=oc: nc.scalar.copy(out=kT[:, oc, 0:bs], in_=p[:, 0:bs]), reads=[p], writes=[kT])
            t0, t1 = tmp[0], tmp[1]
            k.op("dve", lambda oc=oc: V.tensor_scalar_mul(out=t0[:, 0:bs], in0=kT[:, oc, 0:bs], scalar1=vec[:, 4, oc:oc + 1]), reads=[kT, vec], writes=[t0])
            k.op("dve", lambda: V.tensor_tensor(out=t1[:, 0:bs], in0=t0[:, 0:bs], in1=t0[:, 0:bs], op=ALU.mult), reads=[t0], writes=[t1])
            p2 = nps()
            k.op("pe", lambda p2=p2: nc.tensor.matmul(p2[:, 0:bs], bd[:], t1[:, 0:bs], start=True, stop=True), reads=[bd, t1], writes=[p2], same_ok=True)
            k.op("act", lambda p2=p2: nc.scalar.activation(out=t1[:, 0:bs], in_=p2[:, 0:bs], func=AF.Sqrt), reads=[p2], writes=[t1])
            k.op("dve", lambda: V.tensor_scalar_max(out=t1[:, 0:bs], in0=t1[:, 0:bs], scalar1=1e-12), reads=[t1], writes=[t1])
            k.op("dve", lambda: V.reciprocal(out=t1[:, 0:bs], in_=t1[:, 0:bs]), reads=[t1], writes=[t1])
            k.op("dve", lambda oc=oc: V.tensor_tensor(out=kk[:, oc, 0:bs], in0=t0[:, 0:bs], in1=t1[:, 0:bs], op=ALU.mult), reads=[t0, t1], writes=[kk])
            o = nob()
            k.op("dve", lambda o=o, oc=oc: V.tensor_scalar_mul(out=o[:, 0:bs], in0=kk[:, oc, 0:bs], scalar1=-1.0), reads=[kk], writes=[o])
            store("kkneg", oc, o)
        mix(2)
        for oc in range(8):
            p = proj(2, oc); o = nob()
            k.op("act", lambda p=p, o=o: nc.scalar.copy(out=o[:, 0:bs], in_=p[:, 0:bs]), reads=[p], writes=[o])
            store("v", oc, o)

        def lora_in(j, dst, func):
            p = nps()
            for kc in range(8):
                k.op("pe", lambda p=p, kc=kc, j=j: nc.tensor.matmul(p[0:64, 0:bs], w1[j][:, kc, :], xm[:, kc, 0:bs], start=(kc == 0), stop=(kc == 7)),
                     reads=[w1[j], xm], writes=[p], same_ok=True)
            k.op("act", lambda p=p, dst=dst: nc.scalar.activation(out=dst[:, 0:bs], in_=p[0:64, 0:bs], func=func), reads=[p], writes=[dst])

        mix(3)
        for z in range(2):
            lora_in(z, th[z], AF.Tanh)
        for oc in range(8):
            for z in range(2):
                p = nps()
                k.op("pe", lambda p=p, z=z, oc=oc: nc.tensor.matmul(p[:, 0:bs], w2[z][:, oc * 128:(oc + 1) * 128], th[z][:, 0:bs], start=True, stop=True),
                     reads=[w2[z], th[z]], writes=[p], same_ok=True)
                o = nob()
                k.op("act", lambda p=p, o=o, z=z, oc=oc: nc.scalar.activation(out=o[:, 0:bs], in_=p[:, 0:bs], func=AF.Sigmoid, bias=vec[:, z, oc:oc + 1], scale=1.0),
                     reads=[p, vec], writes=[o])
                k.op("dve", lambda o=o: V.tensor_scalar_mul(out=o[:, 0:bs], in0=o[:, 0:bs], scalar1=-0.6065306597126334), reads=[o], writes=[o])
                store(f"logw{z}", oc, o)
        mix(4)
        for z in range(2):
            lora_in(2 + z, th[z], AF.Identity)
        for oc in range(8):
            kt_z = []
            for z in range(2):
                p = nps()
                k.op("pe", lambda p=p, z=z, oc=oc: nc.tensor.matmul(p[:, 0:bs], w2[2 + z][:, oc * 128:(oc + 1) * 128], th[z][:, 0:bs], start=True, stop=True),
                     reads=[w2[2 + z], th[z]], writes=[p], same_ok=True)
                asg = tmp[z]
                k.op("act", lambda p=p, asg=asg, z=z, oc=oc: nc.scalar.activation(out=asg[:, 0:bs], in_=p[:, 0:bs], func=AF.Sigmoid, bias=vec[:, 2 + z, oc:oc + 1], scale=1.0),
                     reads=[p, vec], writes=[asg])
                o = nob()
                k.op("dve", lambda o=o, asg=asg, oc=oc: V.tensor_tensor(out=o[:, 0:bs], in0=kk[:, oc, 0:bs], in1=asg[:, 0:bs], op=ALU.mult), reads=[kk, asg], writes=[o])
                store(f"b{z}", oc, o)
                k.op("dve", lambda asg=asg, oc=oc: V.tensor_scalar(out=asg[:, 0:bs], in0=asg[:, 0:bs], scalar1=vec[:, 5, oc:oc + 1], scalar2=omka[:, oc:oc + 1],
                                                                 op0=ALU.mult, op1=ALU.add), reads=[asg, vec, omka], writes=[asg])
                o2 = nob()
                k.op("dve", lambda o2=o2, asg=asg, oc=oc: V.tensor_tensor(out=o2[:, 0:bs], in0=asg[:, 0:bs], in1=kT[:, oc, 0:bs], op=ALU.mult), reads=[asg, kT], writes=[o2])
                store(f"ktil{z}", oc, o2)
                kt_z.append(o2)
            o3 = nob()
            k.op("dve", lambda o3=o3, a=kt_z[0], b=kt_z[1]: V.tensor_tensor(out=o3[:, 0:bs], in0=a[:, 0:bs], in1=b[:, 0:bs], op=ALU.add), reads=[kt_z[0], kt_z[1]], writes=[o3])
            k.op("dve", lambda o3=o3: V.tensor_scalar_mul(out=o3[:, 0:bs], in0=o3[:, 0:bs], scalar1=0.5), reads=[o3], writes=[o3])
            store("kbar", oc, o3)
        mix(5)
        for (m0, mn, dst) in ((0, 128, gs0), (128, 32, gs1)):
            p = nps()
            for kc in range(8):
                k.op("pe", lambda p=p, kc=kc, m0=m0, mn=mn: nc.tensor.matmul(p[0:mn, 0:bs], g1[:, kc, m0:m0 + mn], xm[:, kc, 0:bs], start=(kc == 0), stop=(kc == 7)),
                     reads=[g1, xm], writes=[p], same_ok=True)
            k.op("act", lambda p=p, dst=dst, mn=mn: nc.scalar.activation(out=dst[:, 0:bs], in_=p[0:mn, 0:bs], func=AF.Sigmoid), reads=[p], writes=[dst])
        for oc in range(8):
            p = nps()
            k.op("pe", lambda p=p, oc=oc: nc.tensor.matmul(p[:, 0:bs], g2a[:, oc * 128:(oc + 1) * 128], gs0[:, 0:bs], start=True, stop=False),
                 reads=[g2a, gs0], writes=[p], same_ok=True)
            k.op("pe", lambda p=p, oc=oc: nc.tensor.matmul(p[:, 0:bs], g2b[:, oc * 128:(oc + 1) * 128], gs1[:, 0:bs], start=False, stop=True),
                 reads=[g2b, gs1], writes=[p], same_ok=True)
            o = nob()
            k.op("act", lambda p=p, o=o: nc.scalar.copy(out=o[:, 0:bs], in_=p[:, 0:bs]), reads=[p], writes=[o])
            store("g", oc, o)
    return k.finish()


def build_B_rw(NCH):
    k = KB(); nc = k.nc; V = nc.vector
    NSC = 4
    T = NCH * 128
    tk_d = {n: k.dram_in(n, [128, NCH, NSC, 64]) for n in ("lw_t", "b_t", "k_t", "v_t")}
    ch_d = {n: k.dram_in(n, [64, NSC, T]) for n in ("r_c", "a_c", "b_c", "k_c")}
    tri_d = k.dram_in("tri", [128, 128]); tris_d = k.dram_in("tris", [128, 128])
    msu_d = k.dram_in("m_su", [128, 128]); msl_d = k.dram_in("m_sl", [128, 128]); miu_d = k.dram_in("m_iu", [128, 128])
    y_d = k.dram_out("y", [128, NCH, NSC, 64])
    ident = _consts(k)
    tri = k.sb("tri_sb", [128, 128]); tris = k.sb("tris_sb", [128, 128])
    msu = k.sb("msu", [128, 128]); msl = k.sb("msl", [128, 128]); miu = k.sb("miu", [128, 128])
    for t, d in ((tri, tri_d), (tris, tris_d), (msu, msu_d), (msl, msl_d), (miu, miu_d)):
        k.dma("sp", t[:], d[:, :])
    NS = 2
    tk = {n: [k.sb(f"{n}_s{j}", [128, NSC, 64]) for j in range(NS)] for n in tk_d}
    ch = {n: [k.sb(f"{n}_s{j}", [64, NSC, 128]) for j in range(NS)] for n in ch_d}
    Pinv = k.sb("Pinv", [128, NSC, 64]); PT = k.sb("PT", [64, NSC, 128]); PinvT = k.sb("PinvT", [64, NSC, 128]); Pm1T = k.sb("Pm1T", [64, NSC, 128])
    At = k.sb("At", [64, NSC, 128]); BtT = k.sb("BtT", [64, NSC, 128]); KtT = k.sb("KtT", [64, NSC, 128]); RtT = k.sb("RtT", [64, NSC, 128])
    Btok = k.sb("Btok", [128, NSC, 64]); Ktok = k.sb("Ktok", [128, NSC, 64])
    Nn = [k.sb(f"Nn{j}", [128, NSC, 128]) for j in range(2)]; NTt = [k.sb(f"NTt{j}", [128, NSC, 128]) for j in range(2)]
    X = k.sb("X", [128, NSC, 128]); XT = k.sb("XT", [128, NSC, 128])
    MakT = k.sb("MakT", [128, NSC, 128]); MrbT = k.sb("MrbT", [128, NSC, 128]); MrkT = k.sb("MrkT", [128, NSC, 128])
    W = k.sb("W", [128, NSC, 64]); U = k.sb("U", [128, NSC, 64]); Yb = [k.sb(f"Yb{j}", [128, NSC, 64]) for j in range(2)]
    Z = k.sb("Z", [64, NSC, 64]); Zt = k.sb("Zt", [64, NSC, 64])
    banks = [k.ps(f"bk{j}", [128, 512]) for j in range(8)]
    bi = [0]

    def bank():
        bi[0] += 1
        return banks[bi[0] % 8]
    k.op("pool", lambda: nc.gpsimd.memset(Z[:], 0.0), writes=[Z])

    def load(c):
        s = c % NS
        for n in tk_d:
            k.dma("sp", tk[n][s][:], tk_d[n][:, c, :, :])
        for n in ch_d:
            k.dma("sp", ch[n][s][:], ch_d[n][:, :, c * 128:(c + 1) * 128])
    load(0)
    bcm = lambda m: m[:, None, :].broadcast_to([128, NSC, 128])
    v3 = lambda b: b[:, :].rearrange("p (s t) -> p s t", s=NSC)
    v64 = lambda b: b[:, 0:NSC * 64].rearrange("p (s t) -> p s t", s=NSC)
    for c in range(NCH):
        s = c % NS
        if c + 1 < NCH:
            load(c + 1)
        LW, Bt_, Kt_, Vt = tk["lw_t"][s], tk["b_t"][s], tk["k_t"][s], tk["v_t"][s]
        Rc, Ac, Bc, Kc = ch["r_c"][s], ch["a_c"][s], ch["b_c"][s], ch["k_c"][s]
        pLP = bank()
        k.op("pe", lambda: nc.tensor.matmul(pLP[:, 0:256], tri[:], LW[:].rearrange("p s c -> p (s c)"), start=True, stop=True), reads=[tri, LW], writes=[pLP], same_ok=True)
        pLT = bank(); pL1 = bank()
        for sc in range(NSC):
            k.op("pe", lambda sc=sc: nc.tensor.matmul(pLT[0:64, sc * 128:(sc + 1) * 128], LW[:, sc, :], tri[:], start=True, stop=True), reads=[LW, tri], writes=[pLT], same_ok=True)
            k.op("pe", lambda sc=sc: nc.tensor.matmul(pL1[0:64, sc * 128:(sc + 1) * 128], LW[:, sc, :], tris[:], start=True, stop=True), reads=[LW, tris], writes=[pL1], same_ok=True)
        k.op("act", lambda: nc.scalar.activation(out=Pinv[:].rearrange("p s c -> p (s c)"), in_=pLP[:, 0:256], func=AF.Exp, scale=-1.0), reads=[pLP], writes=[Pinv])
        k.op("act", lambda: nc.scalar.activation(out=PT[:].rearrange("p s c -> p (s c)"), in_=pLT[0:64, :], func=AF.Exp), reads=[pLT], writes=[PT])
        k.op("act", lambda: nc.scalar.activation(out=PinvT[:].rearrange("p s c -> p (s c)"), in_=pLT[0:64, :], func=AF.Exp, scale=-1.0), reads=[pLT], writes=[PinvT])
        k.op("act", lambda: nc.scalar.activation(out=Pm1T[:].rearrange("p s c -> p (s c)"), in_=pL1[0:64, :], func=AF.Exp), reads=[pL1], writes=[Pm1T])
        k.op("dve", lambda: V.tensor_tensor(out=At[:], in0=Ac[:], in1=Pm1T[:], op=ALU.mult), reads=[Ac, Pm1T], writes=[At])
        k.op("dve", lambda: V.tensor_tensor(out=BtT[:], in0=Bc[:], in1=PinvT[:], op=ALU.mult), reads=[Bc, PinvT], writes=[BtT])
        k.op("dve", lambda: V.tensor_tensor(out=KtT[:], in0=Kc[:], in1=PinvT[:], op=ALU.mult), reads=[Kc, PinvT], writes=[KtT])
        k.op("dve", lambda: V.tensor_tensor(out=RtT[:], in0=Rc[:], in1=PT[:], op=ALU.mult), reads=[Rc, PT], writes=[RtT])
        k.op("pool", lambda: nc.gpsimd.tensor_tensor(out=Btok[:], in0=Bt_[:], in1=Pinv[:], op=ALU.mult), reads=[Bt_, Pinv], writes=[Btok])
        k.op("pool", lambda: nc.gpsimd.tensor_tensor(out=Ktok[:], in0=Kt_[:], in1=Pinv[:], op=ALU.mult), reads=[Kt_, Pinv], writes=[Ktok])
        for (L, Rr, dst, msk) in ((BtT, At, Nn[0], msu), (At, BtT, NTt[0], msl), (KtT, At, MakT, msu), (BtT, RtT, MrbT, miu), (KtT, RtT, MrkT, miu)):
            p = bank()
            for sc in range(NSC):
                k.op("pe", lambda p=p, L=L, Rr=Rr, sc=sc: nc.tensor.matmul(p[:, sc * 128:(sc + 1) * 128], L[:, sc, :], Rr[:, sc, :], start=True, stop=True),
                     reads=[L, Rr], writes=[p], same_ok=True)
            k.op("dve", lambda p=p, dst=dst, msk=msk: V.tensor_tensor(out=dst[:], in0=v3(p), in1=bcm(msk), op=ALU.mult), reads=[p, msk], writes=[dst])
        k.op("dve", lambda: V.tensor_tensor(out=X[:], in0=Nn[0][:], in1=bcm(ident), op=ALU.add), reads=[Nn[0], ident], writes=[X])
        k.op("dve", lambda: V.tensor_tensor(out=XT[:], in0=NTt[0][:], in1=bcm(ident), op=ALU.add), reads=[NTt[0], ident], writes=[XT])
        cur = 0
        for it in range(6):
            nxt = 1 - cur
            last = it == 5
            pN2 = bank()
            for sc in range(NSC):
                k.op("pe", lambda sc=sc, cur=cur, pN2=pN2: nc.tensor.matmul(pN2[:, sc * 128:(sc + 1) * 128], NTt[cur][:, sc, :], Nn[cur][:, sc, :], start=True, stop=True),
                     reads=[NTt[cur], Nn[cur]], writes=[pN2], same_ok=True)
            k.op("act", lambda pN2=pN2, nxt=nxt: nc.scalar.copy(out=Nn[nxt][:], in_=v3(pN2)), reads=[pN2], writes=[Nn[nxt]])
            if not last:
                pT2 = bank()
                for sc in range(NSC):
                    k.op("pe", lambda sc=sc, cur=cur, pT2=pT2: nc.tensor.matmul(pT2[:, sc * 128:(sc + 1) * 128], Nn[cur][:, sc, :], NTt[cur][:, sc, :], start=True, stop=True),
                         reads=[Nn[cur], NTt[cur]], writes=[pT2], same_ok=True)
                k.op("act", lambda pT2=pT2, nxt=nxt: nc.scalar.copy(out=NTt[nxt][:], in_=v3(pT2)), reads=[pT2], writes=[NTt[nxt]])
            pX = bank()
            for sc in range(NSC):
                k.op("pe", lambda sc=sc, nxt=nxt, pX=pX: nc.tensor.matmul(pX[:, sc * 128:(sc + 1) * 128], XT[:, sc, :], Nn[nxt][:, sc, :], start=True, stop=True),
                     reads=[XT, Nn[nxt]], writes=[pX], same_ok=True)
            if not last:
                pXT = bank()
                for sc in range(NSC):
                    k.op("pe", lambda sc=sc, nxt=nxt, pXT=pXT: nc.tensor.matmul(pXT[:, sc * 128:(sc + 1) * 128], X[:, sc, :], NTt[nxt][:, sc, :], start=True, stop=True),
                         reads=[X, NTt[nxt]], writes=[pXT], same_ok=True)
            k.op("dve", lambda pX=pX: V.tensor_tensor(out=X[:], in0=X[:], in1=v3(pX), op=ALU.add), reads=[X, pX], writes=[X])
            if not last:
                k.op("dve", lambda pXT=pXT: V.tensor_tensor(out=XT[:], in0=XT[:], in1=v3(pXT), op=ALU.add), reads=[XT, pXT], writes=[XT])
            cur = nxt
        pW = bank()
        for sc in range(NSC):
            k.op("pe", lambda sc=sc: nc.tensor.matmul(pW[:, sc * 64:(sc + 1) * 64], At[:, sc, :], Z[:, sc, :], start=True, stop=False), reads=[At, Z], writes=[pW], same_ok=True)
            k.op("pe", lambda sc=sc: nc.tensor.matmul(pW[:, sc * 64:(sc + 1) * 64], MakT[:, sc, :], Vt[:, sc, :], start=False, stop=True), reads=[MakT, Vt], writes=[pW], same_ok=True)
        k.op("act", lambda: nc.scalar.copy(out=W[:], in_=v64(pW)), reads=[pW], writes=[W])
        pU = bank()
        for sc in range(NSC):
            k.op("pe", lambda sc=sc: nc.tensor.matmul(pU[:, sc * 64:(sc + 1) * 64], X[:, sc, :], W[:, sc, :], start=True, stop=True), reads=[X, W], writes=[pU], same_ok=True)
        k.op("act", lambda: nc.scalar.copy(out=U[:], in_=v64(pU)), reads=[pU], writes=[U])
        pY = bank()
        for sc in range(NSC):
            k.op("pe", lambda sc=sc: nc.tensor.matmul(pY[:, sc * 64:(sc + 1) * 64], RtT[:, sc, :], Z[:, sc, :], start=True, stop=False), reads=[RtT, Z], writes=[pY], same_ok=True)
            k.op("pe", lambda sc=sc: nc.tensor.matmul(pY[:, sc * 64:(sc + 1) * 64], MrbT[:, sc, :], U[:, sc, :], start=False, stop=False), reads=[MrbT, U], writes=[pY], same_ok=True)
            k.op("pe", lambda sc=sc: nc.tensor.matmul(pY[:, sc * 64:(sc + 1) * 64], MrkT[:, sc, :], Vt[:, sc, :], start=False, stop=True), reads=[MrkT, Vt], writes=[pY], same_ok=True)
        yb = Yb[c % 2]
        k.op("act", lambda yb=yb: nc.scalar.copy(out=yb[:], in_=v64(pY)), reads=[pY], writes=[yb])
        k.dma("sp", y_d[:, c, :, :], yb[:])
        pZ = bank()
        for sc in range(NSC):
            k.op("pe", lambda sc=sc: nc.tensor.matmul(pZ[0:64, sc * 64:(sc + 1) * 64], Btok[:, sc, :], U[:, sc, :], start=True, stop=False), reads=[Btok, U], writes=[pZ], same_ok=True)
            k.op("pe", lambda sc=sc: nc.tensor.matmul(pZ[0:64, sc * 64:(sc + 1) * 64], Ktok[:, sc, :], Vt[:, sc, :], start=False, stop=True), reads=[Ktok, Vt], writes=[pZ], same_ok=True)
        k.op("dve", lambda: V.tensor_tensor(out=Zt[:], in0=Z[:], in1=pZ[0:64, 0:NSC * 64].rearrange("p (s t) -> p s t", s=NSC), op=ALU.add), reads=[Z, pZ], writes=[Zt])
        k.op("dve", lambda: V.tensor_tensor(out=Z[:], in0=Zt[:], in1=PT[:, :, 127:128].broadcast_to([64, NSC, 64]), op=ALU.mult), reads=[Zt, PT], writes=[Z])
    return k.finish()


def run_rwkv(x, ctx, modx, modc, P):
    NX, NC = x.shape[0], ctx.shape[0]
    nxc = NX // NCORES
    xpad = np.concatenate([np.zeros((64, D), np.float32), x, np.zeros((64, D), np.float32)])
    mod = np.zeros((128, 2, 8, 2), np.float32)
    for ty, m in enumerate((modx, modc)):
        mod[:, ty, :, 0] = _fm(m[0]); mod[:, ty, :, 1] = _fm(m[1])
    vecs = np.zeros((128, 7, 8), np.float32)
    for j, v in enumerate((P["w0"][0], P["w0"][1], P["a0"][0], P["a0"][1], P["k_k"], P["k_a"])):
        vecs[:, j, :] = _fm(v)
    bd = np.zeros((128, 128), np.float32); bd[:64, :64] = 1; bd[64:, 64:] = 1
    common = {"mod": mod, "mu": _f(np.stack([_fm(P["mu"][n]) for n in range(6)], axis=1)),
              "w_rkv": _f(np.stack([_kc_layout(P["w_rkv"][n]) for n in range(3)])),
              "w1": _f(np.stack([_kc_layout(P["w1"][0]), _kc_layout(P["w1"][1]), _kc_layout(P["a1"][0]), _kc_layout(P["a1"][1])])),
              "w2": _f(np.stack([P["w2"][0], P["w2"][1], P["a2"][0], P["a2"][1]])),
              "g1": _kc_layout(P["g1"]), "g2a": _f(P["g2"][:128]), "g2b": _f(P["g2"][128:]), "vecs": vecs, "bdones": bd}
    in_maps = []
    for c in range(NCORES):
        win = np.concatenate([xpad[c * nxc:c * nxc + nxc + 128], ctx])
        m = dict(common)
        m["xT"] = _f(win.T.reshape(8, 128, -1).transpose(1, 0, 2))
        hm = np.ones((128, 2), np.float32)
        if c == 0:
            hm[:, 0] = 0
        if c == NCORES - 1:
            hm[:, 1] = 0
        m["hmask"] = hm
        in_maps.append(m)
    res = _run(("A_rw", nxc, NC), lambda: build_A_rw(nxc, NC), in_maps)
    fmx, fmc = {}, {}
    for n in ["r", "v", "kkneg", "b0", "b1", "ktil0", "ktil1", "logw0", "logw1", "kbar", "g"]:
        fmx[n] = np.concatenate([r[n].reshape(1024, -1)[:, :nxc] for r in res], axis=1)
        fmc[n] = res[0][n].reshape(1024, -1)[:, nxc:]
    T = NC + NX
    NCH = T // 128
    iu = np.triu(np.ones((128, 128), np.float32)); su = np.triu(np.ones((128, 128), np.float32), 1)
    consts = {"tri": _f(iu), "tris": _f(su), "m_su": _f(su), "m_sl": _f(su.T), "m_iu": _f(iu), "ident": _IDENT}
    in_maps = []
    for core in range(NCORES):
        tkm = {n: [] for n in ("lw_t", "b_t", "k_t", "v_t")}
        chm = {n: [] for n in ("r_c", "a_c", "b_c", "k_c")}
        for j in range(4):
            sid = core * 4 + j
            hd, z = sid // 2, sid % 2
            hs = slice(hd * 64, (hd + 1) * 64)

            def seq(n):
                ax, ac = fmx[n][hs], fmc[n][hs]
                if z == 0:
                    return np.concatenate([ac, ax], axis=1)
                return np.concatenate([ac[:, ::-1], ax[:, ::-1]], axis=1)
            tkm["lw_t"].append(seq(f"logw{z}").T); tkm["b_t"].append(seq(f"b{z}").T); tkm["k_t"].append(seq(f"ktil{z}").T); tkm["v_t"].append(seq("v").T)
            chm["r_c"].append(seq("r")); chm["a_c"].append(seq("kkneg")); chm["b_c"].append(seq(f"b{z}")); chm["k_c"].append(seq(f"ktil{z}"))
        m = dict(consts)
        for n, lst in tkm.items():
            m[n] = _f(np.stack(lst).reshape(4, NCH, 128, 64).transpose(2, 1, 0, 3))
        for n, lst in chm.items():
            m[n] = _f(np.stack(lst).transpose(1, 0, 2))
        in_maps.append(m)
    res = _run(("B_rw", NCH), lambda: build_B_rw(NCH), in_maps)
    yf = np.zeros((T, D), np.float32); yb = np.zeros((T, D), np.float32)
    for core in range(NCORES):
        yy = res[core]["y"]
        for j in range(4):
            sid = core * 4 + j
            hd, z = sid // 2, sid % 2
            ys = yy[:, :, j, :].transpose(1, 0, 2).reshape(T, 64)
            if z == 0:
                yf[:, hd * 64:(hd + 1) * 64] = ys
            else:
                yb[:NC, hd * 64:(hd + 1) * 64] = ys[:NC][::-1]
                yb[NC:, hd * 64:(hd + 1) * 64] = ys[NC:][::-1]
    px = [yf[NC:], yb[NC:]] + [_f(fmx[n].T) for n in ("r", "kbar", "v", "g")]
    pc = [yf[:NC], yb[:NC]] + [_f(fmc[n].T) for n in ("r", "kbar", "v", "g")]
    return px, pc


def kernel(x, c, ctx, c_ctx, ada_w, ada_b, ln_g, ln_b,
           ml_w_in, ml_b_in, ml_conv_w, ml_conv_b, ml_hn_g, ml_w_out,
           rw_mu, rw_w_rkv, rw_w0, rw_w1, rw_w2, rw_a0, rw_a1, rw_a2, rw_g1, rw_g2,
           rw_k_k, rw_k_a, rw_r_k, rw_lnx_g, rw_lnx_b, rw_w_out,
           pk_wq, pk_keys, pk_u, pk_v):
    A = lambda a: np.asarray(a, dtype=np.float32)
    xs = A(x)[0]; cs = A(ctx)[0]
    depth = ada_w.shape[0]
    mods = run_P0(A(c), A(c_ctx), A(ada_w), A(ada_b))
    for i in range(depth):
        j = i // 2
        modx, modc = mods[i, 0], mods[i, 1]
        if i % 2 == 0:
            px, pc = run_mlstm(xs, cs, modx, modc, A(ml_w_in[j]), A(ml_b_in[j]), A(ml_conv_w[j]), A(ml_conv_b[j]))
            x1, c1 = run_C1("ml", xs, cs, list(px), list(pc), [A(ml_hn_g[j])], modx[2], modc[2], A(ln_g[i, 0]), A(ln_b[i, 0]), A(ml_w_out[j]))
        else:
            P = {"mu": A(rw_mu[j]), "w_rkv": A(rw_w_rkv[j]), "w0": A(rw_w0[j]), "w1": A(rw_w1[j]), "w2": A(rw_w2[j]),
                 "a0": A(rw_a0[j]), "a1": A(rw_a1[j]), "a2": A(rw_a2[j]), "g1": A(rw_g1[j]), "g2": A(rw_g2[j]),
                 "k_k": A(rw_k_k[j]), "k_a": A(rw_k_a[j])}
            px, pc = run_rwkv(xs, cs, modx, modc, P)
            x1, c1 = run_C1("rw", xs, cs, px, pc, [A(rw_lnx_g[j]), A(rw_lnx_b[j]), A(rw_r_k[j])], modx[2], modc[2],
                            A(ln_g[i, 0]), A(ln_b[i, 0]), A(rw_w_out[j]))
        xs, cs = run_C2(x1, c1, modx[3:6], modc[3:6], A(ln_g[i, 1]), A(ln_b[i, 1]), A(pk_wq[i]), A(pk_keys[i]), A(pk_u[i]), A(pk_v[i]))
    return np.ascontiguousarray(xs[None].astype(np.float32))
```

```python
import contextlib
import numpy as np
import concourse.bass as bass
import concourse.mybir as mybir
from concourse.alu_op_type import AluOpType as ALU
from concourse.bass_utils import run_bass_kernel_spmd

F32 = mybir.dt.float32
I32 = mybir.dt.int32
U32 = mybir.dt.uint32
AF = mybir.ActivationFunctionType


class KB:
    def __init__(self):
        self.nc = bass.Bass("TRN2", target_bir_lowering=False)
        nc = self.nc
        self.es = contextlib.ExitStack()
        self.es.enter_context(nc.cleanup_on_exit())
        self.engs = {"pe": nc.tensor, "dve": nc.vector, "act": nc.scalar,
                     "pool": nc.gpsimd, "sp": nc.sync}
        self.esem = {}
        self.ecnt = {}
        for e in self.engs:
            self.esem[e] = nc.alloc_semaphore(name=f"s_{e}")
            self.ecnt[e] = 0
        self.seen = {e: {} for e in self.engs}
        self.tr = {}
        self.dsem = {}
        self.n_inst = 0
        self._uid = 0
        self.rec = None

    def sb(self, name, shape, dt=F32):
        t = self.es.enter_context(self.nc.sbuf_tensor(name, list(shape), dt))
        return t

    def ps(self, name, shape, dt=F32):
        t = self.es.enter_context(self.nc.psum_tensor(name, list(shape), dt))
        return t

    def dram_in(self, name, shape, dt=F32):
        return self.nc.dram_tensor(name, list(shape), dt, kind="ExternalInput").ap()

    def dram_out(self, name, shape, dt=F32):
        return self.nc.dram_tensor(name, list(shape), dt, kind="ExternalOutput").ap()

    @staticmethod
    def _key(ap):
        t = getattr(ap, "tensor", ap)
        return t.name

    def _needs(self, reads, writes):
        needs = {}

        def need(sv):
            sem, val = sv
            k = sem.name if hasattr(sem, "name") else id(sem)
            if k not in needs or needs[k][1] < val:
                needs[k] = (sem, val)

        for ap in reads:
            st = self.tr.get(self._key(ap))
            if st and st[0]:
                need(st[0])
        for ap in writes:
            st = self.tr.get(self._key(ap))
            if st:
                if st[0]:
                    need(st[0])
                for sv in st[1].values():
                    need(sv)
        return needs

    def _emit_waits(self, e, needs, skip_sem=None):
        eng = self.engs[e]
        seen = self.seen[e]
        for k, (sem, val) in needs.items():
            if skip_sem is not None and sem is skip_sem:
                continue
            if seen.get(k, -1) >= val:
                continue
            eng.wait_ge(sem, val)
            seen[k] = val

    def _update(self, reads, writes, sv):
        sem, val = sv
        k = sem.name if hasattr(sem, "name") else id(sem)
        for ap in reads:
            st = self.tr.setdefault(self._key(ap), [None, {}])
            st[1][k] = sv
        for ap in writes:
            st = self.tr.setdefault(self._key(ap), [None, {}])
            st[0] = sv
            st[1] = {}

    def op(self, e, fn, reads=(), writes=(), same_ok=False):
        if self.rec is not None:
            self.rec.append(("op", (e, fn), dict(reads=reads, writes=writes, same_ok=same_ok)))
            return None
        needs = self._needs(reads, writes)
        self._emit_waits(e, needs, skip_sem=self.esem[e] if same_ok else None)
        inst = fn()
        self.ecnt[e] += 1
        inst.then_inc(self.esem[e], 1)
        self._update(reads, writes, (self.esem[e], self.ecnt[e]))
        self.n_inst += 1
        return inst

    def dma(self, q, out, in_, fn=None, extra_reads=(), **kw):
        if self.rec is not None:
            self.rec.append(("dma", (q, out, in_), dict(fn=fn, extra_reads=extra_reads, **kw)))
            return None
        reads, writes = [in_] + list(extra_reads), [out]
        needs = self._needs(reads, writes)
        self._emit_waits(q, needs)
        sbt = None
        for ap in (out, in_):
            if "sbuf" in str(ap.space).lower() or "sb" == str(ap.space).lower():
                sbt = ap
        keyt = self._key(sbt if sbt is not None else out)
        if keyt not in self.dsem:
            self.dsem[keyt] = [self.nc.alloc_semaphore(name=f"d_{len(self.dsem)}"), 0]
        ds = self.dsem[keyt]
        if fn is None:
            inst = self.engs[q].dma_start(out=out, in_=in_, **kw)
        else:
            inst = fn()
        ds[1] += 16
        inst.then_inc(ds[0], 16)
        self._update(reads, writes, (ds[0], ds[1]))
        self.n_inst += 1
        return inst

    def replay(self, lst, n):
        for _ in range(min(n, len(lst))):
            kind, a, kw = lst.pop(0)
            if kind == "op":
                self.op(*a, **kw)
            else:
                self.dma(*a, **kw)

    def finish(self):
        sp = self.engs["sp"]
        for e in self.engs:
            if self.ecnt[e] > 0 and e != "sp":
                sp.wait_ge(self.esem[e], self.ecnt[e])
        for k, (sem, cnt) in self.dsem.items():
            sp.wait_ge(sem, cnt)
        self.nc.all_engine_barrier()
        self.es.close()
        return self.nc


D = 1024
ALPHA = (2.0 * 4) ** 0.25
LN_EPS = 1e-5


def _consts(k, need_iota=False):
    ident_d = k.dram_in("ident", [128, 128])
    ident = k.sb("ident_sb", [128, 128])
    k.dma("sp", ident[:], ident_d[:, :])
    return ident


def _layernorm_tile(k, t, tmp, stats, mv, rstd, epst):
    nc = k.nc
    for j in range(2):
        k.op("dve", lambda j=j: nc.vector.bn_stats(out=stats[:, j * 6:(j + 1) * 6], in_=t[:, j * 512:(j + 1) * 512]),
             reads=[t], writes=[stats])
    k.op("dve", lambda: nc.vector.bn_aggr(out=mv[:, 0:2], in_=stats[:, 0:12]), reads=[stats], writes=[mv])
    k.op("act", lambda: nc.scalar.activation(out=rstd[:, 0:1], in_=mv[:, 1:2], func=AF.Sqrt, bias=epst[:, 0:1], scale=1.0),
         reads=[mv, epst], writes=[rstd])
    k.op("dve", lambda: nc.vector.reciprocal(out=rstd[:, 0:1], in_=rstd[:, 0:1]), reads=[rstd], writes=[rstd])
    k.op("dve", lambda: nc.vector.tensor_scalar(out=t[:], in0=t[:], scalar1=mv[:, 0:1], scalar2=rstd[:, 0:1],
                                                op0=ALU.subtract, op1=ALU.mult), reads=[t, mv, rstd], writes=[t])


def build_C2(NX, NCTX):
    k = KB()
    nc = k.nc
    TOK = NX + NCTX
    x1_d = k.dram_in("x1", [TOK, D])
    mod_d = k.dram_in("mod", [2, 3, 128, D])
    lnp_d = k.dram_in("lnp", [2, 128, D])
    wq_d = k.dram_in("wq", [128, 8, 2048])
    keysT_d = k.dram_in("keysT", [128, 16, 128])
    iota_d = k.dram_in("iota", [128, 256])
    u_d = k.dram_in("u_tab", [16384, D])
    v_d = k.dram_in("v_tab", [16384, D])
    out_d = k.dram_out("xout", [TOK, D])
    ident = _consts(k)

    wq = k.sb("wq_sb", [128, 8, 2048])
    keysT = k.sb("keysT_sb", [128, 16, 128])
    iota = k.sb("iota_sb", [128, 256])
    modts = [[k.sb(f"mod{t}_{j}", [128, D]) for j in range(3)] for t in range(2)]
    lng = k.sb("lng", [128, D]); lnb = k.sb("lnb", [128, D])
    x1ts = [k.sb(f"x1t{q}", [128, D]) for q in range(2)]; h2s = [k.sb(f"h2{q}", [128, D]) for q in range(2)]; acc = k.sb("acc", [128, D])
    junk = k.sb("junk", [128, D]); junk2 = k.sb("junk2", [128, 256])
    NB = 8
    gb = [k.sb(f"gb{j}", [128, D]) for j in range(NB)]
    T = k.sb("T", [128, 8, 128]); qT = k.sb("qT", [128, 16, 128])
    R1 = k.sb("R1", [128, 16, 128]); R2 = k.sb("R2", [128, 16, 128]); R3 = k.sb("R3", [128, 8, 256])
    sv = k.sb("sv", [128, 16, 16]); si = k.sb("si", [128, 16, 16], U32); sif = k.sb("sif", [128, 16, 16])
    fv = k.sb("fv", [128, 8, 16]); fi = k.sb("fi", [128, 8, 16], U32); fif = k.sb("fif", [128, 8, 16])
    eidf = k.sb("eidf", [128, 128]); eids = [k.sb(f"eid{q}", [128, 128], U32) for q in range(2)]
    negm = k.sb("negm", [128, 8]); gs = k.sb("gs", [128, 8]); gates = [k.sb(f"gate{q}", [128, 8, 16]) for q in range(2)]
    actv = k.sb("actv", [128, 128]); wgt = k.sb("wgt", [128, 128])
    stats = k.sb("stats", [128, 12]); mv = k.sb("mv", [128, 2]); rstd = k.sb("rstd", [128, 1])
    epst = k.sb("epst", [128, 1])
    pbank = [k.ps(f"pb{j}", [128, 4, 128]) for j in range(4)]

    k.op("pool", lambda: nc.gpsimd.memset(epst[:], LN_EPS), writes=[epst])
    for q in range(2):
        k.op("pool", lambda q=q: nc.gpsimd.memset(eids[q][:], 0), writes=[eids[q]])
        k.op("pool", lambda q=q: nc.gpsimd.memset(x1ts[q][:], 0.0), writes=[x1ts[q]])
    for kc in range(8):
        k.dma("sp", wq[:, kc, :], wq_d[:, kc, :])
    k.dma("sp", keysT[:], keysT_d[:, :, :])
    k.dma("sp", iota[:], iota_d[:, :])
    k.dma("sp", lng[:], lnp_d[0]); k.dma("sp", lnb[:], lnp_d[1])

    tiles = [(i * 128, 128, 0) for i in range(NX // 128)]
    if NCTX:
        tiles.append((NX, NCTX, 1))
    V = nc.vector
    for t_ in range(2 if NCTX else 1):
        for j in range(3):
            k.dma("sp", modts[t_][j][:], mod_d[t_, j])
        k.op("dve", lambda t_=t_: V.tensor_scalar_add(out=modts[t_][1][:], in0=modts[t_][1][:], scalar1=1.0), reads=[modts[t_][1]], writes=[modts[t_][1]])

    def emit_select(ti, r0, n, ty, x1t, h2, eid, gate, modt):
            k.dma("sp", x1t[:n, :], x1_d[r0:r0 + n, :])
            k.op("dve", lambda: V.tensor_tensor(out=h2[:], in0=x1t[:], in1=modt[1][:], op=ALU.mult), reads=[x1t, modt[1]], writes=[h2])
            k.op("dve", lambda: V.tensor_tensor(out=h2[:], in0=h2[:], in1=modt[0][:], op=ALU.add), reads=[h2, modt[0]], writes=[h2])
            for half in range(2):
                pb = pbank[half]
                for j in range(4):
                    kc = half * 4 + j
                    k.op("pe", lambda pb=pb, j=j, kc=kc: nc.tensor.transpose(out=pb[:, j, :], in_=h2[:, kc * 128:(kc + 1) * 128], identity=ident[:]),
                         reads=[h2, ident], writes=[pb], same_ok=True)
                k.op("act", lambda pb=pb, half=half: nc.scalar.copy(out=T[:, half * 4:(half + 1) * 4, :], in_=pb[:]), reads=[pb], writes=[T])
            for g4 in range(4):
                pb = pbank[g4]
                for j in range(4):
                    hp = g4 * 4 + j
                    for kc in range(8):
                        k.op("pe", lambda pb=pb, j=j, hp=hp, kc=kc: nc.tensor.matmul(pb[:, j, :], wq[:, kc, hp * 128:(hp + 1) * 128], T[:, kc, :],
                                                                                  start=(kc == 0), stop=(kc == 7)),
                             reads=[wq, T], writes=[pb], same_ok=True)
                k.op("act", lambda pb=pb, g4=g4: nc.scalar.copy(out=qT[:, g4 * 4:(g4 + 1) * 4, :], in_=pb[:]), reads=[pb], writes=[qT])
            for g4 in range(4):
                pb = pbank[g4]
                for j in range(4):
                    hp = g4 * 4 + j
                    k.op("pe", lambda pb=pb, j=j, hp=hp: nc.tensor.matmul(pb[:, j, :], qT[:, hp, :], keysT[:, hp, :], start=True, stop=True),
                         reads=[qT, keysT], writes=[pb], same_ok=True)
                k.op("act", lambda pb=pb, g4=g4: nc.scalar.copy(out=R1[:, g4 * 4:(g4 + 1) * 4, :], in_=pb[:]), reads=[pb], writes=[R1])
            for hp in range(16):
                k.op("dve", lambda hp=hp: V.max(out=sv[:, hp, 0:8], in_=R1[:, hp, :]), reads=[R1], writes=[sv])
                k.op("dve", lambda hp=hp: V.max_index(out=si[:, hp, 0:8], in_max=sv[:, hp, 0:8], in_values=R1[:, hp, :]), reads=[R1, sv], writes=[si])
                k.op("dve", lambda hp=hp: V.match_replace(out=R2[:, hp, :], in_to_replace=sv[:, hp, 0:8], in_values=R1[:, hp, :], imm_value=-1e30),
                     reads=[R1, sv], writes=[R2])
                k.op("dve", lambda hp=hp: V.max(out=sv[:, hp, 8:16], in_=R2[:, hp, :]), reads=[R2], writes=[sv])
                k.op("dve", lambda hp=hp: V.max_index(out=si[:, hp, 8:16], in_max=sv[:, hp, 8:16], in_values=R2[:, hp, :]), reads=[R2, sv], writes=[si])
            k.op("dve", lambda: V.tensor_copy(out=sif[:], in_=si[:]), reads=[si], writes=[sif])
            sv4 = sv[:].rearrange("p (h two) k -> p h two k", two=2)
            sif4 = sif[:].rearrange("p (h two) k -> p h two k", two=2)
            cand = R1[:].rearrange("p (h a) (b j) -> p h (a b) j", a=2, b=8)
            cand_flat = R1[:].rearrange("p (h a) m -> p h (a m)", a=2)
            cand2_flat = R2[:].rearrange("p (h a) m -> p h (a m)", a=2)
            cidx = R3[:].rearrange("p h (i j) -> p h i j", i=16)
            k.op("dve", lambda: V.tensor_tensor(out=cand, in0=sv4[:, :, 0, :, None].broadcast_to([128, 8, 16, 16]),
                                                in1=sv4[:, :, 1, None, :].broadcast_to([128, 8, 16, 16]), op=ALU.add),
                 reads=[sv], writes=[R1])
            k.op("dve", lambda: V.tensor_scalar_mul(out=sif4[:, :, 0, :], in0=sif4[:, :, 0, :], scalar1=128.0), reads=[sif], writes=[sif])
            k.op("dve", lambda: V.tensor_tensor(out=cidx, in0=sif4[:, :, 0, :, None].broadcast_to([128, 8, 16, 16]),
                                                in1=sif4[:, :, 1, None, :].broadcast_to([128, 8, 16, 16]), op=ALU.add),
                 reads=[sif], writes=[R3])
            for h in range(8):
                k.op("dve", lambda h=h: V.max(out=fv[:, h, 0:8], in_=cand_flat[:, h, :]), reads=[R1], writes=[fv])
                k.op("dve", lambda h=h: V.max_index(out=fi[:, h, 0:8], in_max=fv[:, h, 0:8], in_values=cand_flat[:, h, :]), reads=[R1, fv], writes=[fi])
                k.op("dve", lambda h=h: V.match_replace(out=cand2_flat[:, h, :], in_to_replace=fv[:, h, 0:8], in_values=cand_flat[:, h, :], imm_value=-1e30),
                     reads=[R1, fv], writes=[R2])
                k.op("dve", lambda h=h: V.max(out=fv[:, h, 8:16], in_=cand2_flat[:, h, :]), reads=[R2], writes=[fv])
                k.op("dve", lambda h=h: V.max_index(out=fi[:, h, 8:16], in_max=fv[:, h, 8:16], in_values=cand2_flat[:, h, :]), reads=[R2, fv], writes=[fi])
            k.op("dve", lambda: V.tensor_copy(out=fif[:], in_=fi[:]), reads=[fi], writes=[fif])
            for h in range(8):
                for j in range(16):
                    e = h * 16 + j
                    k.op("dve", lambda h=h, j=j, e=e: V.scalar_tensor_tensor(out=junk2[:, 0:256], in0=iota[:], scalar=fif[:, h, j:j + 1], in1=R3[:, h, :],
                                                                            op0=ALU.is_equal, op1=ALU.mult, accum_out=eidf[:, e:e + 1]),
                         reads=[iota, fif, R3], writes=[junk2, eidf])
            k.op("dve", lambda: V.tensor_copy(out=eid[:], in_=eidf[:]), reads=[eidf], writes=[eid])
            k.op("dve", lambda: V.tensor_scalar_mul(out=negm[:], in0=fv[:, :, 0], scalar1=-1.0), reads=[fv], writes=[negm])
            for h in range(8):
                k.op("act", lambda h=h: nc.scalar.activation(out=gate[:, h, :], in_=fv[:, h, :], func=AF.Exp, bias=negm[:, h:h + 1], scale=1.0,
                                                             accum_out=gs[:, h:h + 1]), reads=[fv, negm], writes=[gate, gs])
            k.op("dve", lambda: V.reciprocal(out=gs[:], in_=gs[:]), reads=[gs], writes=[gs])
            k.op("dve", lambda: V.tensor_tensor(out=gate[:], in0=gate[:], in1=gs[:, :, None].broadcast_to([128, 8, 16]), op=ALU.mult),
                 reads=[gate, gs], writes=[gate])

    def emit_gather(ti, r0, n, ty, x1t, h2, eid, gate, modt, pending):
            for e in range(128):
                b = gb[e % NB]
                k.dma("pool", b[:], u_d[:, :], fn=lambda b=b, e=e: nc.gpsimd.indirect_dma_start(
                    out=b[:], out_offset=None, in_=u_d[:, :], in_offset=bass.IndirectOffsetOnAxis(ap=eid[:, e:e + 1], axis=0)),
                    extra_reads=[eid])
                k.op("dve", lambda b=b, e=e: V.scalar_tensor_tensor(out=junk[:], in0=b[:], scalar=1.0, in1=h2[:],
                                                                    op0=ALU.mult, op1=ALU.mult, accum_out=actv[:, e:e + 1]),
                     reads=[b, h2], writes=[junk, actv])
                k.replay(pending, 2)
            k.op("act", lambda: nc.scalar.activation(out=wgt[:], in_=actv[:], func=AF.Gelu), reads=[actv], writes=[wgt])
            k.op("dve", lambda: V.tensor_tensor(out=wgt[:], in0=wgt[:], in1=gate[:].rearrange("p h j -> p (h j)"), op=ALU.mult),
                 reads=[wgt, gate], writes=[wgt])
            for e in range(128):
                b = gb[e % NB]
                k.dma("pool", b[:], v_d[:, :], fn=lambda b=b, e=e: nc.gpsimd.indirect_dma_start(
                    out=b[:], out_offset=None, in_=v_d[:, :], in_offset=bass.IndirectOffsetOnAxis(ap=eid[:, e:e + 1], axis=0)),
                    extra_reads=[eid])
                if e == 0:
                    k.op("dve", lambda b=b, e=e: V.tensor_scalar_mul(out=acc[:], in0=b[:], scalar1=wgt[:, 0:1]), reads=[b, wgt], writes=[acc])
                else:
                    k.op("dve", lambda b=b, e=e: V.scalar_tensor_tensor(out=acc[:], in0=b[:], scalar=wgt[:, e:e + 1], in1=acc[:],
                                                                        op0=ALU.mult, op1=ALU.add), reads=[b, wgt, acc], writes=[acc])
                k.replay(pending, 3)
            k.op("dve", lambda: V.tensor_tensor(out=acc[:], in0=acc[:], in1=modt[2][:], op=ALU.mult), reads=[acc, modt[2]], writes=[acc])
            k.op("dve", lambda: V.scalar_tensor_tensor(out=acc[:], in0=x1t[:], scalar=ALPHA, in1=acc[:], op0=ALU.mult, op1=ALU.add),
                 reads=[x1t, acc], writes=[acc])
            _layernorm_tile(k, acc, junk, stats, mv, rstd, epst)
            k.op("dve", lambda: V.tensor_tensor(out=acc[:], in0=acc[:], in1=lng[:], op=ALU.mult), reads=[acc, lng], writes=[acc])
            k.op("dve", lambda: V.tensor_tensor(out=junk[:], in0=acc[:], in1=lnb[:], op=ALU.add), reads=[acc, lnb], writes=[junk])
            k.dma("sp", out_d[r0:r0 + n, :], junk[:n, :])

    bufs = lambda ti, ty: (x1ts[ti % 2], h2s[ti % 2], eids[ti % 2], gates[ti % 2], modts[ty])
    for ti, (r0, n, ty) in enumerate(tiles):
        if ti == 0:
            emit_select(ti, r0, n, ty, *bufs(ti, ty))
        pending = []
        if ti + 1 < len(tiles):
            r1, n1, ty1 = tiles[ti + 1]
            k.rec = pending
            emit_select(ti + 1, r1, n1, ty1, *bufs(ti + 1, ty1))
            k.rec = None
        emit_gather(ti, r0, n, ty, *bufs(ti, ty), pending)
        k.replay(pending, len(pending))
    return k.finish()
def build_P0():
    k = KB(); nc = k.nc
    cT_d = k.dram_in("cT", [128, 8, 2])
    w_d = k.dram_in("w", [128, 8, 3072])
    b_d = k.dram_in("b", [2, 3072])
    out_d = k.dram_out("mod", [2, 3072])
    cT = k.sb("cT_sb", [128, 8, 2]); sT = k.sb("sT", [128, 8, 2])
    w = k.sb("w_sb", [128, 8, 3072]); b = k.sb("b_sb", [2, 3072]); o = k.sb("o_sb", [2, 3072])
    ps = [k.ps(f"ps{j}", [2, 512]) for j in range(2)]
    k.dma("sp", cT[:], cT_d[:, :, :]); k.dma("sp", b[:], b_d[:, :])
    for kc in range(8):
        k.dma("sp", w[:, kc, :], w_d[:, kc, :])
    k.op("act", lambda: nc.scalar.activation(out=sT[:], in_=cT[:], func=AF.Silu), reads=[cT], writes=[sT])
    for j in range(6):
        p = ps[j % 2]
        for kc in range(8):
            k.op("pe", lambda p=p, j=j, kc=kc: nc.tensor.matmul(p[:, :], sT[:, kc, :], w[:, kc, j * 512:(j + 1) * 512], start=(kc == 0), stop=(kc == 7)),
                 reads=[sT, w], writes=[p], same_ok=True)
        k.op("dve", lambda p=p, j=j: nc.vector.tensor_tensor(out=o[:, j * 512:(j + 1) * 512], in0=p[:, :], in1=b[:, j * 512:(j + 1) * 512], op=ALU.add),
             reads=[p, b], writes=[o])
    k.dma("sp", out_d[:, :], o[:])
    return k.finish()


def build_A_ml(NXc, NC):
    k = KB(); nc = k.nc; V = nc.vector
    NXE = NXc + 128
    R = NXc // 64
    NT = NXE + NC
    xT_d = k.dram_in("xT", [128, 8, NT])
    mod_d = k.dram_in("mod", [128, 2, 8, 2])
    w_d = k.dram_in("w_in", [128, 8, 4112])
    b_d = k.dram_in("b_in", [128, 33])
    cw_d = k.dram_in("conv_w", [128, 16, 9])
    cb_d = k.dram_in("conv_b", [128, 16])
    hm_d = k.dram_in("hmask", [128, 2])
    NO = NXc + NC
    q_d = k.dram_out("qk", [16, 128, NO])
    v_d = k.dram_out("v", [8, 128, NO])
    o_d = k.dram_out("o", [8, 128, NO])
    g_d = k.dram_out("g", [16, NO])

    hT = k.sb("hT", [128, 8, NT])
    mod = k.sb("mod_sb", [128, 2, 8, 2]); bsb = k.sb("b_sb", [128, 33]); cw = k.sb("cw", [128, 16, 9]); cb = k.sb("cb", [128, 16])
    hm = k.sb("hm", [128, 2])
    wbuf = [k.sb(f"wb{j}", [128, 8, 128]) for j in range(3)]
    pbuf = [k.sb(f"pbuf{j}", [128, NT]) for j in range(2)]
    cbuf = [k.sb(f"cbuf{j}", [128, NO]) for j in range(2)]
    obuf = [k.sb(f"obuf{j}", [128, NO]) for j in range(2)]
    psb = [k.ps(f"ps{j}", [128, 512]) for j in range(4)]
    for kc in range(8):
        k.dma("sp", hT[:, kc, :], xT_d[:, kc, :])
    k.dma("sp", mod[:], mod_d[:, :, :, :]); k.dma("sp", bsb[:], b_d[:, :]); k.dma("sp", cw[:], cw_d[:, :, :])
    k.dma("sp", cb[:], cb_d[:, :]); k.dma("sp", hm[:], hm_d[:, :])
    k.op("dve", lambda: V.tensor_scalar_add(out=mod[:, :, :, 1], in0=mod[:, :, :, 1], scalar1=1.0), reads=[mod], writes=[mod])
    for kc in range(8):
        k.op("dve", lambda kc=kc: V.tensor_scalar(out=hT[:, kc, 0:NXE], in0=hT[:, kc, 0:NXE], scalar1=mod[:, 0, kc, 1:2], scalar2=mod[:, 0, kc, 0:1],
                                                  op0=ALU.mult, op1=ALU.add), reads=[hT, mod], writes=[hT])
        k.op("dve", lambda kc=kc: V.tensor_scalar(out=hT[:, kc, NXE:NT], in0=hT[:, kc, NXE:NT], scalar1=mod[:, 1, kc, 1:2], scalar2=mod[:, 1, kc, 0:1],
                                                  op0=ALU.mult, op1=ALU.add), reads=[hT, mod], writes=[hT])
    blocks = []
    t0 = 0
    while t0 < NT:
        blocks.append((t0, min(512, NT - t0))); t0 += 512
    pi = 0
    for oc in range(33):
        M = 128 if oc < 32 else 16
        wb = wbuf[oc % 3]
        k.dma("sp", wb[:, :, 0:M], w_d[:, :, oc * 128:oc * 128 + M])
        pb = pbuf[oc % 2]
        for (b0, bn) in blocks:
            p = psb[pi % 4]; pi += 1
            for kc in range(8):
                k.op("pe", lambda p=p, wb=wb, kc=kc, b0=b0, bn=bn, M=M: nc.tensor.matmul(p[0:M, 0:bn], wb[:, kc, 0:M], hT[:, kc, b0:b0 + bn],
                                                                                      start=(kc == 0), stop=(kc == 7)),
                     reads=[wb, hT], writes=[p], same_ok=True)
            fn = AF.Sigmoid if 24 <= oc < 32 else AF.Identity
            k.op("act", lambda p=p, pb=pb, b0=b0, bn=bn, M=M, oc=oc, fn=fn: nc.scalar.activation(out=pb[0:M, b0:b0 + bn], in_=p[0:M, 0:bn], func=fn,
                                                                                           bias=bsb[0:M, oc:oc + 1], scale=1.0),
                 reads=[p, bsb], writes=[pb])
        if oc < 16:
            k.op("dve", lambda pb=pb: V.tensor_scalar_mul(out=pb[:, 0:64], in0=pb[:, 0:64], scalar1=hm[:, 0:1]), reads=[pb, hm], writes=[pb])
            k.op("dve", lambda pb=pb: V.tensor_scalar_mul(out=pb[:, NXE - 64:NXE], in0=pb[:, NXE - 64:NXE], scalar1=hm[:, 1:2]), reads=[pb, hm], writes=[pb])
            cbf = cbuf[oc % 2]
            pg = pb[:, 0:NXE].rearrange("p (r c) -> p r c", c=64)
            cg = cbf[:, 0:NXc].rearrange("p (r c) -> p r c", c=64)
            k.op("dve", lambda pg=pg, cg=cg, oc=oc: V.tensor_scalar(out=cg, in0=pg[:, 1:R + 1, :], scalar1=cw[:, oc, 4:5], scalar2=cb[:, oc:oc + 1],
                                                                 op0=ALU.mult, op1=ALU.add), reads=[pb, cw, cb], writes=[cbf])
            for dr in range(3):
                for dc in range(3):
                    if dr == 1 and dc == 1:
                        continue
                    c0, c1 = (1, 64) if dc == 0 else ((0, 63) if dc == 2 else (0, 64))
                    k.op("dve", lambda pg=pg, cg=cg, oc=oc, dr=dr, dc=dc, c0=c0, c1=c1: V.scalar_tensor_tensor(
                        out=cg[:, :, c0:c1], in0=pg[:, dr:dr + R, c0 + dc - 1:c1 + dc - 1], scalar=cw[:, oc, dr * 3 + dc:dr * 3 + dc + 1],
                        in1=cg[:, :, c0:c1], op0=ALU.mult, op1=ALU.add), reads=[pb, cw, cbf], writes=[cbf])
            k.op("dve", lambda pb=pb, cbf=cbf, oc=oc: V.tensor_scalar(out=cbf[:, NXc:NO], in0=pb[:, NXE:NT], scalar1=cw[:, oc, 4:5], scalar2=cb[:, oc:oc + 1],
                                                                    op0=ALU.mult, op1=ALU.add), reads=[pb, cw, cb], writes=[cbf])
            k.op("dve", lambda pb=pb, cbf=cbf, oc=oc: V.scalar_tensor_tensor(out=cbf[:, NXc + 1:NO], in0=pb[:, NXE:NT - 1], scalar=cw[:, oc, 3:4],
                                                                           in1=cbf[:, NXc + 1:NO], op0=ALU.mult, op1=ALU.add), reads=[pb, cw, cbf], writes=[cbf])
            k.op("dve", lambda pb=pb, cbf=cbf, oc=oc: V.scalar_tensor_tensor(out=cbf[:, NXc:NO - 1], in0=pb[:, NXE + 1:NT], scalar=cw[:, oc, 5:6],
                                                                           in1=cbf[:, NXc:NO - 1], op0=ALU.mult, op1=ALU.add), reads=[pb, cw, cbf], writes=[cbf])
            ob = obuf[oc % 2]
            k.op("act", lambda ob=ob, cbf=cbf: nc.scalar.activation(out=ob[:], in_=cbf[:], func=AF.Silu), reads=[cbf], writes=[ob])
            if oc < 8:
                k.op("dve", lambda ob=ob: V.tensor_scalar_mul(out=ob[:], in0=ob[:], scalar1=1.0 / 16.0), reads=[ob], writes=[ob])
            k.dma("sp", q_d[oc], ob[:])
        elif oc < 32:
            dst = v_d if oc < 24 else o_d
            j = oc - 16 if oc < 24 else oc - 24
            k.dma("sp", dst[j][:, 0:NXc], pb[:, 64:64 + NXc])
            k.dma("sp", dst[j][:, NXc:NO], pb[:, NXE:NT])
        else:
            k.dma("sp", g_d[:, 0:NXc], pb[0:16, 64:64 + NXc])
            k.dma("sp", g_d[:, NXc:NO], pb[0:16, NXE:NT])
    return k.finish()


def build_B_ml(NCH):
    k = KB(); nc = k.nc; V = nc.vector
    qT_d = k.dram_in("qT", [128, 2, NCH * 128])
    kT_d = k.dram_in("kT", [128, 2, NCH * 128])
    k_d = k.dram_in("k", [128, NCH, 256])
    v_d = k.dram_in("v", [128, NCH, 257])
    ig_d = k.dram_in("ig", [128, NCH]); fg_d = k.dram_in("fg", [128, NCH])
    tri_d = k.dram_in("tri", [128, 128]); mk_d = k.dram_in("maskT", [128, 128]); ones_d = k.dram_in("ones", [128, 128])
    h_d = k.dram_out("h", [128, NCH, 256])
    ident = _consts(k)
    tri = k.sb("tri_sb", [128, 128]); mk = k.sb("mk_sb", [128, 128]); ones = k.sb("ones_sb", [128, 128])
    ig = k.sb("ig_sb", [128, NCH]); LF = k.sb("LF", [128, NCH])
    k.dma("sp", tri[:], tri_d[:, :]); k.dma("sp", mk[:], mk_d[:, :]); k.dma("sp", ones[:], ones_d[:, :])
    k.dma("sp", ig[:], ig_d[:, :]); k.dma("sp", LF[:], fg_d[:, :])
    k.op("act", lambda: nc.scalar.activation(out=LF[:], in_=LF[:], func=AF.Exp, scale=-1.0), reads=[LF], writes=[LF])
    k.op("act", lambda: nc.scalar.activation(out=LF[:], in_=LF[:], func=AF.Ln, bias=1.0, scale=1.0), reads=[LF], writes=[LF])
    k.op("dve", lambda: V.tensor_scalar_mul(out=LF[:], in0=LF[:], scalar1=-1.0), reads=[LF], writes=[LF])
    NS = 3
    qb = [k.sb(f"qb{j}", [128, 2, 128]) for j in range(NS)]
    kb = [k.sb(f"kb{j}", [128, 2, 128]) for j in range(NS)]
    kt = [k.sb(f"kt{j}", [128, 256]) for j in range(NS)]
    vb = [k.sb(f"vb{j}", [128, 257]) for j in range(NS)]
    hb = [k.sb(f"hb{j}", [128, 256]) for j in range(2)]
    Cst = [k.sb(f"Cst{j}", [128, 257]) for j in range(2)]
    LFb = k.sb("LFb", [128, 128]); DT = k.sb("DT", [128, 128]); EB = k.sb("EB", [128, 128]); ST = k.sb("ST", [128, 128])
    qs = k.sb("qs", [128, 2, 128]); ka = k.sb("ka", [128, 256])
    wcol = k.sb("wcol", [128, 1]); acol = k.sb("acol", [128, 1]); Gc = k.sb("Gc", [128, 1]); den = k.sb("den", [128, 1])
    psA = k.ps("psA", [128, 128]); psB = k.ps("psB", [128, 128]); psC = k.ps("psC", [128, 2]); psS = k.ps("psS", [128, 128])
    psN = k.ps("psN", [128, 257]); psU = [k.ps(f"psU{j}", [128, 257]) for j in range(2)]
    for j in range(2):
        k.op("pool", lambda j=j: nc.gpsimd.memset(Cst[j][:], 0.0), writes=[Cst[j]])

    def load(c):
        s = c % NS
        k.dma("sp", qb[s][:], qT_d[:, :, c * 128:(c + 1) * 128])
        k.dma("sp", kb[s][:], kT_d[:, :, c * 128:(c + 1) * 128])
        k.dma("sp", kt[s][:], k_d[:, c, :])
        k.dma("sp", vb[s][:], v_d[:, c, :])
    load(0)
    if NCH > 1:
        load(1)
    for c in range(NCH):
        s = c % NS
        if c + 2 < NCH:
            load(c + 2)
        k.op("dve", lambda c=c: V.tensor_scalar_mul(out=LFb[:], in0=ones[:], scalar1=LF[:, c:c + 1]), reads=[ones, LF], writes=[LFb])
        k.op("pe", lambda: nc.tensor.matmul(psA[:, :], LFb[:], tri[:], start=True, stop=False), reads=[LFb, tri], writes=[psA], same_ok=True)
        k.op("pe", lambda: nc.tensor.matmul(psA[:, :], ident[:], mk[:], start=False, stop=True), reads=[ident, mk], writes=[psA], same_ok=True)
        k.op("pe", lambda: nc.tensor.matmul(psB[:, :], LFb[:], tri[:], start=True, stop=True), reads=[LFb, tri], writes=[psB], same_ok=True)
        k.op("pe", lambda c=c: nc.tensor.matmul(psC[:, 0:1], tri[:], LF[:, c:c + 1], start=True, stop=True), reads=[tri, LF], writes=[psC], same_ok=True)
        k.op("pe", lambda c=c: nc.tensor.matmul(psC[:, 1:2], ones[:], LF[:, c:c + 1], start=True, stop=True), reads=[ones, LF], writes=[psC], same_ok=True)
        k.op("dve", lambda c=c: V.tensor_tensor(out=wcol[:], in0=ig[:, c:c + 1], in1=psC[:, 0:1], op=ALU.subtract), reads=[ig, psC], writes=[wcol])
        k.op("act", lambda: nc.scalar.activation(out=DT[:], in_=psA[:, :], func=AF.Exp, bias=wcol[:, 0:1], scale=1.0), reads=[psA, wcol], writes=[DT])
        k.op("act", lambda: nc.scalar.activation(out=EB[:], in_=psB[:, :], func=AF.Exp), reads=[psB], writes=[EB])
        k.op("act", lambda: nc.scalar.activation(out=acol[:], in_=psC[:, 1:2], func=AF.Exp, bias=wcol[:, 0:1], scale=1.0), reads=[psC, wcol], writes=[acol])
        k.op("act", lambda: nc.scalar.activation(out=Gc[:], in_=psC[:, 1:2], func=AF.Exp), reads=[psC], writes=[Gc])
        for dc in range(2):
            k.op("pe", lambda s=s, dc=dc: nc.tensor.matmul(psS[:, :], kb[s][:, dc, :], qb[s][:, dc, :], start=(dc == 0), stop=(dc == 1)),
                 reads=[kb[s], qb[s]], writes=[psS], same_ok=True)
        k.op("dve", lambda: V.tensor_tensor(out=ST[:], in0=psS[:, :], in1=DT[:], op=ALU.mult), reads=[psS, DT], writes=[ST])
        k.op("dve", lambda s=s: V.tensor_tensor(out=qs[:], in0=qb[s][:], in1=EB[:, None, :].broadcast_to([128, 2, 128]), op=ALU.mult),
             reads=[qb[s], EB], writes=[qs])
        k.op("pe", lambda s=s: nc.tensor.matmul(psN[:, :], ST[:], vb[s][:], start=True, stop=False), reads=[ST, vb[s]], writes=[psN], same_ok=True)
        for dc in range(2):
            k.op("pe", lambda dc=dc: nc.tensor.matmul(psN[:, :], qs[:, dc, :], Cst[dc][:], start=False, stop=(dc == 1)),
                 reads=[qs, Cst[dc]], writes=[psN], same_ok=True)
        k.op("act", lambda: nc.scalar.activation(out=den[:], in_=psN[:, 256:257], func=AF.Abs), reads=[psN], writes=[den])
        k.op("dve", lambda: V.tensor_scalar_max(out=den[:], in0=den[:], scalar1=1.0), reads=[den], writes=[den])
        k.op("dve", lambda: V.reciprocal(out=den[:], in_=den[:]), reads=[den], writes=[den])
        hbb = hb[c % 2]
        k.op("dve", lambda hbb=hbb: V.tensor_scalar_mul(out=hbb[:], in0=psN[:, 0:256], scalar1=den[:, 0:1]), reads=[psN, den], writes=[hbb])
        k.dma("sp", h_d[:, c, :], hbb[:])
        k.op("pool", lambda s=s: nc.gpsimd.tensor_scalar(out=ka[:], in0=kt[s][:], scalar1=acol[:, 0:1], scalar2=None, op0=ALU.mult),
             reads=[kt[s], acol], writes=[ka])
        for dc in range(2):
            k.op("pe", lambda s=s, dc=dc: nc.tensor.matmul(psU[dc][:, :], ka[:, dc * 128:(dc + 1) * 128], vb[s][:], start=True, stop=True),
                 reads=[ka, vb[s]], writes=[psU[dc]], same_ok=True)
            k.op("dve", lambda dc=dc: V.scalar_tensor_tensor(out=Cst[dc][:], in0=Cst[dc][:], scalar=Gc[:, 0:1], in1=psU[dc][:, :], op0=ALU.mult, op1=ALU.add),
                 reads=[Cst[dc], Gc, psU[dc]], writes=[Cst[dc]])
    return k.finish()


def build_C1(kind, NX, NCTX):
    k = KB(); nc = k.nc; V = nc.vector
    TOK = NX + NCTX
    ml = kind == "ml"
    H, dh, eps = (4, 256, 1e-5) if ml else (16, 64, 64e-5)
    NPIECE = 3 if ml else 6
    NPRM = 1 if ml else 3
    xres_d = k.dram_in("xres", [TOK, D])
    pc_d = [k.dram_in(f"piece{j}", [TOK, D]) for j in range(NPIECE)]
    prm_d = k.dram_in("prm", [NPRM, 128, D])
    mod_d = k.dram_in("mod", [2, 128, D])
    lnp_d = k.dram_in("lnp", [2, 128, D])
    w_d = k.dram_in("w_out", [128, 8, D])
    out_d = k.dram_out("x1", [TOK, D])
    ident = _consts(k)
    w = k.sb("w_sb", [128, 8, D]); prm = [k.sb(f"prm{j}", [128, D]) for j in range(NPRM)]
    g1 = k.sb("g1", [128, D]); lng = k.sb("lng", [128, D]); lnb = k.sb("lnb", [128, D])
    xr = k.sb("xr", [128, D]); A = k.sb("A", [128, D]); B = k.sb("B", [128, D]); C = k.sb("C", [128, D])
    zT = k.sb("zT", [128, 8, 128]); t1 = k.sb("t1", [128, D]); junk = k.sb("junk", [128, D])
    sm = k.sb("sm", [128, H]); vs = k.sb("vs", [128, H]); bs = k.sb("bs", [128, H])
    stats = k.sb("stats", [128, 12]); mv = k.sb("mv", [128, 2]); rstd = k.sb("rstd", [128, 1])
    epst = k.sb("epst", [128, 1]); epsh = k.sb("epsh", [128, 1])
    pbank = [k.ps(f"pb{j}", [128, 512]) for j in range(4)]
    k.op("pool", lambda: nc.gpsimd.memset(epst[:], LN_EPS), writes=[epst])
    k.op("pool", lambda: nc.gpsimd.memset(epsh[:], eps), writes=[epsh])
    for kc in range(8):
        k.dma("sp", w[:, kc, :], w_d[:, kc, :])
    for j in range(NPRM):
        k.dma("sp", prm[j][:], prm_d[j])
    k.dma("sp", lng[:], lnp_d[0]); k.dma("sp", lnb[:], lnp_d[1])
    tiles = [(i * 128, 128, 0) for i in range(NX // 128)]
    if NCTX:
        tiles.append((NX, NCTX, 1))
    cur_ty = None
    hv = lambda t: t[:].rearrange("p (h e) -> p h e", h=H)
    bc = lambda s: s[:, :, None].broadcast_to([128, H, dh])
    for (r0, n, ty) in tiles:
        if ty != cur_ty:
            k.dma("sp", g1[:], mod_d[ty]); cur_ty = ty
        rs = slice(r0, r0 + n)
        k.dma("sp", xr[:n, :], xres_d[rs, :])
        k.dma("sp", A[:n, :], pc_d[0][rs, :]); k.dma("sp", B[:n, :], pc_d[1][rs, :])
        k.op("dve", lambda: V.tensor_tensor(out=A[:], in0=A[:], in1=B[:], op=ALU.add), reads=[A, B], writes=[A])
        k.op("dve", lambda: V.tensor_reduce(out=sm[:], in_=hv(A), axis=mybir.AxisListType.X, op=ALU.add), reads=[A], writes=[sm])
        k.op("dve", lambda: V.tensor_scalar_mul(out=sm[:], in0=sm[:], scalar1=1.0 / dh), reads=[sm], writes=[sm])
        k.op("dve", lambda: V.tensor_tensor(out=hv(A), in0=hv(A), in1=bc(sm), op=ALU.subtract), reads=[A, sm], writes=[A])
        k.op("dve", lambda: V.tensor_tensor(out=junk[:], in0=A[:], in1=A[:], op=ALU.mult), reads=[A], writes=[junk])
        k.op("dve", lambda: V.tensor_reduce(out=vs[:], in_=hv(junk), axis=mybir.AxisListType.X, op=ALU.add), reads=[junk], writes=[vs])
        k.op("act", lambda: nc.scalar.activation(out=vs[:], in_=vs[:], func=AF.Sqrt, bias=epsh[:, 0:1], scale=1.0 / dh), reads=[vs, epsh], writes=[vs])
        k.op("dve", lambda: V.reciprocal(out=vs[:], in_=vs[:]), reads=[vs], writes=[vs])
        k.op("dve", lambda: V.tensor_tensor(out=hv(A), in0=hv(A), in1=bc(vs), op=ALU.mult), reads=[A, vs], writes=[A])
        if ml:
            k.dma("sp", C[:n, :], pc_d[2][rs, :])
            k.op("dve", lambda: V.tensor_tensor(out=A[:], in0=A[:], in1=C[:], op=ALU.mult), reads=[A, C], writes=[A])
            k.op("dve", lambda: V.tensor_tensor(out=A[:], in0=A[:], in1=prm[0][:], op=ALU.mult), reads=[A, prm[0]], writes=[A])
        else:
            k.op("dve", lambda: V.tensor_tensor(out=A[:], in0=A[:], in1=prm[0][:], op=ALU.mult), reads=[A, prm[0]], writes=[A])
            k.op("dve", lambda: V.tensor_tensor(out=A[:], in0=A[:], in1=prm[1][:], op=ALU.add), reads=[A, prm[1]], writes=[A])
            k.dma("sp", B[:n, :], pc_d[2][rs, :]); k.dma("sp", C[:n, :], pc_d[3][rs, :])
            k.op("dve", lambda: V.tensor_tensor(out=B[:], in0=B[:], in1=C[:], op=ALU.mult), reads=[B, C], writes=[B])
            k.op("dve", lambda: V.tensor_tensor(out=B[:], in0=B[:], in1=prm[2][:], op=ALU.mult), reads=[B, prm[2]], writes=[B])
            k.op("dve", lambda: V.tensor_reduce(out=bs[:], in_=hv(B), axis=mybir.AxisListType.X, op=ALU.add), reads=[B], writes=[bs])
            k.dma("sp", C[:n, :], pc_d[4][rs, :])
            k.op("dve", lambda: V.tensor_tensor(out=hv(C), in0=hv(C), in1=bc(bs), op=ALU.mult), reads=[C, bs], writes=[C])
            k.op("dve", lambda: V.tensor_tensor(out=A[:], in0=A[:], in1=C[:], op=ALU.add), reads=[A, C], writes=[A])
            k.dma("sp", B[:n, :], pc_d[5][rs, :])
            k.op("dve", lambda: V.tensor_tensor(out=A[:], in0=A[:], in1=B[:], op=ALU.mult), reads=[A, B], writes=[A])
        for half in range(2):
            pb = pbank[half]
            for j in range(4):
                kc = half * 4 + j
                k.op("pe", lambda pb=pb, j=j, kc=kc: nc.tensor.transpose(out=pb[:, j * 128:(j + 1) * 128], in_=A[:, kc * 128:(kc + 1) * 128], identity=ident[:]),
                     reads=[A, ident], writes=[pb], same_ok=True)
            k.op("act", lambda pb=pb, half=half: nc.scalar.copy(out=zT[:, half * 4:(half + 1) * 4, :].rearrange("p a b -> p (a b)"), in_=pb[:, :]),
                 reads=[pb], writes=[zT])
        for half in range(2):
            pb = pbank[2 + half]
            for kc in range(8):
                k.op("pe", lambda pb=pb, kc=kc, half=half: nc.tensor.matmul(pb[:, :], zT[:, kc, :], w[:, kc, half * 512:(half + 1) * 512],
                                                                          start=(kc == 0), stop=(kc == 7)), reads=[zT, w], writes=[pb], same_ok=True)
            k.op("dve", lambda pb=pb, half=half: V.tensor_tensor(out=t1[:, half * 512:(half + 1) * 512], in0=pb[:, :], in1=g1[:, half * 512:(half + 1) * 512],
                                                                 op=ALU.mult), reads=[pb, g1], writes=[t1])
        k.op("dve", lambda: V.scalar_tensor_tensor(out=t1[:], in0=xr[:], scalar=ALPHA, in1=t1[:], op0=ALU.mult, op1=ALU.add), reads=[xr, t1], writes=[t1])
        _layernorm_tile(k, t1, junk, stats, mv, rstd, epst)
        k.op("dve", lambda: V.tensor_tensor(out=t1[:], in0=t1[:], in1=lng[:], op=ALU.mult), reads=[t1, lng], writes=[t1])
        k.op("dve", lambda: V.tensor_tensor(out=junk[:], in0=t1[:], in1=lnb[:], op=ALU.add), reads=[t1, lnb], writes=[junk])
        k.dma("sp", out_d[rs, :], junk[:n, :])
    return k.finish()


NCORES = 8
_PROGS = {}


def _run(key, builder, in_maps):
    if key not in _PROGS:
        _PROGS[key] = builder()
    res = run_bass_kernel_spmd(_PROGS[key], in_maps, core_ids=list(range(len(in_maps))))
    return res.results


def _f(a):
    return np.ascontiguousarray(a, dtype=np.float32)


def _bc(v):
    return _f(np.broadcast_to(np.asarray(v, np.float32), (128, v.shape[-1])))


def _kc_layout(w):
    return _f(w.reshape(8, 128, -1).transpose(1, 0, 2))


def _fm(vec):
    return _f(vec.reshape(-1, 128).T)


_IDENT = np.eye(128, dtype=np.float32)


def run_P0(c, c_ctx, ada_w, ada_b):
    depth = ada_w.shape[0]
    cc = np.stack([c.reshape(-1), c_ctx.reshape(-1)], axis=1)
    cT = _f(cc.reshape(8, 128, 2).transpose(1, 0, 2))
    in_maps = []
    for core in range(NCORES):
        i, half = core // 2, core % 2
        i = min(i, depth - 1)
        sl = slice(half * 3072, (half + 1) * 3072)
        in_maps.append({"cT": cT, "w": _kc_layout(ada_w[i][:, sl]), "b": _f(np.stack([ada_b[i][sl], ada_b[i][sl]]))})
    res = _run("P0", build_P0, in_maps)
    mods = np.zeros((depth, 2, 6144), np.float32)
    for core in range(2 * depth):
        i, half = core // 2, core % 2
        mods[i][:, half * 3072:(half + 1) * 3072] = res[core]["mod"]
    return mods.reshape(depth, 2, 6, 1024)


def run_C1(kind, x, ctx, pieces_x, pieces_c, prm, g1x, g1c, ln_g, ln_b, w_out):
    NX, NC = x.shape[0], ctx.shape[0]
    nxc, ncc = NX // NCORES, NC // NCORES
    common = {"prm": _f(np.stack([_bc(p) for p in prm])), "mod": _f(np.stack([_bc(g1x), _bc(g1c)])),
              "lnp": _f(np.stack([_bc(ln_g), _bc(ln_b)])), "w_out": _kc_layout(w_out), "ident": _IDENT}
    in_maps = []
    for c in range(NCORES):
        m = dict(common)
        m["xres"] = _f(np.concatenate([x[c * nxc:(c + 1) * nxc], ctx[c * ncc:(c + 1) * ncc]]))
        for j, (px, pc) in enumerate(zip(pieces_x, pieces_c)):
            m[f"piece{j}"] = _f(np.concatenate([px[c * nxc:(c + 1) * nxc], pc[c * ncc:(c + 1) * ncc]]))
        in_maps.append(m)
    res = _run(("C1", kind, nxc, ncc), lambda: build_C1(kind, nxc, ncc), in_maps)
    x1 = np.concatenate([r["x1"][:nxc] for r in res]); c1 = np.concatenate([r["x1"][nxc:] for r in res])
    return x1, c1


def run_C2(x1, c1, modx, modc, ln_g, ln_b, wq, keys, u_tab, v_tab):
    NX, NC = x1.shape[0], c1.shape[0]
    nxc, ncc = NX // NCORES, NC // NCORES
    common = {"mod": _f(np.stack([np.stack([_bc(m) for m in modx]), np.stack([_bc(m) for m in modc])])),
              "lnp": _f(np.stack([_bc(ln_g), _bc(ln_b)])), "wq": _kc_layout(wq),
              "keysT": _f(keys.reshape(16, 128, 128).transpose(2, 0, 1)),
              "iota": _f(np.broadcast_to(np.arange(256, dtype=np.float32), (128, 256))),
              "u_tab": _f(u_tab), "v_tab": _f(v_tab), "ident": _IDENT}
    in_maps = []
    for c in range(NCORES):
        m = dict(common)
        m["x1"] = _f(np.concatenate([x1[c * nxc:(c + 1) * nxc], c1[c * ncc:(c + 1) * ncc]]))
        in_maps.append(m)
    res = _run(("C2", nxc, ncc), lambda: build_C2(nxc, ncc), in_maps)
    x2 = np.concatenate([r["xout"][:nxc] for r in res]); c2 = np.concatenate([r["xout"][nxc:] for r in res])
    return x2, c2


def run_mlstm(x, ctx, modx, modc, w_in, b_in, conv_w, conv_b):
    NX, NC = x.shape[0], ctx.shape[0]
    nxc = NX // NCORES
    xpad = np.concatenate([np.zeros((64, D), np.float32), x, np.zeros((64, D), np.float32)])
    mod = np.zeros((128, 2, 8, 2), np.float32)
    for ty, m in enumerate((modx, modc)):
        mod[:, ty, :, 0] = _fm(m[0]); mod[:, ty, :, 1] = _fm(m[1])
    b33 = np.zeros((128, 33), np.float32)
    b33[:, :32] = _fm(b_in[:4096]); b33[:16, 32] = b_in[4096:]
    common = {"mod": mod, "w_in": _kc_layout(w_in), "b_in": b33,
              "conv_w": _f(conv_w.reshape(9, 16, 128).transpose(2, 1, 0)), "conv_b": _fm(conv_b)}
    in_maps = []
    for c in range(NCORES):
        win = np.concatenate([xpad[c * nxc:c * nxc + nxc + 128], ctx])
        m = dict(common)
        m["xT"] = _f(win.T.reshape(8, 128, -1).transpose(1, 0, 2))
        hm = np.ones((128, 2), np.float32)
        if c == 0:
            hm[:, 0] = 0
        if c == NCORES - 1:
            hm[:, 1] = 0
        m["hmask"] = hm
        in_maps.append(m)
    res = _run(("A_ml", nxc, NC), lambda: build_A_ml(nxc, NC), in_maps)

    def gather(name, nfeat):
        xs = np.concatenate([r[name].reshape(nfeat, -1)[:, :nxc] for r in res], axis=1)
        cs = res[0][name].reshape(nfeat, -1)[:, nxc:]
        return xs, cs
    qk_x, qk_c = gather("qk", 2048); v_x, v_c = gather("v", 1024); o_x, o_c = gather("o", 1024); g_x, g_c = gather("g", 16)
    T = NC + NX
    NCH = T // 128
    tri = _f(np.triu(np.ones((128, 128), np.float32)))
    maskT = _f(np.where(np.triu(np.ones((128, 128))) > 0, 0.0, -30000.0))
    consts = {"tri": tri, "maskT": maskT, "ones": np.ones((128, 128), np.float32), "ident": _IDENT}
    in_maps = []
    for core in range(NCORES):
        h, d = core % 4, core // 4

        def seqT(ax, ac):
            if d == 0:
                return np.concatenate([ac, ax], axis=1)
            return np.concatenate([ac[:, ::-1], ax[:, ::-1]], axis=1)
        hs = slice(h * 256, (h + 1) * 256)
        qT = seqT(qk_x[hs], qk_c[hs]); kT = seqT(qk_x[1024:][hs], qk_c[1024:][hs]); vT = seqT(v_x[hs], v_c[hs])
        ig = seqT(g_x[d * 4 + h][None], g_c[d * 4 + h][None])[0]
        fg = seqT(g_x[8 + d * 4 + h][None], g_c[8 + d * 4 + h][None])[0]
        m = dict(consts)
        m["qT"] = _f(qT.reshape(2, 128, T).transpose(1, 0, 2)); m["kT"] = _f(kT.reshape(2, 128, T).transpose(1, 0, 2))
        m["k"] = _f(kT.T.reshape(NCH, 128, 256).transpose(1, 0, 2))
        vext = np.concatenate([vT.T, np.ones((T, 1), np.float32)], axis=1)
        m["v"] = _f(vext.reshape(NCH, 128, 257).transpose(1, 0, 2))
        m["ig"] = _f(ig.reshape(NCH, 128).T); m["fg"] = _f(fg.reshape(NCH, 128).T)
        in_maps.append(m)
    res = _run(("B_ml", NCH), lambda: build_B_ml(NCH), in_maps)
    hf = np.zeros((T, D), np.float32); hb = np.zeros((T, D), np.float32)
    for core in range(NCORES):
        h, d = core % 4, core // 4
        hh = res[core]["h"].transpose(1, 0, 2).reshape(T, 256)
        if d == 0:
            hf[:, h * 256:(h + 1) * 256] = hh
        else:
            hb[:NC, h * 256:(h + 1) * 256] = hh[:NC][::-1]
            hb[NC:, h * 256:(h + 1) * 256] = hh[NC:][::-1]
    return (hf[NC:], hb[NC:], _f(o_x.T)), (hf[:NC], hb[:NC], _f(o_c.T))


def build_A_rw(NXc, NC):
    k = KB(); nc = k.nc; V = nc.vector
    NXE = NXc + 128
    NT = NXE + NC
    NO = NXc + NC
    BS = min(512, NXc)
    BW = max(BS, NC)
    xT_d = k.dram_in("xT", [128, 8, NT])
    mod_d = k.dram_in("mod", [128, 2, 8, 2])
    hm_d = k.dram_in("hmask", [128, 2])
    mu_d = k.dram_in("mu", [128, 6, 8])
    wrkv_d = k.dram_in("w_rkv", [3, 128, 8, D])
    w1_d = k.dram_in("w1", [4, 128, 8, 64])
    w2_d = k.dram_in("w2", [4, 64, D])
    g1_d = k.dram_in("g1", [128, 8, 160])
    g2a_d = k.dram_in("g2a", [128, D]); g2b_d = k.dram_in("g2b", [32, D])
    vec_d = k.dram_in("vecs", [128, 7, 8])
    bd_d = k.dram_in("bdones", [128, 128])
    names = ["r", "v", "kkneg", "b0", "b1", "ktil0", "ktil1", "logw0", "logw1", "kbar", "g"]
    outs = {n: k.dram_out(n, [8, 128, NO]) for n in names}

    mod = k.sb("mod_sb", [128, 2, 8, 2]); hm = k.sb("hm", [128, 2]); mu = k.sb("mu_sb", [128, 6, 8])
    w1 = [k.sb(f"w1_{j}", [128, 8, 64]) for j in range(4)]
    w2 = [k.sb(f"w2_{j}", [64, D]) for j in range(4)]
    g1 = k.sb("g1_sb", [128, 8, 160]); g2a = k.sb("g2a_sb", [128, D]); g2b = k.sb("g2b_sb", [32, D])
    vec = k.sb("vec_sb", [128, 7, 8]); bd = k.sb("bd_sb", [128, 128]); omka = k.sb("omka", [128, 8])
    hTb = k.sb("hTb", [128, 8, BW + 128]); sTb = k.sb("sTb", [128, 8, BW]); xm = k.sb("xm", [128, 8, BW])
    kT = k.sb("kT", [128, 8, BW]); kk = k.sb("kk", [128, 8, BW])
    th = [k.sb(f"th{j}", [64, BW]) for j in range(2)]
    gs0 = k.sb("gs0", [128, BW]); gs1 = k.sb("gs1", [32, BW])
    wbuf = [k.sb(f"wb{j}", [128, 8, 128]) for j in range(3)]
    ob = [k.sb(f"ob{j}", [128, BW]) for j in range(6)]
    tmp = [k.sb(f"tmp{j}", [128, BW]) for j in range(3)]
    psb = [k.ps(f"ps{j}", [128, 512]) for j in range(6)]
    cnt = {"ps": 0, "ob": 0, "wb": 0}

    def nps():
        cnt["ps"] += 1; return psb[cnt["ps"] % 6]

    def nob():
        cnt["ob"] += 1; return ob[cnt["ob"] % 6]

    k.dma("sp", mod[:], mod_d[:, :, :, :]); k.dma("sp", hm[:], hm_d[:, :]); k.dma("sp", mu[:], mu_d[:, :, :])
    for j in range(4):
        k.dma("sp", w1[j][:], w1_d[j]); k.dma("sp", w2[j][:], w2_d[j])
    k.dma("sp", g1[:], g1_d[:, :, :]); k.dma("sp", g2a[:], g2a_d[:, :]); k.dma("sp", g2b[:], g2b_d[:, :])
    k.dma("sp", vec[:], vec_d[:, :, :]); k.dma("sp", bd[:], bd_d[:, :])
    k.op("dve", lambda: V.tensor_scalar_add(out=mod[:, :, :, 1], in0=mod[:, :, :, 1], scalar1=1.0), reads=[mod], writes=[mod])
    k.op("dve", lambda: V.tensor_scalar(out=omka[:], in0=vec[:, 5, :], scalar1=-1.0, scalar2=1.0, op0=ALU.mult, op1=ALU.add), reads=[vec], writes=[omka])

    blocks = [(b0, BS, 0) for b0 in range(0, NXc, BS)]
    if NC:
        assert NC <= 512
        blocks.append((0, NC, 1))
    for (b0, bs, ty) in blocks:
        off = 64 if ty == 0 else 0
        wn = bs + 128 if ty == 0 else bs
        src0 = b0 if ty == 0 else NXE
        o0 = b0 if ty == 0 else NXc
        for kc in range(8):
            k.dma("sp", hTb[:, kc, 0:wn], xT_d[:, kc, src0:src0 + wn])
        for kc in range(8):
            k.op("dve", lambda kc=kc: V.tensor_scalar(out=hTb[:, kc, 0:wn], in0=hTb[:, kc, 0:wn], scalar1=mod[:, ty, kc, 1:2], scalar2=mod[:, ty, kc, 0:1],
                                                      op0=ALU.mult, op1=ALU.add), reads=[hTb, mod], writes=[hTb])
        k.op("pool", lambda: nc.gpsimd.memset(sTb[:], 0.0), writes=[sTb])
        if ty == 0:
            if b0 == 0:
                k.op("dve", lambda: V.tensor_scalar_mul(out=hTb[:, :, 0:64], in0=hTb[:, :, 0:64], scalar1=hm[:, 0:1]), reads=[hTb, hm], writes=[hTb])
            if b0 + bs == NXc:
                k.op("dve", lambda: V.tensor_scalar_mul(out=hTb[:, :, bs + 64:bs + 128], in0=hTb[:, :, bs + 64:bs + 128], scalar1=hm[:, 1:2]),
                     reads=[hTb, hm], writes=[hTb])
            hg = lambda kc0, kc1, lo: hTb[:, kc0:kc1, lo:lo + bs].rearrange("p k (r c) -> p k r c", c=64)
            sg = sTb[:, :, 0:bs].rearrange("p k (r c) -> p k r c", c=64)
            for kc in range(2):
                k.op("dve", lambda kc=kc: V.tensor_copy(out=sg[:, kc, :, 1:64], in_=hg(kc, kc + 1, 64)[:, 0, :, 0:63]), reads=[hTb], writes=[sTb])
                k.op("dve", lambda kc=kc: V.tensor_copy(out=sg[:, 2 + kc, :, 0:63], in_=hg(2 + kc, 3 + kc, 64)[:, 0, :, 1:64]), reads=[hTb], writes=[sTb])
            k.op("dve", lambda: V.tensor_copy(out=sTb[:, 4:6, 0:bs], in_=hTb[:, 4:6, 0:bs]), reads=[hTb], writes=[sTb])
            k.op("dve", lambda: V.tensor_copy(out=sTb[:, 6:8, 0:bs], in_=hTb[:, 6:8, 128:128 + bs]), reads=[hTb], writes=[sTb])
        else:
            k.op("dve", lambda: V.tensor_copy(out=sTb[:, 0:4, 1:bs], in_=hTb[:, 0:4, 0:bs - 1]), reads=[hTb], writes=[sTb])
            k.op("dve", lambda: V.tensor_copy(out=sTb[:, 4:8, 0:bs - 1], in_=hTb[:, 4:8, 1:bs]), reads=[hTb], writes=[sTb])
        hc = lambda kc: hTb[:, kc, off:off + bs]
        k.op("dve", lambda: V.tensor_tensor(out=sTb[:, :, 0:bs], in0=sTb[:, :, 0:bs], in1=hTb[:, :, off:off + bs], op=ALU.subtract), reads=[sTb, hTb], writes=[sTb])

        def mix(n):
            for kc in range(8):
                k.op("dve", lambda kc=kc: V.scalar_tensor_tensor(out=xm[:, kc, 0:bs], in0=sTb[:, kc, 0:bs], scalar=mu[:, n, kc:kc + 1], in1=hc(kc),
                                                                 op0=ALU.mult, op1=ALU.add), reads=[sTb, mu, hTb], writes=[xm])

        def proj(n, oc):
            cnt["wb"] += 1
            wb = wbuf[cnt["wb"] % 3]
            k.dma("sp", wb[:], wrkv_d[n][:, :, oc * 128:(oc + 1) * 128])
            p = nps()
            for kc in range(8):
                k.op("pe", lambda p=p, wb=wb, kc=kc: nc.tensor.matmul(p[:, 0:bs], wb[:, kc, :], xm[:, kc, 0:bs], start=(kc == 0), stop=(kc == 7)),
                     reads=[wb, xm], writes=[p], same_ok=True)
            return p

        def store(name, oc, t):
            k.dma("sp", outs[name][oc][:, o0:o0 + bs], t[:, 0:bs])

        mix(0)
        for oc in range(8):
            p = proj(0, oc); o = nob()
            k.op("act", lambda p=p, o=o: nc.scalar.copy(out=o[:, 0:bs], in_=p[:, 0:bs]), reads=[p], writes=[o])
            store("r", oc, o)
        mix(1)
        for oc in range(8):
            p = proj(1, oc)
            k.op("act", lambda p=p, oc=oc: nc.scalar.copy(out=kT[:, oc, 0:bs], in_=p[:, 0:bs]), reads=[p], writes=[kT])
            t0, t1 = tmp[0], tmp[1]
            k.op("dve", lambda oc=oc: V.tensor_scalar_mul(out=t0[:, 0:bs], in0=kT[:, oc, 0:bs], scalar1=vec[:, 4, oc:oc + 1]), reads=[kT, vec], writes=[t0])
            k.op("dve", lambda: V.tensor_tensor(out=t1[:, 0:bs], in0=t0[:, 0:bs], in1=t0[:, 0:bs], op=ALU.mult), reads=[t0], writes=[t1])
            p2 = nps()
            k.op("pe", lambda p2=p2: nc.tensor.matmul(p2[:, 0:bs], bd[:], t1[:, 0:bs], start=True, stop=True), reads=[bd, t1], writes=[p2], same_ok=True)
            k.op("act", lambda p2=p2: nc.scalar.activation(out=t1[:, 0:bs], in_=p2[:, 0:bs], func=AF.Sqrt), reads=[p2], writes=[t1])
            k.op("dve", lambda: V.tensor_scalar_max(out=t1[:, 0:bs], in0=t1[:, 0:bs], scalar1=1e-12), reads=[t1], writes=[t1])
            k.op("dve", lambda: V.reciprocal(out=t1[:, 0:bs], in_=t1[:, 0:bs]), reads=[t1], writes=[t1])
            k.op("dve", lambda oc=oc: V.tensor_tensor(out=kk[:, oc, 0:bs], in0=t0[:, 0:bs], in1=t1[:, 0:bs], op=ALU.mult), reads=[t0, t1], writes=[kk])
            o = nob()
            k.op("dve", lambda o=o, oc=oc: V.tensor_scalar_mul(out=o[:, 0:bs], in0=kk[:, oc, 0:bs], scalar1=-1.0), reads=[kk], writes=[o])
            store("kkneg", oc, o)
        mix(2)
        for oc in range(8):
            p = proj(2, oc); o = nob()
            k.op("act", lambda p=p, o=o: nc.scalar.copy(out=o[:, 0:bs], in_=p[:, 0:bs]), reads=[p], writes=[o])
            store("v", oc, o)

        def lora_in(j, dst, func):
            p = nps()
            for kc in range(8):
                k.op("pe", lambda p=p, kc=kc, j=j: nc.tensor.matmul(p[0:64, 0:bs], w1[j][:, kc, :], xm[:, kc, 0:bs], start=(kc == 0), stop=(kc == 7)),
                     reads=[w1[j], xm], writes=[p], same_ok=True)
            k.op("act", lambda p=p, dst=dst: nc.scalar.activation(out=dst[:, 0:bs], in_=p[0:64, 0:bs], func=func), reads=[p], writes=[dst])

        mix(3)
        for z in range(2):
            lora_in(z, th[z], AF.Tanh)
        for oc in range(8):
            for z in range(2):
                p = nps()
                k.op("pe", lambda p=p, z=z, oc=oc: nc.tensor.matmul(p[:, 0:bs], w2[z][:, oc * 128:(oc + 1) * 128], th[z][:, 0:bs], start=True, stop=True),
                     reads=[w2[z], th[z]], writes=[p], same_ok=True)
                o = nob()
                k.op("act", lambda p=p, o=o, z=z, oc=oc: nc.scalar.activation(out=o[:, 0:bs], in_=p[:, 0:bs], func=AF.Sigmoid, bias=vec[:, z, oc:oc + 1], scale=1.0),
                     reads=[p, vec], writes=[o])
                k.op("dve", lambda o=o: V.tensor_scalar_mul(out=o[:, 0:bs], in0=o[:, 0:bs], scalar1=-0.6065306597126334), reads=[o], writes=[o])
                store(f"logw{z}", oc, o)
        mix(4)
        for z in range(2):
            lora_in(2 + z, th[z], AF.Identity)
        for oc in range(8):
            kt_z = []
            for z in range(2):
                p = nps()
                k.op("pe", lambda p=p, z=z, oc=oc: nc.tensor.matmul(p[:, 0:bs], w2[2 + z][:, oc * 128:(oc + 1) * 128], th[z][:, 0:bs], start=True, stop=True),
                     reads=[w2[2 + z], th[z]], writes=[p], same_ok=True)
                asg = tmp[z]
                k.op("act", lambda p=p, asg=asg, z=z, oc=oc: nc.scalar.activation(out=asg[:, 0:bs], in_=p[:, 0:bs], func=AF.Sigmoid, bias=vec[:, 2 + z, oc:oc + 1], scale=1.0),
                     reads=[p, vec], writes=[asg])
                o = nob()
                k.op("dve", lambda o=o, asg=asg, oc=oc: V.tensor_tensor(out=o[:, 0:bs], in0=kk[:, oc, 0:bs], in1=asg[:, 0:bs], op=ALU.mult), reads=[kk, asg], writes=[o])
                store(f"b{z}", oc, o)
                k.op("dve", lambda asg=asg, oc=oc: V.tensor_scalar(out=asg[:, 0:bs], in0=asg[:, 0:bs], scalar1=vec[:, 5, oc:oc + 1], scalar2=omka[:, oc:oc + 1],
                                                                 op0=ALU.mult, op1=ALU.add), reads=[asg, vec, omka], writes=[asg])
                o2 = nob()
                k.op("dve", lambda o2=o2, asg=asg, oc=oc: V.tensor_tensor(out=o2[:, 0:bs], in0=asg[:, 0:bs], in1=kT[:, oc, 0:bs], op=ALU.mult), reads=[asg, kT], writes=[o2])
                store(f"ktil{z}", oc, o2)
                kt_z.append(o2)
            o3 = nob()
            k.op("dve", lambda o3=o3, a=kt_z[0], b=kt_z[1]: V.tensor_tensor(out=o3[:, 0:bs], in0=a[:, 0:bs], in1=b[:, 0:bs], op=ALU.add), reads=[kt_z[0], kt_z[1]], writes=[o3])
            k.op("dve", lambda o3=o3: V.tensor_scalar_mul(out=o3[:, 0:bs], in0=o3[:, 0:bs], scalar1=0.5), reads=[o3], writes=[o3])
            store("kbar", oc, o3)
        mix(5)
        for (m0, mn, dst) in ((0, 128, gs0), (128, 32, gs1)):
            p = nps()
            for kc in range(8):
                k.op("pe", lambda p=p, kc=kc, m0=m0, mn=mn: nc.tensor.matmul(p[0:mn, 0:bs], g1[:, kc, m0:m0 + mn], xm[:, kc, 0:bs], start=(kc == 0), stop=(kc == 7)),
                     reads=[g1, xm], writes=[p], same_ok=True)
            k.op("act", lambda p=p, dst=dst, mn=mn: nc.scalar.activation(out=dst[:, 0:bs], in_=p[0:mn, 0:bs], func=AF.Sigmoid), reads=[p], writes=[dst])
        for oc in range(8):
            p = nps()
            k.op("pe", lambda p=p, oc=oc: nc.tensor.matmul(p[:, 0:bs], g2a[:, oc * 128:(oc + 1) * 128], gs0[:, 0:bs], start=True, stop=False),
                 reads=[g2a, gs0], writes=[p], same_ok=True)
            k.op("pe", lambda p=p, oc=oc: nc.tensor.matmul(p[:, 0:bs], g2b[:, oc * 128:(oc + 1) * 128], gs1[:, 0:bs], start=False, stop=True),
                 reads=[g2b, gs1], writes=[p], same_ok=True)
            o = nob()
            k.op("act", lambda p=p, o=o: nc.scalar.copy(out=o[:, 0:bs], in_=p[:, 0:bs]), reads=[p], writes=[o])
            store("g", oc, o)
    return k.finish()


def build_B_rw(NCH):
    k = KB(); nc = k.nc; V = nc.vector
    NSC = 4
    T = NCH * 128
    tk_d = {n: k.dram_in(n, [128, NCH, NSC, 64]) for n in ("lw_t", "b_t", "k_t", "v_t")}
    ch_d = {n: k.dram_in(n, [64, NSC, T]) for n in ("r_c", "a_c", "b_c", "k_c")}
    tri_d = k.dram_in("tri", [128, 128]); tris_d = k.dram_in("tris", [128, 128])
    msu_d = k.dram_in("m_su", [128, 128]); msl_d = k.dram_in("m_sl", [128, 128]); miu_d = k.dram_in("m_iu", [128, 128])
    y_d = k.dram_out("y", [128, NCH, NSC, 64])
    ident = _consts(k)
    tri = k.sb("tri_sb", [128, 128]); tris = k.sb("tris_sb", [128, 128])
    msu = k.sb("msu", [128, 128]); msl = k.sb("msl", [128, 128]); miu = k.sb("miu", [128, 128])
    for t, d in ((tri, tri_d), (tris, tris_d), (msu, msu_d), (msl, msl_d), (miu, miu_d)):
        k.dma("sp", t[:], d[:, :])
    NS = 2
    tk = {n: [k.sb(f"{n}_s{j}", [128, NSC, 64]) for j in range(NS)] for n in tk_d}
    ch = {n: [k.sb(f"{n}_s{j}", [64, NSC, 128]) for j in range(NS)] for n in ch_d}
    Pinv = k.sb("Pinv", [128, NSC, 64]); PT = k.sb("PT", [64, NSC, 128]); PinvT = k.sb("PinvT", [64, NSC, 128]); Pm1T = k.sb("Pm1T", [64, NSC, 128])
    At = k.sb("At", [64, NSC, 128]); BtT = k.sb("BtT", [64, NSC, 128]); KtT = k.sb("KtT", [64, NSC, 128]); RtT = k.sb("RtT", [64, NSC, 128])
    Btok = k.sb("Btok", [128, NSC, 64]); Ktok = k.sb("Ktok", [128, NSC, 64])
    Nn = [k.sb(f"Nn{j}", [128, NSC, 128], mybir.dt.float32r) for j in range(2)]; NTt = [k.sb(f"NTt{j}", [128, NSC, 128], mybir.dt.float32r) for j in range(2)]
    X = k.sb("X", [128, NSC, 128], mybir.dt.float32r); XT = k.sb("XT", [128, NSC, 128], mybir.dt.float32r)
    MakT = k.sb("MakT", [128, NSC, 128]); MrbT = k.sb("MrbT", [128, NSC, 128]); MrkT = k.sb("MrkT", [128, NSC, 128])
    W = k.sb("W", [128, NSC, 64]); U = k.sb("U", [128, NSC, 64]); Yb = [k.sb(f"Yb{j}", [128, NSC, 64]) for j in range(2)]
    Z = k.sb("Z", [64, NSC, 64]); Zt = k.sb("Zt", [64, NSC, 64])
    banks = [k.ps(f"bk{j}", [128, 512]) for j in range(8)]
    bi = [0]

    def bank():
        bi[0] += 1
        return banks[bi[0] % 8]
    k.op("pool", lambda: nc.gpsimd.memset(Z[:], 0.0), writes=[Z])

    def load(c):
        s = c % NS
        for n in tk_d:
            k.dma("sp", tk[n][s][:], tk_d[n][:, c, :, :])
        for n in ch_d:
            k.dma("sp", ch[n][s][:], ch_d[n][:, :, c * 128:(c + 1) * 128])
    load(0)
    bcm = lambda m: m[:, None, :].broadcast_to([128, NSC, 128])
    v3 = lambda b: b[:, :].rearrange("p (s t) -> p s t", s=NSC)
    v64 = lambda b: b[:, 0:NSC * 64].rearrange("p (s t) -> p s t", s=NSC)
    for c in range(NCH):
        s = c % NS
        if c + 1 < NCH:
            load(c + 1)
        LW, Bt_, Kt_, Vt = tk["lw_t"][s], tk["b_t"][s], tk["k_t"][s], tk["v_t"][s]
        Rc, Ac, Bc, Kc = ch["r_c"][s], ch["a_c"][s], ch["b_c"][s], ch["k_c"][s]
        pLP = bank()
        k.op("pe", lambda: nc.tensor.matmul(pLP[:, 0:256], tri[:], LW[:].rearrange("p s c -> p (s c)"), start=True, stop=True), reads=[tri, LW], writes=[pLP], same_ok=True)
        pLT = bank(); pL1 = bank()
        for sc in range(NSC):
            k.op("pe", lambda sc=sc: nc.tensor.matmul(pLT[0:64, sc * 128:(sc + 1) * 128], LW[:, sc, :], tri[:], start=True, stop=True), reads=[LW, tri], writes=[pLT], same_ok=True)
            k.op("pe", lambda sc=sc: nc.tensor.matmul(pL1[0:64, sc * 128:(sc + 1) * 128], LW[:, sc, :], tris[:], start=True, stop=True), reads=[LW, tris], writes=[pL1], same_ok=True)
        k.op("act", lambda: nc.scalar.activation(out=Pinv[:].rearrange("p s c -> p (s c)"), in_=pLP[:, 0:256], func=AF.Exp, scale=-1.0), reads=[pLP], writes=[Pinv])
        k.op("act", lambda: nc.scalar.activation(out=PT[:].rearrange("p s c -> p (s c)"), in_=pLT[0:64, :], func=AF.Exp), reads=[pLT], writes=[PT])
        k.op("act", lambda: nc.scalar.activation(out=PinvT[:].rearrange("p s c -> p (s c)"), in_=pLT[0:64, :], func=AF.Exp, scale=-1.0), reads=[pLT], writes=[PinvT])
        k.op("act", lambda: nc.scalar.activation(out=Pm1T[:].rearrange("p s c -> p (s c)"), in_=pL1[0:64, :], func=AF.Exp), reads=[pL1], writes=[Pm1T])
        k.op("dve", lambda: V.tensor_tensor(out=At[:], in0=Ac[:], in1=Pm1T[:], op=ALU.mult), reads=[Ac, Pm1T], writes=[At])
        k.op("dve", lambda: V.tensor_tensor(out=BtT[:], in0=Bc[:], in1=PinvT[:], op=ALU.mult), reads=[Bc, PinvT], writes=[BtT])
        k.op("dve", lambda: V.tensor_tensor(out=KtT[:], in0=Kc[:], in1=PinvT[:], op=ALU.mult), reads=[Kc, PinvT], writes=[KtT])
        k.op("dve", lambda: V.tensor_tensor(out=RtT[:], in0=Rc[:], in1=PT[:], op=ALU.mult), reads=[Rc, PT], writes=[RtT])
        k.op("pool", lambda: nc.gpsimd.tensor_tensor(out=Btok[:], in0=Bt_[:], in1=Pinv[:], op=ALU.mult), reads=[Bt_, Pinv], writes=[Btok])
        k.op("pool", lambda: nc.gpsimd.tensor_tensor(out=Ktok[:], in0=Kt_[:], in1=Pinv[:], op=ALU.mult), reads=[Kt_, Pinv], writes=[Ktok])
        for (L, Rr, dst, msk) in ((BtT, At, Nn[0], msu), (At, BtT, NTt[0], msl), (KtT, At, MakT, msu), (BtT, RtT, MrbT, miu), (KtT, RtT, MrkT, miu)):
            p = bank()
            for sc in range(NSC):
                k.op("pe", lambda p=p, L=L, Rr=Rr, sc=sc: nc.tensor.matmul(p[:, sc * 128:(sc + 1) * 128], L[:, sc, :], Rr[:, sc, :], start=True, stop=True),
                     reads=[L, Rr], writes=[p], same_ok=True)
            k.op("dve", lambda p=p, dst=dst, msk=msk: V.tensor_tensor(out=dst[:], in0=v3(p), in1=bcm(msk), op=ALU.mult), reads=[p, msk], writes=[dst])
        k.op("dve", lambda: V.tensor_tensor(out=X[:], in0=Nn[0][:].bitcast(F32), in1=bcm(ident), op=ALU.add), reads=[Nn[0], ident], writes=[X])
        k.op("dve", lambda: V.tensor_tensor(out=XT[:], in0=NTt[0][:].bitcast(F32), in1=bcm(ident), op=ALU.add), reads=[NTt[0], ident], writes=[XT])
        cur = 0
        for it in range(6):
            nxt = 1 - cur
            last = it == 5
            pN2 = bank()
            for sc in range(NSC):
                k.op("pe", lambda sc=sc, cur=cur, pN2=pN2: nc.tensor.matmul(pN2[:, sc * 128:(sc + 1) * 128], NTt[cur][:, sc, :], Nn[cur][:, sc, :], start=True, stop=True),
                     reads=[NTt[cur], Nn[cur]], writes=[pN2], same_ok=True)
            k.op("act", lambda pN2=pN2, nxt=nxt: nc.scalar.copy(out=Nn[nxt][:], in_=v3(pN2)), reads=[pN2], writes=[Nn[nxt]])
            if not last:
                pT2 = bank()
                for sc in range(NSC):
                    k.op("pe", lambda sc=sc, cur=cur, pT2=pT2: nc.tensor.matmul(pT2[:, sc * 128:(sc + 1) * 128], Nn[cur][:, sc, :], NTt[cur][:, sc, :], start=True, stop=True),
                         reads=[Nn[cur], NTt[cur]], writes=[pT2], same_ok=True)
                k.op("act", lambda pT2=pT2, nxt=nxt: nc.scalar.copy(out=NTt[nxt][:], in_=v3(pT2)), reads=[pT2], writes=[NTt[nxt]])
            pX = bank()
            for sc in range(NSC):
                k.op("pe", lambda sc=sc, nxt=nxt, pX=pX: nc.tensor.matmul(pX[:, sc * 128:(sc + 1) * 128], XT[:, sc, :], Nn[nxt][:, sc, :], start=True, stop=True),
                     reads=[XT, Nn[nxt]], writes=[pX], same_ok=True)
            if not last:
                pXT = bank()
                for sc in range(NSC):
                    k.op("pe", lambda sc=sc, nxt=nxt, pXT=pXT: nc.tensor.matmul(pXT[:, sc * 128:(sc + 1) * 128], X[:, sc, :], NTt[nxt][:, sc, :], start=True, stop=True),
                         reads=[X, NTt[nxt]], writes=[pXT], same_ok=True)
            k.op("dve", lambda pX=pX: V.tensor_tensor(out=X[:], in0=X[:].bitcast(F32), in1=v3(pX), op=ALU.add), reads=[X, pX], writes=[X])
            if not last:
                k.op("dve", lambda pXT=pXT: V.tensor_tensor(out=XT[:], in0=XT[:].bitcast(F32), in1=v3(pXT), op=ALU.add), reads=[XT, pXT], writes=[XT])
            cur = nxt
        pW = bank()
        for sc in range(NSC):
            k.op("pe", lambda sc=sc: nc.tensor.matmul(pW[:, sc * 64:(sc + 1) * 64], At[:, sc, :], Z[:, sc, :], start=True, stop=False), reads=[At, Z], writes=[pW], same_ok=True)
            k.op("pe", lambda sc=sc: nc.tensor.matmul(pW[:, sc * 64:(sc + 1) * 64], MakT[:, sc, :], Vt[:, sc, :], start=False, stop=True), reads=[MakT, Vt], writes=[pW], same_ok=True)
        k.op("act", lambda: nc.scalar.copy(out=W[:], in_=v64(pW)), reads=[pW], writes=[W])
        pU = bank()
        for sc in range(NSC):
            k.op("pe", lambda sc=sc: nc.tensor.matmul(pU[:, sc * 64:(sc + 1) * 64], X[:, sc, :].bitcast(F32), W[:, sc, :], start=True, stop=True), reads=[X, W], writes=[pU], same_ok=True)
        k.op("act", lambda: nc.scalar.copy(out=U[:], in_=v64(pU)), reads=[pU], writes=[U])
        pY = bank()
        for sc in range(NSC):
            k.op("pe", lambda sc=sc: nc.tensor.matmul(pY[:, sc * 64:(sc + 1) * 64], RtT[:, sc, :], Z[:, sc, :], start=True, stop=False), reads=[RtT, Z], writes=[pY], same_ok=True)
            k.op("pe", lambda sc=sc: nc.tensor.matmul(pY[:, sc * 64:(sc + 1) * 64], MrbT[:, sc, :], U[:, sc, :], start=False, stop=False), reads=[MrbT, U], writes=[pY], same_ok=True)
            k.op("pe", lambda sc=sc: nc.tensor.matmul(pY[:, sc * 64:(sc + 1) * 64], MrkT[:, sc, :], Vt[:, sc, :], start=False, stop=True), reads=[MrkT, Vt], writes=[pY], same_ok=True)
        yb = Yb[c % 2]
        k.op("act", lambda yb=yb: nc.scalar.copy(out=yb[:], in_=v64(pY)), reads=[pY], writes=[yb])
        k.dma("sp", y_d[:, c, :, :], yb[:])
        pZ = bank()
        for sc in range(NSC):
            k.op("pe", lambda sc=sc: nc.tensor.matmul(pZ[0:64, sc * 64:(sc + 1) * 64], Btok[:, sc, :], U[:, sc, :], start=True, stop=False), reads=[Btok, U], writes=[pZ], same_ok=True)
            k.op("pe", lambda sc=sc: nc.tensor.matmul(pZ[0:64, sc * 64:(sc + 1) * 64], Ktok[:, sc, :], Vt[:, sc, :], start=False, stop=True), reads=[Ktok, Vt], writes=[pZ], same_ok=True)
        k.op("dve", lambda: V.tensor_tensor(out=Zt[:], in0=Z[:], in1=pZ[0:64, 0:NSC * 64].rearrange("p (s t) -> p s t", s=NSC), op=ALU.add), reads=[Z, pZ], writes=[Zt])
        k.op("dve", lambda: V.tensor_tensor(out=Z[:], in0=Zt[:], in1=PT[:, :, 127:128].broadcast_to([64, NSC, 64]), op=ALU.mult), reads=[Zt, PT], writes=[Z])
    return k.finish()


def run_rwkv(x, ctx, modx, modc, P):
    NX, NC = x.shape[0], ctx.shape[0]
    nxc = NX // NCORES
    xpad = np.concatenate([np.zeros((64, D), np.float32), x, np.zeros((64, D), np.float32)])
    mod = np.zeros((128, 2, 8, 2), np.float32)
    for ty, m in enumerate((modx, modc)):
        mod[:, ty, :, 0] = _fm(m[0]); mod[:, ty, :, 1] = _fm(m[1])
    vecs = np.zeros((128, 7, 8), np.float32)
    for j, v in enumerate((P["w0"][0], P["w0"][1], P["a0"][0], P["a0"][1], P["k_k"], P["k_a"])):
        vecs[:, j, :] = _fm(v)
    bd = np.zeros((128, 128), np.float32); bd[:64, :64] = 1; bd[64:, 64:] = 1
    common = {"mod": mod, "mu": _f(np.stack([_fm(P["mu"][n]) for n in range(6)], axis=1)),
              "w_rkv": _f(np.stack([_kc_layout(P["w_rkv"][n]) for n in range(3)])),
              "w1": _f(np.stack([_kc_layout(P["w1"][0]), _kc_layout(P["w1"][1]), _kc_layout(P["a1"][0]), _kc_layout(P["a1"][1])])),
              "w2": _f(np.stack([P["w2"][0], P["w2"][1], P["a2"][0], P["a2"][1]])),
              "g1": _kc_layout(P["g1"]), "g2a": _f(P["g2"][:128]), "g2b": _f(P["g2"][128:]), "vecs": vecs, "bdones": bd}
    in_maps = []
    for c in range(NCORES):
        win = np.concatenate([xpad[c * nxc:c * nxc + nxc + 128], ctx])
        m = dict(common)
        m["xT"] = _f(win.T.reshape(8, 128, -1).transpose(1, 0, 2))
        hm = np.ones((128, 2), np.float32)
        if c == 0:
            hm[:, 0] = 0
        if c == NCORES - 1:
            hm[:, 1] = 0
        m["hmask"] = hm
        in_maps.append(m)
    res = _run(("A_rw", nxc, NC), lambda: build_A_rw(nxc, NC), in_maps)
    fmx, fmc = {}, {}
    for n in ["r", "v", "kkneg", "b0", "b1", "ktil0", "ktil1", "logw0", "logw1", "kbar", "g"]:
        fmx[n] = np.concatenate([r[n].reshape(1024, -1)[:, :nxc] for r in res], axis=1)
        fmc[n] = res[0][n].reshape(1024, -1)[:, nxc:]
    T = NC + NX
    NCH = T // 128
    iu = np.triu(np.ones((128, 128), np.float32)); su = np.triu(np.ones((128, 128), np.float32), 1)
    consts = {"tri": _f(iu), "tris": _f(su), "m_su": _f(su), "m_sl": _f(su.T), "m_iu": _f(iu), "ident": _IDENT}
    in_maps = []
    for core in range(NCORES):
        tkm = {n: [] for n in ("lw_t", "b_t", "k_t", "v_t")}
        chm = {n: [] for n in ("r_c", "a_c", "b_c", "k_c")}
        for j in range(4):
            sid = core * 4 + j
            hd, z = sid // 2, sid % 2
            hs = slice(hd * 64, (hd + 1) * 64)

            def seq(n):
                ax, ac = fmx[n][hs], fmc[n][hs]
                if z == 0:
                    return np.concatenate([ac, ax], axis=1)
                return np.concatenate([ac[:, ::-1], ax[:, ::-1]], axis=1)
            tkm["lw_t"].append(seq(f"logw{z}").T); tkm["b_t"].append(seq(f"b{z}").T); tkm["k_t"].append(seq(f"ktil{z}").T); tkm["v_t"].append(seq("v").T)
            chm["r_c"].append(seq("r")); chm["a_c"].append(seq("kkneg")); chm["b_c"].append(seq(f"b{z}")); chm["k_c"].append(seq(f"ktil{z}"))
        m = dict(consts)
        for n, lst in tkm.items():
            m[n] = _f(np.stack(lst).reshape(4, NCH, 128, 64).transpose(2, 1, 0, 3))
        for n, lst in chm.items():
            m[n] = _f(np.stack(lst).transpose(1, 0, 2))
        in_maps.append(m)
    res = _run(("B_rw", NCH), lambda: build_B_rw(NCH), in_maps)
    yf = np.zeros((T, D), np.float32); yb = np.zeros((T, D), np.float32)
    for core in range(NCORES):
        yy = res[core]["y"]
        for j in range(4):
            sid = core * 4 + j
            hd, z = sid // 2, sid % 2
            ys = yy[:, :, j, :].transpose(1, 0, 2).reshape(T, 64)
            if z == 0:
                yf[:, hd * 64:(hd + 1) * 64] = ys
            else:
                yb[:NC, hd * 64:(hd + 1) * 64] = ys[:NC][::-1]
                yb[NC:, hd * 64:(hd + 1) * 64] = ys[NC:][::-1]
    px = [yf[NC:], yb[NC:]] + [_f(fmx[n].T) for n in ("r", "kbar", "v", "g")]
    pc = [yf[:NC], yb[:NC]] + [_f(fmc[n].T) for n in ("r", "kbar", "v", "g")]
    return px, pc


def kernel(x, c, ctx, c_ctx, ada_w, ada_b, ln_g, ln_b,
           ml_w_in, ml_b_in, ml_conv_w, ml_conv_b, ml_hn_g, ml_w_out,
           rw_mu, rw_w_rkv, rw_w0, rw_w1, rw_w2, rw_a0, rw_a1, rw_a2, rw_g1, rw_g2,
           rw_k_k, rw_k_a, rw_r_k, rw_lnx_g, rw_lnx_b, rw_w_out,
           pk_wq, pk_keys, pk_u, pk_v):
    A = lambda a: np.asarray(a, dtype=np.float32)
    xs = A(x)[0]; cs = A(ctx)[0]
    depth = ada_w.shape[0]
    mods = run_P0(A(c), A(c_ctx), A(ada_w), A(ada_b))
    for i in range(depth):
        j = i // 2
        modx, modc = mods[i, 0], mods[i, 1]
        if i % 2 == 0:
            px, pc = run_mlstm(xs, cs, modx, modc, A(ml_w_in[j]), A(ml_b_in[j]), A(ml_conv_w[j]), A(ml_conv_b[j]))
            x1, c1 = run_C1("ml", xs, cs, list(px), list(pc), [A(ml_hn_g[j])], modx[2], modc[2], A(ln_g[i, 0]), A(ln_b[i, 0]), A(ml_w_out[j]))
        else:
            P = {"mu": A(rw_mu[j]), "w_rkv": A(rw_w_rkv[j]), "w0": A(rw_w0[j]), "w1": A(rw_w1[j]), "w2": A(rw_w2[j]),
                 "a0": A(rw_a0[j]), "a1": A(rw_a1[j]), "a2": A(rw_a2[j]), "g1": A(rw_g1[j]), "g2": A(rw_g2[j]),
                 "k_k": A(rw_k_k[j]), "k_a": A(rw_k_a[j])}
            px, pc = run_rwkv(xs, cs, modx, modc, P)
            x1, c1 = run_C1("rw", xs, cs, px, pc, [A(rw_lnx_g[j]), A(rw_lnx_b[j]), A(rw_r_k[j])], modx[2], modc[2],
                            A(ln_g[i, 0]), A(ln_b[i, 0]), A(rw_w_out[j]))
        xs, cs = run_C2(x1, c1, modx[3:6], modc[3:6], A(ln_g[i, 1]), A(ln_b[i, 1]), A(pk_wq[i]), A(pk_keys[i]), A(pk_u[i]), A(pk_v[i]))
    return np.ascontiguousarray(xs[None].astype(np.float32))
```
